# Optimizing a Trainium2 kernel written in Bass

```python
import math
import jax, jax.numpy as jnp
from jax import lax
import numpy as np

D_MODEL = 1024
BATCH = 8
SEQ = 4096
DEPTH = 4

N_MIXERS = 4
N_SSD = (DEPTH + 3) // 4
N_RWKV = (DEPTH + 2) // 4
N_GLA = (DEPTH + 1) // 4
N_RET = DEPTH // 4
CHUNK = 64
NORM_EPS = 1e-5

SSD_D_INNER = 2 * D_MODEL
SSD_HEAD_DIM = 64
SSD_HEADS = SSD_D_INNER // SSD_HEAD_DIM
SSD_GROUPS = 8
SSD_STATE = 128
SSD_CONV = 4
SSD_CONV_DIM = SSD_D_INNER + 2 * SSD_GROUPS * SSD_STATE
SSD_IN_DIM = SSD_D_INNER + SSD_CONV_DIM + SSD_HEADS

RWKV_HEAD_DIM = 64
RWKV_HEADS = D_MODEL // RWKV_HEAD_DIM
RWKV_DECAY_LORA = 64
RWKV_AAA_LORA = 64
RWKV_GATE_LORA = 160
RWKV_GN_EPS = 64e-5

GLA_HEADS = 4
GLA_KEY_DIM = D_MODEL // 2
GLA_DK = GLA_KEY_DIM // GLA_HEADS
GLA_VALUE_DIM = D_MODEL
GLA_DV = GLA_VALUE_DIM // GLA_HEADS
GLA_GATE_RANK = 16
GLA_GATE_NORM = 16.0
GLA_IN_DIM = 2 * GLA_KEY_DIM + 2 * GLA_VALUE_DIM + GLA_GATE_RANK

RET_HEADS = 4
RET_QK_DIM = D_MODEL
RET_DK = RET_QK_DIM // RET_HEADS
RET_V_DIM = 2 * D_MODEL
RET_DV = RET_V_DIM // RET_HEADS
RET_IN_DIM = 2 * RET_QK_DIM + 2 * RET_V_DIM
ROPE_BASE = 10000.0

FFN_HIDDEN = 2816
FFN_CONV = 3

kernel_name = 'hybrid_ssd_rwkv7_gla_retnet_convffn'


def rms_norm(x, w, eps=NORM_EPS):
    xf = x.astype(jnp.float32)
    y = xf * lax.rsqrt(jnp.mean(xf * xf, axis=-1, keepdims=True) + eps)
    return (y * w).astype(x.dtype)


def head_rms(x, eps=NORM_EPS):
    xf = x.astype(jnp.float32)
    return xf * lax.rsqrt(jnp.mean(xf * xf, axis=-1, keepdims=True) + eps)


def causal_dwconv(x, w, b):
    K = w.shape[0]
    y = lax.conv_general_dilated(x, w[:, None, :].astype(x.dtype), window_strides=(1,),
                                 padding=[(K - 1, 0)], dimension_numbers=('NWC', 'WIO', 'NWC'),
                                 feature_group_count=x.shape[-1])
    return y + b


def token_shift(x):
    return jnp.pad(x[:, :-1], ((0, 0), (1, 0), (0, 0)))


def chunked_linear_attn(q, k, v, log_decay):
    out_dtype = v.dtype
    B, T, H, Dv = v.shape
    Hk, Dk = q.shape[2], q.shape[3]
    Dg = log_decay.shape[-1]
    rep = H // Hk
    n = T // CHUNK

    def to_chunks(a):
        return jnp.moveaxis(a.astype(jnp.float32).reshape(B, n, CHUNK, *a.shape[2:]), 1, 0)

    qc, kc, vc = to_chunks(q), to_chunks(k), to_chunks(v)
    gc = jnp.cumsum(to_chunks(log_decay), axis=2)
    causal = jnp.tril(jnp.ones((CHUNK, CHUNK), dtype=bool))

    def step(S, inp):
        qb, kb, vb, gb = inp
        if rep > 1:
            qb = jnp.repeat(qb, rep, axis=2)
            kb = jnp.repeat(kb, rep, axis=2)
        if Dg == 1:
            seg = gb[:, :, None, :, 0] - gb[:, None, :, :, 0]
            dec = jnp.exp(jnp.where(causal[None, :, :, None], seg, -jnp.inf))
            scores = jnp.einsum('blhd,bshd->blsh', qb, kb) * dec
        else:
            seg = gb[:, :, None] - gb[:, None]
            dec = jnp.exp(jnp.where(causal[None, :, :, None, None], seg, -jnp.inf))
            scores = jnp.einsum('blhd,bshd,blshd->blsh', qb, kb, dec)
        o = (jnp.einsum('blsh,bshv->blhv', scores, vb)
             + jnp.einsum('blhd,bhdv->blhv', qb * jnp.exp(gb), S))
        g_last = gb[:, -1]
        S = (S * jnp.exp(g_last)[..., None]
             + jnp.einsum('bshd,bshv->bhdv', kb * jnp.exp(g_last[:, None] - gb), vb))
        return S, o

    S0 = jnp.zeros((B, H, Dk, Dv), jnp.float32)
    _, o = lax.scan(step, S0, (qc, kc, vc, gc))
    return jnp.moveaxis(o, 0, 1).reshape(B, T, H, Dv).astype(out_dtype)


def rwkv7_scan(r, w, k, v, a, b):
    out_dtype = v.dtype
    B, T, H, N = v.shape
    xs = tuple(jnp.moveaxis(t.astype(jnp.float32), 1, 0) for t in (r, w, k, v, a, b))

    def step(S, inp):
        rt, wt, kt, vt, at, bt = inp
        sa = jnp.einsum('bhij,bhj->bhi', S, at)
        S = S * wt[:, :, None, :] + sa[..., None] * bt[:, :, None, :] + vt[..., None] * kt[:, :, None, :]
        return S, jnp.einsum('bhij,bhj->bhi', S, rt)

    S0 = jnp.zeros((B, H, N, N), jnp.float32)
    _, y = lax.scan(step, S0, xs)
    return jnp.moveaxis(y, 0, 1).astype(out_dtype)


def ssd_mixer(x, w_in, conv_w, conv_b, dt_bias, a_log, d_skip, norm_w, w_out):
    B, T, _ = x.shape
    zxbcdt = x @ w_in
    z, xbc, dt = jnp.split(zxbcdt, [SSD_D_INNER, SSD_D_INNER + SSD_CONV_DIM], axis=-1)
    xbc = jax.nn.silu(causal_dwconv(xbc, conv_w, conv_b))
    xs, bm, cm = jnp.split(xbc, [SSD_D_INNER, SSD_D_INNER + SSD_GROUPS * SSD_STATE], axis=-1)
    xs = xs.reshape(B, T, SSD_HEADS, SSD_HEAD_DIM)
    bm = bm.reshape(B, T, SSD_GROUPS, SSD_STATE)
    cm = cm.reshape(B, T, SSD_GROUPS, SSD_STATE)
    dt = jax.nn.softplus(dt.astype(jnp.float32) + dt_bias)
    a = -jnp.exp(a_log.astype(jnp.float32))
    y = chunked_linear_attn(cm, bm, xs * dt[..., None].astype(xs.dtype), (dt * a)[..., None])
    y = y + d_skip[:, None] * xs
    y = y.reshape(B, T, SSD_D_INNER) * jax.nn.silu(z)
    y = head_rms(y.reshape(B, T, SSD_GROUPS, SSD_D_INNER // SSD_GROUPS)).reshape(B, T, SSD_D_INNER)
    return (y * norm_w).astype(x.dtype) @ w_out


def rwkv7_mixer(x, mix, w_rkv, w0, w1, w2, a0, a1, a2, g1, g2, k_k, k_a, r_k, ln_w, ln_b, w_out):
    B, T, D = x.shape
    H, N = RWKV_HEADS, RWKV_HEAD_DIM
    xx = token_shift(x) - x
    xr, xw, xk, xv, xa, xg = [x + xx * mix[i] for i in range(6)]
    r = xr @ w_rkv[0]
    k = xk @ w_rkv[1]
    v = xv @ w_rkv[2]
    w_raw = -jax.nn.softplus(-(w0 + jnp.tanh(xw @ w1) @ w2).astype(jnp.float32)) - 0.5
    decay = jnp.exp(-jnp.exp(w_raw))
    a = jax.nn.sigmoid(a0 + (xa @ a1) @ a2)
    g = jax.nn.sigmoid(xg @ g1) @ g2
    kk = (k * k_k).reshape(B, T, H, N).astype(jnp.float32)
    kk = kk / jnp.maximum(jnp.sqrt(jnp.sum(kk * kk, axis=-1, keepdims=True)), 1e-12)
    k = k * (1.0 + (a - 1.0) * k_a)
    ah = a.reshape(B, T, H, N)
    rh, kh, vh = r.reshape(B, T, H, N), k.reshape(B, T, H, N), v.reshape(B, T, H, N)
    y = rwkv7_scan(rh, decay.reshape(B, T, H, N), kh, vh, -kk, kk * ah).astype(jnp.float32)
    mu = jnp.mean(y, axis=-1, keepdims=True)
    var = jnp.mean(jnp.square(y - mu), axis=-1, keepdims=True)
    y = ((y - mu) * lax.rsqrt(var + RWKV_GN_EPS)).reshape(B, T, D) * ln_w + ln_b
    bonus = jnp.sum(rh * kh * r_k, axis=-1, keepdims=True) * vh
    y = y + bonus.reshape(B, T, D)
    return (y * g).astype(x.dtype) @ w_out


def gla_mixer(x, w_in, w_gk2, b_gk2, norm_w, w_out):
    B, T, _ = x.shape
    proj = x @ w_in
    q, k, v, g, gk = jnp.split(proj, [GLA_KEY_DIM, 2 * GLA_KEY_DIM, 2 * GLA_KEY_DIM + GLA_VALUE_DIM,
                                     2 * GLA_KEY_DIM + 2 * GLA_VALUE_DIM], axis=-1)
    log_a = jax.nn.log_sigmoid((gk @ w_gk2 + b_gk2).astype(jnp.float32)) / GLA_GATE_NORM
    q = q.reshape(B, T, GLA_HEADS, GLA_DK) * (GLA_DK ** -0.5)
    k = k.reshape(B, T, GLA_HEADS, GLA_DK)
    v = v.reshape(B, T, GLA_HEADS, GLA_DV)
    o = chunked_linear_attn(q, k, v, log_a.reshape(B, T, GLA_HEADS, GLA_DK))
    o = (head_rms(o) * norm_w).reshape(B, T, GLA_VALUE_DIM) * jax.nn.silu(g.astype(jnp.float32))
    return o.astype(x.dtype) @ w_out


def retnet_rotary(x):
    T, Dk = x.shape[1], x.shape[-1]
    inv = 1.0 / (ROPE_BASE ** jnp.linspace(0.0, 1.0, Dk // 2, dtype=jnp.float32))
    ang = jnp.repeat(jnp.arange(T, dtype=jnp.float32)[:, None] * inv[None], 2, axis=-1)
    sin, cos = jnp.sin(ang)[None, :, None], jnp.cos(ang)[None, :, None]
    xf = x.astype(jnp.float32)
    rot = jnp.stack([-xf[..., 1::2], xf[..., 0::2]], axis=-1).reshape(xf.shape)
    return (xf * cos + rot * sin).astype(x.dtype)


def retention_mixer(x, w_in, w_out):
    B, T, _ = x.shape
    proj = x @ w_in
    q, k, v, g = jnp.split(proj, [RET_QK_DIM, 2 * RET_QK_DIM, 2 * RET_QK_DIM + RET_V_DIM], axis=-1)
    q = retnet_rotary(q.reshape(B, T, RET_HEADS, RET_DK))
    k = retnet_rotary(k.reshape(B, T, RET_HEADS, RET_DK)) * (RET_DK ** -0.5)
    v = v.reshape(B, T, RET_HEADS, RET_DV)
    log_gamma = jnp.log(1.0 - 2.0 ** (-5.0 - jnp.arange(RET_HEADS, dtype=jnp.float32)))
    log_decay = jnp.broadcast_to(log_gamma[None, None, :, None], (B, T, RET_HEADS, 1))
    o = head_rms(chunked_linear_attn(q, k, v, log_decay)).reshape(B, T, RET_V_DIM)
    return (jax.nn.silu(g.astype(jnp.float32)) * o).astype(x.dtype) @ w_out


def conv_ffn(x, w_up, conv_w, conv_b, w_down):
    h = causal_dwconv(x @ w_up, conv_w, conv_b)
    val, gate = jnp.split(h, 2, axis=-1)
    return (jax.nn.silu(gate) * val) @ w_down


def setup_inputs(seed: int = 0) -> dict:
    key = jax.random.key(seed)
    ks = iter(jax.random.split(key, 64))

    def nrm(shape, scale):
        return scale * jax.random.normal(next(ks), shape, jnp.float32)

    def uni(shape, lo, hi):
        return jax.random.uniform(next(ks), shape, jnp.float32, lo, hi)

    D = D_MODEL
    F2 = 2 * FFN_HIDDEN
    dt0 = jnp.exp(uni((N_SSD, SSD_HEADS), math.log(1e-3), math.log(1e-1)))
    return {
        'x': nrm((BATCH, SEQ, D), 1.0),
        'norm_mix': 1.0 + nrm((DEPTH, D), 0.02),
        'norm_ffn': 1.0 + nrm((DEPTH, D), 0.02),
        'norm_final': 1.0 + nrm((D,), 0.02),
        'ssd_w_in': nrm((N_SSD, D, SSD_IN_DIM), D ** -0.5),
        'ssd_conv_w': nrm((N_SSD, SSD_CONV, SSD_CONV_DIM), SSD_CONV ** -0.5),
        'ssd_conv_b': nrm((N_SSD, SSD_CONV_DIM), 0.02),
        'ssd_dt_bias': dt0 + jnp.log(-jnp.expm1(-dt0)),
        'ssd_a_log': jnp.log(uni((N_SSD, SSD_HEADS), 1.0, 16.0)),
        'ssd_d': 1.0 + nrm((N_SSD, SSD_HEADS), 0.1),
        'ssd_norm_w': 1.0 + nrm((N_SSD, SSD_D_INNER), 0.02),
        'ssd_w_out': nrm((N_SSD, SSD_D_INNER, D), SSD_D_INNER ** -0.5),
        'rwkv_mix': uni((N_RWKV, 6, D), 0.0, 1.0),
        'rwkv_w_rkv': nrm((N_RWKV, 3, D, D), D ** -0.5),
        'rwkv_w0': uni((N_RWKV, D), -6.0, -1.0),
        'rwkv_w1': nrm((N_RWKV, D, RWKV_DECAY_LORA), D ** -0.5),
        'rwkv_w2': nrm((N_RWKV, RWKV_DECAY_LORA, D), 0.1 * RWKV_DECAY_LORA ** -0.5),
        'rwkv_a0': nrm((N_RWKV, D), 0.1),
        'rwkv_a1': nrm((N_RWKV, D, RWKV_AAA_LORA), D ** -0.5),
        'rwkv_a2': nrm((N_RWKV, RWKV_AAA_LORA, D), RWKV_AAA_LORA ** -0.5),
        'rwkv_g1': nrm((N_RWKV, D, RWKV_GATE_LORA), D ** -0.5),
        'rwkv_g2': nrm((N_RWKV, RWKV_GATE_LORA, D), RWKV_GATE_LORA ** -0.5),
        'rwkv_k_k': 0.85 + nrm((N_RWKV, D), 0.05),
        'rwkv_k_a': 1.0 + nrm((N_RWKV, D), 0.05),
        'rwkv_r_k': nrm((N_RWKV, RWKV_HEADS, RWKV_HEAD_DIM), 0.1),
        'rwkv_ln_w': 1.0 + nrm((N_RWKV, D), 0.02),
        'rwkv_ln_b': nrm((N_RWKV, D), 0.02),
        'rwkv_w_out': nrm((N_RWKV, D, D), D ** -0.5),
        'gla_w_in': nrm((N_GLA, D, GLA_IN_DIM), D ** -0.5),
        'gla_w_gk2': nrm((N_GLA, GLA_GATE_RANK, GLA_KEY_DIM), GLA_GATE_RANK ** -0.5),
        'gla_b_gk2': nrm((N_GLA, GLA_KEY_DIM), 0.1),
        'gla_norm_w': 1.0 + nrm((N_GLA, GLA_DV), 0.02),
        'gla_w_out': nrm((N_GLA, GLA_VALUE_DIM, D), GLA_VALUE_DIM ** -0.5),
        'ret_w_in': nrm((N_RET, D, RET_IN_DIM), D ** -0.5),
        'ret_w_out': nrm((N_RET, RET_V_DIM, D), RET_V_DIM ** -0.5),
        'ffn_w_up': nrm((DEPTH, D, F2), D ** -0.5),
        'ffn_conv_w': nrm((DEPTH, FFN_CONV, F2), FFN_CONV ** -0.5),
        'ffn_conv_b': nrm((DEPTH, F2), 0.02),
        'ffn_w_down': nrm((DEPTH, FFN_HIDDEN, D), FFN_HIDDEN ** -0.5),
    }


def reference(x, norm_mix, norm_ffn, norm_final,
              ssd_w_in, ssd_conv_w, ssd_conv_b, ssd_dt_bias, ssd_a_log, ssd_d, ssd_norm_w, ssd_w_out,
              rwkv_mix, rwkv_w_rkv, rwkv_w0, rwkv_w1, rwkv_w2, rwkv_a0, rwkv_a1, rwkv_a2,
              rwkv_g1, rwkv_g2, rwkv_k_k, rwkv_k_a, rwkv_r_k, rwkv_ln_w, rwkv_ln_b, rwkv_w_out,
              gla_w_in, gla_w_gk2, gla_b_gk2, gla_norm_w, gla_w_out,
              ret_w_in, ret_w_out,
              ffn_w_up, ffn_conv_w, ffn_conv_b, ffn_w_down):
    for i in range(DEPTH):
        m, j = i % N_MIXERS, i // N_MIXERS
        h = rms_norm(x, norm_mix[i])
        if m == 0:
            h = ssd_mixer(h, ssd_w_in[j], ssd_conv_w[j], ssd_conv_b[j], ssd_dt_bias[j], ssd_a_log[j],
                          ssd_d[j], ssd_norm_w[j], ssd_w_out[j])
        elif m == 1:
            h = rwkv7_mixer(h, rwkv_mix[j], rwkv_w_rkv[j], rwkv_w0[j], rwkv_w1[j], rwkv_w2[j],
                            rwkv_a0[j], rwkv_a1[j], rwkv_a2[j], rwkv_g1[j], rwkv_g2[j],
                            rwkv_k_k[j], rwkv_k_a[j], rwkv_r_k[j], rwkv_ln_w[j], rwkv_ln_b[j],
                            rwkv_w_out[j])
        elif m == 2:
            h = gla_mixer(h, gla_w_in[j], gla_w_gk2[j], gla_b_gk2[j], gla_norm_w[j], gla_w_out[j])
        else:
            h = retention_mixer(h, ret_w_in[j], ret_w_out[j])
        x = x + h
        x = x + conv_ffn(rms_norm(x, norm_ffn[i]), ffn_w_up[i], ffn_conv_w[i], ffn_conv_b[i], ffn_w_down[i])
    return rms_norm(x, norm_final)
```

```python
import numpy as np
import concourse.bass as bass
import concourse.mybir as mybir
from concourse.bass_utils import run_bass_kernel_spmd

F32 = mybir.dt.float32
BF16 = mybir.dt.bfloat16
AF = mybir.ActivationFunctionType
ALU = mybir.AluOpType
AX = mybir.AxisListType

D = 1024
KC = 8
TB = 512
EPS = 1e-5


class V:
    __slots__ = ("ap", "tok")

    def __init__(self, ap, tok):
        self.ap = ap
        self.tok = tok if isinstance(tok, tuple) else (tok,)


class _Op:
    __slots__ = ("eng", "fn", "reads", "writes", "dma_key", "deps", "inc", "val", "sem", "amt")

    def __init__(self, eng, fn, reads, writes, dma_key):
        self.eng = eng
        self.fn = fn
        self.reads = reads
        self.writes = writes
        self.dma_key = dma_key
        self.deps = []
        self.inc = False
        self.val = 0
        self.sem = None
        self.amt = 1


class Sched:
    COMPUTE = ("pe", "act", "dve", "pool")

    def __init__(self, nc):
        self.nc = nc
        self.ops = []
        self.state = {}

    def op(self, eng, fn, reads=(), writes=(), dma_key=None):
        o = _Op(eng, fn, [t if isinstance(t, tuple) else (t,) for t in reads],
                [t if isinstance(t, tuple) else (t,) for t in writes], dma_key)
        self._analyse(o)
        self.ops.append(o)
        return o

    @staticmethod
    def _conf(a, b):
        n = min(len(a), len(b))
        return a[:n] == b[:n]

    def _add_dep(self, o, p, kind):
        if p is None or p is o:
            return
        pd = p.dma_key is not None
        od = o.dma_key is not None
        if not pd and not od:
            if p.eng == "pe" and o.eng == "pe":
                return
            if p.eng == o.eng and kind != "RAW":
                return
        if pd and od and p.eng == o.eng and kind == "WAR" and False:
            return
        o.deps.append(p)

    def _analyse(self, o):
        st = self.state
        for tk in o.reads:
            root = st.setdefault(tk[0], {})
            for k, e in root.items():
                if self._conf(k, tk):
                    self._add_dep(o, e[0], "RAW")
            e = root.get(tk)
            if e is None:
                root[tk] = [None, [o]]
            else:
                e[1].append(o)
        for tk in o.writes:
            root = st.setdefault(tk[0], {})
            dead = []
            for k, e in root.items():
                if self._conf(k, tk):
                    self._add_dep(o, e[0], "WAW")
                    for r in e[1]:
                        self._add_dep(o, r, "WAR")
                    if len(k) > len(tk):
                        dead.append(k)
                    elif len(k) < len(tk):
                        pass
            for k in dead:
                del root[k]
            root[tk] = [o, []]

    def barrier(self):
        lasts = {}
        dmas = {}
        for o in self.ops:
            if o.dma_key is not None:
                dmas[o.dma_key] = o
            elif o.fn is not None:
                lasts[o.eng] = o
        new = []
        for eng in ("pe", "act", "dve", "pool", "sp"):
            b = _Op(eng, None, [], [], None)
            b.deps = [p for e, p in lasts.items() if e != eng] + list(dmas.values())
            new.append(b)
        self.ops.extend(new)
        self.state = {}

    def emit(self, block_ctx, sems):
        nc = self.nc
        for o in self.ops:
            for p in o.deps:
                p.inc = True
        cnt = {}
        for o in self.ops:
            if o.dma_key is not None:
                key = ("dma", o.dma_key)
                cnt[key] = cnt.get(key, 0) + 16
                o.val = cnt[key]
                o.sem = sems[key]
                o.amt = 16
                o.inc = True
            elif o.inc:
                cnt[o.eng] = cnt.get(o.eng, 0) + 1
                o.val = cnt[o.eng]
                o.sem = sems[o.eng]
        engs = {"pe": nc.tensor, "act": nc.scalar, "dve": nc.vector, "pool": nc.gpsimd, "sp": nc.sync}
        per_eng = {k: [] for k in engs}
        for o in self.ops:
            per_eng[o.eng].append(o)

        def run(engname):
            eng = engs[engname]
            waited = {}
            for o in per_eng[engname]:
                need = {}
                for p in o.deps:
                    sid = id(p.sem)
                    if need.get(sid, (None, 0))[1] < p.val:
                        need[sid] = (p.sem, p.val)
                for sid, (sem, val) in need.items():
                    if waited.get(sid, 0) < val:
                        eng.wait_ge(sem, val)
                        waited[sid] = val
                if o.fn is None:
                    continue
                ins = o.fn()
                if o.inc:
                    ins.then_inc(o.sem, o.amt)

        @block_ctx.tensor
        def _(e):
            run("pe")

        @block_ctx.scalar
        def _(e):
            run("act")

        @block_ctx.vector
        def _(e):
            run("dve")

        @block_ctx.gpsimd
        def _(e):
            run("pool")

        @block_ctx.sync
        def _(e):
            run("sp")


class Prog:
    def __init__(self, T, plan):
        self.T = T
        self.plan = plan
        self.nc = bass.Bass("TRN2", target_bir_lowering=False)
        self.S = Sched(self.nc)
        self.ctxs = []
        self.dma_keys = []
        self.inputs = {}
        self.psn = 0

    def dram_in(self, name, shape, dt=F32):
        t = self.nc.dram_tensor(name, list(shape), dt, kind="ExternalInput")
        self.inputs[name] = t
        return t.ap()

    def dram_out(self, name, shape, dt=F32):
        return self.nc.dram_tensor(name, list(shape), dt, kind="ExternalOutput").ap()

    def dram_scratch(self, name, shape, dt=F32):
        return self.nc.dram_tensor(name, list(shape), dt, kind="Internal").ap()

    def sb(self, name, shape, dt=F32):
        g = self.nc.sbuf_tensor(name, list(shape), dt)
        t = g.__enter__()
        self.ctxs.append(g)
        return t

    def ps(self, name, shape=(128, 512), dt=F32):
        g = self.nc.psum_tensor(name, list(shape), dt)
        t = g.__enter__()
        self.ctxs.append(g)
        return t

    def arena_init(self, nbytes):
        self.arena = self.sb("arena", [128, nbytes // 4], F32)
        self.arena_n = nbytes
        self.arena_off = 0

    def alloc(self, shape, dt=F32):
        n = 1
        for d in shape:
            n *= d
        esz = 4 if dt == F32 else 2
        nb = (n * esz + 63) // 64 * 64
        assert self.arena_off + nb <= self.arena_n, ("arena overflow", self.arena_off + nb, self.arena_n)
        a = self.arena[:, self.arena_off // 4:(self.arena_off + nb) // 4]
        self.arena_off += nb
        if dt != F32:
            a = a.bitcast(dt)
        a = a[:, 0:n]
        if len(shape) == 2:
            a = a.rearrange("p (a b) -> p a b", a=shape[0])
        elif len(shape) == 3:
            a = a.rearrange("p (a b c) -> p a b c", a=shape[0], b=shape[1])
        elif len(shape) == 4:
            a = a.rearrange("p (a b c d) -> p a b c d", a=shape[0], b=shape[1], c=shape[2])
        return a

    def arena_reset(self, mark=0):
        self.S.barrier()
        self.arena_off = mark

    def _toks(self, *vs):
        return [v.tok for v in vs if isinstance(v, V)]

    def mm(self, out, lhsT, rhs, start=True, stop=True):
        nc = self.nc
        self.S.op("pe", lambda: nc.tensor.matmul(out.ap, lhsT.ap, rhs.ap, start=start, stop=stop),
                  reads=self._toks(lhsT, rhs), writes=self._toks(out))

    def transpose(self, out, in_, ident):
        nc = self.nc
        self.S.op("pe", lambda: nc.tensor.transpose(out.ap, in_.ap, ident.ap),
                  reads=self._toks(in_, ident), writes=self._toks(out))

    def act(self, out, in_, func, scale=None, bias=None, accum=None, extra_reads=()):
        nc = self.nc
        kw = {}
        if scale is not None:
            kw["scale"] = scale.ap if isinstance(scale, V) else scale
        if bias is not None:
            kw["bias"] = bias.ap if isinstance(bias, V) else bias
        if accum is not None:
            kw["accum_out"] = accum.ap
        w = self._toks(out) + (self._toks(accum) if accum is not None else [])
        self.S.op("act", lambda: nc.scalar.activation(out.ap, in_.ap, func, **kw),
                  reads=self._toks(in_, scale, bias) + list(extra_reads), writes=w)

    def _e(self, eng):
        return {"dve": self.nc.vector, "pool": self.nc.gpsimd, "act": self.nc.scalar}[eng]

    def tt(self, eng, out, a, b, op):
        e = self._e(eng)
        self.S.op(eng, lambda: e.tensor_tensor(out.ap, a.ap, b.ap, op),
                  reads=self._toks(a, b), writes=self._toks(out))

    def ts(self, eng, out, in_, s1, op0, s2=None, op1=None, accum=None):
        e = self._e(eng)
        a1 = s1.ap if isinstance(s1, V) else s1
        a2 = s2.ap if isinstance(s2, V) else s2
        kw = {}
        if op1 is not None:
            kw["op1"] = op1
        if accum is not None:
            kw["accum_out"] = accum.ap
        w = self._toks(out) + (self._toks(accum) if accum is not None else [])
        self.S.op(eng, lambda: e.tensor_scalar(out.ap, in_.ap, a1, a2, op0, **kw),
                  reads=self._toks(in_, s1, s2), writes=w)

    def stt(self, out, in0, scalar, in1, op0, op1):
        nc = self.nc
        sc = scalar.ap if isinstance(scalar, V) else scalar
        self.S.op("dve", lambda: nc.vector.scalar_tensor_tensor(out.ap, in0.ap, sc, in1.ap, op0, op1),
                  reads=self._toks(in0, scalar, in1), writes=self._toks(out))

    def copy(self, eng, out, in_):
        if eng == "act":
            nc = self.nc
            self.S.op("act", lambda: nc.scalar.copy(out.ap, in_.ap), reads=self._toks(in_), writes=self._toks(out))
        else:
            e = self._e(eng)
            self.S.op(eng, lambda: e.tensor_copy(out.ap, in_.ap), reads=self._toks(in_), writes=self._toks(out))

    def memset(self, eng, out, val):
        e = self._e(eng)
        self.S.op(eng, lambda: e.memset(out.ap, val), writes=self._toks(out))

    def recip(self, out, in_):
        nc = self.nc
        self.S.op("dve", lambda: nc.vector.reciprocal(out.ap, in_.ap), reads=self._toks(in_), writes=self._toks(out))

    def dma(self, q, out, in_, key):
        if key not in self.dma_keys:
            self.dma_keys.append(key)
        e = {"sp": self.nc.sync, "pool": self.nc.gpsimd, "act": self.nc.scalar}[q]
        self.S.op(q, lambda: e.dma_start(out=out.ap, in_=in_.ap), reads=self._toks(in_),
                  writes=self._toks(out), dma_key=key)

    def barrier(self, eng, toks):
        self.S.op(eng, None, reads=list(toks))

    def finish(self):
        nc = self.nc
        sems = {}
        gs = []
        for name in ("pe", "act", "dve", "pool"):
            g = nc.semaphore("sem_" + name)
            sems[name] = g.__enter__()
            gs.append(g)
        for i, k in enumerate(self.dma_keys):
            g = nc.semaphore("semd_%d" % i)
            sems[("dma", k)] = g.__enter__()
            gs.append(g)
        blk = nc.Block()
        b = blk.__enter__()
        self.S.emit(b, sems)
        blk.__exit__(None, None, None)
        for g in reversed(gs):
            g.__exit__(None, None, None)
        for g in reversed(self.ctxs):
            g.__exit__(None, None, None)
        return nc


FFN_H = 2816
FFN_NC = 22
ARENA_BYTES = 204 * 1024


def tile_rows(ap, blk, s):
    r0 = blk * TB + s * 128
    return ap[r0:r0 + 128, :]


def alloc_common(P):
    P.xt = [P.alloc([D]) for _ in range(2)]
    P.xr = [P.alloc([D]) for _ in range(2)]
    P.xnT = [P.alloc([KC, TB], BF16) for _ in range(2)]
    P.xs = P.alloc([D], BF16)
    P.junk = P.alloc([D], BF16)
    P.ss = [P.alloc([4]) for _ in range(2)]
    P.rstd = [P.alloc([4]) for _ in range(2)]
    P.xtn = 0
    P.xrn = 0


def norm_stage(P, src, srcname, blk, nidx, slot):
    psT = P.psb[7].bitcast(BF16)
    ss, rstd = P.ss[slot], P.rstd[slot]
    for s in range(4):
        xi = P.xtn % 2
        P.xtn += 1
        xt = P.xt[xi]
        xtv = V(xt, ("xt", xi))
        P.dma("sp", xtv, V(tile_rows(src, blk, s), (srcname, blk, s)), key=("xt", xi))
        P.act(V(P.junk, "junk"), xtv, AF.Square, accum=V(ss[:, s:s + 1], ("ss", slot, s)))
        P.act(V(rstd[:, s:s + 1], ("rstd", slot, s)), V(ss[:, s:s + 1], ("ss", slot, s)), AF.Sqrt,
              scale=1.0 / D, bias=V(P.epsv, "epsv"))
        P.recip(V(rstd[:, s:s + 1], ("rstd", slot, s)), V(rstd[:, s:s + 1], ("rstd", slot, s)))
        P.ts("dve", V(P.xs, "xs"), xtv, V(rstd[:, s:s + 1], ("rstd", slot, s)), ALU.mult)
        for kc in range(KC):
            P.transpose(V(psT[:, kc * 128:(kc + 1) * 128], ("psb", 7)), V(P.xs[:, kc * 128:(kc + 1) * 128], "xs"),
                        V(P.ident, "ident"))
        P.tt("dve", V(P.xnT[slot][:, :, s * 128:(s + 1) * 128], ("xnT", slot, s)),
             V(psT.rearrange("p (k t) -> p k t", k=KC), ("psb", 7)),
             V(P.normw[:, nidx, :].unsqueeze(2).broadcast_to([128, KC, 128]), "normw"), ALU.mult)


def out_stage(P, blk, srcR, srcRname, dst, dstname, lhs_fn, nk, wo, wotok):
    for s in range(4):
        xi = P.xrn % 2
        P.xrn += 1
        xr = P.xr[xi]
        xrv = V(xr, ("xr", xi))
        P.dma("sp", xrv, V(tile_rows(srcR, blk, s), (srcRname, blk, s)), key=("xr", xi))
        for half in range(2):
            b = 3 + (2 * s + half) % 2
            pd = V(P.psb[b], ("psb", b))
            for k in range(nk):
                P.mm(pd, lhs_fn(k, s), V(wo[:, k, half * 512:(half + 1) * 512], wotok),
                     start=(k == 0), stop=(k == nk - 1))
            xh = V(xr[:, half * 512:(half + 1) * 512], ("xr", xi))
            P.tt("dve", xh, pd, xh, ALU.add)
        P.dma("sp", V(tile_rows(dst, blk, s), (dstname, blk, s)), xrv, key=("xr", xi))


def ffn_pass(P, li, c0, c1, srcN, srcNname, srcR, srcRname, dst, dstname):
    mark = P.arena_off
    alloc_common(P)
    nch = c1 - c0
    ncol = nch * 128
    nblk = P.T // TB
    w_up = P.win("ffn_w_up_%d" % li, [D, 2 * FFN_H])
    w_dn = P.win("ffn_w_down_%d" % li, [FFN_H, D])
    c_cw = P.win("c_ffn_cw_%d" % li, [128, 2 * FFN_NC, 3])
    c_cb = P.win("c_ffn_cb_%d" % li, [128, 2 * FFN_NC])
    wupv = P.alloc([KC, ncol], BF16)
    wupg = P.alloc([KC, ncol], BF16)
    wdn = P.alloc([nch, D], BF16)
    cw = P.alloc([2 * FFN_NC, 3])
    cb = P.alloc([2 * FFN_NC])
    spill = P.alloc([2 * FFN_NC, 2])
    A = [P.alloc([TB + 2]) for _ in range(4)]
    G = [P.alloc([TB]) for _ in range(2)]
    hid = P.alloc([nch, TB], BF16)
    upsrc = w_up.rearrange("(k p) n -> p k n", p=128)
    P.dma("pool", V(wupv, ("w", 0)), V(upsrc[:, :, c0 * 128:c1 * 128], "in_w"), key=("w", 0))
    P.dma("pool", V(wupg, ("w", 1)), V(upsrc[:, :, FFN_H + c0 * 128:FFN_H + c1 * 128], "in_w"), key=("w", 1))
    P.dma("pool", V(wdn, ("w", 2)), V(w_dn[c0 * 128:c1 * 128, :].rearrange("(c p) n -> p c n", p=128), "in_w"),
          key=("w", 2))
    P.dma("sp", V(cw, "cw"), V(c_cw, "in_w"), key="cw")
    P.dma("sp", V(cb, "cb"), V(c_cb, "in_w"), key="cb")
    P.memset("pool", V(spill, "spill"), 0.0)

    def stageB(blk, slot):
        xnT = P.xnT[slot]
        for c in range(nch):
            for part in range(2):
                cp = (c0 + c) + part * FFN_NC
                wsel = wupv if part == 0 else wupg
                wtok = ("w", part)
                bi = (2 * c + part) % 3
                pu = V(P.psb[bi], ("psb", bi))
                for kc in range(KC):
                    P.mm(pu, V(wsel[:, kc, c * 128:(c + 1) * 128], wtok),
                         V(xnT[:, kc, :], ("xnT", slot)), start=(kc == 0), stop=(kc == KC - 1))
                ai = (2 * c + part) % 4
                At = A[ai]
                atok = ("A", ai)
                P.act(V(At[:, 0:TB], atok), pu, AF.Identity,
                      scale=V(cw[:, cp, 2:3], "cw"), bias=V(cb[:, cp:cp + 1], "cb"))
                P.memset("pool", V(At[:, TB:TB + 2], atok), 0.0)
                P.stt(V(At[:, 1:TB + 1], atok), pu, V(cw[:, cp, 1:2], "cw"),
                      V(At[:, 1:TB + 1], atok), ALU.mult, ALU.add)
                P.stt(V(At[:, 2:TB + 2], atok), pu, V(cw[:, cp, 0:1], "cw"),
                      V(At[:, 2:TB + 2], atok), ALU.mult, ALU.add)
                P.tt("pool", V(At[:, 0:2], atok), V(At[:, 0:2], atok), V(spill[:, cp, :], ("spill", cp)), ALU.add)
                P.copy("pool", V(spill[:, cp, :], ("spill", cp)), V(At[:, TB:TB + 2], atok))
                if part == 0:
                    Aval, avtok = At, atok
                else:
                    Gt = G[c % 2]
                    gtok = ("G", c % 2)
                    P.act(V(Gt, gtok), V(At[:, 0:TB], atok), AF.Silu)
                    P.tt("pool", V(hid[:, c, :], ("hid", c)), V(Aval[:, 0:TB], avtok), V(Gt, gtok), ALU.mult)
        out_stage(P, blk, srcR, srcRname, dst, dstname,
                  lambda k, s: V(hid[:, k, s * 128:(s + 1) * 128], ("hid", k)), nch, wdn, ("w", 2))

    nidx = 4 + li
    norm_stage(P, srcN, srcNname, 0, nidx, 0)
    for blk in range(nblk):
        if blk + 1 < nblk:
            norm_stage(P, srcN, srcNname, blk + 1, nidx, (blk + 1) % 2)
        stageB(blk, blk % 2)
    P.arena_reset(mark)


def final_norm(P, src, srcname, dst, dstname):
    mark = P.arena_off
    alloc_common(P)
    nfb = P.alloc([D])
    c_nf = P.win("c_nfb", [D])
    P.dma("sp", V(nfb, "nfb"), V(c_nf.partition_broadcast(128), "in_w"), key="const2")
    nblk = P.T // TB
    n = 0
    for blk in range(nblk):
        for s in range(4):
            xi = n % 2
            n += 1
            xt, xo = P.xt[xi], P.xr[xi]
            xtv = V(xt, ("xt", xi))
            P.dma("sp", xtv, V(tile_rows(src, blk, s), (srcname, blk, s)), key=("xt", xi))
            ssv = V(P.ss[xi][:, 0:1], ("ss", xi))
            rv = V(P.rstd[xi][:, 0:1], ("rstd", xi))
            P.act(V(P.junk, "junk"), xtv, AF.Square, accum=ssv)
            P.act(rv, ssv, AF.Sqrt, scale=1.0 / D, bias=V(P.epsv, "epsv"))
            P.recip(rv, rv)
            P.stt(V(xo, ("xr", xi)), xtv, rv, V(nfb, "nfb"), ALU.mult, ALU.mult)
            P.dma("sp", V(tile_rows(dst, blk, s), (dstname, blk, s)), V(xo, ("xr", xi)), key=("xr", xi))
    P.arena_reset(mark)


RET_H = 4
RET_DK = 256
RET_DV = 512


def retnet_pass(P, h0, srcN, srcNname, srcR, srcRname, dst, dstname):
    mark = P.arena_off
    alloc_common(P)
    nblk = P.T // TB
    T = P.T
    w_in = P.win("ret_w_in_p", [D, 6144])
    w_out = P.win("ret_w_out", [2048, D])
    c_cos = P.win("c_ret_cos", [128, 4096])
    c_sin = P.win("c_ret_sin", [128, 4096])
    c_decT = P.win("c_ret_decT", [128, RET_H, 128])
    c_gl = P.win("c_ret_gl", [RET_H, TB])
    c_kdec = P.win("c_ret_kdec", [128, RET_H])
    wq = P.alloc([KC, 512], BF16)
    wk = P.alloc([KC, 512], BF16)
    wv = P.alloc([KC, 1024], BF16)
    wg = P.alloc([KC, 1024], BF16)
    wo = P.alloc([8, D], BF16)
    cos = P.alloc([TB])
    sin = P.alloc([TB])
    qT = P.alloc([2, 2, TB], BF16)
    kT = P.alloc([2, 2, TB], BF16)
    qg = P.alloc([2, 2, TB], BF16)
    rt = [P.alloc([TB]) for _ in range(4)]
    vt = P.alloc([4, 2, 512], BF16)
    khat = P.alloc([4, 2, 256], BF16)
    sg = P.alloc([8, TB], BF16)
    yT = P.alloc([8, TB], BF16)
    S = P.alloc([2, 2, 512])
    Sbf = P.alloc([2, 2, 512], BF16)
    decT = P.alloc([RET_H, 128])
    gl = P.alloc([2, TB])
    kdec = P.alloc([RET_H])
    PT = [P.alloc([128], BF16) for _ in range(2)]
    ysq = [P.alloc([512], BF16) for _ in range(2)]
    rs = [P.alloc([128]) for _ in range(2)]
    tmp = [P.alloc([4, 128]) for _ in range(2)]
    src = w_in.rearrange("(k p) n -> p k n", p=128)
    P.dma("pool", V(wq, ("w", 0)), V(src[:, :, h0 * 256:h0 * 256 + 512], "in_w"), key=("w", 0))
    P.dma("pool", V(wk, ("w", 1)), V(src[:, :, 1024 + h0 * 256:1024 + h0 * 256 + 512], "in_w"), key=("w", 1))
    P.dma("pool", V(wv, ("w", 2)), V(src[:, :, 2048 + h0 * 512:2048 + h0 * 512 + 1024], "in_w"), key=("w", 2))
    P.dma("pool", V(wg, ("w", 3)), V(src[:, :, 4096 + h0 * 512:4096 + h0 * 512 + 1024], "in_w"), key=("w", 3))
    P.dma("pool", V(wo, ("w", 4)), V(w_out[h0 * 512:h0 * 512 + 1024, :].rearrange("(c p) n -> p c n", p=128), "in_w"),
          key=("w", 4))
    P.dma("sp", V(decT, "decT"), V(c_decT, "in_w"), key="c0")
    P.dma("sp", V(kdec, "kdec"), V(c_kdec, "in_w"), key="c1")
    for hl in range(2):
        P.dma("sp", V(gl[:, hl, :], ("gl", hl)), V(c_gl[h0 + hl].partition_broadcast(128), "in_w"), key=("c2", hl))
    P.memset("dve", V(S, "S"), 0.0)
    P.memset("pool", V(Sbf, "Sbf"), 0.0)
    g128 = [float((1.0 - 2.0 ** (-5.0 - (h0 + hl))) ** 128) for hl in range(2)]
    pn = [0]

    def pbank():
        b = pn[0] % 3
        pn[0] += 1
        return V(P.psb[b], ("psb", b))

    def stageB(blk, slot):
        xnT = P.xnT[slot]
        xv = V(xnT, ("xnT", slot))
        P.dma("sp", V(cos, "cos"), V(c_cos[:, blk * TB:(blk + 1) * TB], "in_w"), key="cos")
        P.dma("sp", V(sin, "sin"), V(c_sin[:, blk * TB:(blk + 1) * TB], "in_w"), key="sin")
        for (wsel, wtok, dstT, dname) in ((wq, ("w", 0), qT, "qT"), (wk, ("w", 1), kT, "kT")):
            for hl in range(2):
                p1 = pbank()
                for kc in range(KC):
                    P.mm(p1, V(wsel[:, kc, hl * 256:hl * 256 + 128], wtok), V(xnT[:, kc, :], ("xnT", slot)),
                         start=(kc == 0), stop=(kc == KC - 1))
                p2 = pbank()
                for kc in range(KC):
                    P.mm(p2, V(wsel[:, kc, hl * 256 + 128:hl * 256 + 256], wtok), V(xnT[:, kc, :], ("xnT", slot)),
                         start=(kc == 0), stop=(kc == KC - 1))
                r = [V(rt[i], ("rt", i)) for i in range(4)]
                P.tt("dve", r[0], p1, V(cos, "cos"), ALU.mult)
                P.tt("dve", r[1], p2, V(sin, "sin"), ALU.mult)
                P.tt("dve", r[2], p2, V(cos, "cos"), ALU.mult)
                P.tt("dve", r[3], p1, V(sin, "sin"), ALU.mult)
                P.tt("pool", V(dstT[:, hl, 0, :], (dname, hl, 0)), r[0], r[1], ALU.subtract)
                P.tt("pool", V(dstT[:, hl, 1, :], (dname, hl, 1)), r[2], r[3], ALU.add)
                if dname == "qT":
                    for e in range(2):
                        P.tt("pool", V(qg[:, hl, e, :], ("qg", hl, e)), V(qT[:, hl, e, :], ("qT", hl, e)),
                             V(gl[:, hl, :], ("gl", hl)), ALU.mult)
        for hl in range(2):
            for j in range(4):
                pg = pbank()
                c = hl * 512 + j * 128
                for kc in range(KC):
                    P.mm(pg, V(wg[:, kc, c:c + 128], ("w", 3)), V(xnT[:, kc, :], ("xnT", slot)),
                         start=(kc == 0), stop=(kc == KC - 1))
                P.act(V(sg[:, hl * 4 + j, :], ("sg", hl, j)), pg, AF.Silu)
        for c4 in range(4):
            for hl in range(2):
                pv = pbank()
                for kc in range(KC):
                    P.mm(pv, V(xnT[:, kc, c4 * 128:(c4 + 1) * 128], ("xnT", slot)),
                         V(wv[:, kc, hl * 512:(hl + 1) * 512], ("w", 2)), start=(kc == 0), stop=(kc == KC - 1))
                P.copy("act", V(vt[:, c4, hl, :], ("vt", c4, hl)), pv)
        psT6 = P.psb[6].bitcast(BF16)
        for c4 in range(4):
            for hl in range(2):
                for e in range(2):
                    P.transpose(V(psT6[:, e * 128:(e + 1) * 128], ("psb", 6)),
                                V(kT[:, hl, e, c4 * 128:(c4 + 1) * 128], ("kT", hl, e)), V(P.ident, "ident"))
                P.act(V(khat[:, c4, hl, :], ("khat", c4, hl)), V(psT6[:, 0:256], ("psb", 6)), AF.Identity,
                      scale=V(kdec[:, h0 + hl:h0 + hl + 1], "kdec"))
        n = 0
        for c4 in range(4):
            sl = slice(c4 * 128, (c4 + 1) * 128)
            for hl in range(2):
                h = h0 + hl
                i2 = n % 2
                n += 1
                psS = V(P.psb[3][:, 0:128], ("psb", 3))
                for e in range(2):
                    P.mm(psS, V(kT[:, hl, e, sl], ("kT", hl, e)), V(qT[:, hl, e, sl], ("qT", hl, e)),
                         start=(e == 0), stop=(e == 1))
                ptv = V(PT[i2], ("PT", i2))
                P.tt("dve", ptv, psS, V(decT[:, h, :], "decT"), ALU.mult)
                psO = P.psb[4]
                for j in range(4):
                    po = V(psO[:, j * 128:(j + 1) * 128], ("psb", 4))
                    P.mm(po, V(vt[:, c4, hl, j * 128:(j + 1) * 128], ("vt", c4, hl)), ptv, start=True, stop=False)
                    for e in range(2):
                        P.mm(po, V(Sbf[:, hl, e, j * 128:(j + 1) * 128], ("Sbf", hl, e)),
                             V(qg[:, hl, e, sl], ("qg", hl, e)), start=False, stop=(e == 1))
                pov = V(psO, ("psb", 4))
                yq = V(ysq[i2], ("ysq", i2))
                P.act(yq, pov, AF.Square)
                psN = V(P.psb[3][:, 128:256], ("psb", 3))
                for j in range(4):
                    P.mm(psN, V(P.ones, "ones"), V(ysq[i2][:, j * 128:(j + 1) * 128], ("ysq", i2)),
                         start=(j == 0), stop=(j == 3))
                rv = V(rs[i2], ("rs", i2))
                P.act(rv, psN, AF.Sqrt, scale=1.0 / RET_DV, bias=V(P.epsv, "epsv"))
                P.recip(rv, rv)
                tv = V(tmp[i2], ("tmp", i2))
                P.tt("dve", tv, V(psO.rearrange("p (j l) -> p j l", j=4), ("psb", 4)),
                     V(rs[i2].unsqueeze(1).broadcast_to([128, 4, 128]), ("rs", i2)), ALU.mult)
                P.tt("pool", V(yT[:, hl * 4:(hl + 1) * 4, sl], ("yT", hl, c4)), tv,
                     V(sg[:, hl * 4:(hl + 1) * 4, sl], ("sg", hl)), ALU.mult)
                for e in range(2):
                    pu = V(P.psb[5], ("psb", 5))
                    P.mm(pu, V(khat[:, c4, hl, e * 128:(e + 1) * 128], ("khat", c4, hl)),
                         V(vt[:, c4, hl, :], ("vt", c4, hl)), start=True, stop=True)
                    sv = V(S[:, hl, e, :], ("S", hl, e))
                    P.stt(sv, sv, g128[hl], pu, ALU.mult, ALU.add)
                    P.copy("pool", V(Sbf[:, hl, e, :], ("Sbf", hl, e)), sv)
        out_stage(P, blk, srcR, srcRname, dst, dstname,
                  lambda k, s: V(yT[:, k, s * 128:(s + 1) * 128], ("yT",)), 8, wo, ("w", 4))

    nidx = 3
    norm_stage(P, srcN, srcNname, 0, nidx, 0)
    for blk in range(nblk):
        if blk + 1 < nblk:
            norm_stage(P, srcN, srcNname, blk + 1, nidx, (blk + 1) % 2)
        stageB(blk, blk % 2)
    P.arena_reset(mark)


GLA_H = 4
GLA_DK = 128
GLA_DV = 256


def gla_pass(P, srcN, srcNname, srcR, srcRname, dst, dstname):
    mark = P.arena_off
    alloc_common(P)
    nblk = P.T // TB
    w_in = P.win("gla_w_in", [D, 3088])
    w_out = P.win("gla_w_out", [D, D])
    w_gk2 = P.win("gla_w_gk2", [16, 512])
    c_bgk = P.win("c_gla_bgk", [128, GLA_H])
    c_nw = P.win("c_gla_nw", [128, 2])
    c_maskT = P.win("c_maskT", [128, 128])
    c_scanm = P.win("c_scanm", [TB])
    wq = P.alloc([KC, 512], BF16)
    wk = P.alloc([KC, 512], BF16)
    wv = P.alloc([KC, 1024], BF16)
    wg = P.alloc([KC, 1024], BF16)
    wgk = P.alloc([KC, 16], BF16)
    wo = P.alloc([8, D], BF16)
    wgk2 = P.alloc([512])
    bgk = P.alloc([GLA_H])
    nbgk = P.alloc([GLA_H])
    nw = P.alloc([2])
    maskT = P.alloc([128])
    scanm = P.alloc([TB])
    gkf = P.alloc([TB])
    Gp = P.alloc([GLA_H, TB])
    et = [P.alloc([TB]) for _ in range(2)]
    qT = P.alloc([GLA_H, TB], BF16)
    kT = P.alloc([GLA_H, TB], BF16)
    vt = P.alloc([4, 1024], BF16)
    khat = P.alloc([4, GLA_H, 128], BF16)
    sg = P.alloc([8, TB], BF16)
    yT = P.alloc([8, TB], BF16)
    S = P.alloc([GLA_H, 256])
    Sbf = P.alloc([GLA_H, 256], BF16)
    elast = P.alloc([GLA_H, 4])
    PT = [P.alloc([128], BF16) for _ in range(2)]
    ysq = [P.alloc([256], BF16) for _ in range(2)]
    rs = [P.alloc([128]) for _ in range(2)]
    tmp = [P.alloc([2, 128]) for _ in range(2)]
    src = w_in.rearrange("(k p) n -> p k n", p=128)
    P.dma("pool", V(wq, ("w", 0)), V(src[:, :, 0:512], "in_w"), key=("w", 0))
    P.dma("pool", V(wk, ("w", 1)), V(src[:, :, 512:1024], "in_w"), key=("w", 1))
    P.dma("pool", V(wv, ("w", 2)), V(src[:, :, 1024:2048], "in_w"), key=("w", 2))
    P.dma("pool", V(wg, ("w", 3)), V(src[:, :, 2048:3072], "in_w"), key=("w", 3))
    P.dma("pool", V(wgk, ("w", 5)), V(src[:, :, 3072:3088], "in_w"), key=("w", 5))
    P.dma("pool", V(wo, ("w", 4)), V(w_out.rearrange("(c p) n -> p c n", p=128), "in_w"), key=("w", 4))
    P.dma("sp", V(wgk2[0:16, :], "wgk2"), V(w_gk2, "in_w"), key="c0")
    P.dma("sp", V(bgk, "bgk"), V(c_bgk, "in_w"), key="c1")
    P.dma("sp", V(nw, "nw"), V(c_nw, "in_w"), key="c2")
    P.dma("sp", V(maskT, "maskT"), V(c_maskT, "in_w"), key="c3")
    P.dma("sp", V(scanm, "scanm"), V(c_scanm.partition_broadcast(128), "in_w"), key="c4")
    P.ts("dve", V(nbgk, "nbgk"), V(bgk, "bgk"), -1.0, ALU.mult)
    P.memset("dve", V(S, "S"), 0.0)
    P.memset("pool", V(Sbf, "Sbf"), 0.0)
    lnsc = float(np.log(GLA_DK ** -0.5))
    pn = [0]

    def pbank():
        b = pn[0] % 3
        pn[0] += 1
        return V(P.psb[b], ("psb", b))

    def stageB(blk, slot):
        xnT = P.xnT[slot]
        xtok = ("xnT", slot)
        pg = pbank()
        for kc in range(KC):
            P.mm(V(pg.ap[0:16, :], pg.tok), V(wgk[:, kc, :], ("w", 5)), V(xnT[:, kc, :], xtok),
                 start=(kc == 0), stop=(kc == KC - 1))
        P.copy("act", V(gkf[0:16, :], "gkf"), V(pg.ap[0:16, :], pg.tok))
        for h in range(GLA_H):
            pp = pbank()
            P.mm(pp, V(wgk2[0:16, h * 128:(h + 1) * 128], "wgk2"), V(gkf[0:16, :], "gkf"), start=True, stop=True)
            e0 = V(et[0], ("et", 0))
            P.act(e0, pp, AF.Exp, scale=-1.0, bias=V(nbgk[:, h:h + 1], "nbgk"))
            P.act(e0, e0, AF.Ln, scale=1.0, bias=1.0)
            gph = V(Gp[:, h, :], ("Gp", h))
            nc = P.nc
            P.S.op("dve", (lambda o=gph.ap, a=scanm, b=et[0]: nc.vector.tensor_tensor_scan(o, a, b, 0.0, ALU.mult, ALU.add)),
                   reads=[("scanm",), ("et", 0)], writes=[gph.tok])
            pq = pbank()
            for kc in range(KC):
                P.mm(pq, V(wq[:, kc, h * 128:(h + 1) * 128], ("w", 0)), V(xnT[:, kc, :], xtok),
                     start=(kc == 0), stop=(kc == KC - 1))
            e1 = V(et[1], ("et", 1))
            P.act(e1, gph, AF.Exp, scale=-1.0 / 16.0, bias=lnsc)
            P.tt("dve", V(qT[:, h, :], ("qT", h)), pq, e1, ALU.mult)
            pk = pbank()
            for kc in range(KC):
                P.mm(pk, V(wk[:, kc, h * 128:(h + 1) * 128], ("w", 1)), V(xnT[:, kc, :], xtok),
                     start=(kc == 0), stop=(kc == KC - 1))
            P.act(e1, gph, AF.Exp, scale=1.0 / 16.0)
            P.tt("dve", V(kT[:, h, :], ("kT", h)), pk, e1, ALU.mult)
            P.act(V(elast[:, h, :], ("elast", h)),
                  V(Gp[:, h, :].rearrange("p (c l) -> p c l", c=4)[:, :, 127], ("Gp", h)), AF.Exp, scale=-1.0 / 16.0)
        for c in range(8):
            pg2 = pbank()
            for kc in range(KC):
                P.mm(pg2, V(wg[:, kc, c * 128:(c + 1) * 128], ("w", 3)), V(xnT[:, kc, :], xtok),
                     start=(kc == 0), stop=(kc == KC - 1))
            P.act(V(sg[:, c, :], ("sg", c)), pg2, AF.Silu)
            P.ts("pool", V(sg[:, c, :], ("sg", c)), V(sg[:, c, :], ("sg", c)), V(nw[:, (c % 2):(c % 2) + 1], "nw"), ALU.mult)
        for c4 in range(4):
            for half in range(2):
                pv = pbank()
                for kc in range(KC):
                    P.mm(pv, V(xnT[:, kc, c4 * 128:(c4 + 1) * 128], xtok),
                         V(wv[:, kc, half * 512:(half + 1) * 512], ("w", 2)), start=(kc == 0), stop=(kc == KC - 1))
                P.copy("act", V(vt[:, c4, half * 512:(half + 1) * 512], ("vt", c4, half)), pv)
        psT6 = P.psb[6].bitcast(BF16)
        for c4 in range(4):
            for h in range(GLA_H):
                P.transpose(V(psT6[:, h * 128:(h + 1) * 128], ("psb", 6)),
                            V(kT[:, h, c4 * 128:(c4 + 1) * 128], ("kT", h)), V(P.ident, "ident"))
            P.copy("act", V(khat[:, c4, :, :], ("khat", c4)),
                   V(psT6[:, 0:512].rearrange("p (h d) -> p h d", h=GLA_H), ("psb", 6)))
        n = 0
        for c4 in range(4):
            sl = slice(c4 * 128, (c4 + 1) * 128)
            for h in range(GLA_H):
                i2 = n % 2
                n += 1
                psS = V(P.psb[3][:, 0:128], ("psb", 3))
                P.mm(psS, V(kT[:, h, sl], ("kT", h)), V(qT[:, h, sl], ("qT", h)), start=True, stop=True)
                ptv = V(PT[i2], ("PT", i2))
                P.tt("dve", ptv, psS, V(maskT, "maskT"), ALU.mult)
                psO = P.psb[4]
                for j in range(2):
                    po = V(psO[:, j * 128:(j + 1) * 128], ("psb", 4))
                    vc = h * 256 + j * 128
                    P.mm(po, V(vt[:, c4, vc:vc + 128], ("vt", c4, vc // 512)), ptv, start=True, stop=False)
                    P.mm(po, V(Sbf[:, h, j * 128:(j + 1) * 128], ("Sbf", h)), V(qT[:, h, sl], ("qT", h)),
                         start=False, stop=True)
                pov = V(psO[:, 0:256], ("psb", 4))
                yq = V(ysq[i2], ("ysq", i2))
                P.act(yq, pov, AF.Square)
                psN = V(P.psb[3][:, 128:256], ("psb", 3))
                for j in range(2):
                    P.mm(psN, V(P.ones, "ones"), V(ysq[i2][:, j * 128:(j + 1) * 128], ("ysq", i2)),
                         start=(j == 0), stop=(j == 1))
                rv = V(rs[i2], ("rs", i2))
                P.act(rv, psN, AF.Sqrt, scale=1.0 / GLA_DV, bias=V(P.epsv, "epsv"))
                P.recip(rv, rv)
                tv = V(tmp[i2], ("tmp", i2))
                P.tt("dve", tv, V(psO[:, 0:256].rearrange("p (j l) -> p j l", j=2), ("psb", 4)),
                     V(rs[i2].unsqueeze(1).broadcast_to([128, 2, 128]), ("rs", i2)), ALU.mult)
                P.tt("pool", V(yT[:, h * 2:(h + 1) * 2, sl], ("yT", h, c4)), tv,
                     V(sg[:, h * 2:(h + 1) * 2, sl], ("sg",)), ALU.mult)
                pu = V(P.psb[5][:, 0:256], ("psb", 5))
                P.mm(pu, V(khat[:, c4, h, :], ("khat", c4)), V(vt[:, c4, h * 256:(h + 1) * 256], ("vt", c4, h // 2)),
                     start=True, stop=True)
                sv = V(S[:, h, :], ("S", h))
                P.tt("dve", sv, sv, pu, ALU.add)
                P.ts("dve", sv, sv, V(elast[:, h, c4:c4 + 1], ("elast", h)), ALU.mult)
                P.copy("pool", V(Sbf[:, h, :], ("Sbf", h)), sv)
        out_stage(P, blk, srcR, srcRname, dst, dstname,
                  lambda k, s: V(yT[:, k, s * 128:(s + 1) * 128], ("yT",)), 8, wo, ("w", 4))

    nidx = 2
    norm_stage(P, srcN, srcNname, 0, nidx, 0)
    for blk in range(nblk):
        if blk + 1 < nblk:
            norm_stage(P, srcN, srcNname, blk + 1, nidx, (blk + 1) % 2)
        stageB(blk, blk % 2)
    P.arena_reset(mark)


def ssd_pass(P, p, srcN, srcNname, srcR, srcRname, dst, dstname):
    mark = P.arena_off
    alloc_common(P)
    nc = P.nc
    nblk = P.T // TB
    w_in = P.win("ssd_w_in", [D, 6176])
    w_out = P.win("ssd_w_out", [2048, D])
    c_cw = P.win("c_ssd_cw", [128, 32, 4])
    c_cb = P.win("c_ssd_cb", [128, 32])
    c_dtb = P.win("ssd_dt_bias", [32])
    c_alog = P.win("ssd_a_log", [32])
    c_dsk = P.win("ssd_d", [32])
    c_nw = P.win("ssd_norm_w", [2048])
    c_maskT = P.win("c_maskT", [128, 128])
    c_SU = P.win("c_SU", [128, 128])
    wz = P.alloc([KC, 1024], BF16)
    wxs = P.alloc([KC, 1024], BF16)
    wB = P.alloc([KC, 512], BF16)
    wC = P.alloc([KC, 512], BF16)
    wdt = P.alloc([KC, 16], BF16)
    wo = P.alloc([8, D], BF16)
    cw = P.alloc([32, 4])
    cb = P.alloc([32])
    spill = P.alloc([16, 3])
    dtb = P.alloc([16])
    abc = P.alloc([16])
    dsk = P.alloc([16])
    nwc = P.alloc([1024])
    maskT = P.alloc([128])
    SU = P.alloc([128])
    onesf = P.alloc([128])
    A = [P.alloc([TB + 3]) for _ in range(3)]
    xsT = P.alloc([8, TB], BF16)
    kT = P.alloc([4, TB], BF16)
    qT = P.alloc([4, TB], BF16)
    sz = P.alloc([4, 1024], BF16)
    dtt = P.alloc([16])
    ld = P.alloc([16])
    Gs = P.alloc([16])
    eG = P.alloc([16])
    eGl = P.alloc([16])
    wdec = P.alloc([16])
    tiny = P.alloc([16])
    R = [P.alloc([4, 128]) for _ in range(2)]
    dec = [P.alloc([4, 128]) for _ in range(2)]
    sm = [P.alloc([128]) for _ in range(2)]
    PT = [P.alloc([4, 128], BF16) for _ in range(2)]
    xk = [P.alloc([384], BF16) for _ in range(2)]
    vv = [P.alloc([256], BF16) for _ in range(2)]
    vh = [P.alloc([256], BF16) for _ in range(2)]
    ot = [P.alloc([256]) for _ in range(2)]
    t2 = [P.alloc([256]) for _ in range(2)]
    yv = [P.alloc([256]) for _ in range(2)]
    yn = [P.alloc([256], BF16) for _ in range(2)]
    ssq = [P.alloc([1]) for _ in range(2)]
    yT = P.alloc([8, TB], BF16)
    S = P.alloc([4, 256])
    Sbf = P.alloc([4, 256], BF16)
    src = w_in.rearrange("(k p) n -> p k n", p=128)
    P.dma("pool", V(wz, ("w", 0)), V(src[:, :, p * 1024:(p + 1) * 1024], "in_w"), key=("w", 0))
    P.dma("pool", V(wxs, ("w", 1)), V(src[:, :, 2048 + p * 1024:2048 + (p + 1) * 1024], "in_w"), key=("w", 1))
    P.dma("pool", V(wB, ("w", 2)), V(src[:, :, 4096 + p * 512:4096 + (p + 1) * 512], "in_w"), key=("w", 2))
    P.dma("pool", V(wC, ("w", 3)), V(src[:, :, 5120 + p * 512:5120 + (p + 1) * 512], "in_w"), key=("w", 3))
    P.dma("pool", V(wdt, ("w", 5)), V(src[:, :, 6144 + p * 16:6144 + (p + 1) * 16], "in_w"), key=("w", 5))
    P.dma("pool", V(wo, ("w", 4)), V(w_out[p * 1024:(p + 1) * 1024, :].rearrange("(c p) n -> p c n", p=128), "in_w"),
          key=("w", 4))
    P.dma("sp", V(cw, "cw"), V(c_cw, "in_w"), key="c0")
    P.dma("sp", V(cb, "cb"), V(c_cb, "in_w"), key="c1")
    P.dma("sp", V(dtb, "dtb"), V(c_dtb[p * 16:(p + 1) * 16].partition_broadcast(128), "in_w"), key="c2")
    P.dma("sp", V(abc, "abc"), V(c_alog[p * 16:(p + 1) * 16].partition_broadcast(128), "in_w"), key="c3")
    P.dma("sp", V(dsk, "dsk"), V(c_dsk[p * 16:(p + 1) * 16].partition_broadcast(128), "in_w"), key="c4")
    P.dma("sp", V(nwc, "nwc"), V(c_nw[p * 1024:(p + 1) * 1024].partition_broadcast(128), "in_w"), key="c5")
    P.dma("sp", V(maskT, "maskT"), V(c_maskT, "in_w"), key="c6")
    P.dma("sp", V(SU, "SU"), V(c_SU, "in_w"), key="c7")
    P.memset("pool", V(onesf, "onesf"), 1.0)
    P.memset("pool", V(spill, "spill"), 0.0)
    P.act(V(abc, "abc"), V(abc, "abc"), AF.Exp)
    P.ts("dve", V(abc, "abc"), V(abc, "abc"), -1.0, ALU.mult)
    P.memset("dve", V(S, "S"), 0.0)
    P.memset("pool", V(Sbf, "Sbf"), 0.0)
    pn = [0]

    def pbank():
        b = pn[0] % 3
        pn[0] += 1
        return V(P.psb[b], ("psb", b))

    def conv_chunk(lc, wsel, wtok, col, xnT, slot, dst):
        cc = (8 * p + lc) if lc < 8 else ((16 + 4 * p + lc - 8) if lc < 12 else (24 + 4 * p + lc - 12))
        pu = pbank()
        for kc in range(KC):
            P.mm(pu, V(wsel[:, kc, col:col + 128], wtok), V(xnT[:, kc, :], ("xnT", slot)),
                 start=(kc == 0), stop=(kc == KC - 1))
        ai = lc % 3
        At, atok = A[ai], ("A", ai)
        P.act(V(At[:, 0:TB], atok), pu, AF.Identity, scale=V(cw[:, cc, 3:4], "cw"), bias=V(cb[:, cc:cc + 1], "cb"))
        P.memset("pool", V(At[:, TB:TB + 3], atok), 0.0)
        for sh in (1, 2, 3):
            P.stt(V(At[:, sh:TB + sh], atok), pu, V(cw[:, cc, 3 - sh:4 - sh], "cw"), V(At[:, sh:TB + sh], atok),
                  ALU.mult, ALU.add)
        P.tt("pool", V(At[:, 0:3], atok), V(At[:, 0:3], atok), V(spill[:, lc, :], ("spill", lc)), ALU.add)
        P.copy("pool", V(spill[:, lc, :], ("spill", lc)), V(At[:, TB:TB + 3], atok))
        P.act(dst, V(At[:, 0:TB], atok), AF.Silu)

    def stageB(blk, slot):
        xnT = P.xnT[slot]
        xtok = ("xnT", slot)
        for lc in range(8):
            conv_chunk(lc, wxs, ("w", 1), lc * 128, xnT, slot, V(xsT[:, lc, :], ("xsT", lc)))
        for gl in range(4):
            conv_chunk(8 + gl, wB, ("w", 2), gl * 128, xnT, slot, V(kT[:, gl, :], ("kT", gl)))
            conv_chunk(12 + gl, wC, ("w", 3), gl * 128, xnT, slot, V(qT[:, gl, :], ("qT", gl)))
        for c4 in range(4):
            for half in range(2):
                pz = pbank()
                for kc in range(KC):
                    P.mm(pz, V(xnT[:, kc, c4 * 128:(c4 + 1) * 128], xtok),
                         V(wz[:, kc, half * 512:(half + 1) * 512], ("w", 0)), start=(kc == 0), stop=(kc == KC - 1))
                P.act(V(sz[:, c4, half * 512:(half + 1) * 512], ("sz", c4, half)), pz, AF.Silu)
        n = 0
        psT6 = P.psb[6].bitcast(BF16)
        for c4 in range(4):
            sl = slice(c4 * 128, (c4 + 1) * 128)
            pdt = V(P.psb[4][:, 128:144], ("psb", 4, "d"))
            for kc in range(KC):
                P.mm(pdt, V(xnT[:, kc, sl], xtok), V(wdt[:, kc, :], ("w", 5)), start=(kc == 0), stop=(kc == KC - 1))
            tn = V(tiny, "tiny")
            P.tt("dve", tn, pdt, V(dtb, "dtb"), ALU.add)
            P.act(tn, tn, AF.Exp)
            P.act(V(dtt, "dtt"), tn, AF.Ln, scale=1.0, bias=1.0)
            P.tt("dve", V(ld, "ld"), V(dtt, "dtt"), V(abc, "abc"), ALU.mult)
            pG = V(P.psb[4][:, 144:160], ("psb", 4, "d"))
            P.mm(pG, V(maskT, "maskT"), V(ld, "ld"), start=True, stop=True)
            pGl = V(P.psb[4][:, 160:176], ("psb", 4, "d"))
            P.mm(pGl, V(onesf, "onesf"), V(ld, "ld"), start=True, stop=True)
            P.act(V(Gs, "Gs"), pG, AF.Identity)
            P.act(V(eG, "eG"), pG, AF.Exp)
            P.act(V(eGl, "eGl"), pGl, AF.Exp)
            P.tt("dve", tn, pGl, V(Gs, "Gs"), ALU.subtract)
            P.act(V(wdec, "wdec"), tn, AF.Exp)
            for gl in range(4):
                i2 = n % 2
                n += 1
                hs = slice(gl * 4, gl * 4 + 4)
                Rv = V(R[i2], ("R", i2))
                P.tt("dve", Rv, V(maskT.unsqueeze(1).broadcast_to([128, 4, 128]), "maskT"),
                     V(ld[:, hs].unsqueeze(2).broadcast_to([128, 4, 128]), "ld"), ALU.mult)
                pSeg = V(P.psb[3], ("psb", 3))
                P.mm(pSeg, V(SU, "SU"), V(R[i2].rearrange("p h l -> p (h l)"), ("R", i2)), start=True, stop=True)
                dv_ = V(dec[i2], ("dec", i2))
                P.act(V(dec[i2].rearrange("p h l -> p (h l)"), ("dec", i2)), pSeg, AF.Exp)
                pS = V(P.psb[4][:, 0:128], ("psb", 4, "s"))
                P.mm(pS, V(kT[:, gl, sl], ("kT", gl)), V(qT[:, gl, sl], ("qT", gl)), start=True, stop=True)
                smv = V(sm[i2], ("sm", i2))
                P.tt("dve", smv, pS, V(maskT, "maskT"), ALU.mult)
                ptv = V(PT[i2], ("PT", i2))
                P.tt("pool", ptv, dv_, V(sm[i2].unsqueeze(1).broadcast_to([128, 4, 128]), ("sm", i2)), ALU.mult)
                P.transpose(V(psT6[:, 0:128], ("psb", 6, "a")), V(xsT[:, gl * 2, sl], ("xsT", gl * 2)), V(P.ident, "ident"))
                P.transpose(V(psT6[:, 128:256], ("psb", 6, "a")), V(xsT[:, gl * 2 + 1, sl], ("xsT", gl * 2 + 1)),
                            V(P.ident, "ident"))
                P.transpose(V(psT6[:, 256:384], ("psb", 6, "a")), V(kT[:, gl, sl], ("kT", gl)), V(P.ident, "ident"))
                xkv = V(xk[i2], ("xk", i2))
                P.copy("act", xkv, V(psT6[:, 0:384], ("psb", 6, "a")))
                xs4 = V(xk[i2][:, 0:256].rearrange("p (h d) -> p h d", h=4), ("xk", i2))
                v4 = V(vv[i2].rearrange("p (h d) -> p h d", h=4), ("vv", i2))
                P.tt("dve", v4, xs4, V(dtt[:, hs].unsqueeze(2).broadcast_to([128, 4, 64]), "dtt"), ALU.mult)
                vh4 = V(vh[i2].rearrange("p (h d) -> p h d", h=4), ("vh", i2))
                P.tt("pool", vh4, v4, V(wdec[:, hs].unsqueeze(2).broadcast_to([128, 4, 64]), "wdec"), ALU.mult)
                for hh in range(4):
                    P.mm(V(P.psb[5][:, hh * 64:(hh + 1) * 64], ("psb", 5, "a")), V(PT[i2][:, hh, :], ("PT", i2)),
                         V(vv[i2][:, hh * 64:(hh + 1) * 64], ("vv", i2)), start=True, stop=True)
                pB = V(P.psb[5][:, 256:512], ("psb", 5, "b"))
                P.mm(pB, V(qT[:, gl, sl], ("qT", gl)), V(Sbf[:, gl, :], ("Sbf", gl)), start=True, stop=True)
                o4 = V(ot[i2].rearrange("p (h d) -> p h d", h=4), ("ot", i2))
                P.tt("dve", o4, V(P.psb[5][:, 256:512].rearrange("p (h d) -> p h d", h=4), ("psb", 5, "b")),
                     V(eG[:, hs].unsqueeze(2).broadcast_to([128, 4, 64]), "eG"), ALU.mult)
                ov = V(ot[i2], ("ot", i2))
                P.tt("dve", ov, ov, V(P.psb[5][:, 0:256], ("psb", 5, "a")), ALU.add)
                t24 = V(t2[i2].rearrange("p (h d) -> p h d", h=4), ("t2", i2))
                P.tt("pool", t24, xs4, V(dsk[:, hs].unsqueeze(2).broadcast_to([128, 4, 64]), "dsk"), ALU.mult)
                P.tt("pool", ov, ov, V(t2[i2], ("t2", i2)), ALU.add)
                yvv = V(yv[i2], ("yv", i2))
                P.tt("pool", yvv, ov, V(sz[:, c4, gl * 256:(gl + 1) * 256], ("sz", c4, gl // 2)), ALU.mult)
                sq = V(ssq[i2], ("ssq", i2))
                P.act(V(P.junk[:, 0:256], "junk"), yvv, AF.Square, accum=sq)
                P.act(sq, sq, AF.Sqrt, scale=1.0 / 256.0, bias=V(P.epsv, "epsv"))
                P.recip(sq, sq)
                ynv = V(yn[i2], ("yn", i2))
                P.stt(ynv, yvv, sq, V(nwc[:, gl * 256:(gl + 1) * 256], "nwc"), ALU.mult, ALU.mult)
                for j in range(2):
                    P.transpose(V(psT6[:, 512 + j * 128:512 + (j + 1) * 128], ("psb", 6, "b")),
                                V(yn[i2][:, j * 128:(j + 1) * 128], ("yn", i2)), V(P.ident, "ident"))
                P.copy("act", V(yT[:, gl * 2:(gl + 1) * 2, sl], ("yT", gl, c4)),
                       V(psT6[:, 512:768].rearrange("p (j l) -> p j l", j=2), ("psb", 6, "b")))
                pU = V(P.psb[4][:, 256:512], ("psb", 4, "u"))
                P.mm(pU, V(xk[i2][:, 256:384], ("xk", i2)), V(vh[i2], ("vh", i2)), start=True, stop=True)
                s4 = V(S[:, gl, :].rearrange("p (h d) -> p h d", h=4), ("S", gl))
                P.tt("dve", s4, s4, V(eGl[:, hs].unsqueeze(2).broadcast_to([128, 4, 64]), "eGl"), ALU.mult)
                sv = V(S[:, gl, :], ("S", gl))
                P.tt("dve", sv, sv, pU, ALU.add)
                P.copy("pool", V(Sbf[:, gl, :], ("Sbf", gl)), sv)
        out_stage(P, blk, srcR, srcRname, dst, dstname,
                  lambda k, s: V(yT[:, k, s * 128:(s + 1) * 128], ("yT",)), 8, wo, ("w", 4))

    nidx = 0
    norm_stage(P, srcN, srcNname, 0, nidx, 0)
    for blk in range(nblk):
        if blk + 1 < nblk:
            norm_stage(P, srcN, srcNname, blk + 1, nidx, (blk + 1) % 2)
        stageB(blk, blk % 2)
    P.arena_reset(mark)


RW_C = 64
RW_GN_EPS = 64e-5


def rwkv_pass(P, p, srcN, srcNname, srcR, srcRname, dst, dstname):
    mark = P.arena_off
    alloc_common(P)
    nc = P.nc
    nblk = P.T // TB
    NK = 4
    c0 = p * 512
    w_rkv = P.win("rwkv_w_rkv", [3, D, D])
    w_out = P.win("rwkv_w_out", [D, D])
    w1d, a1d, g1d = P.win("rwkv_w1", [D, 64]), P.win("rwkv_a1", [D, 64]), P.win("rwkv_g1", [D, 160])
    w2d, a2d, g2d = P.win("rwkv_w2", [64, D]), P.win("rwkv_a2", [64, D]), P.win("rwkv_g2", [160, D])
    c_vec = P.win("c_rwkv_vec", [128, 12, KC])
    c_lnw, c_lnb = P.win("rwkv_ln_w", [D]), P.win("rwkv_ln_b", [D])
    c_m = P.win("c_rwkv_masks", [64, 4, 64])
    c_E = P.win("c_rwkv_E", [128, 2], BF16)
    c_bo = P.win("c_rwkv_bo", [128, 128], BF16)
    c_scm = P.win("c_rwkv_scanm", [TB])
    Wr = P.alloc([KC, 512], BF16)
    Wk = P.alloc([KC, 512], BF16)
    Wv = P.alloc([KC, 512], BF16)
    wo = P.alloc([NK, D], BF16)
    w1 = P.alloc([KC, 64], BF16)
    a1 = P.alloc([KC, 64], BF16)
    g1 = P.alloc([KC, 160], BF16)
    w2 = P.alloc([512], BF16)
    a2 = P.alloc([512], BF16)
    g2A = P.alloc([512], BF16)
    g2B = P.alloc([512], BF16)
    vec = P.alloc([12, KC])
    omka = P.alloc([KC])
    nw0 = P.alloc([KC])
    lnw = P.alloc([512])
    lnb = P.alloc([512])
    msk = P.alloc([4, 64])
    identb = P.alloc([64], BF16)
    Eh = P.alloc([2], BF16)
    bo = P.alloc([128], BF16)
    scm = P.alloc([TB])
    epsg = P.alloc([1])
    tinyb = P.alloc([1])
    xx = P.alloc([KC, TB], BF16)
    xi = [P.alloc([KC, TB], BF16) for _ in range(2)]
    xlast = P.alloc([KC], BF16)
    h1 = P.alloc([TB], BF16)
    ha = P.alloc([TB], BF16)
    hgA = P.alloc([TB], BF16)
    hgB = P.alloc([TB], BF16)
    aTm = [P.alloc([NK, TB], BF16) for _ in range(2)]
    rTm = [P.alloc([NK, TB], BF16) for _ in range(2)]
    bT = P.alloc([NK, TB], BF16)
    kT = P.alloc([NK, TB], BF16)
    rkT = P.alloc([NK, TB], BF16)
    yT = P.alloc([NK, TB], BF16)
    WC = P.alloc([NK, 8])
    f32t = [P.alloc([TB]) for _ in range(8)]
    NS = 2
    Lm = [P.alloc([8, 64], BF16) for _ in range(2)]
    LTm = [P.alloc([8, 64], BF16) for _ in range(2)]
    XT = [P.alloc([8, 64], BF16) for _ in range(2 + NS)]
    AkT = [P.alloc([8, 64], BF16) for _ in range(NS)]
    ArbT = [P.alloc([8, 64], BF16) for _ in range(NS)]
    ArkT = [P.alloc([8, 64], BF16) for _ in range(NS)]
    Zs = P.alloc([512], BF16)
    Us = P.alloc([512], BF16)
    Vtm = [P.alloc([512], BF16) for _ in range(2)]
    BKtm = P.alloc([2, 512], BF16)
    yc = P.alloc([512])
    sq = P.alloc([512])
    bon = P.alloc([512])
    ytm = P.alloc([512], BF16)
    st8 = [P.alloc([8]) for _ in range(4)]
    S = P.alloc([NK, 64])
    Sbf = P.alloc([NK, 64], BF16)
    wsrc = lambda i_: w_rkv[i_].rearrange("(k p) n -> p k n", p=128)
    P.dma("pool", V(Wr, ("w", 0)), V(wsrc(0)[:, :, c0:c0 + 512], "in_w"), key=("w", 0))
    P.dma("pool", V(Wk, ("w", 1)), V(wsrc(1)[:, :, c0:c0 + 512], "in_w"), key=("w", 1))
    P.dma("pool", V(Wv, ("w", 2)), V(wsrc(2)[:, :, c0:c0 + 512], "in_w"), key=("w", 2))
    P.dma("pool", V(wo, ("w", 3)), V(w_out[c0:c0 + 512, :].rearrange("(c p) n -> p c n", p=128), "in_w"), key=("w", 3))
    P.dma("pool", V(w1, ("w", 4)), V(w1d.rearrange("(k p) n -> p k n", p=128), "in_w"), key=("w", 4))
    P.dma("pool", V(a1, ("w", 5)), V(a1d.rearrange("(k p) n -> p k n", p=128), "in_w"), key=("w", 5))
    P.dma("pool", V(g1, ("w", 6)), V(g1d.rearrange("(k p) n -> p k n", p=128), "in_w"), key=("w", 6))
    P.dma("pool", V(w2[0:64, :], ("w", 7)), V(w2d[:, c0:c0 + 512], "in_w"), key=("w", 7))
    P.dma("pool", V(a2[0:64, :], ("w", 8)), V(a2d[:, c0:c0 + 512], "in_w"), key=("w", 8))
    P.dma("pool", V(g2A, ("w", 9)), V(g2d[0:128, c0:c0 + 512], "in_w"), key=("w", 9))
    P.dma("pool", V(g2B[0:32, :], ("w", 10)), V(g2d[128:160, c0:c0 + 512], "in_w"), key=("w", 10))
    P.dma("sp", V(vec, "vec"), V(c_vec, "in_w"), key="c0")
    P.dma("sp", V(lnw[0:64, :], "lnw"), V(c_lnw[c0:c0 + 512].partition_broadcast(64), "in_w"), key="c1")
    P.dma("sp", V(lnb[0:64, :], "lnb"), V(c_lnb[c0:c0 + 512].partition_broadcast(64), "in_w"), key="c2")
    P.dma("sp", V(msk[0:64, :, :], "msk"), V(c_m, "in_w"), key="c3")
    P.dma("sp", V(Eh, "Eh"), V(c_E, "in_w"), key="c4")
    P.dma("sp", V(bo, "bo"), V(c_bo, "in_w"), key="c5")
    P.dma("sp", V(scm, "scm"), V(c_scm.partition_broadcast(128), "in_w"), key="c6")
    P.ts("dve", V(omka, "omka"), V(vec[:, 9, :], "vec"), -1.0, ALU.mult, 1.0, ALU.add)
    P.ts("dve", V(nw0, "nw0"), V(vec[:, 6, :], "vec"), -1.0, ALU.mult)
    P.copy("dve", V(identb[0:64, :], "identb"), V(msk[0:64, 3, :], "msk"))
    P.memset("pool", V(epsg, "epsg"), RW_GN_EPS)
    P.memset("pool", V(tinyb, "tinyb"), 1e-24)
    P.memset("pool", V(xlast, "xlast"), 0.0)
    P.memset("dve", V(S, "S"), 0.0)
    P.memset("pool", V(Sbf, "Sbf"), 0.0)
    for e_ in range(2):
        P.memset("pool", V(aTm[e_], ("aT", e_)), 0.0)
        P.memset("pool", V(rTm[e_], ("rT", e_)), 0.0)
    pn = [0]

    def pbank():
        b = pn[0] % 3
        pn[0] += 1
        return V(P.psb[b], ("psb", b))

    def mask(i_):
        return V(msk[0:64, i_, :].unsqueeze(1).broadcast_to([64, 8, 64]), "msk")

    def vcol(i_, kc):
        return V(vec[:, i_, kc:kc + 1], "vec")

    def mix(i_, xnT, xtok, buf):
        o = xi[buf]
        for kc in range(KC):
            if kc % 2 == 0:
                P.stt(V(o[:, kc, :], ("xi", buf, kc)), V(xx[:, kc, :], ("xx", kc)), vcol(i_, kc),
                      V(xnT[:, kc, :], xtok), ALU.mult, ALU.add)
            else:
                P.ts("pool", V(o[:, kc, :], ("xi", buf, kc)), V(xx[:, kc, :], ("xx", kc)), vcol(i_, kc), ALU.mult)
                P.tt("pool", V(o[:, kc, :], ("xi", buf, kc)), V(o[:, kc, :], ("xi", buf, kc)), V(xnT[:, kc, :], xtok), ALU.add)
        return o, ("xi", buf)

    def stage1(blk, slot):
        xnT = P.xnT[slot]
        xtok = ("xnT", slot)
        P.tt("dve", V(xx[:, :, 1:TB], "xx"), V(xnT[:, :, 0:TB - 1], xtok), V(xnT[:, :, 1:TB], xtok), ALU.subtract)
        P.tt("dve", V(xx[:, :, 0:1], "xx"), V(xlast.unsqueeze(2), "xlast"), V(xnT[:, :, 0:1], xtok), ALU.subtract)
        P.copy("pool", V(xlast.unsqueeze(2), "xlast"), V(xnT[:, :, TB - 1:TB], xtok))
        xw, xwtok = mix(1, xnT, xtok, 0)
        pw = pbank()
        for kc in range(KC):
            P.mm(V(pw.ap[0:64, :], pw.tok), V(w1[:, kc, :], ("w", 4)), V(xw[:, kc, :], xwtok), start=(kc == 0), stop=(kc == KC - 1))
        P.act(V(h1[0:64, :], "h1"), V(pw.ap[0:64, :], pw.tok), AF.Tanh)
        xa, xatok = mix(4, xnT, xtok, 1)
        pa = pbank()
        for kc in range(KC):
            P.mm(V(pa.ap[0:64, :], pa.tok), V(a1[:, kc, :], ("w", 5)), V(xa[:, kc, :], xatok), start=(kc == 0), stop=(kc == KC - 1))
        P.copy("act", V(ha[0:64, :], "ha"), V(pa.ap[0:64, :], pa.tok))
        xg, xgtok = mix(5, xnT, xtok, 0)
        pg = pbank()
        for kc in range(KC):
            P.mm(pg, V(g1[:, kc, 0:128], ("w", 6)), V(xg[:, kc, :], xgtok), start=(kc == 0), stop=(kc == KC - 1))
        P.act(V(hgA, "hgA"), pg, AF.Sigmoid)
        pg = pbank()
        for kc in range(KC):
            P.mm(V(pg.ap[0:32, :], pg.tok), V(g1[:, kc, 128:160], ("w", 6)), V(xg[:, kc, :], xgtok), start=(kc == 0), stop=(kc == KC - 1))
        P.act(V(hgB[0:32, :], "hgB"), V(pg.ap[0:32, :], pg.tok), AF.Sigmoid)
        xk_, xktok = mix(2, xnT, xtok, 1)
        xr_, xrtok = mix(0, xnT, xtok, 0)
        for kc in range(NK):
            gk = 4 * p + kc
            cs = slice(kc * 128, (kc + 1) * 128)
            t = [V(f32t[j], ("f32t", j)) for j in range(8)]
            pz = pbank()
            P.mm(pz, V(w2[0:64, cs], ("w", 7)), V(h1[0:64, :], "h1"), start=True, stop=True)
            P.act(t[0], pz, AF.Exp, scale=-1.0, bias=V(nw0[:, gk:gk + 1], "nw0"))
            P.act(t[0], t[0], AF.Ln, scale=1.0, bias=1.0)
            P.act(t[0], t[0], AF.Exp, scale=-1.0, bias=-0.5)
            P.S.op("dve", (lambda o=f32t[1], a_=scm, b_=f32t[0]: nc.vector.tensor_tensor_scan(o, a_, b_, 0.0, ALU.mult, ALU.add)),
                   reads=[("scm",), ("f32t", 0)], writes=[("f32t", 1)])
            P.tt("pool", t[2], t[1], t[0], ALU.subtract)
            P.act(t[2], t[2], AF.Exp, scale=-1.0)
            P.act(t[3], t[1], AF.Exp, scale=1.0)
            P.act(t[1], t[1], AF.Exp, scale=-1.0)
            P.copy("pool", V(WC[:, kc, :], ("WC", kc)), V(f32t[1].rearrange("p (c l) -> p c l", c=8)[:, :, RW_C - 1], ("f32t", 1)))
            pa2 = pbank()
            P.mm(pa2, V(a2[0:64, cs], ("w", 8)), V(ha[0:64, :], "ha"), start=True, stop=True)
            P.act(t[4], pa2, AF.Sigmoid, scale=1.0, bias=vcol(7, gk))
            pk = pbank()
            for k8 in range(KC):
                P.mm(pk, V(Wk[:, k8, cs], ("w", 1)), V(xk_[:, k8, :], xktok), start=(k8 == 0), stop=(k8 == KC - 1))
            P.ts("dve", t[5], pk, vcol(8, gk), ALU.mult)
            P.act(V(P.junk[:, 0:TB], "junk"), t[5], AF.Square)
            pss = pbank()
            P.mm(pss, V(bo, "bo"), V(P.junk[:, 0:TB], "junk"), start=True, stop=True)
            P.act(t[6], pss, AF.Ln, scale=1.0, bias=V(tinyb, "tinyb"))
            P.act(t[6], t[6], AF.Exp, scale=-0.5)
            P.tt("dve", t[5], t[5], t[6], ALU.mult)
            for e_ in range(2):
                ps_ = slice(e_ * 64, (e_ + 1) * 64)
                P.stt(V(aTm[e_][ps_, kc, :], ("aT", e_, kc)), V(f32t[5][ps_, :], ("f32t", 5)), -1.0,
                      V(f32t[2][ps_, :], ("f32t", 2)), ALU.mult, ALU.mult)
            P.tt("pool", t[6], t[5], t[4], ALU.mult)
            P.tt("pool", V(bT[:, kc, :], ("bT", kc)), t[6], t[3], ALU.mult)
            P.ts("dve", t[4], t[4], vcol(9, gk), ALU.mult, V(omka[:, gk:gk + 1], "omka"), ALU.add)
            P.tt("dve", t[4], pk, t[4], ALU.mult)
            P.tt("pool", V(kT[:, kc, :], ("kT", kc)), t[4], t[3], ALU.mult)
            pr = pbank()
            for k8 in range(KC):
                P.mm(pr, V(Wr[:, k8, cs], ("w", 0)), V(xr_[:, k8, :], xrtok), start=(k8 == 0), stop=(k8 == KC - 1))
            for e_ in range(2):
                ps_ = slice(e_ * 64, (e_ + 1) * 64)
                P.tt("dve", V(rTm[e_][ps_, kc, :], ("rT", e_, kc)), V(pr.ap[ps_, :], pr.tok),
                     V(f32t[1][ps_, :], ("f32t", 1)), ALU.mult)
            P.stt(V(rkT[:, kc, :], ("rkT", kc)), pr, vcol(10, gk), t[4], ALU.mult, ALU.mult)
        xv_, xvtok = mix(3, xnT, xtok, 1)
        return xv_, xvtok

    def phaseAB(c8, sl, ab):
        banks = [V(P.psb[b][0:64, :], ("psb", b)) for b in range(5)]
        for hl in range(8):
            kc, e_ = hl // 2, hl % 2
            a_ = V(aTm[e_][:, kc, sl], ("aT", e_, kc))
            b_ = V(bT[:, kc, sl], ("bT", kc))
            k_ = V(kT[:, kc, sl], ("kT", kc))
            r_ = V(rTm[e_][:, kc, sl], ("rT", e_, kc))
            hs = slice(hl * 64, (hl + 1) * 64)
            for bi, (l_, r2) in enumerate(((a_, b_), (b_, a_), (k_, a_), (b_, r_), (k_, r_))):
                P.mm(V(P.psb[bi][0:64, hs], ("psb", bi)), l_, r2, start=True, stop=True)
        v8 = lambda ap_: ap_.rearrange("p (h s) -> p h s", h=8)
        P.tt("dve", V(Lm[0][0:64], ("Lm", 0)), V(v8(P.psb[0][0:64, :]), ("psb", 0)), mask(0), ALU.mult)
        P.tt("dve", V(LTm[0][0:64], ("LTm", 0)), V(v8(P.psb[1][0:64, :]), ("psb", 1)), mask(1), ALU.mult)
        P.tt("dve", V(AkT[ab][0:64], ("AkT", ab)), V(v8(P.psb[2][0:64, :]), ("psb", 2)), mask(1), ALU.mult)
        P.tt("dve", V(ArbT[ab][0:64], ("ArbT", ab)), V(v8(P.psb[3][0:64, :]), ("psb", 3)), mask(2), ALU.mult)
        P.tt("dve", V(ArkT[ab][0:64], ("ArkT", ab)), V(v8(P.psb[4][0:64, :]), ("psb", 4)), mask(2), ALU.mult)
        P.tt("pool", V(XT[0][0:64], ("XT", 0)), V(LTm[0][0:64], ("LTm", 0)), mask(3), ALU.add)
        cur, xc = 0, 0
        for lvl in range(5):
            nx = 1 - cur
            last = (lvl == 4)
            for hl in range(8):
                hs = slice(hl * 64, (hl + 1) * 64)
                P.mm(V(P.psb[0][0:64, hs], ("psb", 0)), V(LTm[cur][0:64, hl, :], ("LTm", cur)),
                     V(Lm[cur][0:64, hl, :], ("Lm", cur)), start=True, stop=True)
                if not last:
                    P.mm(V(P.psb[1][0:64, hs], ("psb", 1)), V(Lm[cur][0:64, hl, :], ("Lm", cur)),
                         V(LTm[cur][0:64, hl, :], ("LTm", cur)), start=True, stop=True)
            P.copy("act", V(Lm[nx][0:64], ("Lm", nx)), V(v8(P.psb[0][0:64, :]), ("psb", 0)))
            if not last:
                P.copy("act", V(LTm[nx][0:64], ("LTm", nx)), V(v8(P.psb[1][0:64, :]), ("psb", 1)))
            xn_ = (2 + ab) if last else (1 - xc)
            for hl in range(8):
                hs = slice(hl * 64, (hl + 1) * 64)
                P.mm(V(P.psb[2][0:64, hs], ("psb", 2)), V(Lm[nx][0:64, hl, :], ("Lm", nx)),
                     V(XT[xc][0:64, hl, :], ("XT", xc)), start=True, stop=False)
                P.mm(V(P.psb[2][0:64, hs], ("psb", 2)), V(identb[0:64, :], "identb"),
                     V(XT[xc][0:64, hl, :], ("XT", xc)), start=False, stop=True)
            P.copy("act", V(XT[xn_][0:64], ("XT", xn_)), V(v8(P.psb[2][0:64, :]), ("psb", 2)))
            cur = nx
            xc = xn_

    def phaseCD(blk, c8, sl, ab, xv_, xvtok):
        b5 = V(P.psb[5][0:64, :], ("psb", 5))
        vt_ = Vtm[c8 % 2]
        vtok = ("Vtm", c8 % 2)
        for k8 in range(KC):
            P.mm(b5, V(xv_[:, k8, sl], xvtok), V(Wv[:, k8, :], ("w", 2)), start=(k8 == 0), stop=(k8 == KC - 1))
        P.copy("act", V(vt_[0:64, :], vtok), b5)
        for hl in range(8):
            kc, e_ = hl // 2, hl % 2
            hs = slice(hl * 64, (hl + 1) * 64)
            P.mm(V(P.psb[5][0:64, hs], ("psb", 5)), V(aTm[e_][:, kc, sl], ("aT", e_, kc)),
                 V(Sbf[:, kc, :], ("Sbf", kc)), start=True, stop=False)
            P.mm(V(P.psb[5][0:64, hs], ("psb", 5)), V(AkT[ab][0:64, hl, :], ("AkT", ab)),
                 V(vt_[0:64, hs], vtok), start=False, stop=True)
        P.copy("act", V(Zs[0:64, :], "Zs"), b5)
        for hl in range(8):
            hs = slice(hl * 64, (hl + 1) * 64)
            P.mm(V(P.psb[5][0:64, hs], ("psb", 5)), V(XT[2 + ab][0:64, hl, :], ("XT", 2 + ab)),
                 V(Zs[0:64, hs], "Zs"), start=True, stop=True)
        P.copy("act", V(Us[0:64, :], "Us"), b5)
        for hl in range(8):
            kc, e_ = hl // 2, hl % 2
            hs = slice(hl * 64, (hl + 1) * 64)
            P.mm(V(P.psb[5][0:64, hs], ("psb", 5)), V(rTm[e_][:, kc, sl], ("rT", e_, kc)),
                 V(Sbf[:, kc, :], ("Sbf", kc)), start=True, stop=False)
            P.mm(V(P.psb[5][0:64, hs], ("psb", 5)), V(ArbT[ab][0:64, hl, :], ("ArbT", ab)),
                 V(Us[0:64, hs], "Us"), start=False, stop=False)
            P.mm(V(P.psb[5][0:64, hs], ("psb", 5)), V(ArkT[ab][0:64, hl, :], ("ArkT", ab)),
                 V(vt_[0:64, hs], vtok), start=False, stop=True)
        y8 = P.psb[5][0:64, :].rearrange("p (h v) -> p h v", h=8)
        s0, s1 = V(st8[0][0:64, :], ("st8", 0)), V(st8[1][0:64, :], ("st8", 1))
        P.S.op("dve", (lambda o=st8[0][0:64, :], i_=y8: nc.vector.tensor_reduce(o, i_, AX.X, ALU.add)),
               reads=[("psb", 5)], writes=[("st8", 0)])
        P.ts("dve", s0, s0, -1.0 / 64.0, ALU.mult)
        ycv = V(yc[0:64, :], "yc")
        yc8 = yc[0:64, :].rearrange("p (h v) -> p h v", h=8)
        P.tt("dve", V(yc8, "yc"), V(y8, ("psb", 5)), V(st8[0][0:64, :].unsqueeze(2).broadcast_to([64, 8, 64]), ("st8", 0)), ALU.add)
        P.act(V(sq[0:64, :], "sq"), ycv, AF.Square)
        P.S.op("dve", (lambda o=st8[1][0:64, :], i_=sq[0:64, :].rearrange("p (h v) -> p h v", h=8): nc.vector.tensor_reduce(o, i_, AX.X, ALU.add)),
               reads=[("sq",)], writes=[("st8", 1)])
        P.act(s1, s1, AF.Ln, scale=1.0 / 64.0, bias=V(epsg[0:64, :], "epsg"))
        P.act(s1, s1, AF.Exp, scale=-0.5)
        P.tt("dve", V(yc8, "yc"), V(yc8, "yc"), V(st8[1][0:64, :].unsqueeze(2).broadcast_to([64, 8, 64]), ("st8", 1)), ALU.mult)
        P.tt("pool", ycv, ycv, V(lnw[0:64, :], "lnw"), ALU.mult)
        P.tt("pool", ycv, ycv, V(lnb[0:64, :], "lnb"), ALU.add)
        b7f = P.psb[7]
        pBs = V(b7f[0:64, 384:392], ("psb", 7, "s"))
        for kc in range(NK):
            P.mm(V(b7f[0:64, 384 + 2 * kc:386 + 2 * kc], ("psb", 7, "s")), V(rkT[:, kc, sl], ("rkT", kc)), V(Eh, "Eh"),
                 start=True, stop=True)
        s2 = V(st8[2][0:64, :], ("st8", 2))
        P.copy("act", s2, pBs)
        P.tt("pool", V(bon[0:64, :].rearrange("p (h v) -> p h v", h=8), "bon"),
             V(vt_[0:64, :].rearrange("p (h v) -> p h v", h=8), vtok),
             V(st8[2][0:64, :].unsqueeze(2).broadcast_to([64, 8, 64]), ("st8", 2)), ALU.mult)
        P.tt("pool", ycv, ycv, V(bon[0:64, :], "bon"), ALU.add)
        psT7 = P.psb[7].bitcast(BF16)
        for kc in range(NK):
            P.transpose(V(psT7[0:64, kc * 128:(kc + 1) * 128], ("psb", 7, "t")), V(bT[:, kc, sl], ("bT", kc)), V(P.ident, "ident"))
        P.copy("act", V(BKtm[0:64, 0, :], ("BKtm", 0)), V(psT7[0:64, 0:512], ("psb", 7, "t")))
        for kc in range(NK):
            P.transpose(V(psT7[0:64, kc * 128:(kc + 1) * 128], ("psb", 7, "t")), V(kT[:, kc, sl], ("kT", kc)), V(P.ident, "ident"))
        P.copy("act", V(BKtm[0:64, 1, :], ("BKtm", 1)), V(psT7[0:64, 0:512], ("psb", 7, "t")))
        for kc in range(NK):
            cs = slice(kc * 128, (kc + 1) * 128)
            P.mm(V(P.psb[6][:, cs], ("psb", 6)), V(BKtm[0:64, 0, cs], ("BKtm", 0)), V(Us[0:64, cs], "Us"), start=True, stop=False)
            P.mm(V(P.psb[6][:, cs], ("psb", 6)), V(BKtm[0:64, 1, cs], ("BKtm", 1)), V(vt_[0:64, cs], vtok), start=False, stop=True)
        for hp in range(2):
            ps_ = slice(hp * 64, (hp + 1) * 64)
            sv = V(S[ps_, :, :], ("S", hp))
            pst = V(P.psb[6][ps_, :].rearrange("p (k c) -> p k c", k=NK)[:, :, hp * 64:(hp + 1) * 64], ("psb", 6))
            P.tt("dve", sv, sv, pst, ALU.add)
            P.tt("dve", sv, sv, V(WC[ps_, :, c8:c8 + 1].broadcast_to([64, NK, 64]), ("WC",)), ALU.mult)
            P.copy("pool", V(Sbf[ps_, :, :], ("Sbf",)), sv)
        for (lh, rh, kk_) in ((V(hgA[:, sl], "hgA"), V(g2A, ("w", 9)), 0), (V(hgB[0:32, sl], "hgB"), V(g2B[0:32, :], ("w", 10)), 1)):
            P.mm(b5, lh, rh, start=(kk_ == 0), stop=(kk_ == 1))
        P.tt("dve", V(ytm[0:64, :], "ytm"), b5, ycv, ALU.mult)
        for kc in range(NK):
            P.transpose(V(psT7[:, 512 + kc * 64:512 + (kc + 1) * 64], ("psb", 7, "y")), V(ytm[0:64, kc * 128:(kc + 1) * 128], "ytm"),
                        V(P.ident[0:64, 0:64], "ident"))
        P.copy("act", V(yT[:, :, sl], ("yT", c8)), V(psT7[:, 512:768].rearrange("p (k t) -> p k t", k=NK), ("psb", 7, "y")))

    def stageB(blk, slot):
        xv_, xvtok = stage1(blk, slot)
        sls = [slice(c8 * RW_C, (c8 + 1) * RW_C) for c8 in range(8)]
        phaseAB(0, sls[0], 0)
        for c8 in range(8):
            if c8 + 1 < 8:
                phaseAB(c8 + 1, sls[c8 + 1], (c8 + 1) % 2)
            phaseCD(blk, c8, sls[c8], c8 % 2, xv_, xvtok)
        out_stage(P, blk, srcR, srcRname, dst, dstname,
                  lambda k, s: V(yT[:, k, s * 128:(s + 1) * 128], ("yT",)), NK, wo, ("w", 3))

    nidx = 1
    norm_stage(P, srcN, srcNname, 0, nidx, 0)
    for blk in range(nblk):
        if blk + 1 < nblk:
            norm_stage(P, srcN, srcNname, blk + 1, nidx, (blk + 1) % 2)
        stageB(blk, blk % 2)
    P.arena_reset(mark)


def build(T, plan):
    P = Prog(T, plan)
    P.w = {}

    def win(name, shape, dt=F32):
        if name not in P.w:
            P.w[name] = P.dram_in(name, shape, dt)
        return P.w[name]

    P.win = win
    x_in = P.dram_in("x", [T, D])
    out = P.dram_out("out", [T, D])
    P.arena_init(ARENA_BYTES)
    P.psb = [P.ps("psb%d" % i)[:, :] for i in range(8)]
    P.ident = P.alloc([128], BF16)
    P.ones = P.alloc([128], BF16)
    P.normw = P.alloc([9, KC])
    P.epsv = P.alloc([1])
    P.dma("sp", V(P.ident, "ident"), V(win("c_ident", [128, 128], BF16), "in_w"), key="const0")
    P.dma("sp", V(P.normw, "normw"), V(win("c_normw", [128, 9, KC]), "in_w"), key="const1")
    P.memset("pool", V(P.ones, "ones"), 1.0)
    P.memset("pool", V(P.epsv, "epsv"), EPS)
    P.S.barrier()
    scr = [P.dram_scratch("scr%d" % i, [T, D]) for i in range(3)]
    bufs = [(x_in, "x")] + [(scr[i], "scr%d" % i) for i in range(3)]
    cur = 0

    def nxt(*busy):
        for i in (1, 2, 3):
            if i not in busy:
                return i

    for item in plan:
        kind = item[0]
        a = cur
        b = nxt(a)
        c = nxt(a, b)
        A_, B_, C_ = bufs[a], bufs[b], bufs[c]
        if kind == "ffn":
            li = item[1]
            ffn_pass(P, li, 0, 11, A_[0], A_[1], A_[0], A_[1], B_[0], B_[1])
            ffn_pass(P, li, 11, 22, A_[0], A_[1], B_[0], B_[1], C_[0], C_[1])
            cur = c
        elif kind == "mix" and item[1] == 3:
            retnet_pass(P, 0, A_[0], A_[1], A_[0], A_[1], B_[0], B_[1])
            retnet_pass(P, 2, A_[0], A_[1], B_[0], B_[1], C_[0], C_[1])
            cur = c
        elif kind == "mix" and item[1] == 0:
            ssd_pass(P, 0, A_[0], A_[1], A_[0], A_[1], B_[0], B_[1])
            ssd_pass(P, 1, A_[0], A_[1], B_[0], B_[1], C_[0], C_[1])
            cur = c
        elif kind == "mix" and item[1] == 1:
            rwkv_pass(P, 0, A_[0], A_[1], A_[0], A_[1], B_[0], B_[1])
            rwkv_pass(P, 1, A_[0], A_[1], B_[0], B_[1], C_[0], C_[1])
            cur = c
        elif kind == "mix" and item[1] == 2:
            gla_pass(P, A_[0], A_[1], A_[0], A_[1], B_[0], B_[1])
            cur = b
        elif kind == "final":
            final_norm(P, bufs[cur][0], bufs[cur][1], out, "out")
    P.barrier("sp", [("out",)])
    global LAST_INPUT_NAMES
    LAST_INPUT_NAMES = list(P.inputs.keys())
    return P.finish()


def ret_perm():
    idx = []
    for part in range(2):
        for h in range(RET_H):
            base = part * 1024 + h * RET_DK
            idx += [base + 2 * i for i in range(128)] + [base + 2 * i + 1 for i in range(128)]
    return np.array(idx + list(range(2048, 6144)))


def host_consts(inputs):
    import ml_dtypes
    f = lambda a: np.ascontiguousarray(np.asarray(a, dtype=np.float32))
    c = {}
    c["c_ident"] = np.eye(128, dtype=np.float32).astype(ml_dtypes.bfloat16)
    nw = np.concatenate([f(inputs["norm_mix"]), f(inputs["norm_ffn"]), f(inputs["norm_final"])[None]], 0)
    c["c_normw"] = np.ascontiguousarray(nw.reshape(9, KC, 128).transpose(2, 0, 1))
    c["c_nfb"] = f(inputs["norm_final"])
    cw = f(inputs["ffn_conv_w"])
    cwl = cw.reshape(4, 3, 44, 128).transpose(0, 3, 2, 1)
    cb = f(inputs["ffn_conv_b"]).reshape(4, 44, 128).transpose(0, 2, 1)
    for li in range(4):
        c["ffn_w_up_%d" % li] = f(inputs["ffn_w_up"][li])
        c["ffn_w_down_%d" % li] = f(inputs["ffn_w_down"][li])
        c["c_ffn_cw_%d" % li] = np.ascontiguousarray(cwl[li])
        c["c_ffn_cb_%d" % li] = np.ascontiguousarray(cb[li])
    c["ret_w_in_p"] = np.ascontiguousarray(f(inputs["ret_w_in"][0])[:, ret_perm()])
    c["ret_w_out"] = f(inputs["ret_w_out"][0])
    inv = (1.0 / (np.float32(10000.0) ** np.linspace(0.0, 1.0, 128, dtype=np.float32))).astype(np.float32)
    ang = (np.arange(4096, dtype=np.float32)[None, :] * inv[:, None]).astype(np.float32)
    c["c_ret_cos"] = np.cos(ang).astype(np.float32)
    c["c_ret_sin"] = np.sin(ang).astype(np.float32)
    gam = 1.0 - 2.0 ** (-5.0 - np.arange(4, dtype=np.float64))
    s_ = np.arange(128)[:, None]
    l_ = np.arange(128)[None, :]
    decT = np.zeros((128, 4, 128), np.float64)
    for h in range(4):
        decT[:, h, :] = np.where(l_ >= s_, gam[h] ** (l_ - s_), 0.0) / 16.0
    c["c_ret_decT"] = decT.astype(np.float32)
    c["c_ret_gl"] = np.stack([gam[h] ** ((np.arange(TB) % 128) + 1) for h in range(4)]).astype(np.float32)
    c["c_ret_kdec"] = np.stack([gam[h] ** (127 - np.arange(128)) / 16.0 for h in range(4)], 1).astype(np.float32)
    c["ssd_w_in"] = f(inputs["ssd_w_in"][0])
    c["ssd_w_out"] = f(inputs["ssd_w_out"][0])
    c["c_ssd_cw"] = np.ascontiguousarray(f(inputs["ssd_conv_w"][0]).reshape(4, 32, 128).transpose(2, 1, 0))
    c["c_ssd_cb"] = np.ascontiguousarray(f(inputs["ssd_conv_b"][0]).reshape(32, 128).T)
    for n_ in ("ssd_dt_bias", "ssd_a_log", "ssd_d", "ssd_norm_w"):
        c[n_] = f(inputs[n_][0])
    c["c_SU"] = (s_ < l_).T.astype(np.float32).copy()
    for n_ in ("w_rkv", "w_out", "w1", "w2", "a1", "a2", "g1", "g2", "ln_w", "ln_b"):
        c["rwkv_" + n_] = f(inputs["rwkv_" + n_][0])
    vecs = [f(inputs["rwkv_mix"][0])[i_] for i_ in range(6)] + [f(inputs["rwkv_" + n_][0]).reshape(-1) for n_ in
                                                                 ("w0", "a0", "k_k", "k_a", "r_k")] + [np.zeros(1024, np.float32)]
    c["c_rwkv_vec"] = np.ascontiguousarray(np.stack(vecs).reshape(12, KC, 128).transpose(2, 0, 1))
    t64 = np.arange(64)[:, None]
    u64 = np.arange(64)[None, :]
    c["c_rwkv_masks"] = np.ascontiguousarray(np.stack([(u64 < t64), (t64 < u64), (t64 <= u64), (t64 == u64)], 1).astype(np.float32))
    E = np.zeros((128, 2), np.float32)
    E[:64, 0] = 1.0
    E[64:, 1] = 1.0
    c["c_rwkv_E"] = E.astype(ml_dtypes.bfloat16)
    c["c_rwkv_bo"] = (E @ E.T).astype(ml_dtypes.bfloat16)
    sm64 = np.ones(TB, np.float32)
    sm64[::64] = 0.0
    c["c_rwkv_scanm"] = sm64
    c["gla_w_in"] = f(inputs["gla_w_in"][0])
    c["gla_w_out"] = f(inputs["gla_w_out"][0])
    c["gla_w_gk2"] = f(inputs["gla_w_gk2"][0])
    c["c_gla_bgk"] = np.ascontiguousarray(f(inputs["gla_b_gk2"][0]).reshape(4, 128).T)
    c["c_gla_nw"] = np.ascontiguousarray(f(inputs["gla_norm_w"][0]).reshape(2, 128).T)
    c["c_maskT"] = (l_ >= s_).astype(np.float32)
    sm = np.ones(TB, np.float32)
    sm[::128] = 0.0
    c["c_scanm"] = sm
    return c


FULL_PLAN = [("mix", 0), ("ffn", 0), ("mix", 1), ("ffn", 1), ("mix", 2), ("ffn", 2), ("mix", 3), ("ffn", 3), ("final",)]
_CACHE = {}


def kernel(**inputs):
    T = 4096
    n_cores = 8
    if "nc" not in _CACHE:
        _CACHE["nc"] = build(T, FULL_PLAN)
        _CACHE["names"] = list(LAST_INPUT_NAMES)
    nc = _CACHE["nc"]
    names = _CACHE["names"]
    consts = host_consts(inputs)
    x = np.ascontiguousarray(np.asarray(inputs["x"], dtype=np.float32))
    shared = {n: consts[n] for n in names if n != "x"}
    in_maps = []
    for b in range(n_cores):
        m = dict(shared)
        m["x"] = np.ascontiguousarray(x[b])
        in_maps.append(m)
    res = run_bass_kernel_spmd(nc, in_maps, core_ids=list(range(n_cores)))
    return np.stack([np.asarray(r["out"], dtype=np.float32) for r in res.results], axis=0)
```

```python
import numpy as np
import concourse.bass as bass
import concourse.mybir as mybir
from concourse.bass_utils import run_bass_kernel_spmd

F32 = mybir.dt.float32
BF16 = mybir.dt.bfloat16
AF = mybir.ActivationFunctionType
ALU = mybir.AluOpType
AX = mybir.AxisListType

D = 1024
KC = 8
TB = 512
SCHEDULE = True
KEEP_ORDER = ()
EPS = 1e-5


class V:
    __slots__ = ("ap", "tok")

    def __init__(self, ap, tok):
        self.ap = ap
        self.tok = tok if isinstance(tok, tuple) else (tok,)


class _Op:
    __slots__ = ("eng", "fn", "reads", "writes", "dma_key", "deps", "inc", "val", "sem", "amt",
                 "odeps", "cost", "lat", "bar", "idx", "grp", "gend")

    def __init__(self, eng, fn, reads, writes, dma_key, cost=300.0, lat=0.0):
        self.odeps = []
        self.cost = cost
        self.lat = lat
        self.bar = False
        self.idx = 0
        self.grp = None
        self.gend = True
        self.eng = eng
        self.fn = fn
        self.reads = reads
        self.writes = writes
        self.dma_key = dma_key
        self.deps = []
        self.inc = False
        self.val = 0
        self.sem = None
        self.amt = 1


class Sched:
    COMPUTE = ("pe", "act", "dve", "pool")

    def __init__(self, nc):
        self.nc = nc
        self.ops = []
        self.state = {}

    def op(self, eng, fn, reads=(), writes=(), dma_key=None, cost=300.0, lat=0.0):
        reads = [t if isinstance(t, tuple) else (t,) for t in reads]
        writes = [t if isinstance(t, tuple) else (t,) for t in writes]
        writes = [t[:2] if t[0] == "psb" else t for t in writes]
        writes += [t[:2] for t in reads if t[0] == "psb" and t[:2] not in writes]
        reads = [t for t in reads if t[0] != "psb"]
        o = _Op(eng, fn, reads, writes, dma_key, cost, lat)
        self._analyse(o)
        self.ops.append(o)
        return o

    @staticmethod
    def _conf(a, b):
        n = min(len(a), len(b))
        return a[:n] == b[:n]

    def _add_dep(self, o, p, kind):
        if p is None or p is o:
            return
        pd = p.dma_key is not None
        od = o.dma_key is not None
        if not pd and not od:
            if p.eng == "pe" and o.eng == "pe":
                o.odeps.append(p)
                return
            if p.eng == o.eng and kind != "RAW":
                o.odeps.append(p)
                return
        if pd and od and p.eng == o.eng and kind == "WAR" and False:
            return
        o.deps.append(p)

    def _analyse(self, o):
        st = self.state
        for tk in o.reads:
            root = st.setdefault(tk[0], {})
            for k, e in root.items():
                if self._conf(k, tk):
                    self._add_dep(o, e[0], "RAW")
            e = root.get(tk)
            if e is None:
                root[tk] = [None, [o]]
            else:
                e[1].append(o)
        for tk in o.writes:
            root = st.setdefault(tk[0], {})
            dead = []
            for k, e in root.items():
                if self._conf(k, tk):
                    self._add_dep(o, e[0], "WAW")
                    for r in e[1]:
                        self._add_dep(o, r, "WAR")
                    if len(k) > len(tk):
                        dead.append(k)
                    elif len(k) < len(tk):
                        pass
            for k in dead:
                del root[k]
            root[tk] = [o, []]

    def barrier(self):
        lasts = {}
        dmas = {}
        for o in self.ops:
            if o.dma_key is not None:
                dmas[o.dma_key] = o
            elif o.fn is not None:
                lasts[o.eng] = o
        new = []
        for eng in ("pe", "act", "dve", "pool", "sp"):
            b = _Op(eng, None, [], [], None)
            b.deps = [p for e, p in lasts.items() if e != eng] + list(dmas.values())
            b.bar = True
            new.append(b)
        self.ops.extend(new)
        self.state = {}

    def schedule(self, window=16, xlat=300.0):
        import bisect
        segs, cur = [], []
        for o in self.ops:
            if o.bar:
                if cur:
                    segs.append(cur)
                    cur = []
                segs.append([o])
            else:
                cur.append(o)
        if cur:
            segs.append(cur)
        out = []
        prev_lasts, prev_dmas = {}, {}
        for seg in segs:
            if len(seg) == 1:
                b = seg[0]
                if b.bar:
                    b.deps = [p_ for e_, p_ in prev_lasts.items() if e_ != b.eng] + list(prev_dmas.values())
                out.extend(seg)
                continue
            seg_start = len(out)
            lastof = {}
            for i, o in enumerate(seg):
                o.idx = i
                if o.eng in KEEP_ORDER:
                    if o.eng in lastof:
                        o.odeps.append(lastof[o.eng])
                    lastof[o.eng] = o
            inseg = set(id(o) for o in seg)
            groups = {}
            for o in seg:
                if o.grp is not None:
                    groups.setdefault(o.grp, []).append(o)
            for g, mem in groups.items():
                if len(mem) > 1:
                    ids = set(id(m) for m in mem)
                    first = mem[0]
                    for m in mem[1:]:
                        for d in m.deps + m.odeps:
                            if id(d) not in ids:
                                first.odeps.append(d)
            pe_lock = None
            npred = {}
            succ = {}
            for o in seg:
                ds = [d for d in (o.deps + o.odeps) if id(d) in inseg]
                npred[id(o)] = len(ds)
                for d in ds:
                    succ.setdefault(id(d), []).append(o)
            fin = {}
            rtime = {}
            ready = {e: [] for e in ("pe", "act", "dve", "pool", "sp")}
            free = {e: 0.0 for e in ready}
            for o in seg:
                if npred[id(o)] == 0:
                    rtime[id(o)] = 0.0
                    ready[o.eng].append((o.idx, o))
            for e in ready:
                ready[e].sort(key=lambda t: t[0])
            done = 0
            n = len(seg)
            while done < n:
                best = None
                for e, lst in ready.items():
                    fe = free[e]
                    cand = lst[:window]
                    if e == "pe" and pe_lock is not None:
                        cand = [t for t in lst if t[1].grp == pe_lock][:1]
                    for (ix, o) in cand:
                        st = rtime[id(o)]
                        if st < fe:
                            st = fe
                        if best is None or (st, ix) < best[0]:
                            best = ((st, ix), o)
                (st, ix), o = best
                lst = ready[o.eng]
                lst.pop(bisect.bisect_left(lst, (ix,), key=lambda t: (t[0],)))
                if o.eng == "pe" and o.grp is not None:
                    pe_lock = None if o.gend else o.grp
                free[o.eng] = st + o.cost
                f = st + o.cost + o.lat
                fin[id(o)] = f
                out.append(o)
                done += 1
                for s_ in succ.get(id(o), ()):
                    k = id(s_)
                    npred[k] -= 1
                    t_ = f + (xlat if s_.eng != o.eng else 0.0)
                    if rtime.get(k, 0.0) < t_:
                        rtime[k] = t_
                    if npred[k] == 0:
                        bisect.insort(ready[s_.eng], (s_.idx, s_), key=lambda t: t[0])
            prev_lasts, prev_dmas = {}, {}
            for o in out[seg_start:]:
                if o.dma_key is not None:
                    prev_dmas[o.dma_key] = o
                elif o.fn is not None:
                    prev_lasts[o.eng] = o
        assert len(out) == len(self.ops)
        self.ops = out

    def emit(self, block_ctx, sems):
        nc = self.nc
        for o in self.ops:
            for p in o.deps:
                p.inc = True
        cnt = {}
        for o in self.ops:
            if o.dma_key is not None:
                key = ("dma", o.dma_key)
                cnt[key] = cnt.get(key, 0) + 16
                o.val = cnt[key]
                o.sem = sems[key]
                o.amt = 16
                o.inc = True
            elif o.inc:
                cnt[o.eng] = cnt.get(o.eng, 0) + 1
                o.val = cnt[o.eng]
                o.sem = sems[o.eng]
        engs = {"pe": nc.tensor, "act": nc.scalar, "dve": nc.vector, "pool": nc.gpsimd, "sp": nc.sync}
        per_eng = {k: [] for k in engs}
        for o in self.ops:
            per_eng[o.eng].append(o)

        def run(engname):
            eng = engs[engname]
            waited = {}
            for o in per_eng[engname]:
                need = {}
                for p in o.deps:
                    sid = id(p.sem)
                    if need.get(sid, (None, 0))[1] < p.val:
                        need[sid] = (p.sem, p.val)
                for sid, (sem, val) in need.items():
                    if waited.get(sid, 0) < val:
                        eng.wait_ge(sem, val)
                        waited[sid] = val
                if o.fn is None:
                    continue
                ins = o.fn()
                if o.inc:
                    ins.then_inc(o.sem, o.amt)

        @block_ctx.tensor
        def _(e):
            run("pe")

        @block_ctx.scalar
        def _(e):
            run("act")

        @block_ctx.vector
        def _(e):
            run("dve")

        @block_ctx.gpsimd
        def _(e):
            run("pool")

        @block_ctx.sync
        def _(e):
            run("sp")


class Prog:
    def __init__(self, T, plan):
        self.T = T
        self.plan = plan
        self.nc = bass.Bass("TRN2", target_bir_lowering=False)
        self.S = Sched(self.nc)
        self.ctxs = []
        self.dma_keys = []
        self.inputs = {}
        self.psn = 0

    def dram_in(self, name, shape, dt=F32):
        t = self.nc.dram_tensor(name, list(shape), dt, kind="ExternalInput")
        self.inputs[name] = t
        return t.ap()

    def dram_out(self, name, shape, dt=F32):
        return self.nc.dram_tensor(name, list(shape), dt, kind="ExternalOutput").ap()

    def dram_scratch(self, name, shape, dt=F32):
        return self.nc.dram_tensor(name, list(shape), dt, kind="Internal").ap()

    def sb(self, name, shape, dt=F32):
        g = self.nc.sbuf_tensor(name, list(shape), dt)
        t = g.__enter__()
        self.ctxs.append(g)
        return t

    def ps(self, name, shape=(128, 512), dt=F32):
        g = self.nc.psum_tensor(name, list(shape), dt)
        t = g.__enter__()
        self.ctxs.append(g)
        return t

    def arena_init(self, nbytes):
        self.arena = self.sb("arena", [128, nbytes // 4], F32)
        self.arena_n = nbytes
        self.arena_off = 0

    def alloc(self, shape, dt=F32):
        n = 1
        for d in shape:
            n *= d
        esz = 4 if dt == F32 else 2
        nb = (n * esz + 63) // 64 * 64
        assert self.arena_off + nb <= self.arena_n, ("arena overflow", self.arena_off + nb, self.arena_n)
        a = self.arena[:, self.arena_off // 4:(self.arena_off + nb) // 4]
        self.arena_off += nb
        if dt != F32:
            a = a.bitcast(dt)
        a = a[:, 0:n]
        if len(shape) == 2:
            a = a.rearrange("p (a b) -> p a b", a=shape[0])
        elif len(shape) == 3:
            a = a.rearrange("p (a b c) -> p a b c", a=shape[0], b=shape[1])
        elif len(shape) == 4:
            a = a.rearrange("p (a b c d) -> p a b c d", a=shape[0], b=shape[1], c=shape[2])
        return a

    def arena_reset(self, mark=0):
        self.S.barrier()
        self.arena_off = mark

    def _toks(self, *vs):
        return [v.tok for v in vs if isinstance(v, V)]

    @staticmethod
    def _n(ap):
        n = 1
        for d in list(ap.shape)[1:]:
            n *= int(d)
        return n

    def mm(self, out, lhsT, rhs, start=True, stop=True):
        nc = self.nc
        otok = out.tok
        if not (start and stop):
            otok = otok[:2]
        n = self._n(rhs.ap)
        mult = 4.0 if rhs.ap.dtype == F32 else 1.0
        o = self.S.op("pe", lambda: nc.tensor.matmul(out.ap, lhsT.ap, rhs.ap, start=start, stop=stop),
                      reads=self._toks(lhsT, rhs), writes=[otok], cost=mult * (max(64, n) * 0.42 + 20.0), lat=250.0)
        if start:
            self.gid = getattr(self, "gid", 0) + 1
        o.grp = self.gid
        o.gend = bool(stop)

    def transpose(self, out, in_, ident):
        nc = self.nc
        self.S.op("pe", lambda: nc.tensor.transpose(out.ap, in_.ap, ident.ap),
                  reads=self._toks(in_, ident), writes=self._toks(out), cost=80.0, lat=250.0)

    def act(self, out, in_, func, scale=None, bias=None, accum=None, extra_reads=()):
        nc = self.nc
        kw = {}
        if scale is not None:
            kw["scale"] = scale.ap if isinstance(scale, V) else scale
        if bias is not None:
            kw["bias"] = bias.ap if isinstance(bias, V) else bias
        if accum is not None:
            kw["accum_out"] = accum.ap
        w = self._toks(out) + (self._toks(accum) if accum is not None else [])
        self.S.op("act", lambda: nc.scalar.activation(out.ap, in_.ap, func, **kw),
                  reads=self._toks(in_, scale, bias) + list(extra_reads), writes=w,
                  cost=230.0 + 0.83 * self._n(in_.ap) + (90.0 if accum is not None else 0.0))

    def _e(self, eng):
        return {"dve": self.nc.vector, "pool": self.nc.gpsimd, "act": self.nc.scalar}[eng]

    def _c(self, eng, ap, per=1.04):
        n = self._n(ap)
        if eng == "pool":
            return 300.0 + 1.6 * n
        if eng == "act":
            return 230.0 + 0.83 * n
        return 120.0 + per * n

    def tt(self, eng, out, a, b, op):
        e = self._e(eng)
        self.S.op(eng, lambda: e.tensor_tensor(out.ap, a.ap, b.ap, op),
                  reads=self._toks(a, b), writes=self._toks(out), cost=self._c(eng, out.ap))

    def ts(self, eng, out, in_, s1, op0, s2=None, op1=None, accum=None):
        e = self._e(eng)
        a1 = s1.ap if isinstance(s1, V) else s1
        a2 = s2.ap if isinstance(s2, V) else s2
        kw = {}
        if op1 is not None:
            kw["op1"] = op1
        if accum is not None:
            kw["accum_out"] = accum.ap
        w = self._toks(out) + (self._toks(accum) if accum is not None else [])
        self.S.op(eng, lambda: e.tensor_scalar(out.ap, in_.ap, a1, a2, op0, **kw),
                  reads=self._toks(in_, s1, s2), writes=w, cost=self._c(eng, out.ap, 0.7))

    def stt(self, out, in0, scalar, in1, op0, op1):
        nc = self.nc
        sc = scalar.ap if isinstance(scalar, V) else scalar
        self.S.op("dve", lambda: nc.vector.scalar_tensor_tensor(out.ap, in0.ap, sc, in1.ap, op0, op1),
                  reads=self._toks(in0, scalar, in1), writes=self._toks(out), cost=self._c("dve", out.ap))

    def copy(self, eng, out, in_):
        if eng == "act":
            nc = self.nc
            self.S.op("act", lambda: nc.scalar.copy(out.ap, in_.ap), reads=self._toks(in_), writes=self._toks(out),
                      cost=self._c("act", out.ap))
        else:
            e = self._e(eng)
            self.S.op(eng, lambda: e.tensor_copy(out.ap, in_.ap), reads=self._toks(in_), writes=self._toks(out),
                      cost=self._c(eng, out.ap, 0.7))

    def memset(self, eng, out, val):
        e = self._e(eng)
        self.S.op(eng, lambda: e.memset(out.ap, val), writes=self._toks(out), cost=self._c(eng, out.ap, 0.7))

    def recip(self, out, in_):
        nc = self.nc
        self.S.op("dve", lambda: nc.vector.reciprocal(out.ap, in_.ap), reads=self._toks(in_), writes=self._toks(out),
                  cost=self._c("dve", out.ap, 8.4))

    def dma(self, q, out, in_, key):
        if key not in self.dma_keys:
            self.dma_keys.append(key)
        e = {"sp": self.nc.sync, "pool": self.nc.gpsimd, "act": self.nc.scalar}[q]
        nb = self._n(out.ap) * int(list(out.ap.shape)[0]) * (4 if out.ap.dtype == F32 else 2)
        self.S.op(q, lambda: e.dma_start(out=out.ap, in_=in_.ap), reads=self._toks(in_),
                  writes=self._toks(out), dma_key=key, cost=(400.0 if q == "pool" else 60.0), lat=2000.0 + nb / 100.0)

    def barrier(self, eng, toks):
        self.S.op(eng, None, reads=list(toks))

    def finish(self):
        nc = self.nc
        sems = {}
        gs = []
        for name in ("pe", "act", "dve", "pool"):
            g = nc.semaphore("sem_" + name)
            sems[name] = g.__enter__()
            gs.append(g)
        for i, k in enumerate(self.dma_keys):
            g = nc.semaphore("semd_%d" % i)
            sems[("dma", k)] = g.__enter__()
            gs.append(g)
        if SCHEDULE:
            self.S.schedule()
        blk = nc.Block()
        b = blk.__enter__()
        self.S.emit(b, sems)
        blk.__exit__(None, None, None)
        for g in reversed(gs):
            g.__exit__(None, None, None)
        for g in reversed(self.ctxs):
            g.__exit__(None, None, None)
        return nc


FFN_H = 2816
FFN_NC = 22
ARENA_BYTES = 204 * 1024


def tile_rows(ap, blk, s):
    r0 = blk * TB + s * 128
    return ap[r0:r0 + 128, :]


def alloc_common(P):
    P.xt = [P.alloc([D]) for _ in range(2)]
    P.xr = [P.alloc([D]) for _ in range(2)]
    P.xnT = [P.alloc([KC, TB], BF16) for _ in range(2)]
    P.xs = P.alloc([D], BF16)
    P.junk = P.alloc([D], BF16)
    P.ss = [P.alloc([4]) for _ in range(2)]
    P.rstd = [P.alloc([4]) for _ in range(2)]
    P.xtn = 0
    P.xrn = 0


def norm_tile(P, src, srcname, blk, s, nidx, slot):
    psT = P.psb[7].bitcast(BF16)
    ss, rstd = P.ss[slot], P.rstd[slot]
    xi = P.xtn % 2
    P.xtn += 1
    xt = P.xt[xi]
    xtv = V(xt, ("xt", xi))
    P.dma("sp", xtv, V(tile_rows(src, blk, s), (srcname, blk, s)), key=("xt", xi))
    P.act(V(P.junk, "junk"), xtv, AF.Square, accum=V(ss[:, s:s + 1], ("ss", slot, s)))
    P.act(V(rstd[:, s:s + 1], ("rstd", slot, s)), V(ss[:, s:s + 1], ("ss", slot, s)), AF.Sqrt,
          scale=1.0 / D, bias=V(P.epsv, "epsv"))
    P.recip(V(rstd[:, s:s + 1], ("rstd", slot, s)), V(rstd[:, s:s + 1], ("rstd", slot, s)))
    P.ts("dve", V(P.xs, "xs"), xtv, V(rstd[:, s:s + 1], ("rstd", slot, s)), ALU.mult)
    for kc in range(KC):
        P.transpose(V(psT[:, kc * 128:(kc + 1) * 128], ("psb", 7)), V(P.xs[:, kc * 128:(kc + 1) * 128], "xs"),
                    V(P.ident, "ident"))
    P.tt("dve", V(P.xnT[slot][:, :, s * 128:(s + 1) * 128], ("xnT", slot, s)),
         V(psT.rearrange("p (k t) -> p k t", k=KC), ("psb", 7)),
         V(P.normw[:, nidx, :].unsqueeze(2).broadcast_to([128, KC, 128]), "normw"), ALU.mult)


def run_blocks(P, srcN, srcNname, nidx, stageB):
    nblk = P.T // TB
    for s in range(4):
        norm_tile(P, srcN, srcNname, 0, s, nidx, 0)
    for blk in range(nblk):
        pending = [(blk + 1, s) for s in range(4)] if blk + 1 < nblk else []

        def tick():
            if pending:
                b, s_ = pending.pop(0)
                norm_tile(P, srcN, srcNname, b, s_, nidx, b % 2)

        stageB(blk, blk % 2, tick)
        while pending:
            tick()


def out_stage(P, blk, srcR, srcRname, dst, dstname, lhs_fn, nk, wo, wotok):
    for s in range(4):
        xi = P.xrn % 2
        P.xrn += 1
        xr = P.xr[xi]
        xrv = V(xr, ("xr", xi))
        P.dma("sp", xrv, V(tile_rows(srcR, blk, s), (srcRname, blk, s)), key=("xr", xi))
        for half in range(2):
            b = 3 + (2 * s + half) % 2
            pd = V(P.psb[b], ("psb", b))
            for k in range(nk):
                P.mm(pd, lhs_fn(k, s), V(wo[:, k, half * 512:(half + 1) * 512], wotok),
                     start=(k == 0), stop=(k == nk - 1))
            xh = V(xr[:, half * 512:(half + 1) * 512], ("xr", xi))
            P.tt("dve", xh, pd, xh, ALU.add)
        P.dma("sp", V(tile_rows(dst, blk, s), (dstname, blk, s)), xrv, key=("xr", xi))


def ffn_pass(P, li, c0, c1, srcN, srcNname, srcR, srcRname, dst, dstname):
    mark = P.arena_off
    alloc_common(P)
    nch = c1 - c0
    ncol = nch * 128
    nblk = P.T // TB
    w_up = P.win("ffn_w_up_%d" % li, [D, 2 * FFN_H])
    w_dn = P.win("ffn_w_down_%d" % li, [FFN_H, D])
    c_cw = P.win("c_ffn_cw_%d" % li, [128, 2 * FFN_NC, 3])
    c_cb = P.win("c_ffn_cb_%d" % li, [128, 2 * FFN_NC])
    wupv = P.alloc([KC, ncol], BF16)
    wupg = P.alloc([KC, ncol], BF16)
    wdn = P.alloc([nch, D], BF16)
    cw = P.alloc([2 * FFN_NC, 3])
    cb = P.alloc([2 * FFN_NC])
    hs = [P.alloc([2 * FFN_NC, 2]) for _ in range(2)]
    A = [P.alloc([TB]) for _ in range(6)]
    G = [P.alloc([TB]) for _ in range(2)]
    hid = P.alloc([nch, TB], BF16)
    upsrc = w_up.rearrange("(k p) n -> p k n", p=128)
    P.dma("pool", V(wupv, ("w", 0)), V(upsrc[:, :, c0 * 128:c1 * 128], "in_w"), key=("w", 0))
    P.dma("pool", V(wupg, ("w", 1)), V(upsrc[:, :, FFN_H + c0 * 128:FFN_H + c1 * 128], "in_w"), key=("w", 1))
    P.dma("pool", V(wdn, ("w", 2)), V(w_dn[c0 * 128:c1 * 128, :].rearrange("(c p) n -> p c n", p=128), "in_w"),
          key=("w", 2))
    P.dma("sp", V(cw, "cw"), V(c_cw, "in_w"), key="cw")
    P.dma("sp", V(cb, "cb"), V(c_cb, "in_w"), key="cb")
    P.memset("pool", V(hs[0], ("hs", 0)), 0.0)
    P.memset("pool", V(hs[1], ("hs", 1)), 0.0)

    def stageB(blk, slot, tick):
        xnT = P.xnT[slot]
        par = blk % 2
        ubanks = (0, 1, 2, 5, 6)
        tick_at = set(int(round(x)) for x in np.linspace(1, 2 * nch - 2, 4))
        for c in range(nch):
            for part in range(2):
                q = 2 * c + part
                if q in tick_at:
                    tick()
                cp = (c0 + c) + part * FFN_NC
                wsel = wupv if part == 0 else wupg
                wtok = ("w", part)
                bi = ubanks[q % 5]
                pu = V(P.psb[bi], ("psb", bi))
                for kc in range(KC):
                    P.mm(pu, V(wsel[:, kc, c * 128:(c + 1) * 128], wtok),
                         V(xnT[:, kc, :], ("xnT", slot)), start=(kc == 0), stop=(kc == KC - 1))
                ai = q % 6
                At = A[ai]
                atok = ("A", ai)
                P.act(V(At[:, 0:TB], atok), pu, AF.Identity,
                      scale=V(cw[:, cp, 2:3], "cw"), bias=V(cb[:, cp:cp + 1], "cb"))
                P.copy("act", V(hs[par][:, cp, :], ("hs", par, cp)), V(P.psb[bi][:, TB - 2:TB], ("psb", bi)))
                P.stt(V(At[:, 1:TB], atok), V(P.psb[bi][:, 0:TB - 1], ("psb", bi)), V(cw[:, cp, 1:2], "cw"),
                      V(At[:, 1:TB], atok), ALU.mult, ALU.add)
                P.stt(V(At[:, 2:TB], atok), V(P.psb[bi][:, 0:TB - 2], ("psb", bi)), V(cw[:, cp, 0:1], "cw"),
                      V(At[:, 2:TB], atok), ALU.mult, ALU.add)
                hp = V(hs[1 - par][:, cp, :], ("hs", 1 - par, cp))
                P.stt(V(At[:, 0:2], atok), hp, V(cw[:, cp, 0:1], "cw"), V(At[:, 0:2], atok), ALU.mult, ALU.add)
                P.stt(V(At[:, 0:1], atok), V(hs[1 - par][:, cp, 1:2], ("hs", 1 - par, cp)), V(cw[:, cp, 1:2], "cw"),
                      V(At[:, 0:1], atok), ALU.mult, ALU.add)
                if part == 0:
                    Aval, avtok = At, atok
                else:
                    Gt = G[c % 2]
                    gtok = ("G", c % 2)
                    P.act(V(Gt, gtok), V(At[:, 0:TB], atok), AF.Silu)
                    P.tt("pool", V(hid[:, c, :], ("hid", c)), V(Aval[:, 0:TB], avtok), V(Gt, gtok), ALU.mult)
        out_stage(P, blk, srcR, srcRname, dst, dstname,
                  lambda k, s: V(hid[:, k, s * 128:(s + 1) * 128], ("hid", k)), nch, wdn, ("w", 2))

    run_blocks(P, srcN, srcNname, 4 + li, stageB)
    P.arena_reset(mark)


def final_norm(P, src, srcname, dst, dstname):
    mark = P.arena_off
    alloc_common(P)
    nfb = P.alloc([D])
    c_nf = P.win("c_nfb", [D])
    P.dma("sp", V(nfb, "nfb"), V(c_nf.partition_broadcast(128), "in_w"), key="const2")
    nblk = P.T // TB
    n = 0
    for blk in range(nblk):
        for s in range(4):
            xi = n % 2
            n += 1
            xt, xo = P.xt[xi], P.xr[xi]
            xtv = V(xt, ("xt", xi))
            P.dma("sp", xtv, V(tile_rows(src, blk, s), (srcname, blk, s)), key=("xt", xi))
            ssv = V(P.ss[xi][:, 0:1], ("ss", xi))
            rv = V(P.rstd[xi][:, 0:1], ("rstd", xi))
            P.act(V(P.junk, "junk"), xtv, AF.Square, accum=ssv)
            P.act(rv, ssv, AF.Sqrt, scale=1.0 / D, bias=V(P.epsv, "epsv"))
            P.recip(rv, rv)
            P.stt(V(xo, ("xr", xi)), xtv, rv, V(nfb, "nfb"), ALU.mult, ALU.mult)
            P.dma("sp", V(tile_rows(dst, blk, s), (dstname, blk, s)), V(xo, ("xr", xi)), key=("xr", xi))
    P.arena_reset(mark)


RET_H = 4
RET_DK = 256
RET_DV = 512


def retnet_pass(P, h0, srcN, srcNname, srcR, srcRname, dst, dstname):
    mark = P.arena_off
    alloc_common(P)
    nblk = P.T // TB
    T = P.T
    w_in = P.win("ret_w_in_p", [D, 6144])
    w_out = P.win("ret_w_out", [2048, D])
    c_cos = P.win("c_ret_cos", [128, 4096])
    c_sin = P.win("c_ret_sin", [128, 4096])
    c_decT = P.win("c_ret_decT", [128, RET_H, 128])
    c_gl = P.win("c_ret_gl", [RET_H, TB])
    c_kdec = P.win("c_ret_kdec", [128, RET_H])
    wq = P.alloc([KC, 512], BF16)
    wk = P.alloc([KC, 512], BF16)
    wv = P.alloc([KC, 1024], BF16)
    wg = P.alloc([KC, 1024], BF16)
    wo = P.alloc([8, D], BF16)
    cos = P.alloc([TB])
    sin = P.alloc([TB])
    qT = P.alloc([2, 2, TB], BF16)
    kT = P.alloc([2, 2, TB], BF16)
    qg = P.alloc([2, 2, TB], BF16)
    rt = [P.alloc([TB]) for _ in range(4)]
    vt = P.alloc([4, 2, 512], BF16)
    khat = P.alloc([4, 2, 256], BF16)
    sg = P.alloc([8, TB], BF16)
    yT = P.alloc([8, TB], BF16)
    S = P.alloc([2, 2, 512])
    Sbf = P.alloc([2, 2, 512], BF16)
    decT = P.alloc([RET_H, 128])
    gl = P.alloc([2, TB])
    kdec = P.alloc([RET_H])
    PT = [P.alloc([128], BF16) for _ in range(2)]
    ysq = [P.alloc([512], BF16) for _ in range(2)]
    rs = [P.alloc([128]) for _ in range(2)]
    tmp = [P.alloc([4, 128]) for _ in range(2)]
    src = w_in.rearrange("(k p) n -> p k n", p=128)
    P.dma("pool", V(wq, ("w", 0)), V(src[:, :, h0 * 256:h0 * 256 + 512], "in_w"), key=("w", 0))
    P.dma("pool", V(wk, ("w", 1)), V(src[:, :, 1024 + h0 * 256:1024 + h0 * 256 + 512], "in_w"), key=("w", 1))
    P.dma("pool", V(wv, ("w", 2)), V(src[:, :, 2048 + h0 * 512:2048 + h0 * 512 + 1024], "in_w"), key=("w", 2))
    P.dma("pool", V(wg, ("w", 3)), V(src[:, :, 4096 + h0 * 512:4096 + h0 * 512 + 1024], "in_w"), key=("w", 3))
    P.dma("pool", V(wo, ("w", 4)), V(w_out[h0 * 512:h0 * 512 + 1024, :].rearrange("(c p) n -> p c n", p=128), "in_w"),
          key=("w", 4))
    P.dma("sp", V(decT, "decT"), V(c_decT, "in_w"), key="c0")
    P.dma("sp", V(kdec, "kdec"), V(c_kdec, "in_w"), key="c1")
    for hl in range(2):
        P.dma("sp", V(gl[:, hl, :], ("gl", hl)), V(c_gl[h0 + hl].partition_broadcast(128), "in_w"), key=("c2", hl))
    P.memset("dve", V(S, "S"), 0.0)
    P.memset("pool", V(Sbf, "Sbf"), 0.0)
    g128 = [float((1.0 - 2.0 ** (-5.0 - (h0 + hl))) ** 128) for hl in range(2)]
    pn = [0]

    def pbank():
        b = pn[0] % 3
        pn[0] += 1
        return V(P.psb[b], ("psb", b))

    def stageB(blk, slot, tick):
        xnT = P.xnT[slot]
        xv = V(xnT, ("xnT", slot))
        P.dma("sp", V(cos, "cos"), V(c_cos[:, blk * TB:(blk + 1) * TB], "in_w"), key="cos")
        P.dma("sp", V(sin, "sin"), V(c_sin[:, blk * TB:(blk + 1) * TB], "in_w"), key="sin")
        for (wsel, wtok, dstT, dname) in ((wq, ("w", 0), qT, "qT"), (wk, ("w", 1), kT, "kT")):
            for hl in range(2):
                p1 = pbank()
                for kc in range(KC):
                    P.mm(p1, V(wsel[:, kc, hl * 256:hl * 256 + 128], wtok), V(xnT[:, kc, :], ("xnT", slot)),
                         start=(kc == 0), stop=(kc == KC - 1))
                p2 = pbank()
                for kc in range(KC):
                    P.mm(p2, V(wsel[:, kc, hl * 256 + 128:hl * 256 + 256], wtok), V(xnT[:, kc, :], ("xnT", slot)),
                         start=(kc == 0), stop=(kc == KC - 1))
                r = [V(rt[i], ("rt", i)) for i in range(4)]
                P.tt("dve", r[0], p1, V(cos, "cos"), ALU.mult)
                P.tt("dve", r[1], p2, V(sin, "sin"), ALU.mult)
                P.tt("dve", r[2], p2, V(cos, "cos"), ALU.mult)
                P.tt("dve", r[3], p1, V(sin, "sin"), ALU.mult)
                P.tt("pool", V(dstT[:, hl, 0, :], (dname, hl, 0)), r[0], r[1], ALU.subtract)
                P.tt("pool", V(dstT[:, hl, 1, :], (dname, hl, 1)), r[2], r[3], ALU.add)
                if dname == "qT":
                    for e in range(2):
                        P.tt("pool", V(qg[:, hl, e, :], ("qg", hl, e)), V(qT[:, hl, e, :], ("qT", hl, e)),
                             V(gl[:, hl, :], ("gl", hl)), ALU.mult)
        tick()
        for hl in range(2):
            for j in range(4):
                pg = pbank()
                c = hl * 512 + j * 128
                for kc in range(KC):
                    P.mm(pg, V(wg[:, kc, c:c + 128], ("w", 3)), V(xnT[:, kc, :], ("xnT", slot)),
                         start=(kc == 0), stop=(kc == KC - 1))
                P.act(V(sg[:, hl * 4 + j, :], ("sg", hl, j)), pg, AF.Silu)
        tick()
        for c4 in range(4):
            for hl in range(2):
                pv = pbank()
                for kc in range(KC):
                    P.mm(pv, V(xnT[:, kc, c4 * 128:(c4 + 1) * 128], ("xnT", slot)),
                         V(wv[:, kc, hl * 512:(hl + 1) * 512], ("w", 2)), start=(kc == 0), stop=(kc == KC - 1))
                P.copy("act", V(vt[:, c4, hl, :], ("vt", c4, hl)), pv)
        tick()
        psT6 = P.psb[6].bitcast(BF16)
        for c4 in range(4):
            for hl in range(2):
                for e in range(2):
                    P.transpose(V(psT6[:, e * 128:(e + 1) * 128], ("psb", 6)),
                                V(kT[:, hl, e, c4 * 128:(c4 + 1) * 128], ("kT", hl, e)), V(P.ident, "ident"))
                P.act(V(khat[:, c4, hl, :], ("khat", c4, hl)), V(psT6[:, 0:256], ("psb", 6)), AF.Identity,
                      scale=V(kdec[:, h0 + hl:h0 + hl + 1], "kdec"))
        tick()
        n = 0
        for c4 in range(4):
            sl = slice(c4 * 128, (c4 + 1) * 128)
            for hl in range(2):
                h = h0 + hl
                i2 = n % 2
                n += 1
                psS = V(P.psb[3][:, 0:128], ("psb", 3))
                for e in range(2):
                    P.mm(psS, V(kT[:, hl, e, sl], ("kT", hl, e)), V(qT[:, hl, e, sl], ("qT", hl, e)),
                         start=(e == 0), stop=(e == 1))
                ptv = V(PT[i2], ("PT", i2))
                P.tt("dve", ptv, psS, V(decT[:, h, :], "decT"), ALU.mult)
                psO = P.psb[4]
                for j in range(4):
                    po = V(psO[:, j * 128:(j + 1) * 128], ("psb", 4))
                    P.mm(po, V(vt[:, c4, hl, j * 128:(j + 1) * 128], ("vt", c4, hl)), ptv, start=True, stop=False)
                    for e in range(2):
                        P.mm(po, V(Sbf[:, hl, e, j * 128:(j + 1) * 128], ("Sbf", hl, e)),
                             V(qg[:, hl, e, sl], ("qg", hl, e)), start=False, stop=(e == 1))
                pov = V(psO, ("psb", 4))
                yq = V(ysq[i2], ("ysq", i2))
                P.act(yq, pov, AF.Square)
                psN = V(P.psb[3][:, 128:256], ("psb", 3))
                for j in range(4):
                    P.mm(psN, V(P.ones, "ones"), V(ysq[i2][:, j * 128:(j + 1) * 128], ("ysq", i2)),
                         start=(j == 0), stop=(j == 3))
                rv = V(rs[i2], ("rs", i2))
                P.act(rv, psN, AF.Sqrt, scale=1.0 / RET_DV, bias=V(P.epsv, "epsv"))
                P.recip(rv, rv)
                tv = V(tmp[i2], ("tmp", i2))
                P.tt("dve", tv, V(psO.rearrange("p (j l) -> p j l", j=4), ("psb", 4)),
                     V(rs[i2].unsqueeze(1).broadcast_to([128, 4, 128]), ("rs", i2)), ALU.mult)
                P.tt("pool", V(yT[:, hl * 4:(hl + 1) * 4, sl], ("yT", hl, c4)), tv,
                     V(sg[:, hl * 4:(hl + 1) * 4, sl], ("sg", hl)), ALU.mult)
                for e in range(2):
                    pu = V(P.psb[5], ("psb", 5))
                    P.mm(pu, V(khat[:, c4, hl, e * 128:(e + 1) * 128], ("khat", c4, hl)),
                         V(vt[:, c4, hl, :], ("vt", c4, hl)), start=True, stop=True)
                    sv = V(S[:, hl, e, :], ("S", hl, e))
                    P.stt(sv, sv, g128[hl], pu, ALU.mult, ALU.add)
                    P.copy("pool", V(Sbf[:, hl, e, :], ("Sbf", hl, e)), sv)
        out_stage(P, blk, srcR, srcRname, dst, dstname,
                  lambda k, s: V(yT[:, k, s * 128:(s + 1) * 128], ("yT",)), 8, wo, ("w", 4))

    run_blocks(P, srcN, srcNname, 3, stageB)
    P.arena_reset(mark)


GLA_H = 4
GLA_DK = 128
GLA_DV = 256


def gla_pass(P, srcN, srcNname, srcR, srcRname, dst, dstname):
    mark = P.arena_off
    alloc_common(P)
    nblk = P.T // TB
    w_in = P.win("gla_w_in", [D, 3088])
    w_out = P.win("gla_w_out", [D, D])
    w_gk2 = P.win("gla_w_gk2", [16, 512])
    c_bgk = P.win("c_gla_bgk", [128, GLA_H])
    c_nw = P.win("c_gla_nw", [128, 2])
    c_maskT = P.win("c_maskT", [128, 128])
    c_scanm = P.win("c_scanm", [TB])
    wq = P.alloc([KC, 512], BF16)
    wk = P.alloc([KC, 512], BF16)
    wv = P.alloc([KC, 1024], BF16)
    wg = P.alloc([KC, 1024], BF16)
    wgk = P.alloc([KC, 16], BF16)
    wo = P.alloc([8, D], BF16)
    wgk2 = P.alloc([512])
    bgk = P.alloc([GLA_H])
    nbgk = P.alloc([GLA_H])
    nw = P.alloc([2])
    maskT = P.alloc([128])
    scanm = P.alloc([TB])
    gkf = P.alloc([TB])
    Gp = P.alloc([GLA_H, TB])
    et = [P.alloc([TB]) for _ in range(2)]
    qT = P.alloc([GLA_H, TB], BF16)
    kT = P.alloc([GLA_H, TB], BF16)
    vt = P.alloc([4, 1024], BF16)
    khat = P.alloc([4, GLA_H, 128], BF16)
    sg = P.alloc([8, TB], BF16)
    yT = P.alloc([8, TB], BF16)
    S = P.alloc([GLA_H, 256])
    Sbf = P.alloc([GLA_H, 256], BF16)
    elast = P.alloc([GLA_H, 4])
    PT = [P.alloc([128], BF16) for _ in range(2)]
    ysq = [P.alloc([256], BF16) for _ in range(2)]
    rs = [P.alloc([128]) for _ in range(2)]
    tmp = [P.alloc([2, 128]) for _ in range(2)]
    src = w_in.rearrange("(k p) n -> p k n", p=128)
    P.dma("pool", V(wq, ("w", 0)), V(src[:, :, 0:512], "in_w"), key=("w", 0))
    P.dma("pool", V(wk, ("w", 1)), V(src[:, :, 512:1024], "in_w"), key=("w", 1))
    P.dma("pool", V(wv, ("w", 2)), V(src[:, :, 1024:2048], "in_w"), key=("w", 2))
    P.dma("pool", V(wg, ("w", 3)), V(src[:, :, 2048:3072], "in_w"), key=("w", 3))
    P.dma("pool", V(wgk, ("w", 5)), V(src[:, :, 3072:3088], "in_w"), key=("w", 5))
    P.dma("pool", V(wo, ("w", 4)), V(w_out.rearrange("(c p) n -> p c n", p=128), "in_w"), key=("w", 4))
    P.dma("sp", V(wgk2[0:16, :], "wgk2"), V(w_gk2, "in_w"), key="c0")
    P.dma("sp", V(bgk, "bgk"), V(c_bgk, "in_w"), key="c1")
    P.dma("sp", V(nw, "nw"), V(c_nw, "in_w"), key="c2")
    P.dma("sp", V(maskT, "maskT"), V(c_maskT, "in_w"), key="c3")
    P.dma("sp", V(scanm, "scanm"), V(c_scanm.partition_broadcast(128), "in_w"), key="c4")
    P.ts("dve", V(nbgk, "nbgk"), V(bgk, "bgk"), -1.0, ALU.mult)
    P.memset("dve", V(S, "S"), 0.0)
    P.memset("pool", V(Sbf, "Sbf"), 0.0)
    lnsc = float(np.log(GLA_DK ** -0.5))
    pn = [0]

    def pbank():
        b = pn[0] % 3
        pn[0] += 1
        return V(P.psb[b], ("psb", b))

    def stageB(blk, slot, tick):
        xnT = P.xnT[slot]
        xtok = ("xnT", slot)
        pg = pbank()
        for kc in range(KC):
            P.mm(V(pg.ap[0:16, :], pg.tok), V(wgk[:, kc, :], ("w", 5)), V(xnT[:, kc, :], xtok),
                 start=(kc == 0), stop=(kc == KC - 1))
        P.copy("act", V(gkf[0:16, :], "gkf"), V(pg.ap[0:16, :], pg.tok))
        for h in range(GLA_H):
            pp = pbank()
            P.mm(pp, V(wgk2[0:16, h * 128:(h + 1) * 128], "wgk2"), V(gkf[0:16, :], "gkf"), start=True, stop=True)
            e0 = V(et[0], ("et", 0))
            P.act(e0, pp, AF.Exp, scale=-1.0, bias=V(nbgk[:, h:h + 1], "nbgk"))
            P.act(e0, e0, AF.Ln, scale=1.0, bias=1.0)
            gph = V(Gp[:, h, :], ("Gp", h))
            nc = P.nc
            P.S.op("dve", (lambda o=gph.ap, a=scanm, b=et[0]: nc.vector.tensor_tensor_scan(o, a, b, 0.0, ALU.mult, ALU.add)),
                   reads=[("scanm",), ("et", 0)], writes=[gph.tok])
            pq = pbank()
            for kc in range(KC):
                P.mm(pq, V(wq[:, kc, h * 128:(h + 1) * 128], ("w", 0)), V(xnT[:, kc, :], xtok),
                     start=(kc == 0), stop=(kc == KC - 1))
            e1 = V(et[1], ("et", 1))
            P.act(e1, gph, AF.Exp, scale=-1.0 / 16.0, bias=lnsc)
            P.tt("dve", V(qT[:, h, :], ("qT", h)), pq, e1, ALU.mult)
            pk = pbank()
            for kc in range(KC):
                P.mm(pk, V(wk[:, kc, h * 128:(h + 1) * 128], ("w", 1)), V(xnT[:, kc, :], xtok),
                     start=(kc == 0), stop=(kc == KC - 1))
            P.act(e1, gph, AF.Exp, scale=1.0 / 16.0)
            P.tt("dve", V(kT[:, h, :], ("kT", h)), pk, e1, ALU.mult)
            P.act(V(elast[:, h, :], ("elast", h)),
                  V(Gp[:, h, :].rearrange("p (c l) -> p c l", c=4)[:, :, 127], ("Gp", h)), AF.Exp, scale=-1.0 / 16.0)
        tick()
        for c in range(8):
            pg2 = pbank()
            for kc in range(KC):
                P.mm(pg2, V(wg[:, kc, c * 128:(c + 1) * 128], ("w", 3)), V(xnT[:, kc, :], xtok),
                     start=(kc == 0), stop=(kc == KC - 1))
            P.act(V(sg[:, c, :], ("sg", c)), pg2, AF.Silu)
            P.ts("pool", V(sg[:, c, :], ("sg", c)), V(sg[:, c, :], ("sg", c)), V(nw[:, (c % 2):(c % 2) + 1], "nw"), ALU.mult)
        tick()
        for c4 in range(4):
            for half in range(2):
                pv = pbank()
                for kc in range(KC):
                    P.mm(pv, V(xnT[:, kc, c4 * 128:(c4 + 1) * 128], xtok),
                         V(wv[:, kc, half * 512:(half + 1) * 512], ("w", 2)), start=(kc == 0), stop=(kc == KC - 1))
                P.copy("act", V(vt[:, c4, half * 512:(half + 1) * 512], ("vt", c4, half)), pv)
        tick()
        psT6 = P.psb[6].bitcast(BF16)
        for c4 in range(4):
            for h in range(GLA_H):
                P.transpose(V(psT6[:, h * 128:(h + 1) * 128], ("psb", 6)),
                            V(kT[:, h, c4 * 128:(c4 + 1) * 128], ("kT", h)), V(P.ident, "ident"))
            P.copy("act", V(khat[:, c4, :, :], ("khat", c4)),
                   V(psT6[:, 0:512].rearrange("p (h d) -> p h d", h=GLA_H), ("psb", 6)))
        tick()
        n = 0
        for c4 in range(4):
            sl = slice(c4 * 128, (c4 + 1) * 128)
            for h in range(GLA_H):
                i2 = n % 2
                n += 1
                psS = V(P.psb[3][:, 0:128], ("psb", 3))
                P.mm(psS, V(kT[:, h, sl], ("kT", h)), V(qT[:, h, sl], ("qT", h)), start=True, stop=True)
                ptv = V(PT[i2], ("PT", i2))
                P.tt("dve", ptv, psS, V(maskT, "maskT"), ALU.mult)
                psO = P.psb[4]
                for j in range(2):
                    po = V(psO[:, j * 128:(j + 1) * 128], ("psb", 4))
                    vc = h * 256 + j * 128
                    P.mm(po, V(vt[:, c4, vc:vc + 128], ("vt", c4, vc // 512)), ptv, start=True, stop=False)
                    P.mm(po, V(Sbf[:, h, j * 128:(j + 1) * 128], ("Sbf", h)), V(qT[:, h, sl], ("qT", h)),
                         start=False, stop=True)
                pov = V(psO[:, 0:256], ("psb", 4))
                yq = V(ysq[i2], ("ysq", i2))
                P.act(yq, pov, AF.Square)
                psN = V(P.psb[3][:, 128:256], ("psb", 3))
                for j in range(2):
                    P.mm(psN, V(P.ones, "ones"), V(ysq[i2][:, j * 128:(j + 1) * 128], ("ysq", i2)),
                         start=(j == 0), stop=(j == 1))
                rv = V(rs[i2], ("rs", i2))
                P.act(rv, psN, AF.Sqrt, scale=1.0 / GLA_DV, bias=V(P.epsv, "epsv"))
                P.recip(rv, rv)
                tv = V(tmp[i2], ("tmp", i2))
                P.tt("dve", tv, V(psO[:, 0:256].rearrange("p (j l) -> p j l", j=2), ("psb", 4)),
                     V(rs[i2].unsqueeze(1).broadcast_to([128, 2, 128]), ("rs", i2)), ALU.mult)
                P.tt("pool", V(yT[:, h * 2:(h + 1) * 2, sl], ("yT", h, c4)), tv,
                     V(sg[:, h * 2:(h + 1) * 2, sl], ("sg",)), ALU.mult)
                pu = V(P.psb[5][:, 0:256], ("psb", 5))
                P.mm(pu, V(khat[:, c4, h, :], ("khat", c4)), V(vt[:, c4, h * 256:(h + 1) * 256], ("vt", c4, h // 2)),
                     start=True, stop=True)
                sv = V(S[:, h, :], ("S", h))
                P.tt("dve", sv, sv, pu, ALU.add)
                P.ts("dve", sv, sv, V(elast[:, h, c4:c4 + 1], ("elast", h)), ALU.mult)
                P.copy("pool", V(Sbf[:, h, :], ("Sbf", h)), sv)
        out_stage(P, blk, srcR, srcRname, dst, dstname,
                  lambda k, s: V(yT[:, k, s * 128:(s + 1) * 128], ("yT",)), 8, wo, ("w", 4))

    run_blocks(P, srcN, srcNname, 2, stageB)
    P.arena_reset(mark)


def ssd_pass(P, p, srcN, srcNname, srcR, srcRname, dst, dstname):
    mark = P.arena_off
    alloc_common(P)
    nc = P.nc
    nblk = P.T // TB
    w_in = P.win("ssd_w_in", [D, 6176])
    w_out = P.win("ssd_w_out", [2048, D])
    c_cw = P.win("c_ssd_cw", [128, 32, 4])
    c_cb = P.win("c_ssd_cb", [128, 32])
    c_dtb = P.win("ssd_dt_bias", [32])
    c_alog = P.win("ssd_a_log", [32])
    c_dsk = P.win("ssd_d", [32])
    c_nw = P.win("ssd_norm_w", [2048])
    c_maskT = P.win("c_maskT", [128, 128])
    c_SU = P.win("c_SU", [128, 128])
    wz = P.alloc([KC, 1024], BF16)
    wxs = P.alloc([KC, 1024], BF16)
    wB = P.alloc([KC, 512], BF16)
    wC = P.alloc([KC, 512], BF16)
    wdt = P.alloc([KC, 16], BF16)
    wo = P.alloc([8, D], BF16)
    cw = P.alloc([32, 4])
    cb = P.alloc([32])
    spill = P.alloc([16, 3])
    dtb = P.alloc([16])
    abc = P.alloc([16])
    dsk = P.alloc([16])
    nwc = P.alloc([1024])
    maskT = P.alloc([128])
    SU = P.alloc([128])
    onesf = P.alloc([128])
    A = [P.alloc([TB + 3]) for _ in range(3)]
    xsT = P.alloc([8, TB], BF16)
    kT = P.alloc([4, TB], BF16)
    qT = P.alloc([4, TB], BF16)
    sz = P.alloc([4, 1024], BF16)
    dtt = P.alloc([16])
    ld = P.alloc([16])
    Gs = P.alloc([16])
    eG = P.alloc([16])
    eGl = P.alloc([16])
    wdec = P.alloc([16])
    tiny = P.alloc([16])
    R = [P.alloc([4, 128]) for _ in range(2)]
    dec = [P.alloc([4, 128]) for _ in range(2)]
    sm = [P.alloc([128]) for _ in range(2)]
    PT = [P.alloc([4, 128], BF16) for _ in range(2)]
    xk = [P.alloc([384], BF16) for _ in range(2)]
    vv = [P.alloc([256], BF16) for _ in range(2)]
    vh = [P.alloc([256], BF16) for _ in range(2)]
    ot = [P.alloc([256]) for _ in range(2)]
    t2 = [P.alloc([256]) for _ in range(2)]
    yv = [P.alloc([256]) for _ in range(2)]
    yn = [P.alloc([256], BF16) for _ in range(2)]
    ssq = [P.alloc([1]) for _ in range(2)]
    yT = P.alloc([8, TB], BF16)
    S = P.alloc([4, 256])
    Sbf = P.alloc([4, 256], BF16)
    src = w_in.rearrange("(k p) n -> p k n", p=128)
    P.dma("pool", V(wz, ("w", 0)), V(src[:, :, p * 1024:(p + 1) * 1024], "in_w"), key=("w", 0))
    P.dma("pool", V(wxs, ("w", 1)), V(src[:, :, 2048 + p * 1024:2048 + (p + 1) * 1024], "in_w"), key=("w", 1))
    P.dma("pool", V(wB, ("w", 2)), V(src[:, :, 4096 + p * 512:4096 + (p + 1) * 512], "in_w"), key=("w", 2))
    P.dma("pool", V(wC, ("w", 3)), V(src[:, :, 5120 + p * 512:5120 + (p + 1) * 512], "in_w"), key=("w", 3))
    P.dma("pool", V(wdt, ("w", 5)), V(src[:, :, 6144 + p * 16:6144 + (p + 1) * 16], "in_w"), key=("w", 5))
    P.dma("pool", V(wo, ("w", 4)), V(w_out[p * 1024:(p + 1) * 1024, :].rearrange("(c p) n -> p c n", p=128), "in_w"),
          key=("w", 4))
    P.dma("sp", V(cw, "cw"), V(c_cw, "in_w"), key="c0")
    P.dma("sp", V(cb, "cb"), V(c_cb, "in_w"), key="c1")
    P.dma("sp", V(dtb, "dtb"), V(c_dtb[p * 16:(p + 1) * 16].partition_broadcast(128), "in_w"), key="c2")
    P.dma("sp", V(abc, "abc"), V(c_alog[p * 16:(p + 1) * 16].partition_broadcast(128), "in_w"), key="c3")
    P.dma("sp", V(dsk, "dsk"), V(c_dsk[p * 16:(p + 1) * 16].partition_broadcast(128), "in_w"), key="c4")
    P.dma("sp", V(nwc, "nwc"), V(c_nw[p * 1024:(p + 1) * 1024].partition_broadcast(128), "in_w"), key="c5")
    P.dma("sp", V(maskT, "maskT"), V(c_maskT, "in_w"), key="c6")
    P.dma("sp", V(SU, "SU"), V(c_SU, "in_w"), key="c7")
    P.memset("pool", V(onesf, "onesf"), 1.0)
    P.memset("pool", V(spill, "spill"), 0.0)
    P.act(V(abc, "abc"), V(abc, "abc"), AF.Exp)
    P.ts("dve", V(abc, "abc"), V(abc, "abc"), -1.0, ALU.mult)
    P.memset("dve", V(S, "S"), 0.0)
    P.memset("pool", V(Sbf, "Sbf"), 0.0)
    pn = [0]

    def pbank():
        b = pn[0] % 3
        pn[0] += 1
        return V(P.psb[b], ("psb", b))

    def conv_chunk(lc, wsel, wtok, col, xnT, slot, dst):
        cc = (8 * p + lc) if lc < 8 else ((16 + 4 * p + lc - 8) if lc < 12 else (24 + 4 * p + lc - 12))
        pu = pbank()
        for kc in range(KC):
            P.mm(pu, V(wsel[:, kc, col:col + 128], wtok), V(xnT[:, kc, :], ("xnT", slot)),
                 start=(kc == 0), stop=(kc == KC - 1))
        ai = lc % 3
        At, atok = A[ai], ("A", ai)
        P.act(V(At[:, 0:TB], atok), pu, AF.Identity, scale=V(cw[:, cc, 3:4], "cw"), bias=V(cb[:, cc:cc + 1], "cb"))
        P.memset("pool", V(At[:, TB:TB + 3], atok), 0.0)
        for sh in (1, 2, 3):
            P.stt(V(At[:, sh:TB + sh], atok), pu, V(cw[:, cc, 3 - sh:4 - sh], "cw"), V(At[:, sh:TB + sh], atok),
                  ALU.mult, ALU.add)
        P.tt("pool", V(At[:, 0:3], atok), V(At[:, 0:3], atok), V(spill[:, lc, :], ("spill", lc)), ALU.add)
        P.copy("pool", V(spill[:, lc, :], ("spill", lc)), V(At[:, TB:TB + 3], atok))
        P.act(dst, V(At[:, 0:TB], atok), AF.Silu)

    def stageB(blk, slot, tick):
        xnT = P.xnT[slot]
        xtok = ("xnT", slot)
        for lc in range(8):
            conv_chunk(lc, wxs, ("w", 1), lc * 128, xnT, slot, V(xsT[:, lc, :], ("xsT", lc)))
        tick()
        for gl in range(4):
            conv_chunk(8 + gl, wB, ("w", 2), gl * 128, xnT, slot, V(kT[:, gl, :], ("kT", gl)))
            conv_chunk(12 + gl, wC, ("w", 3), gl * 128, xnT, slot, V(qT[:, gl, :], ("qT", gl)))
        tick()
        for c4 in range(4):
            for half in range(2):
                pz = pbank()
                for kc in range(KC):
                    P.mm(pz, V(xnT[:, kc, c4 * 128:(c4 + 1) * 128], xtok),
                         V(wz[:, kc, half * 512:(half + 1) * 512], ("w", 0)), start=(kc == 0), stop=(kc == KC - 1))
                P.act(V(sz[:, c4, half * 512:(half + 1) * 512], ("sz", c4, half)), pz, AF.Silu)
        tick()
        n = 0
        psT6 = P.psb[6].bitcast(BF16)
        for c4 in range(4):
            sl = slice(c4 * 128, (c4 + 1) * 128)
            if c4 == 2:
                tick()
            pdt = V(P.psb[4][:, 128:144], ("psb", 4, "d"))
            for kc in range(KC):
                P.mm(pdt, V(xnT[:, kc, sl], xtok), V(wdt[:, kc, :], ("w", 5)), start=(kc == 0), stop=(kc == KC - 1))
            tn = V(tiny, "tiny")
            P.tt("dve", tn, pdt, V(dtb, "dtb"), ALU.add)
            P.act(tn, tn, AF.Exp)
            P.act(V(dtt, "dtt"), tn, AF.Ln, scale=1.0, bias=1.0)
            P.tt("dve", V(ld, "ld"), V(dtt, "dtt"), V(abc, "abc"), ALU.mult)
            pG = V(P.psb[4][:, 144:160], ("psb", 4, "d"))
            P.mm(pG, V(maskT, "maskT"), V(ld, "ld"), start=True, stop=True)
            pGl = V(P.psb[4][:, 160:176], ("psb", 4, "d"))
            P.mm(pGl, V(onesf, "onesf"), V(ld, "ld"), start=True, stop=True)
            P.act(V(Gs, "Gs"), pG, AF.Identity)
            P.act(V(eG, "eG"), pG, AF.Exp)
            P.act(V(eGl, "eGl"), pGl, AF.Exp)
            P.tt("dve", tn, pGl, V(Gs, "Gs"), ALU.subtract)
            P.act(V(wdec, "wdec"), tn, AF.Exp)
            for gl in range(4):
                i2 = n % 2
                n += 1
                hs = slice(gl * 4, gl * 4 + 4)
                Rv = V(R[i2], ("R", i2))
                P.tt("dve", Rv, V(maskT.unsqueeze(1).broadcast_to([128, 4, 128]), "maskT"),
                     V(ld[:, hs].unsqueeze(2).broadcast_to([128, 4, 128]), "ld"), ALU.mult)
                pSeg = V(P.psb[3], ("psb", 3))
                P.mm(pSeg, V(SU, "SU"), V(R[i2].rearrange("p h l -> p (h l)"), ("R", i2)), start=True, stop=True)
                dv_ = V(dec[i2], ("dec", i2))
                P.act(V(dec[i2].rearrange("p h l -> p (h l)"), ("dec", i2)), pSeg, AF.Exp)
                pS = V(P.psb[4][:, 0:128], ("psb", 4, "s"))
                P.mm(pS, V(kT[:, gl, sl], ("kT", gl)), V(qT[:, gl, sl], ("qT", gl)), start=True, stop=True)
                smv = V(sm[i2], ("sm", i2))
                P.tt("dve", smv, pS, V(maskT, "maskT"), ALU.mult)
                ptv = V(PT[i2], ("PT", i2))
                P.tt("pool", ptv, dv_, V(sm[i2].unsqueeze(1).broadcast_to([128, 4, 128]), ("sm", i2)), ALU.mult)
                P.transpose(V(psT6[:, 0:128], ("psb", 6, "a")), V(xsT[:, gl * 2, sl], ("xsT", gl * 2)), V(P.ident, "ident"))
                P.transpose(V(psT6[:, 128:256], ("psb", 6, "a")), V(xsT[:, gl * 2 + 1, sl], ("xsT", gl * 2 + 1)),
                            V(P.ident, "ident"))
                P.transpose(V(psT6[:, 256:384], ("psb", 6, "a")), V(kT[:, gl, sl], ("kT", gl)), V(P.ident, "ident"))
                xkv = V(xk[i2], ("xk", i2))
                P.copy("act", xkv, V(psT6[:, 0:384], ("psb", 6, "a")))
                xs4 = V(xk[i2][:, 0:256].rearrange("p (h d) -> p h d", h=4), ("xk", i2))
                v4 = V(vv[i2].rearrange("p (h d) -> p h d", h=4), ("vv", i2))
                P.tt("dve", v4, xs4, V(dtt[:, hs].unsqueeze(2).broadcast_to([128, 4, 64]), "dtt"), ALU.mult)
                vh4 = V(vh[i2].rearrange("p (h d) -> p h d", h=4), ("vh", i2))
                P.tt("pool", vh4, v4, V(wdec[:, hs].unsqueeze(2).broadcast_to([128, 4, 64]), "wdec"), ALU.mult)
                for hh in range(4):
                    P.mm(V(P.psb[5][:, hh * 64:(hh + 1) * 64], ("psb", 5, "a")), V(PT[i2][:, hh, :], ("PT", i2)),
                         V(vv[i2][:, hh * 64:(hh + 1) * 64], ("vv", i2)), start=True, stop=True)
                pB = V(P.psb[5][:, 256:512], ("psb", 5, "b"))
                P.mm(pB, V(qT[:, gl, sl], ("qT", gl)), V(Sbf[:, gl, :], ("Sbf", gl)), start=True, stop=True)
                o4 = V(ot[i2].rearrange("p (h d) -> p h d", h=4), ("ot", i2))
                P.tt("dve", o4, V(P.psb[5][:, 256:512].rearrange("p (h d) -> p h d", h=4), ("psb", 5, "b")),
                     V(eG[:, hs].unsqueeze(2).broadcast_to([128, 4, 64]), "eG"), ALU.mult)
                ov = V(ot[i2], ("ot", i2))
                P.tt("dve", ov, ov, V(P.psb[5][:, 0:256], ("psb", 5, "a")), ALU.add)
                t24 = V(t2[i2].rearrange("p (h d) -> p h d", h=4), ("t2", i2))
                P.tt("pool", t24, xs4, V(dsk[:, hs].unsqueeze(2).broadcast_to([128, 4, 64]), "dsk"), ALU.mult)
                P.tt("pool", ov, ov, V(t2[i2], ("t2", i2)), ALU.add)
                yvv = V(yv[i2], ("yv", i2))
                P.tt("pool", yvv, ov, V(sz[:, c4, gl * 256:(gl + 1) * 256], ("sz", c4, gl // 2)), ALU.mult)
                sq = V(ssq[i2], ("ssq", i2))
                P.act(V(P.junk[:, 0:256], "junk"), yvv, AF.Square, accum=sq)
                P.act(sq, sq, AF.Sqrt, scale=1.0 / 256.0, bias=V(P.epsv, "epsv"))
                P.recip(sq, sq)
                ynv = V(yn[i2], ("yn", i2))
                P.stt(ynv, yvv, sq, V(nwc[:, gl * 256:(gl + 1) * 256], "nwc"), ALU.mult, ALU.mult)
                for j in range(2):
                    P.transpose(V(psT6[:, 512 + j * 128:512 + (j + 1) * 128], ("psb", 6, "b")),
                                V(yn[i2][:, j * 128:(j + 1) * 128], ("yn", i2)), V(P.ident, "ident"))
                P.copy("act", V(yT[:, gl * 2:(gl + 1) * 2, sl], ("yT", gl, c4)),
                       V(psT6[:, 512:768].rearrange("p (j l) -> p j l", j=2), ("psb", 6, "b")))
                pU = V(P.psb[4][:, 256:512], ("psb", 4, "u"))
                P.mm(pU, V(xk[i2][:, 256:384], ("xk", i2)), V(vh[i2], ("vh", i2)), start=True, stop=True)
                s4 = V(S[:, gl, :].rearrange("p (h d) -> p h d", h=4), ("S", gl))
                P.tt("dve", s4, s4, V(eGl[:, hs].unsqueeze(2).broadcast_to([128, 4, 64]), "eGl"), ALU.mult)
                sv = V(S[:, gl, :], ("S", gl))
                P.tt("dve", sv, sv, pU, ALU.add)
                P.copy("pool", V(Sbf[:, gl, :], ("Sbf", gl)), sv)
        out_stage(P, blk, srcR, srcRname, dst, dstname,
                  lambda k, s: V(yT[:, k, s * 128:(s + 1) * 128], ("yT",)), 8, wo, ("w", 4))

    run_blocks(P, srcN, srcNname, 0, stageB)
    P.arena_reset(mark)


RW_C = 64
RW_GN_EPS = 64e-5


def rwkv_pass(P, p, srcN, srcNname, srcR, srcRname, dst, dstname):
    mark = P.arena_off
    alloc_common(P)
    nc = P.nc
    nblk = P.T // TB
    NK = 4
    c0 = p * 512
    w_rkv = P.win("rwkv_w_rkv", [3, D, D])
    w_out = P.win("rwkv_w_out", [D, D])
    w1d, a1d, g1d = P.win("rwkv_w1", [D, 64]), P.win("rwkv_a1", [D, 64]), P.win("rwkv_g1", [D, 160])
    w2d, a2d, g2d = P.win("rwkv_w2", [64, D]), P.win("rwkv_a2", [64, D]), P.win("rwkv_g2", [160, D])
    c_vec = P.win("c_rwkv_vec", [128, 12, KC])
    c_lnw, c_lnb = P.win("rwkv_ln_w", [D]), P.win("rwkv_ln_b", [D])
    c_m = P.win("c_rwkv_masks", [64, 4, 64])
    c_E = P.win("c_rwkv_E", [128, 2], BF16)
    c_bo = P.win("c_rwkv_bo", [128, 128], BF16)
    c_scm = P.win("c_rwkv_scanm", [TB])
    Wr = P.alloc([KC, 512], BF16)
    Wk = P.alloc([KC, 512], BF16)
    Wv = P.alloc([KC, 512], BF16)
    wo = P.alloc([NK, D], BF16)
    w1 = P.alloc([KC, 64], BF16)
    a1 = P.alloc([KC, 64], BF16)
    g1 = P.alloc([KC, 160], BF16)
    w2 = P.alloc([512], BF16)
    a2 = P.alloc([512], BF16)
    g2A = P.alloc([512], BF16)
    g2B = P.alloc([512], BF16)
    vec = P.alloc([12, KC])
    omka = P.alloc([KC])
    nw0 = P.alloc([KC])
    lnw = P.alloc([512])
    lnb = P.alloc([512])
    msk = P.alloc([4, 64])
    identb = P.alloc([64], BF16)
    Eh = P.alloc([2], BF16)
    bo = P.alloc([128], BF16)
    scm = P.alloc([TB])
    epsg = P.alloc([1])
    tinyb = P.alloc([1])
    xx = P.alloc([KC, TB], BF16)
    xi = [P.alloc([KC, TB], BF16) for _ in range(2)]
    xlast = P.alloc([KC], BF16)
    h1 = P.alloc([TB], BF16)
    ha = P.alloc([TB], BF16)
    hgA = P.alloc([TB], BF16)
    hgB = P.alloc([TB], BF16)
    aTm = [P.alloc([NK, TB], BF16) for _ in range(2)]
    rTm = [P.alloc([NK, TB], BF16) for _ in range(2)]
    bT = P.alloc([NK, TB], BF16)
    kT = P.alloc([NK, TB], BF16)
    rkT = P.alloc([NK, TB], BF16)
    yT = P.alloc([NK, TB], BF16)
    WC = P.alloc([NK, 8])
    f32t = [P.alloc([TB]) for _ in range(8)]
    NS = 2
    Lm = [P.alloc([8, 64], BF16) for _ in range(2)]
    LTm = [P.alloc([8, 64], BF16) for _ in range(2)]
    XT = [P.alloc([8, 64], BF16) for _ in range(2 + NS)]
    AkT = [P.alloc([8, 64], BF16) for _ in range(NS)]
    ArbT = [P.alloc([8, 64], BF16) for _ in range(NS)]
    ArkT = [P.alloc([8, 64], BF16) for _ in range(NS)]
    Zs = P.alloc([512], BF16)
    Us = P.alloc([512], BF16)
    Vtm = [P.alloc([512], BF16) for _ in range(2)]
    BKtm = P.alloc([2, 512], BF16)
    yc = P.alloc([512])
    sq = P.alloc([512])
    bon = P.alloc([512])
    ytm = P.alloc([512], BF16)
    st8 = [P.alloc([8]) for _ in range(4)]
    S = P.alloc([NK, 64])
    Sbf = P.alloc([NK, 64], BF16)
    wsrc = lambda i_: w_rkv[i_].rearrange("(k p) n -> p k n", p=128)
    P.dma("pool", V(Wr, ("w", 0)), V(wsrc(0)[:, :, c0:c0 + 512], "in_w"), key=("w", 0))
    P.dma("pool", V(Wk, ("w", 1)), V(wsrc(1)[:, :, c0:c0 + 512], "in_w"), key=("w", 1))
    P.dma("pool", V(Wv, ("w", 2)), V(wsrc(2)[:, :, c0:c0 + 512], "in_w"), key=("w", 2))
    P.dma("pool", V(wo, ("w", 3)), V(w_out[c0:c0 + 512, :].rearrange("(c p) n -> p c n", p=128), "in_w"), key=("w", 3))
    P.dma("pool", V(w1, ("w", 4)), V(w1d.rearrange("(k p) n -> p k n", p=128), "in_w"), key=("w", 4))
    P.dma("pool", V(a1, ("w", 5)), V(a1d.rearrange("(k p) n -> p k n", p=128), "in_w"), key=("w", 5))
    P.dma("pool", V(g1, ("w", 6)), V(g1d.rearrange("(k p) n -> p k n", p=128), "in_w"), key=("w", 6))
    P.dma("pool", V(w2[0:64, :], ("w", 7)), V(w2d[:, c0:c0 + 512], "in_w"), key=("w", 7))
    P.dma("pool", V(a2[0:64, :], ("w", 8)), V(a2d[:, c0:c0 + 512], "in_w"), key=("w", 8))
    P.dma("pool", V(g2A, ("w", 9)), V(g2d[0:128, c0:c0 + 512], "in_w"), key=("w", 9))
    P.dma("pool", V(g2B[0:32, :], ("w", 10)), V(g2d[128:160, c0:c0 + 512], "in_w"), key=("w", 10))
    P.dma("sp", V(vec, "vec"), V(c_vec, "in_w"), key="c0")
    P.dma("sp", V(lnw[0:64, :], "lnw"), V(c_lnw[c0:c0 + 512].partition_broadcast(64), "in_w"), key="c1")
    P.dma("sp", V(lnb[0:64, :], "lnb"), V(c_lnb[c0:c0 + 512].partition_broadcast(64), "in_w"), key="c2")
    P.dma("sp", V(msk[0:64, :, :], "msk"), V(c_m, "in_w"), key="c3")
    P.dma("sp", V(Eh, "Eh"), V(c_E, "in_w"), key="c4")
    P.dma("sp", V(bo, "bo"), V(c_bo, "in_w"), key="c5")
    P.dma("sp", V(scm, "scm"), V(c_scm.partition_broadcast(128), "in_w"), key="c6")
    P.ts("dve", V(omka, "omka"), V(vec[:, 9, :], "vec"), -1.0, ALU.mult, 1.0, ALU.add)
    P.ts("dve", V(nw0, "nw0"), V(vec[:, 6, :], "vec"), -1.0, ALU.mult)
    P.copy("dve", V(identb[0:64, :], "identb"), V(msk[0:64, 3, :], "msk"))
    P.memset("pool", V(epsg, "epsg"), RW_GN_EPS)
    P.memset("pool", V(tinyb, "tinyb"), 1e-24)
    P.memset("pool", V(xlast, "xlast"), 0.0)
    P.memset("dve", V(S, "S"), 0.0)
    P.memset("pool", V(Sbf, "Sbf"), 0.0)
    for e_ in range(2):
        P.memset("pool", V(aTm[e_], ("aT", e_)), 0.0)
        P.memset("pool", V(rTm[e_], ("rT", e_)), 0.0)
    pn = [0]

    def pbank():
        b = pn[0] % 3
        pn[0] += 1
        return V(P.psb[b], ("psb", b))

    def mask(i_):
        return V(msk[0:64, i_, :].unsqueeze(1).broadcast_to([64, 8, 64]), "msk")

    def vcol(i_, kc):
        return V(vec[:, i_, kc:kc + 1], "vec")

    def mix(i_, xnT, xtok, buf):
        o = xi[buf]
        for kc in range(KC):
            if kc % 2 == 0:
                P.stt(V(o[:, kc, :], ("xi", buf, kc)), V(xx[:, kc, :], ("xx", kc)), vcol(i_, kc),
                      V(xnT[:, kc, :], xtok), ALU.mult, ALU.add)
            else:
                P.ts("pool", V(o[:, kc, :], ("xi", buf, kc)), V(xx[:, kc, :], ("xx", kc)), vcol(i_, kc), ALU.mult)
                P.tt("pool", V(o[:, kc, :], ("xi", buf, kc)), V(o[:, kc, :], ("xi", buf, kc)), V(xnT[:, kc, :], xtok), ALU.add)
        return o, ("xi", buf)

    def stage1(blk, slot, tick):
        xnT = P.xnT[slot]
        xtok = ("xnT", slot)
        P.tt("dve", V(xx[:, :, 1:TB], "xx"), V(xnT[:, :, 0:TB - 1], xtok), V(xnT[:, :, 1:TB], xtok), ALU.subtract)
        P.tt("dve", V(xx[:, :, 0:1], "xx"), V(xlast.unsqueeze(2), "xlast"), V(xnT[:, :, 0:1], xtok), ALU.subtract)
        P.copy("pool", V(xlast.unsqueeze(2), "xlast"), V(xnT[:, :, TB - 1:TB], xtok))
        xw, xwtok = mix(1, xnT, xtok, 0)
        pw = pbank()
        for kc in range(KC):
            P.mm(V(pw.ap[0:64, :], pw.tok), V(w1[:, kc, :], ("w", 4)), V(xw[:, kc, :], xwtok), start=(kc == 0), stop=(kc == KC - 1))
        P.act(V(h1[0:64, :], "h1"), V(pw.ap[0:64, :], pw.tok), AF.Tanh)
        xa, xatok = mix(4, xnT, xtok, 1)
        pa = pbank()
        for kc in range(KC):
            P.mm(V(pa.ap[0:64, :], pa.tok), V(a1[:, kc, :], ("w", 5)), V(xa[:, kc, :], xatok), start=(kc == 0), stop=(kc == KC - 1))
        P.copy("act", V(ha[0:64, :], "ha"), V(pa.ap[0:64, :], pa.tok))
        xg, xgtok = mix(5, xnT, xtok, 0)
        pg = pbank()
        for kc in range(KC):
            P.mm(pg, V(g1[:, kc, 0:128], ("w", 6)), V(xg[:, kc, :], xgtok), start=(kc == 0), stop=(kc == KC - 1))
        P.act(V(hgA, "hgA"), pg, AF.Sigmoid)
        pg = pbank()
        for kc in range(KC):
            P.mm(V(pg.ap[0:32, :], pg.tok), V(g1[:, kc, 128:160], ("w", 6)), V(xg[:, kc, :], xgtok), start=(kc == 0), stop=(kc == KC - 1))
        P.act(V(hgB[0:32, :], "hgB"), V(pg.ap[0:32, :], pg.tok), AF.Sigmoid)
        tick()
        xk_, xktok = mix(2, xnT, xtok, 1)
        xr_, xrtok = mix(0, xnT, xtok, 0)
        for kc in range(NK):
            gk = 4 * p + kc
            if kc == 2:
                tick()
            cs = slice(kc * 128, (kc + 1) * 128)
            t = [V(f32t[j], ("f32t", j)) for j in range(8)]
            pz = pbank()
            P.mm(pz, V(w2[0:64, cs], ("w", 7)), V(h1[0:64, :], "h1"), start=True, stop=True)
            P.act(t[0], pz, AF.Exp, scale=-1.0, bias=V(nw0[:, gk:gk + 1], "nw0"))
            P.act(t[0], t[0], AF.Ln, scale=1.0, bias=1.0)
            P.act(t[0], t[0], AF.Exp, scale=-1.0, bias=-0.5)
            P.S.op("dve", (lambda o=f32t[1], a_=scm, b_=f32t[0]: nc.vector.tensor_tensor_scan(o, a_, b_, 0.0, ALU.mult, ALU.add)),
                   reads=[("scm",), ("f32t", 0)], writes=[("f32t", 1)])
            P.tt("pool", t[2], t[1], t[0], ALU.subtract)
            P.act(t[2], t[2], AF.Exp, scale=-1.0)
            P.act(t[3], t[1], AF.Exp, scale=1.0)
            P.act(t[1], t[1], AF.Exp, scale=-1.0)
            P.copy("pool", V(WC[:, kc, :], ("WC", kc)), V(f32t[1].rearrange("p (c l) -> p c l", c=8)[:, :, RW_C - 1], ("f32t", 1)))
            pa2 = pbank()
            P.mm(pa2, V(a2[0:64, cs], ("w", 8)), V(ha[0:64, :], "ha"), start=True, stop=True)
            P.act(t[4], pa2, AF.Sigmoid, scale=1.0, bias=vcol(7, gk))
            pk = pbank()
            for k8 in range(KC):
                P.mm(pk, V(Wk[:, k8, cs], ("w", 1)), V(xk_[:, k8, :], xktok), start=(k8 == 0), stop=(k8 == KC - 1))
            P.ts("dve", t[5], pk, vcol(8, gk), ALU.mult)
            P.act(V(P.junk[:, 0:TB], "junk"), t[5], AF.Square)
            pss = pbank()
            P.mm(pss, V(bo, "bo"), V(P.junk[:, 0:TB], "junk"), start=True, stop=True)
            P.act(t[6], pss, AF.Ln, scale=1.0, bias=V(tinyb, "tinyb"))
            P.act(t[6], t[6], AF.Exp, scale=-0.5)
            P.tt("dve", t[5], t[5], t[6], ALU.mult)
            for e_ in range(2):
                ps_ = slice(e_ * 64, (e_ + 1) * 64)
                P.stt(V(aTm[e_][ps_, kc, :], ("aT", e_, kc)), V(f32t[5][ps_, :], ("f32t", 5)), -1.0,
                      V(f32t[2][ps_, :], ("f32t", 2)), ALU.mult, ALU.mult)
            P.tt("pool", t[6], t[5], t[4], ALU.mult)
            P.tt("pool", V(bT[:, kc, :], ("bT", kc)), t[6], t[3], ALU.mult)
            P.ts("dve", t[4], t[4], vcol(9, gk), ALU.mult, V(omka[:, gk:gk + 1], "omka"), ALU.add)
            P.tt("dve", t[4], pk, t[4], ALU.mult)
            P.tt("pool", V(kT[:, kc, :], ("kT", kc)), t[4], t[3], ALU.mult)
            pr = pbank()
            for k8 in range(KC):
                P.mm(pr, V(Wr[:, k8, cs], ("w", 0)), V(xr_[:, k8, :], xrtok), start=(k8 == 0), stop=(k8 == KC - 1))
            for e_ in range(2):
                ps_ = slice(e_ * 64, (e_ + 1) * 64)
                P.tt("dve", V(rTm[e_][ps_, kc, :], ("rT", e_, kc)), V(pr.ap[ps_, :], pr.tok),
                     V(f32t[1][ps_, :], ("f32t", 1)), ALU.mult)
            P.stt(V(rkT[:, kc, :], ("rkT", kc)), pr, vcol(10, gk), t[4], ALU.mult, ALU.mult)
        xv_, xvtok = mix(3, xnT, xtok, 1)
        return xv_, xvtok

    def phaseAB(c8, sl, ab):
        banks = [V(P.psb[b][0:64, :], ("psb", b)) for b in range(5)]
        for hl in range(8):
            kc, e_ = hl // 2, hl % 2
            a_ = V(aTm[e_][:, kc, sl], ("aT", e_, kc))
            b_ = V(bT[:, kc, sl], ("bT", kc))
            k_ = V(kT[:, kc, sl], ("kT", kc))
            r_ = V(rTm[e_][:, kc, sl], ("rT", e_, kc))
            hs = slice(hl * 64, (hl + 1) * 64)
            for bi, (l_, r2) in enumerate(((a_, b_), (b_, a_), (k_, a_), (b_, r_), (k_, r_))):
                P.mm(V(P.psb[bi][0:64, hs], ("psb", bi)), l_, r2, start=True, stop=True)
        v8 = lambda ap_: ap_.rearrange("p (h s) -> p h s", h=8)
        P.tt("dve", V(Lm[0][0:64], ("Lm", 0)), V(v8(P.psb[0][0:64, :]), ("psb", 0)), mask(0), ALU.mult)
        P.tt("dve", V(LTm[0][0:64], ("LTm", 0)), V(v8(P.psb[1][0:64, :]), ("psb", 1)), mask(1), ALU.mult)
        P.tt("dve", V(AkT[ab][0:64], ("AkT", ab)), V(v8(P.psb[2][0:64, :]), ("psb", 2)), mask(1), ALU.mult)
        P.tt("dve", V(ArbT[ab][0:64], ("ArbT", ab)), V(v8(P.psb[3][0:64, :]), ("psb", 3)), mask(2), ALU.mult)
        P.tt("dve", V(ArkT[ab][0:64], ("ArkT", ab)), V(v8(P.psb[4][0:64, :]), ("psb", 4)), mask(2), ALU.mult)
        P.tt("pool", V(XT[0][0:64], ("XT", 0)), V(LTm[0][0:64], ("LTm", 0)), mask(3), ALU.add)
        cur, xc = 0, 0
        for lvl in range(5):
            nx = 1 - cur
            last = (lvl == 4)
            for hl in range(8):
                hs = slice(hl * 64, (hl + 1) * 64)
                P.mm(V(P.psb[0][0:64, hs], ("psb", 0)), V(LTm[cur][0:64, hl, :], ("LTm", cur)),
                     V(Lm[cur][0:64, hl, :], ("Lm", cur)), start=True, stop=True)
                if not last:
                    P.mm(V(P.psb[1][0:64, hs], ("psb", 1)), V(Lm[cur][0:64, hl, :], ("Lm", cur)),
                         V(LTm[cur][0:64, hl, :], ("LTm", cur)), start=True, stop=True)
            P.copy("act", V(Lm[nx][0:64], ("Lm", nx)), V(v8(P.psb[0][0:64, :]), ("psb", 0)))
            if not last:
                P.copy("act", V(LTm[nx][0:64], ("LTm", nx)), V(v8(P.psb[1][0:64, :]), ("psb", 1)))
            xn_ = (2 + ab) if last else (1 - xc)
            for hl in range(8):
                hs = slice(hl * 64, (hl + 1) * 64)
                P.mm(V(P.psb[2][0:64, hs], ("psb", 2)), V(Lm[nx][0:64, hl, :], ("Lm", nx)),
                     V(XT[xc][0:64, hl, :], ("XT", xc)), start=True, stop=False)
                P.mm(V(P.psb[2][0:64, hs], ("psb", 2)), V(identb[0:64, :], "identb"),
                     V(XT[xc][0:64, hl, :], ("XT", xc)), start=False, stop=True)
            P.copy("act", V(XT[xn_][0:64], ("XT", xn_)), V(v8(P.psb[2][0:64, :]), ("psb", 2)))
            cur = nx
            xc = xn_

    def phaseCD(blk, c8, sl, ab, xv_, xvtok):
        b5 = V(P.psb[5][0:64, :], ("psb", 5))
        vt_ = Vtm[c8 % 2]
        vtok = ("Vtm", c8 % 2)
        for k8 in range(KC):
            P.mm(b5, V(xv_[:, k8, sl], xvtok), V(Wv[:, k8, :], ("w", 2)), start=(k8 == 0), stop=(k8 == KC - 1))
        P.copy("act", V(vt_[0:64, :], vtok), b5)
        for hl in range(8):
            kc, e_ = hl // 2, hl % 2
            hs = slice(hl * 64, (hl + 1) * 64)
            P.mm(V(P.psb[5][0:64, hs], ("psb", 5)), V(aTm[e_][:, kc, sl], ("aT", e_, kc)),
                 V(Sbf[:, kc, :], ("Sbf", kc)), start=True, stop=False)
            P.mm(V(P.psb[5][0:64, hs], ("psb", 5)), V(AkT[ab][0:64, hl, :], ("AkT", ab)),
                 V(vt_[0:64, hs], vtok), start=False, stop=True)
        P.copy("act", V(Zs[0:64, :], "Zs"), b5)
        for hl in range(8):
            hs = slice(hl * 64, (hl + 1) * 64)
            P.mm(V(P.psb[5][0:64, hs], ("psb", 5)), V(XT[2 + ab][0:64, hl, :], ("XT", 2 + ab)),
                 V(Zs[0:64, hs], "Zs"), start=True, stop=True)
        P.copy("act", V(Us[0:64, :], "Us"), b5)
        for hl in range(8):
            kc, e_ = hl // 2, hl % 2
            hs = slice(hl * 64, (hl + 1) * 64)
            P.mm(V(P.psb[5][0:64, hs], ("psb", 5)), V(rTm[e_][:, kc, sl], ("rT", e_, kc)),
                 V(Sbf[:, kc, :], ("Sbf", kc)), start=True, stop=False)
            P.mm(V(P.psb[5][0:64, hs], ("psb", 5)), V(ArbT[ab][0:64, hl, :], ("ArbT", ab)),
                 V(Us[0:64, hs], "Us"), start=False, stop=False)
            P.mm(V(P.psb[5][0:64, hs], ("psb", 5)), V(ArkT[ab][0:64, hl, :], ("ArkT", ab)),
                 V(vt_[0:64, hs], vtok), start=False, stop=True)
        y8 = P.psb[5][0:64, :].rearrange("p (h v) -> p h v", h=8)
        s0, s1 = V(st8[0][0:64, :], ("st8", 0)), V(st8[1][0:64, :], ("st8", 1))
        P.S.op("dve", (lambda o=st8[0][0:64, :], i_=y8: nc.vector.tensor_reduce(o, i_, AX.X, ALU.add)),
               reads=[("psb", 5)], writes=[("st8", 0)])
        P.ts("dve", s0, s0, -1.0 / 64.0, ALU.mult)
        ycv = V(yc[0:64, :], "yc")
        yc8 = yc[0:64, :].rearrange("p (h v) -> p h v", h=8)
        P.tt("dve", V(yc8, "yc"), V(y8, ("psb", 5)), V(st8[0][0:64, :].unsqueeze(2).broadcast_to([64, 8, 64]), ("st8", 0)), ALU.add)
        P.act(V(sq[0:64, :], "sq"), ycv, AF.Square)
        P.S.op("dve", (lambda o=st8[1][0:64, :], i_=sq[0:64, :].rearrange("p (h v) -> p h v", h=8): nc.vector.tensor_reduce(o, i_, AX.X, ALU.add)),
               reads=[("sq",)], writes=[("st8", 1)])
        P.act(s1, s1, AF.Ln, scale=1.0 / 64.0, bias=V(epsg[0:64, :], "epsg"))
        P.act(s1, s1, AF.Exp, scale=-0.5)
        P.tt("dve", V(yc8, "yc"), V(yc8, "yc"), V(st8[1][0:64, :].unsqueeze(2).broadcast_to([64, 8, 64]), ("st8", 1)), ALU.mult)
        P.tt("pool", ycv, ycv, V(lnw[0:64, :], "lnw"), ALU.mult)
        P.tt("pool", ycv, ycv, V(lnb[0:64, :], "lnb"), ALU.add)
        b7f = P.psb[7]
        pBs = V(b7f[0:64, 384:392], ("psb", 7, "s"))
        for kc in range(NK):
            P.mm(V(b7f[0:64, 384 + 2 * kc:386 + 2 * kc], ("psb", 7, "s")), V(rkT[:, kc, sl], ("rkT", kc)), V(Eh, "Eh"),
                 start=True, stop=True)
        s2 = V(st8[2][0:64, :], ("st8", 2))
        P.copy("act", s2, pBs)
        P.tt("pool", V(bon[0:64, :].rearrange("p (h v) -> p h v", h=8), "bon"),
             V(vt_[0:64, :].rearrange("p (h v) -> p h v", h=8), vtok),
             V(st8[2][0:64, :].unsqueeze(2).broadcast_to([64, 8, 64]), ("st8", 2)), ALU.mult)
        P.tt("pool", ycv, ycv, V(bon[0:64, :], "bon"), ALU.add)
        psT7 = P.psb[7].bitcast(BF16)
        for kc in range(NK):
            P.transpose(V(psT7[0:64, kc * 128:(kc + 1) * 128], ("psb", 7, "t")), V(bT[:, kc, sl], ("bT", kc)), V(P.ident, "ident"))
        P.copy("act", V(BKtm[0:64, 0, :], ("BKtm", 0)), V(psT7[0:64, 0:512], ("psb", 7, "t")))
        for kc in range(NK):
            P.transpose(V(psT7[0:64, kc * 128:(kc + 1) * 128], ("psb", 7, "t")), V(kT[:, kc, sl], ("kT", kc)), V(P.ident, "ident"))
        P.copy("act", V(BKtm[0:64, 1, :], ("BKtm", 1)), V(psT7[0:64, 0:512], ("psb", 7, "t")))
        for kc in range(NK):
            cs = slice(kc * 128, (kc + 1) * 128)
            P.mm(V(P.psb[6][:, cs], ("psb", 6)), V(BKtm[0:64, 0, cs], ("BKtm", 0)), V(Us[0:64, cs], "Us"), start=True, stop=False)
            P.mm(V(P.psb[6][:, cs], ("psb", 6)), V(BKtm[0:64, 1, cs], ("BKtm", 1)), V(vt_[0:64, cs], vtok), start=False, stop=True)
        for hp in range(2):
            ps_ = slice(hp * 64, (hp + 1) * 64)
            sv = V(S[ps_, :, :], ("S", hp))
            pst = V(P.psb[6][ps_, :].rearrange("p (k c) -> p k c", k=NK)[:, :, hp * 64:(hp + 1) * 64], ("psb", 6))
            P.tt("dve", sv, sv, pst, ALU.add)
            P.tt("dve", sv, sv, V(WC[ps_, :, c8:c8 + 1].broadcast_to([64, NK, 64]), ("WC",)), ALU.mult)
            P.copy("pool", V(Sbf[ps_, :, :], ("Sbf",)), sv)
        for (lh, rh, kk_) in ((V(hgA[:, sl], "hgA"), V(g2A, ("w", 9)), 0), (V(hgB[0:32, sl], "hgB"), V(g2B[0:32, :], ("w", 10)), 1)):
            P.mm(b5, lh, rh, start=(kk_ == 0), stop=(kk_ == 1))
        P.tt("dve", V(ytm[0:64, :], "ytm"), b5, ycv, ALU.mult)
        for kc in range(NK):
            P.transpose(V(psT7[:, 512 + kc * 64:512 + (kc + 1) * 64], ("psb", 7, "y")), V(ytm[0:64, kc * 128:(kc + 1) * 128], "ytm"),
                        V(P.ident[0:64, 0:64], "ident"))
        P.copy("act", V(yT[:, :, sl], ("yT", c8)), V(psT7[:, 512:768].rearrange("p (k t) -> p k t", k=NK), ("psb", 7, "y")))

    def stageB(blk, slot, tick):
        xv_, xvtok = stage1(blk, slot, tick)
        sls = [slice(c8 * RW_C, (c8 + 1) * RW_C) for c8 in range(8)]
        phaseAB(0, sls[0], 0)
        for c8 in range(8):
            if c8 + 1 < 8:
                phaseAB(c8 + 1, sls[c8 + 1], (c8 + 1) % 2)
            phaseCD(blk, c8, sls[c8], c8 % 2, xv_, xvtok)
            if c8 in (1, 4):
                tick()
        out_stage(P, blk, srcR, srcRname, dst, dstname,
                  lambda k, s: V(yT[:, k, s * 128:(s + 1) * 128], ("yT",)), NK, wo, ("w", 3))

    run_blocks(P, srcN, srcNname, 1, stageB)
    P.arena_reset(mark)


def build(T, plan):
    P = Prog(T, plan)
    P.w = {}

    def win(name, shape, dt=F32):
        if name not in P.w:
            P.w[name] = P.dram_in(name, shape, dt)
        return P.w[name]

    P.win = win
    x_in = P.dram_in("x", [T, D])
    out = P.dram_out("out", [T, D])
    P.arena_init(ARENA_BYTES)
    P.psb = [P.ps("psb%d" % i)[:, :] for i in range(8)]
    P.ident = P.alloc([128], BF16)
    P.ones = P.alloc([128], BF16)
    P.normw = P.alloc([9, KC])
    P.epsv = P.alloc([1])
    P.dma("sp", V(P.ident, "ident"), V(win("c_ident", [128, 128], BF16), "in_w"), key="const0")
    P.dma("sp", V(P.normw, "normw"), V(win("c_normw", [128, 9, KC]), "in_w"), key="const1")
    P.memset("pool", V(P.ones, "ones"), 1.0)
    P.memset("pool", V(P.epsv, "epsv"), EPS)
    P.S.barrier()
    scr = [P.dram_scratch("scr%d" % i, [T, D]) for i in range(3)]
    bufs = [(x_in, "x")] + [(scr[i], "scr%d" % i) for i in range(3)]
    cur = 0

    def nxt(*busy):
        for i in (1, 2, 3):
            if i not in busy:
                return i

    for item in plan:
        kind = item[0]
        a = cur
        b = nxt(a)
        c = nxt(a, b)
        A_, B_, C_ = bufs[a], bufs[b], bufs[c]
        if kind == "ffn":
            li = item[1]
            ffn_pass(P, li, 0, 11, A_[0], A_[1], A_[0], A_[1], B_[0], B_[1])
            ffn_pass(P, li, 11, 22, A_[0], A_[1], B_[0], B_[1], C_[0], C_[1])
            cur = c
        elif kind == "mix" and item[1] == 3:
            retnet_pass(P, 0, A_[0], A_[1], A_[0], A_[1], B_[0], B_[1])
            retnet_pass(P, 2, A_[0], A_[1], B_[0], B_[1], C_[0], C_[1])
            cur = c
        elif kind == "mix" and item[1] == 0:
            ssd_pass(P, 0, A_[0], A_[1], A_[0], A_[1], B_[0], B_[1])
            ssd_pass(P, 1, A_[0], A_[1], B_[0], B_[1], C_[0], C_[1])
            cur = c
        elif kind == "mix" and item[1] == 1:
            rwkv_pass(P, 0, A_[0], A_[1], A_[0], A_[1], B_[0], B_[1])
            rwkv_pass(P, 1, A_[0], A_[1], B_[0], B_[1], C_[0], C_[1])
            cur = c
        elif kind == "mix" and item[1] == 2:
            gla_pass(P, A_[0], A_[1], A_[0], A_[1], B_[0], B_[1])
            cur = b
        elif kind == "final":
            final_norm(P, bufs[cur][0], bufs[cur][1], out, "out")
    P.barrier("sp", [("out",)])
    global LAST_INPUT_NAMES
    LAST_INPUT_NAMES = list(P.inputs.keys())
    return P.finish()


def ret_perm():
    idx = []
    for part in range(2):
        for h in range(RET_H):
            base = part * 1024 + h * RET_DK
            idx += [base + 2 * i for i in range(128)] + [base + 2 * i + 1 for i in range(128)]
    return np.array(idx + list(range(2048, 6144)))


def host_consts(inputs):
    import ml_dtypes
    f = lambda a: np.ascontiguousarray(np.asarray(a, dtype=np.float32))
    c = {}
    c["c_ident"] = np.eye(128, dtype=np.float32).astype(ml_dtypes.bfloat16)
    nw = np.concatenate([f(inputs["norm_mix"]), f(inputs["norm_ffn"]), f(inputs["norm_final"])[None]], 0)
    c["c_normw"] = np.ascontiguousarray(nw.reshape(9, KC, 128).transpose(2, 0, 1))
    c["c_nfb"] = f(inputs["norm_final"])
    cw = f(inputs["ffn_conv_w"])
    cwl = cw.reshape(4, 3, 44, 128).transpose(0, 3, 2, 1)
    cb = f(inputs["ffn_conv_b"]).reshape(4, 44, 128).transpose(0, 2, 1)
    for li in range(4):
        c["ffn_w_up_%d" % li] = f(inputs["ffn_w_up"][li])
        c["ffn_w_down_%d" % li] = f(inputs["ffn_w_down"][li])
        c["c_ffn_cw_%d" % li] = np.ascontiguousarray(cwl[li])
        c["c_ffn_cb_%d" % li] = np.ascontiguousarray(cb[li])
    c["ret_w_in_p"] = np.ascontiguousarray(f(inputs["ret_w_in"][0])[:, ret_perm()])
    c["ret_w_out"] = f(inputs["ret_w_out"][0])
    inv = (1.0 / (np.float32(10000.0) ** np.linspace(0.0, 1.0, 128, dtype=np.float32))).astype(np.float32)
    ang = (np.arange(4096, dtype=np.float32)[None, :] * inv[:, None]).astype(np.float32)
    c["c_ret_cos"] = np.cos(ang).astype(np.float32)
    c["c_ret_sin"] = np.sin(ang).astype(np.float32)
    gam = 1.0 - 2.0 ** (-5.0 - np.arange(4, dtype=np.float64))
    s_ = np.arange(128)[:, None]
    l_ = np.arange(128)[None, :]
    decT = np.zeros((128, 4, 128), np.float64)
    for h in range(4):
        decT[:, h, :] = np.where(l_ >= s_, gam[h] ** (l_ - s_), 0.0) / 16.0
    c["c_ret_decT"] = decT.astype(np.float32)
    c["c_ret_gl"] = np.stack([gam[h] ** ((np.arange(TB) % 128) + 1) for h in range(4)]).astype(np.float32)
    c["c_ret_kdec"] = np.stack([gam[h] ** (127 - np.arange(128)) / 16.0 for h in range(4)], 1).astype(np.float32)
    c["ssd_w_in"] = f(inputs["ssd_w_in"][0])
    c["ssd_w_out"] = f(inputs["ssd_w_out"][0])
    c["c_ssd_cw"] = np.ascontiguousarray(f(inputs["ssd_conv_w"][0]).reshape(4, 32, 128).transpose(2, 1, 0))
    c["c_ssd_cb"] = np.ascontiguousarray(f(inputs["ssd_conv_b"][0]).reshape(32, 128).T)
    for n_ in ("ssd_dt_bias", "ssd_a_log", "ssd_d", "ssd_norm_w"):
        c[n_] = f(inputs[n_][0])
    c["c_SU"] = (s_ < l_).T.astype(np.float32).copy()
    for n_ in ("w_rkv", "w_out", "w1", "w2", "a1", "a2", "g1", "g2", "ln_w", "ln_b"):
        c["rwkv_" + n_] = f(inputs["rwkv_" + n_][0])
    vecs = [f(inputs["rwkv_mix"][0])[i_] for i_ in range(6)] + [f(inputs["rwkv_" + n_][0]).reshape(-1) for n_ in
                                                                 ("w0", "a0", "k_k", "k_a", "r_k")] + [np.zeros(1024, np.float32)]
    c["c_rwkv_vec"] = np.ascontiguousarray(np.stack(vecs).reshape(12, KC, 128).transpose(2, 0, 1))
    t64 = np.arange(64)[:, None]
    u64 = np.arange(64)[None, :]
    c["c_rwkv_masks"] = np.ascontiguousarray(np.stack([(u64 < t64), (t64 < u64), (t64 <= u64), (t64 == u64)], 1).astype(np.float32))
    E = np.zeros((128, 2), np.float32)
    E[:64, 0] = 1.0
    E[64:, 1] = 1.0
    c["c_rwkv_E"] = E.astype(ml_dtypes.bfloat16)
    c["c_rwkv_bo"] = (E @ E.T).astype(ml_dtypes.bfloat16)
    sm64 = np.ones(TB, np.float32)
    sm64[::64] = 0.0
    c["c_rwkv_scanm"] = sm64
    c["gla_w_in"] = f(inputs["gla_w_in"][0])
    c["gla_w_out"] = f(inputs["gla_w_out"][0])
    c["gla_w_gk2"] = f(inputs["gla_w_gk2"][0])
    c["c_gla_bgk"] = np.ascontiguousarray(f(inputs["gla_b_gk2"][0]).reshape(4, 128).T)
    c["c_gla_nw"] = np.ascontiguousarray(f(inputs["gla_norm_w"][0]).reshape(2, 128).T)
    c["c_maskT"] = (l_ >= s_).astype(np.float32)
    sm = np.ones(TB, np.float32)
    sm[::128] = 0.0
    c["c_scanm"] = sm
    return c


FULL_PLAN = [("mix", 0), ("ffn", 0), ("mix", 1), ("ffn", 1), ("mix", 2), ("ffn", 2), ("mix", 3), ("ffn", 3), ("final",)]
_CACHE = {}


def kernel(**inputs):
    T = 4096
    n_cores = 8
    if "nc" not in _CACHE:
        _CACHE["nc"] = build(T, FULL_PLAN)
        _CACHE["names"] = list(LAST_INPUT_NAMES)
    nc = _CACHE["nc"]
    names = _CACHE["names"]
    consts = host_consts(inputs)
    x = np.ascontiguousarray(np.asarray(inputs["x"], dtype=np.float32))
    shared = {n: consts[n] for n in names if n != "x"}
    in_maps = []
    for b in range(n_cores):
        m = dict(shared)
        m["x"] = np.ascontiguousarray(x[b])
        in_maps.append(m)
    res = run_bass_kernel_spmd(nc, in_maps, core_ids=list(range(n_cores)))
    return np.stack([np.asarray(r["out"], dtype=np.float32) for r in res.results], axis=0)
```

```python
import numpy as np
import concourse.bass as bass
import concourse.mybir as mybir
from concourse.bass_utils import run_bass_kernel_spmd

F32 = mybir.dt.float32
BF16 = mybir.dt.bfloat16
AF = mybir.ActivationFunctionType
ALU = mybir.AluOpType
AX = mybir.AxisListType

D = 1024
KC = 8
TB = 512
SCHEDULE = True
KEEP_ORDER = ()
EPS = 1e-5


class V:
    __slots__ = ("ap", "tok")

    def __init__(self, ap, tok):
        self.ap = ap
        self.tok = tok if isinstance(tok, tuple) else (tok,)


class _Op:
    __slots__ = ("eng", "fn", "reads", "writes", "dma_key", "deps", "inc", "val", "sem", "amt",
                 "odeps", "cost", "lat", "bar", "idx", "grp", "gend", "st", "bind")

    def __init__(self, eng, fn, reads, writes, dma_key, cost=300.0, lat=0.0):
        self.odeps = []
        self.cost = cost
        self.lat = lat
        self.bar = False
        self.idx = 0
        self.grp = None
        self.gend = True
        self.st = 0.0
        self.bind = None
        self.eng = eng
        self.fn = fn
        self.reads = reads
        self.writes = writes
        self.dma_key = dma_key
        self.deps = []
        self.inc = False
        self.val = 0
        self.sem = None
        self.amt = 1


class Sched:
    COMPUTE = ("pe", "act", "dve", "pool")

    def __init__(self, nc):
        self.nc = nc
        self.ops = []
        self.state = {}

    def op(self, eng, fn, reads=(), writes=(), dma_key=None, cost=300.0, lat=0.0):
        reads = [t if isinstance(t, tuple) else (t,) for t in reads]
        writes = [t if isinstance(t, tuple) else (t,) for t in writes]
        writes = [t[:2] if t[0] == "psb" else t for t in writes]
        writes += [t[:2] for t in reads if t[0] == "psb" and t[:2] not in writes]
        reads = [t for t in reads if t[0] != "psb"]
        o = _Op(eng, fn, reads, writes, dma_key, cost, lat)
        self._analyse(o)
        self.ops.append(o)
        return o

    @staticmethod
    def _conf(a, b):
        n = min(len(a), len(b))
        return a[:n] == b[:n]

    def _add_dep(self, o, p, kind):
        if p is None or p is o:
            return
        pd = p.dma_key is not None
        od = o.dma_key is not None
        if not pd and not od:
            if p.eng == "pe" and o.eng == "pe":
                o.odeps.append(p)
                return
            if p.eng == o.eng and kind != "RAW":
                o.odeps.append(p)
                return
        if pd and od and p.eng == o.eng and kind == "WAR" and False:
            return
        o.deps.append(p)

    def _analyse(self, o):
        st = self.state
        for tk in o.reads:
            root = st.setdefault(tk[0], {})
            for k, e in root.items():
                if self._conf(k, tk):
                    self._add_dep(o, e[0], "RAW")
            e = root.get(tk)
            if e is None:
                root[tk] = [None, [o]]
            else:
                e[1].append(o)
        for tk in o.writes:
            root = st.setdefault(tk[0], {})
            dead = []
            for k, e in root.items():
                if self._conf(k, tk):
                    self._add_dep(o, e[0], "WAW")
                    for r in e[1]:
                        self._add_dep(o, r, "WAR")
                    if len(k) > len(tk):
                        dead.append(k)
                    elif len(k) < len(tk):
                        pass
            for k in dead:
                del root[k]
            root[tk] = [o, []]

    def barrier(self):
        lasts = {}
        dmas = {}
        for o in self.ops:
            if o.dma_key is not None:
                dmas[o.dma_key] = o
            elif o.fn is not None:
                lasts[o.eng] = o
        new = []
        for eng in ("pe", "act", "dve", "pool", "sp"):
            b = _Op(eng, None, [], [], None)
            b.deps = [p for e, p in lasts.items() if e != eng] + list(dmas.values())
            b.bar = True
            new.append(b)
        self.ops.extend(new)
        self.state = {}

    def schedule(self, window=16, xlat=300.0):
        import bisect
        segs, cur = [], []
        for o in self.ops:
            if o.bar:
                if cur:
                    segs.append(cur)
                    cur = []
                segs.append([o])
            else:
                cur.append(o)
        if cur:
            segs.append(cur)
        out = []
        self.seg_times = []
        prev_lasts, prev_dmas = {}, {}
        for seg in segs:
            if len(seg) == 1:
                b = seg[0]
                if b.bar:
                    b.deps = [p_ for e_, p_ in prev_lasts.items() if e_ != b.eng] + list(prev_dmas.values())
                out.extend(seg)
                continue
            seg_start = len(out)
            lastof = {}
            for i, o in enumerate(seg):
                o.idx = i
                if o.eng in KEEP_ORDER:
                    if o.eng in lastof:
                        o.odeps.append(lastof[o.eng])
                    lastof[o.eng] = o
            inseg = set(id(o) for o in seg)
            groups = {}
            for o in seg:
                if o.grp is not None:
                    groups.setdefault(o.grp, []).append(o)
            for g, mem in groups.items():
                if len(mem) > 1:
                    ids = set(id(m) for m in mem)
                    first = mem[0]
                    for m in mem[1:]:
                        for d in m.deps + m.odeps:
                            if id(d) not in ids:
                                first.odeps.append(d)
            pe_lock = None
            busy = {}
            npred = {}
            succ = {}
            for o in seg:
                ds = [(d, True) for d in o.deps if id(d) in inseg] + [(d, False) for d in o.odeps if id(d) in inseg]
                npred[id(o)] = len(ds)
                for d, hard in ds:
                    succ.setdefault(id(d), []).append((o, hard))
            fin = {}
            rtime = {}
            ready = {e: [] for e in ("pe", "act", "dve", "pool", "sp")}
            free = {e: 0.0 for e in ready}
            for o in seg:
                if npred[id(o)] == 0:
                    rtime[id(o)] = 0.0
                    ready[o.eng].append((o.idx, o))
            for e in ready:
                ready[e].sort(key=lambda t: t[0])
            done = 0
            n = len(seg)
            while done < n:
                best = None
                for e, lst in ready.items():
                    fe = free[e]
                    cand = lst[:window]
                    if e == "pe" and pe_lock is not None:
                        cand = [t for t in lst if t[1].grp == pe_lock][:1]
                    for (ix, o) in cand:
                        st = rtime[id(o)]
                        if st < fe:
                            st = fe
                        if best is None or (st, ix) < best[0]:
                            best = ((st, ix), o)
                (st, ix), o = best
                lst = ready[o.eng]
                lst.pop(bisect.bisect_left(lst, (ix,), key=lambda t: (t[0],)))
                if o.eng == "pe" and o.grp is not None:
                    pe_lock = None if o.gend else o.grp
                free[o.eng] = st + o.cost
                busy[o.eng] = busy.get(o.eng, 0.0) + o.cost
                f = st + o.cost + o.lat
                fin[id(o)] = f
                o.st = st
                out.append(o)
                done += 1
                for s_, hard in succ.get(id(o), ()):
                    k = id(s_)
                    npred[k] -= 1
                    if hard or s_.eng != o.eng:
                        t_ = f + (xlat if s_.eng != o.eng else 0.0)
                    else:
                        t_ = st + o.cost
                    if rtime.get(k, 0.0) < t_:
                        rtime[k] = t_
                        s_.bind = o
                    if npred[k] == 0:
                        bisect.insort(ready[s_.eng], (s_.idx, s_), key=lambda t: t[0])
            self.seg_times.append((max(fin.values()) if fin else 0.0, dict(busy), len(seg)))
            prev_lasts, prev_dmas = {}, {}
            for o in out[seg_start:]:
                if o.dma_key is not None:
                    prev_dmas[o.dma_key] = o
                elif o.fn is not None:
                    prev_lasts[o.eng] = o
        assert len(out) == len(self.ops)
        self.ops = out

    def emit(self, block_ctx, sems):
        nc = self.nc
        for o in self.ops:
            for p in o.deps:
                p.inc = True
        cnt = {}
        for o in self.ops:
            if o.dma_key is not None:
                key = ("dma", o.dma_key)
                cnt[key] = cnt.get(key, 0) + 16
                o.val = cnt[key]
                o.sem = sems[key]
                o.amt = 16
                o.inc = True
            elif o.inc:
                cnt[o.eng] = cnt.get(o.eng, 0) + 1
                o.val = cnt[o.eng]
                o.sem = sems[o.eng]
        engs = {"pe": nc.tensor, "act": nc.scalar, "dve": nc.vector, "pool": nc.gpsimd, "sp": nc.sync}
        per_eng = {k: [] for k in engs}
        for o in self.ops:
            per_eng[o.eng].append(o)

        def run(engname):
            eng = engs[engname]
            waited = {}
            for o in per_eng[engname]:
                need = {}
                for p in o.deps:
                    sid = id(p.sem)
                    if need.get(sid, (None, 0))[1] < p.val:
                        need[sid] = (p.sem, p.val)
                for sid, (sem, val) in need.items():
                    if waited.get(sid, 0) < val:
                        eng.wait_ge(sem, val)
                        waited[sid] = val
                if o.fn is None:
                    continue
                ins = o.fn()
                if o.inc:
                    ins.then_inc(o.sem, o.amt)

        @block_ctx.tensor
        def _(e):
            run("pe")

        @block_ctx.scalar
        def _(e):
            run("act")

        @block_ctx.vector
        def _(e):
            run("dve")

        @block_ctx.gpsimd
        def _(e):
            run("pool")

        @block_ctx.sync
        def _(e):
            run("sp")


class Prog:
    def __init__(self, T, plan):
        self.T = T
        self.plan = plan
        self.nc = bass.Bass("TRN2", target_bir_lowering=False)
        self.S = Sched(self.nc)
        self.ctxs = []
        self.dma_keys = []
        self.inputs = {}
        self.psn = 0

    def dram_in(self, name, shape, dt=F32):
        t = self.nc.dram_tensor(name, list(shape), dt, kind="ExternalInput")
        self.inputs[name] = t
        return t.ap()

    def dram_out(self, name, shape, dt=F32):
        return self.nc.dram_tensor(name, list(shape), dt, kind="ExternalOutput").ap()

    def dram_scratch(self, name, shape, dt=F32):
        return self.nc.dram_tensor(name, list(shape), dt, kind="Internal").ap()

    def sb(self, name, shape, dt=F32):
        g = self.nc.sbuf_tensor(name, list(shape), dt)
        t = g.__enter__()
        self.ctxs.append(g)
        return t

    def ps(self, name, shape=(128, 512), dt=F32):
        g = self.nc.psum_tensor(name, list(shape), dt)
        t = g.__enter__()
        self.ctxs.append(g)
        return t

    def arena_init(self, nbytes):
        self.arena = self.sb("arena", [128, nbytes // 4], F32)
        self.arena_n = nbytes
        self.arena_off = 0

    def alloc(self, shape, dt=F32):
        n = 1
        for d in shape:
            n *= d
        esz = 4 if dt == F32 else 2
        nb = (n * esz + 63) // 64 * 64
        assert self.arena_off + nb <= self.arena_n, ("arena overflow", self.arena_off + nb, self.arena_n)
        a = self.arena[:, self.arena_off // 4:(self.arena_off + nb) // 4]
        self.arena_off += nb
        if dt != F32:
            a = a.bitcast(dt)
        a = a[:, 0:n]
        if len(shape) == 2:
            a = a.rearrange("p (a b) -> p a b", a=shape[0])
        elif len(shape) == 3:
            a = a.rearrange("p (a b c) -> p a b c", a=shape[0], b=shape[1])
        elif len(shape) == 4:
            a = a.rearrange("p (a b c d) -> p a b c d", a=shape[0], b=shape[1], c=shape[2])
        return a

    def arena_reset(self, mark=0):
        self.S.barrier()
        self.arena_off = mark

    def _toks(self, *vs):
        return [v.tok for v in vs if isinstance(v, V)]

    @staticmethod
    def _n(ap):
        n = 1
        for d in list(ap.shape)[1:]:
            n *= int(d)
        return n

    def mm(self, out, lhsT, rhs, start=True, stop=True):
        nc = self.nc
        otok = out.tok
        if not (start and stop):
            otok = otok[:2]
        n = self._n(rhs.ap)
        mult = 4.0 if rhs.ap.dtype == F32 else 1.0
        o = self.S.op("pe", lambda: nc.tensor.matmul(out.ap, lhsT.ap, rhs.ap, start=start, stop=stop),
                      reads=self._toks(lhsT, rhs), writes=[otok], cost=mult * (max(64, n) * 0.42 + 20.0), lat=250.0)
        if start:
            self.gid = getattr(self, "gid", 0) + 1
        o.grp = self.gid
        o.gend = bool(stop)

    def transpose(self, out, in_, ident):
        nc = self.nc
        self.S.op("pe", lambda: nc.tensor.transpose(out.ap, in_.ap, ident.ap),
                  reads=self._toks(in_, ident), writes=self._toks(out), cost=80.0, lat=250.0)

    def act(self, out, in_, func, scale=None, bias=None, accum=None, extra_reads=()):
        nc = self.nc
        kw = {}
        if scale is not None:
            kw["scale"] = scale.ap if isinstance(scale, V) else scale
        if bias is not None:
            kw["bias"] = bias.ap if isinstance(bias, V) else bias
        if accum is not None:
            kw["accum_out"] = accum.ap
        w = self._toks(out) + (self._toks(accum) if accum is not None else [])
        self.S.op("act", lambda: nc.scalar.activation(out.ap, in_.ap, func, **kw),
                  reads=self._toks(in_, scale, bias) + list(extra_reads), writes=w,
                  cost=230.0 + 0.83 * self._n(in_.ap) + (90.0 if accum is not None else 0.0))

    def _e(self, eng):
        return {"dve": self.nc.vector, "pool": self.nc.gpsimd, "act": self.nc.scalar}[eng]

    def _c(self, eng, ap, per=1.04):
        n = self._n(ap)
        if eng == "pool":
            return 300.0 + 1.6 * n
        if eng == "act":
            return 230.0 + 0.83 * n
        return 120.0 + per * n

    def tt(self, eng, out, a, b, op):
        e = self._e(eng)
        self.S.op(eng, lambda: e.tensor_tensor(out.ap, a.ap, b.ap, op),
                  reads=self._toks(a, b), writes=self._toks(out), cost=self._c(eng, out.ap))

    def ts(self, eng, out, in_, s1, op0, s2=None, op1=None, accum=None):
        e = self._e(eng)
        a1 = s1.ap if isinstance(s1, V) else s1
        a2 = s2.ap if isinstance(s2, V) else s2
        kw = {}
        if op1 is not None:
            kw["op1"] = op1
        if accum is not None:
            kw["accum_out"] = accum.ap
        w = self._toks(out) + (self._toks(accum) if accum is not None else [])
        self.S.op(eng, lambda: e.tensor_scalar(out.ap, in_.ap, a1, a2, op0, **kw),
                  reads=self._toks(in_, s1, s2), writes=w, cost=self._c(eng, out.ap, 0.7))

    def stt(self, out, in0, scalar, in1, op0, op1):
        nc = self.nc
        sc = scalar.ap if isinstance(scalar, V) else scalar
        self.S.op("dve", lambda: nc.vector.scalar_tensor_tensor(out.ap, in0.ap, sc, in1.ap, op0, op1),
                  reads=self._toks(in0, scalar, in1), writes=self._toks(out), cost=self._c("dve", out.ap))

    def copy(self, eng, out, in_):
        if eng == "act":
            nc = self.nc
            self.S.op("act", lambda: nc.scalar.copy(out.ap, in_.ap), reads=self._toks(in_), writes=self._toks(out),
                      cost=self._c("act", out.ap))
        else:
            e = self._e(eng)
            self.S.op(eng, lambda: e.tensor_copy(out.ap, in_.ap), reads=self._toks(in_), writes=self._toks(out),
                      cost=self._c(eng, out.ap, 0.7))

    def memset(self, eng, out, val):
        e = self._e(eng)
        self.S.op(eng, lambda: e.memset(out.ap, val), writes=self._toks(out), cost=self._c(eng, out.ap, 0.7))

    def recip(self, out, in_):
        nc = self.nc
        self.S.op("dve", lambda: nc.vector.reciprocal(out.ap, in_.ap), reads=self._toks(in_), writes=self._toks(out),
                  cost=self._c("dve", out.ap, 8.4))

    def dma(self, q, out, in_, key):
        if key not in self.dma_keys:
            self.dma_keys.append(key)
        e = {"sp": self.nc.sync, "pool": self.nc.gpsimd, "act": self.nc.scalar}[q]
        nb = self._n(out.ap) * int(list(out.ap.shape)[0]) * (4 if out.ap.dtype == F32 else 2)
        self.S.op(q, lambda: e.dma_start(out=out.ap, in_=in_.ap), reads=self._toks(in_),
                  writes=self._toks(out), dma_key=key, cost=(400.0 if q == "pool" else 60.0), lat=2000.0 + nb / 100.0)

    def barrier(self, eng, toks):
        self.S.op(eng, None, reads=list(toks))

    def finish(self):
        nc = self.nc
        sems = {}
        gs = []
        for name in ("pe", "act", "dve", "pool"):
            g = nc.semaphore("sem_" + name)
            sems[name] = g.__enter__()
            gs.append(g)
        for i, k in enumerate(self.dma_keys):
            g = nc.semaphore("semd_%d" % i)
            sems[("dma", k)] = g.__enter__()
            gs.append(g)
        if SCHEDULE:
            self.S.schedule()
            self.seg_times = self.S.seg_times
        blk = nc.Block()
        b = blk.__enter__()
        self.S.emit(b, sems)
        blk.__exit__(None, None, None)
        for g in reversed(gs):
            g.__exit__(None, None, None)
        for g in reversed(self.ctxs):
            g.__exit__(None, None, None)
        return nc


FFN_H = 2816
FFN_NC = 22
ARENA_BYTES = 204 * 1024


def tile_rows(ap, blk, s):
    r0 = blk * TB + s * 128
    return ap[r0:r0 + 128, :]


def alloc_common(P):
    P.xt = [P.alloc([D]) for _ in range(2)]
    P.xr = [P.alloc([D]) for _ in range(2)]
    P.xnT = [P.alloc([KC, TB], BF16) for _ in range(2)]
    P.xs = P.alloc([D], BF16)
    P.junk = P.alloc([D], BF16)
    P.ss = [P.alloc([4]) for _ in range(2)]
    P.rstd = [P.alloc([4]) for _ in range(2)]
    P.xtn = 0
    P.xrn = 0


def norm_tile(P, src, srcname, blk, s, nidx, slot):
    psT = P.psb[7].bitcast(BF16)
    ss, rstd = P.ss[slot], P.rstd[slot]
    xi = P.xtn % 2
    P.xtn += 1
    xt = P.xt[xi]
    xtv = V(xt, ("xt", xi))
    P.dma("sp", xtv, V(tile_rows(src, blk, s), (srcname, blk, s)), key=("xt", xi))
    P.act(V(P.junk, "junk"), xtv, AF.Square, accum=V(ss[:, s:s + 1], ("ss", slot, s)))
    P.act(V(rstd[:, s:s + 1], ("rstd", slot, s)), V(ss[:, s:s + 1], ("ss", slot, s)), AF.Sqrt,
          scale=1.0 / D, bias=V(P.epsv, "epsv"))
    P.recip(V(rstd[:, s:s + 1], ("rstd", slot, s)), V(rstd[:, s:s + 1], ("rstd", slot, s)))
    P.ts("dve", V(P.xs, "xs"), xtv, V(rstd[:, s:s + 1], ("rstd", slot, s)), ALU.mult)
    for kc in range(KC):
        P.transpose(V(psT[:, kc * 128:(kc + 1) * 128], ("psb", 7)), V(P.xs[:, kc * 128:(kc + 1) * 128], "xs"),
                    V(P.ident, "ident"))
    P.tt("dve", V(P.xnT[slot][:, :, s * 128:(s + 1) * 128], ("xnT", slot, s)),
         V(psT.rearrange("p (k t) -> p k t", k=KC), ("psb", 7)),
         V(P.normw[:, nidx, :].unsqueeze(2).broadcast_to([128, KC, 128]), "normw"), ALU.mult)


def run_blocks(P, srcN, srcNname, nidx, stageB):
    nblk = P.T // TB
    for s in range(4):
        norm_tile(P, srcN, srcNname, 0, s, nidx, 0)
    for blk in range(nblk):
        pending = [(blk + 1, s) for s in range(4)] if blk + 1 < nblk else []

        def tick():
            if pending:
                b, s_ = pending.pop(0)
                norm_tile(P, srcN, srcNname, b, s_, nidx, b % 2)

        stageB(blk, blk % 2, tick)
        while pending:
            tick()


def out_stage(P, blk, srcR, srcRname, dst, dstname, lhs_fn, nk, wo, wotok):
    for s in range(4):
        xi = P.xrn % 2
        P.xrn += 1
        xr = P.xr[xi]
        xrv = V(xr, ("xr", xi))
        P.dma("sp", xrv, V(tile_rows(srcR, blk, s), (srcRname, blk, s)), key=("xr", xi))
        for half in range(2):
            b = 3 + (2 * s + half) % 2
            pd = V(P.psb[b], ("psb", b))
            for k in range(nk):
                P.mm(pd, lhs_fn(k, s), V(wo[:, k, half * 512:(half + 1) * 512], wotok),
                     start=(k == 0), stop=(k == nk - 1))
            xh = V(xr[:, half * 512:(half + 1) * 512], ("xr", xi))
            P.tt("dve", xh, pd, xh, ALU.add)
        P.dma("sp", V(tile_rows(dst, blk, s), (dstname, blk, s)), xrv, key=("xr", xi))


def ffn_pass(P, li, c0, c1, srcN, srcNname, srcR, srcRname, dst, dstname):
    mark = P.arena_off
    alloc_common(P)
    nch = c1 - c0
    ncol = nch * 128
    nblk = P.T // TB
    w_up = P.win("ffn_w_up_%d" % li, [D, 2 * FFN_H])
    w_dn = P.win("ffn_w_down_%d" % li, [FFN_H, D])
    c_cw = P.win("c_ffn_cw_%d" % li, [128, 2 * FFN_NC, 3])
    c_cb = P.win("c_ffn_cb_%d" % li, [128, 2 * FFN_NC])
    wupv = P.alloc([KC, ncol], BF16)
    wupg = P.alloc([KC, ncol], BF16)
    wdn = P.alloc([nch, D], BF16)
    cw = P.alloc([2 * FFN_NC, 3])
    cb = P.alloc([2 * FFN_NC])
    hs = [P.alloc([2 * FFN_NC, 2]) for _ in range(2)]
    A = [P.alloc([TB]) for _ in range(6)]
    G = [P.alloc([TB]) for _ in range(4)]
    hid = P.alloc([nch, TB], BF16)
    upsrc = w_up.rearrange("(k p) n -> p k n", p=128)
    P.dma("pool", V(wupv, ("w", 0)), V(upsrc[:, :, c0 * 128:c1 * 128], "in_w"), key=("w", 0))
    P.dma("pool", V(wupg, ("w", 1)), V(upsrc[:, :, FFN_H + c0 * 128:FFN_H + c1 * 128], "in_w"), key=("w", 1))
    P.dma("pool", V(wdn, ("w", 2)), V(w_dn[c0 * 128:c1 * 128, :].rearrange("(c p) n -> p c n", p=128), "in_w"),
          key=("w", 2))
    P.dma("sp", V(cw, "cw"), V(c_cw, "in_w"), key="cw")
    P.dma("sp", V(cb, "cb"), V(c_cb, "in_w"), key="cb")
    P.memset("pool", V(hs[0], ("hs", 0)), 0.0)
    P.memset("pool", V(hs[1], ("hs", 1)), 0.0)

    def stageB(blk, slot, tick):
        xnT = P.xnT[slot]
        par = blk % 2
        ubanks = (0, 1, 2, 5, 6)
        tick_at = set(int(round(x)) for x in np.linspace(1, 2 * nch - 2, 4))
        for c in range(nch):
            for part in range(2):
                q = 2 * c + part
                if q in tick_at:
                    tick()
                cp = (c0 + c) + part * FFN_NC
                wsel = wupv if part == 0 else wupg
                wtok = ("w", part)
                bi = ubanks[q % 5]
                pu = V(P.psb[bi], ("psb", bi))
                for kc in range(KC):
                    P.mm(pu, V(wsel[:, kc, c * 128:(c + 1) * 128], wtok),
                         V(xnT[:, kc, :], ("xnT", slot)), start=(kc == 0), stop=(kc == KC - 1))
                ai = q % 6
                At = A[ai]
                atok = ("A", ai)
                P.act(V(At[:, 0:TB], atok), pu, AF.Identity,
                      scale=V(cw[:, cp, 2:3], "cw"), bias=V(cb[:, cp:cp + 1], "cb"))
                P.copy("act", V(hs[par][:, cp, :], ("hs", par, cp)), V(P.psb[bi][:, TB - 2:TB], ("psb", bi)))
                P.stt(V(At[:, 1:TB], atok), V(P.psb[bi][:, 0:TB - 1], ("psb", bi)), V(cw[:, cp, 1:2], "cw"),
                      V(At[:, 1:TB], atok), ALU.mult, ALU.add)
                P.stt(V(At[:, 2:TB], atok), V(P.psb[bi][:, 0:TB - 2], ("psb", bi)), V(cw[:, cp, 0:1], "cw"),
                      V(At[:, 2:TB], atok), ALU.mult, ALU.add)
                hp = V(hs[1 - par][:, cp, :], ("hs", 1 - par, cp))
                P.stt(V(At[:, 0:2], atok), hp, V(cw[:, cp, 0:1], "cw"), V(At[:, 0:2], atok), ALU.mult, ALU.add)
                P.stt(V(At[:, 0:1], atok), V(hs[1 - par][:, cp, 1:2], ("hs", 1 - par, cp)), V(cw[:, cp, 1:2], "cw"),
                      V(At[:, 0:1], atok), ALU.mult, ALU.add)
                if part == 0:
                    Aval, avtok = At, atok
                else:
                    Gt = G[c % 4]
                    gtok = ("G", c % 4)
                    P.act(V(Gt, gtok), V(At[:, 0:TB], atok), AF.Silu)
                    P.tt("pool", V(hid[:, c, :], ("hid", c)), V(Aval[:, 0:TB], avtok), V(Gt, gtok), ALU.mult)
        out_stage(P, blk, srcR, srcRname, dst, dstname,
                  lambda k, s: V(hid[:, k, s * 128:(s + 1) * 128], ("hid", k)), nch, wdn, ("w", 2))

    run_blocks(P, srcN, srcNname, 4 + li, stageB)
    P.arena_reset(mark)


def final_norm(P, src, srcname, dst, dstname):
    mark = P.arena_off
    alloc_common(P)
    nfb = P.alloc([D])
    c_nf = P.win("c_nfb", [D])
    P.dma("sp", V(nfb, "nfb"), V(c_nf.partition_broadcast(128), "in_w"), key="const2")
    nblk = P.T // TB
    n = 0
    for blk in range(nblk):
        for s in range(4):
            xi = n % 2
            n += 1
            xt, xo = P.xt[xi], P.xr[xi]
            xtv = V(xt, ("xt", xi))
            P.dma("sp", xtv, V(tile_rows(src, blk, s), (srcname, blk, s)), key=("xt", xi))
            ssv = V(P.ss[xi][:, 0:1], ("ss", xi))
            rv = V(P.rstd[xi][:, 0:1], ("rstd", xi))
            P.act(V(P.junk, "junk"), xtv, AF.Square, accum=ssv)
            P.act(rv, ssv, AF.Sqrt, scale=1.0 / D, bias=V(P.epsv, "epsv"))
            P.recip(rv, rv)
            P.stt(V(xo, ("xr", xi)), xtv, rv, V(nfb, "nfb"), ALU.mult, ALU.mult)
            P.dma("sp", V(tile_rows(dst, blk, s), (dstname, blk, s)), V(xo, ("xr", xi)), key=("xr", xi))
    P.arena_reset(mark)


RET_H = 4
RET_DK = 256
RET_DV = 512


def retnet_pass(P, h0, srcN, srcNname, srcR, srcRname, dst, dstname):
    mark = P.arena_off
    alloc_common(P)
    nblk = P.T // TB
    T = P.T
    w_in = P.win("ret_w_in_p", [D, 6144])
    w_out = P.win("ret_w_out", [2048, D])
    c_cos = P.win("c_ret_cos", [128, 4096])
    c_sin = P.win("c_ret_sin", [128, 4096])
    c_decT = P.win("c_ret_decT", [128, RET_H, 128])
    c_gl = P.win("c_ret_gl", [RET_H, TB])
    c_kdec = P.win("c_ret_kdec", [128, RET_H])
    wq = P.alloc([KC, 512], BF16)
    wk = P.alloc([KC, 512], BF16)
    wv = P.alloc([KC, 1024], BF16)
    wg = P.alloc([KC, 1024], BF16)
    wo = P.alloc([8, D], BF16)
    cos = P.alloc([TB])
    sin = P.alloc([TB])
    qT = P.alloc([2, 2, TB], BF16)
    kT = P.alloc([2, 2, TB], BF16)
    qg = P.alloc([2, 2, TB], BF16)
    rt = [P.alloc([TB]) for _ in range(4)]
    vt = P.alloc([4, 2, 512], BF16)
    khat = P.alloc([4, 2, 256], BF16)
    sg = P.alloc([8, TB], BF16)
    yT = P.alloc([8, TB], BF16)
    S = P.alloc([2, 2, 512])
    Sbf = P.alloc([2, 2, 512], BF16)
    decT = P.alloc([RET_H, 128])
    gl = P.alloc([2, TB])
    kdec = P.alloc([RET_H])
    PT = [P.alloc([128], BF16) for _ in range(4)]
    ysq = [P.alloc([512], BF16) for _ in range(4)]
    rs = [P.alloc([128]) for _ in range(4)]
    tmp = [P.alloc([4, 128]) for _ in range(4)]
    src = w_in.rearrange("(k p) n -> p k n", p=128)
    P.dma("pool", V(wq, ("w", 0)), V(src[:, :, h0 * 256:h0 * 256 + 512], "in_w"), key=("w", 0))
    P.dma("pool", V(wk, ("w", 1)), V(src[:, :, 1024 + h0 * 256:1024 + h0 * 256 + 512], "in_w"), key=("w", 1))
    P.dma("pool", V(wv, ("w", 2)), V(src[:, :, 2048 + h0 * 512:2048 + h0 * 512 + 1024], "in_w"), key=("w", 2))
    P.dma("pool", V(wg, ("w", 3)), V(src[:, :, 4096 + h0 * 512:4096 + h0 * 512 + 1024], "in_w"), key=("w", 3))
    P.dma("pool", V(wo, ("w", 4)), V(w_out[h0 * 512:h0 * 512 + 1024, :].rearrange("(c p) n -> p c n", p=128), "in_w"),
          key=("w", 4))
    P.dma("sp", V(decT, "decT"), V(c_decT, "in_w"), key="c0")
    P.dma("sp", V(kdec, "kdec"), V(c_kdec, "in_w"), key="c1")
    for hl in range(2):
        P.dma("sp", V(gl[:, hl, :], ("gl", hl)), V(c_gl[h0 + hl].partition_broadcast(128), "in_w"), key=("c2", hl))
    P.memset("dve", V(S, "S"), 0.0)
    P.memset("pool", V(Sbf, "Sbf"), 0.0)
    g128 = [float((1.0 - 2.0 ** (-5.0 - (h0 + hl))) ** 128) for hl in range(2)]
    pn = [0]

    def pbank():
        b = pn[0] % 3
        pn[0] += 1
        return V(P.psb[b], ("psb", b))

    def stageB(blk, slot, tick):
        xnT = P.xnT[slot]
        xv = V(xnT, ("xnT", slot))
        P.dma("sp", V(cos, "cos"), V(c_cos[:, blk * TB:(blk + 1) * TB], "in_w"), key="cos")
        P.dma("sp", V(sin, "sin"), V(c_sin[:, blk * TB:(blk + 1) * TB], "in_w"), key="sin")
        for (wsel, wtok, dstT, dname) in ((wq, ("w", 0), qT, "qT"), (wk, ("w", 1), kT, "kT")):
            for hl in range(2):
                p1 = pbank()
                for kc in range(KC):
                    P.mm(p1, V(wsel[:, kc, hl * 256:hl * 256 + 128], wtok), V(xnT[:, kc, :], ("xnT", slot)),
                         start=(kc == 0), stop=(kc == KC - 1))
                p2 = pbank()
                for kc in range(KC):
                    P.mm(p2, V(wsel[:, kc, hl * 256 + 128:hl * 256 + 256], wtok), V(xnT[:, kc, :], ("xnT", slot)),
                         start=(kc == 0), stop=(kc == KC - 1))
                r = [V(rt[i], ("rt", i)) for i in range(4)]
                P.tt("dve", r[0], p1, V(cos, "cos"), ALU.mult)
                P.tt("dve", r[1], p2, V(sin, "sin"), ALU.mult)
                P.tt("dve", r[2], p2, V(cos, "cos"), ALU.mult)
                P.tt("dve", r[3], p1, V(sin, "sin"), ALU.mult)
                P.tt("pool", V(dstT[:, hl, 0, :], (dname, hl, 0)), r[0], r[1], ALU.subtract)
                P.tt("pool", V(dstT[:, hl, 1, :], (dname, hl, 1)), r[2], r[3], ALU.add)
                if dname == "qT":
                    for e in range(2):
                        P.tt("pool", V(qg[:, hl, e, :], ("qg", hl, e)), V(qT[:, hl, e, :], ("qT", hl, e)),
                             V(gl[:, hl, :], ("gl", hl)), ALU.mult)
        tick()
        for hl in range(2):
            for j in range(4):
                pg = pbank()
                c = hl * 512 + j * 128
                for kc in range(KC):
                    P.mm(pg, V(wg[:, kc, c:c + 128], ("w", 3)), V(xnT[:, kc, :], ("xnT", slot)),
                         start=(kc == 0), stop=(kc == KC - 1))
                P.act(V(sg[:, hl * 4 + j, :], ("sg", hl, j)), pg, AF.Silu)
        tick()
        for c4 in range(4):
            for hl in range(2):
                pv = pbank()
                for kc in range(KC):
                    P.mm(pv, V(xnT[:, kc, c4 * 128:(c4 + 1) * 128], ("xnT", slot)),
                         V(wv[:, kc, hl * 512:(hl + 1) * 512], ("w", 2)), start=(kc == 0), stop=(kc == KC - 1))
                P.copy("act", V(vt[:, c4, hl, :], ("vt", c4, hl)), pv)
        tick()
        psT6 = P.psb[6].bitcast(BF16)
        for c4 in range(4):
            for hl in range(2):
                for e in range(2):
                    P.transpose(V(psT6[:, e * 128:(e + 1) * 128], ("psb", 6)),
                                V(kT[:, hl, e, c4 * 128:(c4 + 1) * 128], ("kT", hl, e)), V(P.ident, "ident"))
                P.act(V(khat[:, c4, hl, :], ("khat", c4, hl)), V(psT6[:, 0:256], ("psb", 6)), AF.Identity,
                      scale=V(kdec[:, h0 + hl:h0 + hl + 1], "kdec"))
        tick()
        n = 0
        for c4 in range(4):
            sl = slice(c4 * 128, (c4 + 1) * 128)
            for hl in range(2):
                h = h0 + hl
                i2 = n % 4
                n += 1
                psS = V(P.psb[3][:, 0:128], ("psb", 3))
                for e in range(2):
                    P.mm(psS, V(kT[:, hl, e, sl], ("kT", hl, e)), V(qT[:, hl, e, sl], ("qT", hl, e)),
                         start=(e == 0), stop=(e == 1))
                ptv = V(PT[i2], ("PT", i2))
                P.tt("dve", ptv, psS, V(decT[:, h, :], "decT"), ALU.mult)
                psO = P.psb[4]
                for j in range(4):
                    po = V(psO[:, j * 128:(j + 1) * 128], ("psb", 4))
                    P.mm(po, V(vt[:, c4, hl, j * 128:(j + 1) * 128], ("vt", c4, hl)), ptv, start=True, stop=False)
                    for e in range(2):
                        P.mm(po, V(Sbf[:, hl, e, j * 128:(j + 1) * 128], ("Sbf", hl, e)),
                             V(qg[:, hl, e, sl], ("qg", hl, e)), start=False, stop=(e == 1))
                pov = V(psO, ("psb", 4))
                yq = V(ysq[i2], ("ysq", i2))
                P.act(yq, pov, AF.Square)
                psN = V(P.psb[3][:, 128:256], ("psb", 3))
                for j in range(4):
                    P.mm(psN, V(P.ones, "ones"), V(ysq[i2][:, j * 128:(j + 1) * 128], ("ysq", i2)),
                         start=(j == 0), stop=(j == 3))
                rv = V(rs[i2], ("rs", i2))
                P.act(rv, psN, AF.Sqrt, scale=1.0 / RET_DV, bias=V(P.epsv, "epsv"))
                P.recip(rv, rv)
                tv = V(tmp[i2], ("tmp", i2))
                P.tt("dve", tv, V(psO.rearrange("p (j l) -> p j l", j=4), ("psb", 4)),
                     V(rs[i2].unsqueeze(1).broadcast_to([128, 4, 128]), ("rs", i2)), ALU.mult)
                P.tt("pool", V(yT[:, hl * 4:(hl + 1) * 4, sl], ("yT", hl, c4)), tv,
                     V(sg[:, hl * 4:(hl + 1) * 4, sl], ("sg", hl)), ALU.mult)
                for e in range(2):
                    pu = V(P.psb[5], ("psb", 5))
                    P.mm(pu, V(khat[:, c4, hl, e * 128:(e + 1) * 128], ("khat", c4, hl)),
                         V(vt[:, c4, hl, :], ("vt", c4, hl)), start=True, stop=True)
                    sv = V(S[:, hl, e, :], ("S", hl, e))
                    P.stt(sv, sv, g128[hl], pu, ALU.mult, ALU.add)
                    P.copy("pool", V(Sbf[:, hl, e, :], ("Sbf", hl, e)), sv)
        out_stage(P, blk, srcR, srcRname, dst, dstname,
                  lambda k, s: V(yT[:, k, s * 128:(s + 1) * 128], ("yT",)), 8, wo, ("w", 4))

    run_blocks(P, srcN, srcNname, 3, stageB)
    P.arena_reset(mark)


GLA_H = 4
GLA_DK = 128
GLA_DV = 256


def gla_pass(P, srcN, srcNname, srcR, srcRname, dst, dstname):
    mark = P.arena_off
    alloc_common(P)
    nblk = P.T // TB
    w_in = P.win("gla_w_in", [D, 3088])
    w_out = P.win("gla_w_out", [D, D])
    w_gk2 = P.win("gla_w_gk2", [16, 512])
    c_bgk = P.win("c_gla_bgk", [128, GLA_H])
    c_nw = P.win("c_gla_nw", [128, 2])
    c_maskT = P.win("c_maskT", [128, 128])
    c_scanm = P.win("c_scanm", [TB])
    wq = P.alloc([KC, 512], BF16)
    wk = P.alloc([KC, 512], BF16)
    wv = P.alloc([KC, 1024], BF16)
    wg = P.alloc([KC, 1024], BF16)
    wgk = P.alloc([KC, 16], BF16)
    wo = P.alloc([8, D], BF16)
    wgk2 = P.alloc([512])
    bgk = P.alloc([GLA_H])
    nbgk = P.alloc([GLA_H])
    nw = P.alloc([2])
    maskT = P.alloc([128])
    scanm = P.alloc([TB])
    gkf = P.alloc([TB])
    Gp = P.alloc([GLA_H, TB])
    et = [P.alloc([TB]) for _ in range(2)]
    qT = P.alloc([GLA_H, TB], BF16)
    kT = P.alloc([GLA_H, TB], BF16)
    vt = P.alloc([4, 1024], BF16)
    khat = P.alloc([4, GLA_H, 128], BF16)
    sg = P.alloc([8, TB], BF16)
    yT = P.alloc([8, TB], BF16)
    S = P.alloc([GLA_H, 256])
    Sbf = P.alloc([GLA_H, 256], BF16)
    elast = P.alloc([GLA_H, 4])
    PT = [P.alloc([128], BF16) for _ in range(4)]
    ysq = [P.alloc([256], BF16) for _ in range(4)]
    rs = [P.alloc([128]) for _ in range(4)]
    tmp = [P.alloc([2, 128]) for _ in range(4)]
    src = w_in.rearrange("(k p) n -> p k n", p=128)
    P.dma("pool", V(wq, ("w", 0)), V(src[:, :, 0:512], "in_w"), key=("w", 0))
    P.dma("pool", V(wk, ("w", 1)), V(src[:, :, 512:1024], "in_w"), key=("w", 1))
    P.dma("pool", V(wv, ("w", 2)), V(src[:, :, 1024:2048], "in_w"), key=("w", 2))
    P.dma("pool", V(wg, ("w", 3)), V(src[:, :, 2048:3072], "in_w"), key=("w", 3))
    P.dma("pool", V(wgk, ("w", 5)), V(src[:, :, 3072:3088], "in_w"), key=("w", 5))
    P.dma("pool", V(wo, ("w", 4)), V(w_out.rearrange("(c p) n -> p c n", p=128), "in_w"), key=("w", 4))
    P.dma("sp", V(wgk2[0:16, :], "wgk2"), V(w_gk2, "in_w"), key="c0")
    P.dma("sp", V(bgk, "bgk"), V(c_bgk, "in_w"), key="c1")
    P.dma("sp", V(nw, "nw"), V(c_nw, "in_w"), key="c2")
    P.dma("sp", V(maskT, "maskT"), V(c_maskT, "in_w"), key="c3")
    P.dma("sp", V(scanm, "scanm"), V(c_scanm.partition_broadcast(128), "in_w"), key="c4")
    P.ts("dve", V(nbgk, "nbgk"), V(bgk, "bgk"), -1.0, ALU.mult)
    P.memset("dve", V(S, "S"), 0.0)
    P.memset("pool", V(Sbf, "Sbf"), 0.0)
    lnsc = float(np.log(GLA_DK ** -0.5))
    pn = [0]

    def pbank():
        b = pn[0] % 3
        pn[0] += 1
        return V(P.psb[b], ("psb", b))

    def stageB(blk, slot, tick):
        xnT = P.xnT[slot]
        xtok = ("xnT", slot)
        pg = pbank()
        for kc in range(KC):
            P.mm(V(pg.ap[0:16, :], pg.tok), V(wgk[:, kc, :], ("w", 5)), V(xnT[:, kc, :], xtok),
                 start=(kc == 0), stop=(kc == KC - 1))
        P.copy("act", V(gkf[0:16, :], "gkf"), V(pg.ap[0:16, :], pg.tok))
        for h in range(GLA_H):
            pp = pbank()
            P.mm(pp, V(wgk2[0:16, h * 128:(h + 1) * 128], "wgk2"), V(gkf[0:16, :], "gkf"), start=True, stop=True)
            e0 = V(et[0], ("et", 0))
            P.act(e0, pp, AF.Exp, scale=-1.0, bias=V(nbgk[:, h:h + 1], "nbgk"))
            P.act(e0, e0, AF.Ln, scale=1.0, bias=1.0)
            gph = V(Gp[:, h, :], ("Gp", h))
            nc = P.nc
            P.S.op("dve", (lambda o=gph.ap, a=scanm, b=et[0]: nc.vector.tensor_tensor_scan(o, a, b, 0.0, ALU.mult, ALU.add)),
                   reads=[("scanm",), ("et", 0)], writes=[gph.tok])
            pq = pbank()
            for kc in range(KC):
                P.mm(pq, V(wq[:, kc, h * 128:(h + 1) * 128], ("w", 0)), V(xnT[:, kc, :], xtok),
                     start=(kc == 0), stop=(kc == KC - 1))
            e1 = V(et[1], ("et", 1))
            P.act(e1, gph, AF.Exp, scale=-1.0 / 16.0, bias=lnsc)
            P.tt("dve", V(qT[:, h, :], ("qT", h)), pq, e1, ALU.mult)
            pk = pbank()
            for kc in range(KC):
                P.mm(pk, V(wk[:, kc, h * 128:(h + 1) * 128], ("w", 1)), V(xnT[:, kc, :], xtok),
                     start=(kc == 0), stop=(kc == KC - 1))
            P.act(e1, gph, AF.Exp, scale=1.0 / 16.0)
            P.tt("dve", V(kT[:, h, :], ("kT", h)), pk, e1, ALU.mult)
            P.act(V(elast[:, h, :], ("elast", h)),
                  V(Gp[:, h, :].rearrange("p (c l) -> p c l", c=4)[:, :, 127], ("Gp", h)), AF.Exp, scale=-1.0 / 16.0)
        tick()
        for c in range(8):
            pg2 = pbank()
            for kc in range(KC):
                P.mm(pg2, V(wg[:, kc, c * 128:(c + 1) * 128], ("w", 3)), V(xnT[:, kc, :], xtok),
                     start=(kc == 0), stop=(kc == KC - 1))
            P.act(V(sg[:, c, :], ("sg", c)), pg2, AF.Silu)
            P.ts("pool", V(sg[:, c, :], ("sg", c)), V(sg[:, c, :], ("sg", c)), V(nw[:, (c % 2):(c % 2) + 1], "nw"), ALU.mult)
        tick()
        for c4 in range(4):
            for half in range(2):
                pv = pbank()
                for kc in range(KC):
                    P.mm(pv, V(xnT[:, kc, c4 * 128:(c4 + 1) * 128], xtok),
                         V(wv[:, kc, half * 512:(half + 1) * 512], ("w", 2)), start=(kc == 0), stop=(kc == KC - 1))
                P.copy("act", V(vt[:, c4, half * 512:(half + 1) * 512], ("vt", c4, half)), pv)
        tick()
        psT6 = P.psb[6].bitcast(BF16)
        for c4 in range(4):
            for h in range(GLA_H):
                P.transpose(V(psT6[:, h * 128:(h + 1) * 128], ("psb", 6)),
                            V(kT[:, h, c4 * 128:(c4 + 1) * 128], ("kT", h)), V(P.ident, "ident"))
            P.copy("act", V(khat[:, c4, :, :], ("khat", c4)),
                   V(psT6[:, 0:512].rearrange("p (h d) -> p h d", h=GLA_H), ("psb", 6)))
        tick()
        n = 0
        for c4 in range(4):
            sl = slice(c4 * 128, (c4 + 1) * 128)
            for h in range(GLA_H):
                i2 = n % 4
                n += 1
                psS = V(P.psb[3][:, 0:128], ("psb", 3))
                P.mm(psS, V(kT[:, h, sl], ("kT", h)), V(qT[:, h, sl], ("qT", h)), start=True, stop=True)
                ptv = V(PT[i2], ("PT", i2))
                P.tt("dve", ptv, psS, V(maskT, "maskT"), ALU.mult)
                psO = P.psb[4]
                for j in range(2):
                    po = V(psO[:, j * 128:(j + 1) * 128], ("psb", 4))
                    vc = h * 256 + j * 128
                    P.mm(po, V(vt[:, c4, vc:vc + 128], ("vt", c4, vc // 512)), ptv, start=True, stop=False)
                    P.mm(po, V(Sbf[:, h, j * 128:(j + 1) * 128], ("Sbf", h)), V(qT[:, h, sl], ("qT", h)),
                         start=False, stop=True)
                pov = V(psO[:, 0:256], ("psb", 4))
                yq = V(ysq[i2], ("ysq", i2))
                P.act(yq, pov, AF.Square)
                psN = V(P.psb[3][:, 128:256], ("psb", 3))
                for j in range(2):
                    P.mm(psN, V(P.ones, "ones"), V(ysq[i2][:, j * 128:(j + 1) * 128], ("ysq", i2)),
                         start=(j == 0), stop=(j == 1))
                rv = V(rs[i2], ("rs", i2))
                P.act(rv, psN, AF.Sqrt, scale=1.0 / GLA_DV, bias=V(P.epsv, "epsv"))
                P.recip(rv, rv)
                tv = V(tmp[i2], ("tmp", i2))
                P.tt("dve", tv, V(psO[:, 0:256].rearrange("p (j l) -> p j l", j=2), ("psb", 4)),
                     V(rs[i2].unsqueeze(1).broadcast_to([128, 2, 128]), ("rs", i2)), ALU.mult)
                P.tt("pool", V(yT[:, h * 2:(h + 1) * 2, sl], ("yT", h, c4)), tv,
                     V(sg[:, h * 2:(h + 1) * 2, sl], ("sg",)), ALU.mult)
                pu = V(P.psb[5][:, 0:256], ("psb", 5))
                P.mm(pu, V(khat[:, c4, h, :], ("khat", c4)), V(vt[:, c4, h * 256:(h + 1) * 256], ("vt", c4, h // 2)),
                     start=True, stop=True)
                sv = V(S[:, h, :], ("S", h))
                P.tt("dve", sv, sv, pu, ALU.add)
                P.ts("dve", sv, sv, V(elast[:, h, c4:c4 + 1], ("elast", h)), ALU.mult)
                P.copy("pool", V(Sbf[:, h, :], ("Sbf", h)), sv)
        out_stage(P, blk, srcR, srcRname, dst, dstname,
                  lambda k, s: V(yT[:, k, s * 128:(s + 1) * 128], ("yT",)), 8, wo, ("w", 4))

    run_blocks(P, srcN, srcNname, 2, stageB)
    P.arena_reset(mark)


def ssd_pass(P, p, srcN, srcNname, srcR, srcRname, dst, dstname):
    mark = P.arena_off
    alloc_common(P)
    nc = P.nc
    NSL = 4
    nblk = P.T // TB
    w_in = P.win("ssd_w_in", [D, 6176])
    w_out = P.win("ssd_w_out", [2048, D])
    c_cw = P.win("c_ssd_cw", [128, 32, 4])
    c_cb = P.win("c_ssd_cb", [128, 32])
    c_dtb = P.win("ssd_dt_bias", [32])
    c_alog = P.win("ssd_a_log", [32])
    c_dsk = P.win("ssd_d", [32])
    c_nw = P.win("ssd_norm_w", [2048])
    c_maskT = P.win("c_maskT", [128, 128])
    c_SU = P.win("c_SU", [128, 128])
    wz = P.alloc([KC, 1024], BF16)
    wxs = P.alloc([KC, 1024], BF16)
    wB = P.alloc([KC, 512], BF16)
    wC = P.alloc([KC, 512], BF16)
    wdt = P.alloc([KC, 16], BF16)
    wo = P.alloc([8, D], BF16)
    cw = P.alloc([32, 4])
    cb = P.alloc([32])
    spill = P.alloc([16, 3])
    dtb = P.alloc([16])
    abc = P.alloc([16])
    dsk = P.alloc([16])
    nwc = P.alloc([1024])
    maskT = P.alloc([128])
    SU = P.alloc([128])
    onesf = P.alloc([128])
    A = [P.alloc([TB + 3]) for _ in range(3)]
    xsT = P.alloc([8, TB], BF16)
    kT = P.alloc([4, TB], BF16)
    qT = P.alloc([4, TB], BF16)
    sz = P.alloc([4, 1024], BF16)
    dtt = P.alloc([16])
    ld = P.alloc([16])
    Gs = P.alloc([16])
    eG = P.alloc([16])
    eGl = P.alloc([16])
    wdec = P.alloc([16])
    tiny = P.alloc([16])
    R = [P.alloc([4, 128]) for _ in range(NSL)]
    dec = [P.alloc([4, 128]) for _ in range(NSL)]
    sm = [P.alloc([128]) for _ in range(NSL)]
    PT = [P.alloc([4, 128], BF16) for _ in range(NSL)]
    xk = [P.alloc([384], BF16) for _ in range(NSL)]
    vv = [P.alloc([256], BF16) for _ in range(NSL)]
    vh = [P.alloc([256], BF16) for _ in range(NSL)]
    ot = [P.alloc([256]) for _ in range(NSL)]
    t2 = [P.alloc([256]) for _ in range(NSL)]
    yv = [P.alloc([256]) for _ in range(NSL)]
    yn = [P.alloc([256], BF16) for _ in range(NSL)]
    ssq = [P.alloc([1]) for _ in range(NSL)]
    yT = P.alloc([8, TB], BF16)
    S = P.alloc([4, 256])
    Sbf = P.alloc([4, 256], BF16)
    src = w_in.rearrange("(k p) n -> p k n", p=128)
    P.dma("pool", V(wz, ("w", 0)), V(src[:, :, p * 1024:(p + 1) * 1024], "in_w"), key=("w", 0))
    P.dma("pool", V(wxs, ("w", 1)), V(src[:, :, 2048 + p * 1024:2048 + (p + 1) * 1024], "in_w"), key=("w", 1))
    P.dma("pool", V(wB, ("w", 2)), V(src[:, :, 4096 + p * 512:4096 + (p + 1) * 512], "in_w"), key=("w", 2))
    P.dma("pool", V(wC, ("w", 3)), V(src[:, :, 5120 + p * 512:5120 + (p + 1) * 512], "in_w"), key=("w", 3))
    P.dma("pool", V(wdt, ("w", 5)), V(src[:, :, 6144 + p * 16:6144 + (p + 1) * 16], "in_w"), key=("w", 5))
    P.dma("pool", V(wo, ("w", 4)), V(w_out[p * 1024:(p + 1) * 1024, :].rearrange("(c p) n -> p c n", p=128), "in_w"),
          key=("w", 4))
    P.dma("sp", V(cw, "cw"), V(c_cw, "in_w"), key="c0")
    P.dma("sp", V(cb, "cb"), V(c_cb, "in_w"), key="c1")
    P.dma("sp", V(dtb, "dtb"), V(c_dtb[p * 16:(p + 1) * 16].partition_broadcast(128), "in_w"), key="c2")
    P.dma("sp", V(abc, "abc"), V(c_alog[p * 16:(p + 1) * 16].partition_broadcast(128), "in_w"), key="c3")
    P.dma("sp", V(dsk, "dsk"), V(c_dsk[p * 16:(p + 1) * 16].partition_broadcast(128), "in_w"), key="c4")
    P.dma("sp", V(nwc, "nwc"), V(c_nw[p * 1024:(p + 1) * 1024].partition_broadcast(128), "in_w"), key="c5")
    P.dma("sp", V(maskT, "maskT"), V(c_maskT, "in_w"), key="c6")
    P.dma("sp", V(SU, "SU"), V(c_SU, "in_w"), key="c7")
    P.memset("pool", V(onesf, "onesf"), 1.0)
    P.memset("pool", V(spill, "spill"), 0.0)
    P.act(V(abc, "abc"), V(abc, "abc"), AF.Exp)
    P.ts("dve", V(abc, "abc"), V(abc, "abc"), -1.0, ALU.mult)
    P.memset("dve", V(S, "S"), 0.0)
    P.memset("pool", V(Sbf, "Sbf"), 0.0)
    pn = [0]

    def pbank():
        b = pn[0] % 3
        pn[0] += 1
        return V(P.psb[b], ("psb", b))

    def conv_chunk(lc, wsel, wtok, col, xnT, slot, dst):
        cc = (8 * p + lc) if lc < 8 else ((16 + 4 * p + lc - 8) if lc < 12 else (24 + 4 * p + lc - 12))
        pu = pbank()
        for kc in range(KC):
            P.mm(pu, V(wsel[:, kc, col:col + 128], wtok), V(xnT[:, kc, :], ("xnT", slot)),
                 start=(kc == 0), stop=(kc == KC - 1))
        ai = lc % 3
        At, atok = A[ai], ("A", ai)
        P.act(V(At[:, 0:TB], atok), pu, AF.Identity, scale=V(cw[:, cc, 3:4], "cw"), bias=V(cb[:, cc:cc + 1], "cb"))
        P.memset("pool", V(At[:, TB:TB + 3], atok), 0.0)
        for sh in (1, 2, 3):
            P.stt(V(At[:, sh:TB + sh], atok), pu, V(cw[:, cc, 3 - sh:4 - sh], "cw"), V(At[:, sh:TB + sh], atok),
                  ALU.mult, ALU.add)
        P.tt("pool", V(At[:, 0:3], atok), V(At[:, 0:3], atok), V(spill[:, lc, :], ("spill", lc)), ALU.add)
        P.copy("pool", V(spill[:, lc, :], ("spill", lc)), V(At[:, TB:TB + 3], atok))
        P.act(dst, V(At[:, 0:TB], atok), AF.Silu)

    def stageB(blk, slot, tick):
        xnT = P.xnT[slot]
        xtok = ("xnT", slot)
        for lc in range(8):
            conv_chunk(lc, wxs, ("w", 1), lc * 128, xnT, slot, V(xsT[:, lc, :], ("xsT", lc)))
        tick()
        for gl in range(4):
            conv_chunk(8 + gl, wB, ("w", 2), gl * 128, xnT, slot, V(kT[:, gl, :], ("kT", gl)))
            conv_chunk(12 + gl, wC, ("w", 3), gl * 128, xnT, slot, V(qT[:, gl, :], ("qT", gl)))
        tick()
        for c4 in range(4):
            for half in range(2):
                pz = pbank()
                for kc in range(KC):
                    P.mm(pz, V(xnT[:, kc, c4 * 128:(c4 + 1) * 128], xtok),
                         V(wz[:, kc, half * 512:(half + 1) * 512], ("w", 0)), start=(kc == 0), stop=(kc == KC - 1))
                P.act(V(sz[:, c4, half * 512:(half + 1) * 512], ("sz", c4, half)), pz, AF.Silu)
        tick()
        n = 0
        psT6 = P.psb[6].bitcast(BF16)
        for c4 in range(4):
            sl = slice(c4 * 128, (c4 + 1) * 128)
            if c4 == 2:
                tick()
            pdt = V(P.psb[4][:, 128:144], ("psb", 4, "d"))
            for kc in range(KC):
                P.mm(pdt, V(xnT[:, kc, sl], xtok), V(wdt[:, kc, :], ("w", 5)), start=(kc == 0), stop=(kc == KC - 1))
            tn = V(tiny, "tiny")
            P.tt("dve", tn, pdt, V(dtb, "dtb"), ALU.add)
            P.act(tn, tn, AF.Exp)
            P.act(V(dtt, "dtt"), tn, AF.Ln, scale=1.0, bias=1.0)
            P.tt("dve", V(ld, "ld"), V(dtt, "dtt"), V(abc, "abc"), ALU.mult)
            pG = V(P.psb[4][:, 144:160], ("psb", 4, "d"))
            P.mm(pG, V(maskT, "maskT"), V(ld, "ld"), start=True, stop=True)
            pGl = V(P.psb[4][:, 160:176], ("psb", 4, "d"))
            P.mm(pGl, V(onesf, "onesf"), V(ld, "ld"), start=True, stop=True)
            P.act(V(Gs, "Gs"), pG, AF.Identity)
            P.act(V(eG, "eG"), pG, AF.Exp)
            P.act(V(eGl, "eGl"), pGl, AF.Exp)
            P.tt("dve", tn, pGl, V(Gs, "Gs"), ALU.subtract)
            P.act(V(wdec, "wdec"), tn, AF.Exp)
            for gl in range(4):
                i2 = n % NSL
                n += 1
                hs = slice(gl * 4, gl * 4 + 4)
                Rv = V(R[i2], ("R", i2))
                P.tt("dve", Rv, V(maskT.unsqueeze(1).broadcast_to([128, 4, 128]), "maskT"),
                     V(ld[:, hs].unsqueeze(2).broadcast_to([128, 4, 128]), "ld"), ALU.mult)
                pSeg = V(P.psb[3], ("psb", 3))
                P.mm(pSeg, V(SU, "SU"), V(R[i2].rearrange("p h l -> p (h l)"), ("R", i2)), start=True, stop=True)
                dv_ = V(dec[i2], ("dec", i2))
                P.act(V(dec[i2].rearrange("p h l -> p (h l)"), ("dec", i2)), pSeg, AF.Exp)
                pS = V(P.psb[4][:, 0:128], ("psb", 4, "s"))
                P.mm(pS, V(kT[:, gl, sl], ("kT", gl)), V(qT[:, gl, sl], ("qT", gl)), start=True, stop=True)
                smv = V(sm[i2], ("sm", i2))
                P.tt("dve", smv, pS, V(maskT, "maskT"), ALU.mult)
                ptv = V(PT[i2], ("PT", i2))
                P.tt("pool", ptv, dv_, V(sm[i2].unsqueeze(1).broadcast_to([128, 4, 128]), ("sm", i2)), ALU.mult)
                P.transpose(V(psT6[:, 0:128], ("psb", 6, "a")), V(xsT[:, gl * 2, sl], ("xsT", gl * 2)), V(P.ident, "ident"))
                P.transpose(V(psT6[:, 128:256], ("psb", 6, "a")), V(xsT[:, gl * 2 + 1, sl], ("xsT", gl * 2 + 1)),
                            V(P.ident, "ident"))
                P.transpose(V(psT6[:, 256:384], ("psb", 6, "a")), V(kT[:, gl, sl], ("kT", gl)), V(P.ident, "ident"))
                xkv = V(xk[i2], ("xk", i2))
                P.copy("act", xkv, V(psT6[:, 0:384], ("psb", 6, "a")))
                xs4 = V(xk[i2][:, 0:256].rearrange("p (h d) -> p h d", h=4), ("xk", i2))
                v4 = V(vv[i2].rearrange("p (h d) -> p h d", h=4), ("vv", i2))
                P.tt("dve", v4, xs4, V(dtt[:, hs].unsqueeze(2).broadcast_to([128, 4, 64]), "dtt"), ALU.mult)
                vh4 = V(vh[i2].rearrange("p (h d) -> p h d", h=4), ("vh", i2))
                P.tt("pool", vh4, v4, V(wdec[:, hs].unsqueeze(2).broadcast_to([128, 4, 64]), "wdec"), ALU.mult)
                for hh in range(4):
                    P.mm(V(P.psb[5][:, hh * 64:(hh + 1) * 64], ("psb", 5, "a")), V(PT[i2][:, hh, :], ("PT", i2)),
                         V(vv[i2][:, hh * 64:(hh + 1) * 64], ("vv", i2)), start=True, stop=True)
                pB = V(P.psb[5][:, 256:512], ("psb", 5, "b"))
                P.mm(pB, V(qT[:, gl, sl], ("qT", gl)), V(Sbf[:, gl, :], ("Sbf", gl)), start=True, stop=True)
                o4 = V(ot[i2].rearrange("p (h d) -> p h d", h=4), ("ot", i2))
                P.tt("dve", o4, V(P.psb[5][:, 256:512].rearrange("p (h d) -> p h d", h=4), ("psb", 5, "b")),
                     V(eG[:, hs].unsqueeze(2).broadcast_to([128, 4, 64]), "eG"), ALU.mult)
                ov = V(ot[i2], ("ot", i2))
                P.tt("dve", ov, ov, V(P.psb[5][:, 0:256], ("psb", 5, "a")), ALU.add)
                t24 = V(t2[i2].rearrange("p (h d) -> p h d", h=4), ("t2", i2))
                P.tt("pool", t24, xs4, V(dsk[:, hs].unsqueeze(2).broadcast_to([128, 4, 64]), "dsk"), ALU.mult)
                P.tt("pool", ov, ov, V(t2[i2], ("t2", i2)), ALU.add)
                yvv = V(yv[i2], ("yv", i2))
                P.tt("pool", yvv, ov, V(sz[:, c4, gl * 256:(gl + 1) * 256], ("sz", c4, gl // 2)), ALU.mult)
                sq = V(ssq[i2], ("ssq", i2))
                P.act(V(P.junk[:, 0:256], "junk"), yvv, AF.Square, accum=sq)
                P.act(sq, sq, AF.Sqrt, scale=1.0 / 256.0, bias=V(P.epsv, "epsv"))
                P.recip(sq, sq)
                ynv = V(yn[i2], ("yn", i2))
                P.stt(ynv, yvv, sq, V(nwc[:, gl * 256:(gl + 1) * 256], "nwc"), ALU.mult, ALU.mult)
                for j in range(2):
                    P.transpose(V(psT6[:, 512 + j * 128:512 + (j + 1) * 128], ("psb", 6, "b")),
                                V(yn[i2][:, j * 128:(j + 1) * 128], ("yn", i2)), V(P.ident, "ident"))
                P.copy("act", V(yT[:, gl * 2:(gl + 1) * 2, sl], ("yT", gl, c4)),
                       V(psT6[:, 512:768].rearrange("p (j l) -> p j l", j=2), ("psb", 6, "b")))
                pU = V(P.psb[4][:, 256:512], ("psb", 4, "u"))
                P.mm(pU, V(xk[i2][:, 256:384], ("xk", i2)), V(vh[i2], ("vh", i2)), start=True, stop=True)
                s4 = V(S[:, gl, :].rearrange("p (h d) -> p h d", h=4), ("S", gl))
                P.tt("dve", s4, s4, V(eGl[:, hs].unsqueeze(2).broadcast_to([128, 4, 64]), "eGl"), ALU.mult)
                sv = V(S[:, gl, :], ("S", gl))
                P.tt("dve", sv, sv, pU, ALU.add)
                P.copy("pool", V(Sbf[:, gl, :], ("Sbf", gl)), sv)
        out_stage(P, blk, srcR, srcRname, dst, dstname,
                  lambda k, s: V(yT[:, k, s * 128:(s + 1) * 128], ("yT",)), 8, wo, ("w", 4))

    run_blocks(P, srcN, srcNname, 0, stageB)
    P.arena_reset(mark)


RW_C = 64
RW_GN_EPS = 64e-5


def rwkv_pass(P, p, srcN, srcNname, srcR, srcRname, dst, dstname):
    mark = P.arena_off
    alloc_common(P)
    nc = P.nc
    nblk = P.T // TB
    NK = 4
    c0 = p * 512
    w_rkv = P.win("rwkv_w_rkv", [3, D, D])
    w_out = P.win("rwkv_w_out", [D, D])
    w1d, a1d, g1d = P.win("rwkv_w1", [D, 64]), P.win("rwkv_a1", [D, 64]), P.win("rwkv_g1", [D, 160])
    w2d, a2d, g2d = P.win("rwkv_w2", [64, D]), P.win("rwkv_a2", [64, D]), P.win("rwkv_g2", [160, D])
    c_vec = P.win("c_rwkv_vec", [128, 12, KC])
    c_lnw, c_lnb = P.win("rwkv_ln_w", [D]), P.win("rwkv_ln_b", [D])
    c_m = P.win("c_rwkv_masks", [64, 4, 64])
    c_E = P.win("c_rwkv_E", [128, 2], BF16)
    c_bo = P.win("c_rwkv_bo", [128, 128], BF16)
    c_scm = P.win("c_rwkv_scanm", [TB])
    Wr = P.alloc([KC, 512], BF16)
    Wk = P.alloc([KC, 512], BF16)
    Wv = P.alloc([KC, 512], BF16)
    wo = P.alloc([NK, D], BF16)
    w1 = P.alloc([KC, 64], BF16)
    a1 = P.alloc([KC, 64], BF16)
    g1 = P.alloc([KC, 160], BF16)
    w2 = P.alloc([512], BF16)
    a2 = P.alloc([512], BF16)
    g2A = P.alloc([512], BF16)
    g2B = P.alloc([512], BF16)
    vec = P.alloc([12, KC])
    omka = P.alloc([KC])
    nw0 = P.alloc([KC])
    lnw = P.alloc([512])
    lnb = P.alloc([512])
    msk = P.alloc([4, 64])
    identb = P.alloc([64], BF16)
    Eh = P.alloc([2], BF16)
    bo = P.alloc([128], BF16)
    scm = P.alloc([TB])
    epsg = P.alloc([1])
    tinyb = P.alloc([1])
    xx = P.alloc([KC, TB], BF16)
    xi = [P.alloc([KC, TB], BF16) for _ in range(2)]
    xlast = P.alloc([KC], BF16)
    h1 = P.alloc([TB], BF16)
    ha = P.alloc([TB], BF16)
    hgA = P.alloc([TB], BF16)
    hgB = P.alloc([TB], BF16)
    aTm = [P.alloc([NK, TB], BF16) for _ in range(2)]
    rTm = [P.alloc([NK, TB], BF16) for _ in range(2)]
    bT = P.alloc([NK, TB], BF16)
    kT = P.alloc([NK, TB], BF16)
    rkT = P.alloc([NK, TB], BF16)
    yT = P.alloc([NK, TB], BF16)
    WC = P.alloc([NK, 8])
    f32t = [P.alloc([TB]) for _ in range(8)]
    NS = 2
    LmS = [[P.alloc([8, 64], BF16) for _ in range(2)] for _ in range(NS)]
    LTmS = [[P.alloc([8, 64], BF16) for _ in range(2)] for _ in range(NS)]
    XTS = [[P.alloc([8, 64], BF16) for _ in range(2)] for _ in range(NS)]
    XTF = [P.alloc([8, 64], BF16) for _ in range(NS)]
    AkT = [P.alloc([8, 64], BF16) for _ in range(NS)]
    ArbT = [P.alloc([8, 64], BF16) for _ in range(NS)]
    ArkT = [P.alloc([8, 64], BF16) for _ in range(NS)]
    Zs = P.alloc([512], BF16)
    Us = P.alloc([512], BF16)
    Vtm = [P.alloc([512], BF16) for _ in range(2)]
    BKtm = P.alloc([2, 512], BF16)
    yc = P.alloc([512])
    sq = P.alloc([512])
    bon = P.alloc([512])
    ytm = P.alloc([512], BF16)
    st8 = [P.alloc([8]) for _ in range(4)]
    S = P.alloc([NK, 64])
    Sbf = P.alloc([NK, 64], BF16)
    wsrc = lambda i_: w_rkv[i_].rearrange("(k p) n -> p k n", p=128)
    P.dma("pool", V(Wr, ("w", 0)), V(wsrc(0)[:, :, c0:c0 + 512], "in_w"), key=("w", 0))
    P.dma("pool", V(Wk, ("w", 1)), V(wsrc(1)[:, :, c0:c0 + 512], "in_w"), key=("w", 1))
    P.dma("pool", V(Wv, ("w", 2)), V(wsrc(2)[:, :, c0:c0 + 512], "in_w"), key=("w", 2))
    P.dma("pool", V(wo, ("w", 3)), V(w_out[c0:c0 + 512, :].rearrange("(c p) n -> p c n", p=128), "in_w"), key=("w", 3))
    P.dma("pool", V(w1, ("w", 4)), V(w1d.rearrange("(k p) n -> p k n", p=128), "in_w"), key=("w", 4))
    P.dma("pool", V(a1, ("w", 5)), V(a1d.rearrange("(k p) n -> p k n", p=128), "in_w"), key=("w", 5))
    P.dma("pool", V(g1, ("w", 6)), V(g1d.rearrange("(k p) n -> p k n", p=128), "in_w"), key=("w", 6))
    P.dma("pool", V(w2[0:64, :], ("w", 7)), V(w2d[:, c0:c0 + 512], "in_w"), key=("w", 7))
    P.dma("pool", V(a2[0:64, :], ("w", 8)), V(a2d[:, c0:c0 + 512], "in_w"), key=("w", 8))
    P.dma("pool", V(g2A, ("w", 9)), V(g2d[0:128, c0:c0 + 512], "in_w"), key=("w", 9))
    P.dma("pool", V(g2B[0:32, :], ("w", 10)), V(g2d[128:160, c0:c0 + 512], "in_w"), key=("w", 10))
    P.dma("sp", V(vec, "vec"), V(c_vec, "in_w"), key="c0")
    P.dma("sp", V(lnw[0:64, :], "lnw"), V(c_lnw[c0:c0 + 512].partition_broadcast(64), "in_w"), key="c1")
    P.dma("sp", V(lnb[0:64, :], "lnb"), V(c_lnb[c0:c0 + 512].partition_broadcast(64), "in_w"), key="c2")
    P.dma("sp", V(msk[0:64, :, :], "msk"), V(c_m, "in_w"), key="c3")
    P.dma("sp", V(Eh, "Eh"), V(c_E, "in_w"), key="c4")
    P.dma("sp", V(bo, "bo"), V(c_bo, "in_w"), key="c5")
    P.dma("sp", V(scm, "scm"), V(c_scm.partition_broadcast(128), "in_w"), key="c6")
    P.ts("dve", V(omka, "omka"), V(vec[:, 9, :], "vec"), -1.0, ALU.mult, 1.0, ALU.add)
    P.ts("dve", V(nw0, "nw0"), V(vec[:, 6, :], "vec"), -1.0, ALU.mult)
    P.copy("dve", V(identb[0:64, :], "identb"), V(msk[0:64, 3, :], "msk"))
    P.memset("pool", V(epsg, "epsg"), RW_GN_EPS)
    P.memset("pool", V(tinyb, "tinyb"), 1e-24)
    P.memset("pool", V(xlast, "xlast"), 0.0)
    P.memset("dve", V(S, "S"), 0.0)
    P.memset("pool", V(Sbf, "Sbf"), 0.0)
    for e_ in range(2):
        P.memset("pool", V(aTm[e_], ("aT", e_)), 0.0)
        P.memset("pool", V(rTm[e_], ("rT", e_)), 0.0)
    pn = [0]

    def pbank():
        b = pn[0] % 3
        pn[0] += 1
        return V(P.psb[b], ("psb", b))

    def mask(i_):
        return V(msk[0:64, i_, :].unsqueeze(1).broadcast_to([64, 8, 64]), "msk")

    def vcol(i_, kc):
        return V(vec[:, i_, kc:kc + 1], "vec")

    def mix(i_, xnT, xtok, buf):
        o = xi[buf]
        for kc in range(KC):
            if kc % 2 == 0:
                P.stt(V(o[:, kc, :], ("xi", buf, kc)), V(xx[:, kc, :], ("xx", kc)), vcol(i_, kc),
                      V(xnT[:, kc, :], xtok), ALU.mult, ALU.add)
            else:
                P.ts("pool", V(o[:, kc, :], ("xi", buf, kc)), V(xx[:, kc, :], ("xx", kc)), vcol(i_, kc), ALU.mult)
                P.tt("pool", V(o[:, kc, :], ("xi", buf, kc)), V(o[:, kc, :], ("xi", buf, kc)), V(xnT[:, kc, :], xtok), ALU.add)
        return o, ("xi", buf)

    def stage1(blk, slot, tick):
        xnT = P.xnT[slot]
        xtok = ("xnT", slot)
        P.tt("dve", V(xx[:, :, 1:TB], "xx"), V(xnT[:, :, 0:TB - 1], xtok), V(xnT[:, :, 1:TB], xtok), ALU.subtract)
        P.tt("dve", V(xx[:, :, 0:1], "xx"), V(xlast.unsqueeze(2), "xlast"), V(xnT[:, :, 0:1], xtok), ALU.subtract)
        P.copy("pool", V(xlast.unsqueeze(2), "xlast"), V(xnT[:, :, TB - 1:TB], xtok))
        xw, xwtok = mix(1, xnT, xtok, 0)
        pw = pbank()
        for kc in range(KC):
            P.mm(V(pw.ap[0:64, :], pw.tok), V(w1[:, kc, :], ("w", 4)), V(xw[:, kc, :], xwtok), start=(kc == 0), stop=(kc == KC - 1))
        P.act(V(h1[0:64, :], "h1"), V(pw.ap[0:64, :], pw.tok), AF.Tanh)
        xa, xatok = mix(4, xnT, xtok, 1)
        pa = pbank()
        for kc in range(KC):
            P.mm(V(pa.ap[0:64, :], pa.tok), V(a1[:, kc, :], ("w", 5)), V(xa[:, kc, :], xatok), start=(kc == 0), stop=(kc == KC - 1))
        P.copy("act", V(ha[0:64, :], "ha"), V(pa.ap[0:64, :], pa.tok))
        xg, xgtok = mix(5, xnT, xtok, 0)
        pg = pbank()
        for kc in range(KC):
            P.mm(pg, V(g1[:, kc, 0:128], ("w", 6)), V(xg[:, kc, :], xgtok), start=(kc == 0), stop=(kc == KC - 1))
        P.act(V(hgA, "hgA"), pg, AF.Sigmoid)
        pg = pbank()
        for kc in range(KC):
            P.mm(V(pg.ap[0:32, :], pg.tok), V(g1[:, kc, 128:160], ("w", 6)), V(xg[:, kc, :], xgtok), start=(kc == 0), stop=(kc == KC - 1))
        P.act(V(hgB[0:32, :], "hgB"), V(pg.ap[0:32, :], pg.tok), AF.Sigmoid)
        tick()
        xk_, xktok = mix(2, xnT, xtok, 1)
        xr_, xrtok = mix(0, xnT, xtok, 0)
        for kc in range(NK):
            gk = 4 * p + kc
            if kc == 2:
                tick()
            cs = slice(kc * 128, (kc + 1) * 128)
            t = [V(f32t[j], ("f32t", j)) for j in range(8)]
            pz = pbank()
            P.mm(pz, V(w2[0:64, cs], ("w", 7)), V(h1[0:64, :], "h1"), start=True, stop=True)
            P.act(t[0], pz, AF.Exp, scale=-1.0, bias=V(nw0[:, gk:gk + 1], "nw0"))
            P.act(t[0], t[0], AF.Ln, scale=1.0, bias=1.0)
            P.act(t[0], t[0], AF.Exp, scale=-1.0, bias=-0.5)
            P.S.op("dve", (lambda o=f32t[1], a_=scm, b_=f32t[0]: nc.vector.tensor_tensor_scan(o, a_, b_, 0.0, ALU.mult, ALU.add)),
                   reads=[("scm",), ("f32t", 0)], writes=[("f32t", 1)])
            P.tt("pool", t[2], t[1], t[0], ALU.subtract)
            P.act(t[2], t[2], AF.Exp, scale=-1.0)
            P.act(t[3], t[1], AF.Exp, scale=1.0)
            P.act(t[1], t[1], AF.Exp, scale=-1.0)
            P.copy("pool", V(WC[:, kc, :], ("WC", kc)), V(f32t[1].rearrange("p (c l) -> p c l", c=8)[:, :, RW_C - 1], ("f32t", 1)))
            pa2 = pbank()
            P.mm(pa2, V(a2[0:64, cs], ("w", 8)), V(ha[0:64, :], "ha"), start=True, stop=True)
            P.act(t[4], pa2, AF.Sigmoid, scale=1.0, bias=vcol(7, gk))
            pk = pbank()
            for k8 in range(KC):
                P.mm(pk, V(Wk[:, k8, cs], ("w", 1)), V(xk_[:, k8, :], xktok), start=(k8 == 0), stop=(k8 == KC - 1))
            P.ts("dve", t[5], pk, vcol(8, gk), ALU.mult)
            P.act(V(P.junk[:, 0:TB], "junk"), t[5], AF.Square)
            pss = pbank()
            P.mm(pss, V(bo, "bo"), V(P.junk[:, 0:TB], "junk"), start=True, stop=True)
            P.act(t[6], pss, AF.Ln, scale=1.0, bias=V(tinyb, "tinyb"))
            P.act(t[6], t[6], AF.Exp, scale=-0.5)
            P.tt("dve", t[5], t[5], t[6], ALU.mult)
            for e_ in range(2):
                ps_ = slice(e_ * 64, (e_ + 1) * 64)
                P.stt(V(aTm[e_][ps_, kc, :], ("aT", e_, kc)), V(f32t[5][ps_, :], ("f32t", 5)), -1.0,
                      V(f32t[2][ps_, :], ("f32t", 2)), ALU.mult, ALU.mult)
            P.tt("pool", t[6], t[5], t[4], ALU.mult)
            P.tt("pool", V(bT[:, kc, :], ("bT", kc)), t[6], t[3], ALU.mult)
            P.ts("dve", t[4], t[4], vcol(9, gk), ALU.mult, V(omka[:, gk:gk + 1], "omka"), ALU.add)
            P.tt("dve", t[4], pk, t[4], ALU.mult)
            P.tt("pool", V(kT[:, kc, :], ("kT", kc)), t[4], t[3], ALU.mult)
            pr = pbank()
            for k8 in range(KC):
                P.mm(pr, V(Wr[:, k8, cs], ("w", 0)), V(xr_[:, k8, :], xrtok), start=(k8 == 0), stop=(k8 == KC - 1))
            for e_ in range(2):
                ps_ = slice(e_ * 64, (e_ + 1) * 64)
                P.tt("dve", V(rTm[e_][ps_, kc, :], ("rT", e_, kc)), V(pr.ap[ps_, :], pr.tok),
                     V(f32t[1][ps_, :], ("f32t", 1)), ALU.mult)
            P.stt(V(rkT[:, kc, :], ("rkT", kc)), pr, vcol(10, gk), t[4], ALU.mult, ALU.mult)
        xv_, xvtok = mix(3, xnT, xtok, 1)
        return xv_, xvtok

    def phaseAB(c8, sl, ab):
        Lm, LTm = LmS[ab], LTmS[ab]
        XT = XTS[ab] + [None, None]
        XT[2 + ab] = XTF[ab]
        lt = lambda nm, i_: (nm, ab, i_)
        banks = [V(P.psb[b][0:64, :], ("psb", b)) for b in range(5)]
        for hl in range(8):
            kc, e_ = hl // 2, hl % 2
            a_ = V(aTm[e_][:, kc, sl], ("aT", e_, kc))
            b_ = V(bT[:, kc, sl], ("bT", kc))
            k_ = V(kT[:, kc, sl], ("kT", kc))
            r_ = V(rTm[e_][:, kc, sl], ("rT", e_, kc))
            hs = slice(hl * 64, (hl + 1) * 64)
            for bi, (l_, r2) in enumerate(((a_, b_), (b_, a_), (k_, a_), (b_, r_), (k_, r_))):
                P.mm(V(P.psb[bi][0:64, hs], ("psb", bi)), l_, r2, start=True, stop=True)
        v8 = lambda ap_: ap_.rearrange("p (h s) -> p h s", h=8)
        P.tt("dve", V(Lm[0][0:64], lt("Lm", 0)), V(v8(P.psb[0][0:64, :]), ("psb", 0)), mask(0), ALU.mult)
        P.tt("dve", V(LTm[0][0:64], lt("LTm", 0)), V(v8(P.psb[1][0:64, :]), ("psb", 1)), mask(1), ALU.mult)
        P.tt("dve", V(AkT[ab][0:64], ("AkT", ab)), V(v8(P.psb[2][0:64, :]), ("psb", 2)), mask(1), ALU.mult)
        P.tt("dve", V(ArbT[ab][0:64], ("ArbT", ab)), V(v8(P.psb[3][0:64, :]), ("psb", 3)), mask(2), ALU.mult)
        P.tt("dve", V(ArkT[ab][0:64], ("ArkT", ab)), V(v8(P.psb[4][0:64, :]), ("psb", 4)), mask(2), ALU.mult)
        P.tt("pool", V(XT[0][0:64], lt("XT", 0)), V(LTm[0][0:64], lt("LTm", 0)), mask(3), ALU.add)
        cur, xc = 0, 0
        for lvl in range(5):
            nx = 1 - cur
            last = (lvl == 4)
            for hl in range(8):
                hs = slice(hl * 64, (hl + 1) * 64)
                P.mm(V(P.psb[0][0:64, hs], ("psb", 0)), V(LTm[cur][0:64, hl, :], lt("LTm", cur)),
                     V(Lm[cur][0:64, hl, :], lt("Lm", cur)), start=True, stop=True)
                if not last:
                    P.mm(V(P.psb[1][0:64, hs], ("psb", 1)), V(Lm[cur][0:64, hl, :], lt("Lm", cur)),
                         V(LTm[cur][0:64, hl, :], lt("LTm", cur)), start=True, stop=True)
            P.copy("act", V(Lm[nx][0:64], lt("Lm", nx)), V(v8(P.psb[0][0:64, :]), ("psb", 0)))
            if not last:
                P.copy("act", V(LTm[nx][0:64], lt("LTm", nx)), V(v8(P.psb[1][0:64, :]), ("psb", 1)))
            xn_ = (2 + ab) if last else (1 - xc)
            for hl in range(8):
                hs = slice(hl * 64, (hl + 1) * 64)
                P.mm(V(P.psb[2][0:64, hs], ("psb", 2)), V(Lm[nx][0:64, hl, :], lt("Lm", nx)),
                     V(XT[xc][0:64, hl, :], lt("XT", xc)), start=True, stop=False)
                P.mm(V(P.psb[2][0:64, hs], ("psb", 2)), V(identb[0:64, :], "identb"),
                     V(XT[xc][0:64, hl, :], lt("XT", xc)), start=False, stop=True)
            P.copy("act", V(XT[xn_][0:64], lt("XT", xn_)), V(v8(P.psb[2][0:64, :]), ("psb", 2)))
            cur = nx
            xc = xn_

    def phaseCD(blk, c8, sl, ab, xv_, xvtok):
        b5 = V(P.psb[5][0:64, :], ("psb", 5))
        vt_ = Vtm[c8 % 2]
        vtok = ("Vtm", c8 % 2)
        for k8 in range(KC):
            P.mm(b5, V(xv_[:, k8, sl], xvtok), V(Wv[:, k8, :], ("w", 2)), start=(k8 == 0), stop=(k8 == KC - 1))
        P.copy("act", V(vt_[0:64, :], vtok), b5)
        for hl in range(8):
            kc, e_ = hl // 2, hl % 2
            hs = slice(hl * 64, (hl + 1) * 64)
            P.mm(V(P.psb[5][0:64, hs], ("psb", 5)), V(aTm[e_][:, kc, sl], ("aT", e_, kc)),
                 V(Sbf[:, kc, :], ("Sbf", kc)), start=True, stop=False)
            P.mm(V(P.psb[5][0:64, hs], ("psb", 5)), V(AkT[ab][0:64, hl, :], ("AkT", ab)),
                 V(vt_[0:64, hs], vtok), start=False, stop=True)
        P.copy("act", V(Zs[0:64, :], "Zs"), b5)
        for hl in range(8):
            hs = slice(hl * 64, (hl + 1) * 64)
            P.mm(V(P.psb[5][0:64, hs], ("psb", 5)), V(XTF[ab][0:64, hl, :], ("XT", ab, 2 + ab)),
                 V(Zs[0:64, hs], "Zs"), start=True, stop=True)
        P.copy("act", V(Us[0:64, :], "Us"), b5)
        for hl in range(8):
            kc, e_ = hl // 2, hl % 2
            hs = slice(hl * 64, (hl + 1) * 64)
            P.mm(V(P.psb[5][0:64, hs], ("psb", 5)), V(rTm[e_][:, kc, sl], ("rT", e_, kc)),
                 V(Sbf[:, kc, :], ("Sbf", kc)), start=True, stop=False)
            P.mm(V(P.psb[5][0:64, hs], ("psb", 5)), V(ArbT[ab][0:64, hl, :], ("ArbT", ab)),
                 V(Us[0:64, hs], "Us"), start=False, stop=False)
            P.mm(V(P.psb[5][0:64, hs], ("psb", 5)), V(ArkT[ab][0:64, hl, :], ("ArkT", ab)),
                 V(vt_[0:64, hs], vtok), start=False, stop=True)
        y8 = P.psb[5][0:64, :].rearrange("p (h v) -> p h v", h=8)
        s0, s1 = V(st8[0][0:64, :], ("st8", 0)), V(st8[1][0:64, :], ("st8", 1))
        P.S.op("dve", (lambda o=st8[0][0:64, :], i_=y8: nc.vector.tensor_reduce(o, i_, AX.X, ALU.add)),
               reads=[("psb", 5)], writes=[("st8", 0)])
        P.ts("dve", s0, s0, -1.0 / 64.0, ALU.mult)
        ycv = V(yc[0:64, :], "yc")
        yc8 = yc[0:64, :].rearrange("p (h v) -> p h v", h=8)
        P.tt("dve", V(yc8, "yc"), V(y8, ("psb", 5)), V(st8[0][0:64, :].unsqueeze(2).broadcast_to([64, 8, 64]), ("st8", 0)), ALU.add)
        P.act(V(sq[0:64, :], "sq"), ycv, AF.Square)
        P.S.op("dve", (lambda o=st8[1][0:64, :], i_=sq[0:64, :].rearrange("p (h v) -> p h v", h=8): nc.vector.tensor_reduce(o, i_, AX.X, ALU.add)),
               reads=[("sq",)], writes=[("st8", 1)])
        P.act(s1, s1, AF.Ln, scale=1.0 / 64.0, bias=V(epsg[0:64, :], "epsg"))
        P.act(s1, s1, AF.Exp, scale=-0.5)
        P.tt("dve", V(yc8, "yc"), V(yc8, "yc"), V(st8[1][0:64, :].unsqueeze(2).broadcast_to([64, 8, 64]), ("st8", 1)), ALU.mult)
        P.tt("pool", ycv, ycv, V(lnw[0:64, :], "lnw"), ALU.mult)
        P.tt("pool", ycv, ycv, V(lnb[0:64, :], "lnb"), ALU.add)
        b7f = P.psb[7]
        pBs = V(b7f[0:64, 384:392], ("psb", 7, "s"))
        for kc in range(NK):
            P.mm(V(b7f[0:64, 384 + 2 * kc:386 + 2 * kc], ("psb", 7, "s")), V(rkT[:, kc, sl], ("rkT", kc)), V(Eh, "Eh"),
                 start=True, stop=True)
        s2 = V(st8[2][0:64, :], ("st8", 2))
        P.copy("act", s2, pBs)
        P.tt("pool", V(bon[0:64, :].rearrange("p (h v) -> p h v", h=8), "bon"),
             V(vt_[0:64, :].rearrange("p (h v) -> p h v", h=8), vtok),
             V(st8[2][0:64, :].unsqueeze(2).broadcast_to([64, 8, 64]), ("st8", 2)), ALU.mult)
        P.tt("pool", ycv, ycv, V(bon[0:64, :], "bon"), ALU.add)
        psT7 = P.psb[7].bitcast(BF16)
        for kc in range(NK):
            P.transpose(V(psT7[0:64, kc * 128:(kc + 1) * 128], ("psb", 7, "t")), V(bT[:, kc, sl], ("bT", kc)), V(P.ident, "ident"))
        P.copy("act", V(BKtm[0:64, 0, :], ("BKtm", 0)), V(psT7[0:64, 0:512], ("psb", 7, "t")))
        for kc in range(NK):
            P.transpose(V(psT7[0:64, kc * 128:(kc + 1) * 128], ("psb", 7, "t")), V(kT[:, kc, sl], ("kT", kc)), V(P.ident, "ident"))
        P.copy("act", V(BKtm[0:64, 1, :], ("BKtm", 1)), V(psT7[0:64, 0:512], ("psb", 7, "t")))
        for kc in range(NK):
            cs = slice(kc * 128, (kc + 1) * 128)
            P.mm(V(P.psb[6][:, cs], ("psb", 6)), V(BKtm[0:64, 0, cs], ("BKtm", 0)), V(Us[0:64, cs], "Us"), start=True, stop=False)
            P.mm(V(P.psb[6][:, cs], ("psb", 6)), V(BKtm[0:64, 1, cs], ("BKtm", 1)), V(vt_[0:64, cs], vtok), start=False, stop=True)
        for hp in range(2):
            ps_ = slice(hp * 64, (hp + 1) * 64)
            sv = V(S[ps_, :, :], ("S", hp))
            pst = V(P.psb[6][ps_, :].rearrange("p (k c) -> p k c", k=NK)[:, :, hp * 64:(hp + 1) * 64], ("psb", 6))
            P.tt("dve", sv, sv, pst, ALU.add)
            P.tt("dve", sv, sv, V(WC[ps_, :, c8:c8 + 1].broadcast_to([64, NK, 64]), ("WC",)), ALU.mult)
            P.copy("pool", V(Sbf[ps_, :, :], ("Sbf",)), sv)
        for (lh, rh, kk_) in ((V(hgA[:, sl], "hgA"), V(g2A, ("w", 9)), 0), (V(hgB[0:32, sl], "hgB"), V(g2B[0:32, :], ("w", 10)), 1)):
            P.mm(b5, lh, rh, start=(kk_ == 0), stop=(kk_ == 1))
        P.tt("dve", V(ytm[0:64, :], "ytm"), b5, ycv, ALU.mult)
        for kc in range(NK):
            P.transpose(V(psT7[:, 512 + kc * 64:512 + (kc + 1) * 64], ("psb", 7, "y")), V(ytm[0:64, kc * 128:(kc + 1) * 128], "ytm"),
                        V(P.ident[0:64, 0:64], "ident"))
        P.copy("act", V(yT[:, :, sl], ("yT", c8)), V(psT7[:, 512:768].rearrange("p (k t) -> p k t", k=NK), ("psb", 7, "y")))

    def stageB(blk, slot, tick):
        xv_, xvtok = stage1(blk, slot, tick)
        sls = [slice(c8 * RW_C, (c8 + 1) * RW_C) for c8 in range(8)]
        phaseAB(0, sls[0], 0)
        for c8 in range(8):
            if c8 + 1 < 8:
                phaseAB(c8 + 1, sls[c8 + 1], (c8 + 1) % 2)
            phaseCD(blk, c8, sls[c8], c8 % 2, xv_, xvtok)
            if c8 in (1, 4):
                tick()
        out_stage(P, blk, srcR, srcRname, dst, dstname,
                  lambda k, s: V(yT[:, k, s * 128:(s + 1) * 128], ("yT",)), NK, wo, ("w", 3))

    run_blocks(P, srcN, srcNname, 1, stageB)
    P.arena_reset(mark)


def build(T, plan):
    P = Prog(T, plan)
    P.w = {}

    def win(name, shape, dt=F32):
        if name not in P.w:
            P.w[name] = P.dram_in(name, shape, dt)
        return P.w[name]

    P.win = win
    x_in = P.dram_in("x", [T, D])
    out = P.dram_out("out", [T, D])
    P.arena_init(ARENA_BYTES)
    P.psb = [P.ps("psb%d" % i)[:, :] for i in range(8)]
    P.ident = P.alloc([128], BF16)
    P.ones = P.alloc([128], BF16)
    P.normw = P.alloc([9, KC])
    P.epsv = P.alloc([1])
    P.dma("sp", V(P.ident, "ident"), V(win("c_ident", [128, 128], BF16), "in_w"), key="const0")
    P.dma("sp", V(P.normw, "normw"), V(win("c_normw", [128, 9, KC]), "in_w"), key="const1")
    P.memset("pool", V(P.ones, "ones"), 1.0)
    P.memset("pool", V(P.epsv, "epsv"), EPS)
    P.S.barrier()
    scr = [P.dram_scratch("scr%d" % i, [T, D]) for i in range(3)]
    bufs = [(x_in, "x")] + [(scr[i], "scr%d" % i) for i in range(3)]
    cur = 0

    def nxt(*busy):
        for i in (1, 2, 3):
            if i not in busy:
                return i

    for item in plan:
        kind = item[0]
        a = cur
        b = nxt(a)
        c = nxt(a, b)
        A_, B_, C_ = bufs[a], bufs[b], bufs[c]
        if kind == "ffn":
            li = item[1]
            ffn_pass(P, li, 0, 11, A_[0], A_[1], A_[0], A_[1], B_[0], B_[1])
            ffn_pass(P, li, 11, 22, A_[0], A_[1], B_[0], B_[1], C_[0], C_[1])
            cur = c
        elif kind == "mix" and item[1] == 3:
            retnet_pass(P, 0, A_[0], A_[1], A_[0], A_[1], B_[0], B_[1])
            retnet_pass(P, 2, A_[0], A_[1], B_[0], B_[1], C_[0], C_[1])
            cur = c
        elif kind == "mix" and item[1] == 0:
            ssd_pass(P, 0, A_[0], A_[1], A_[0], A_[1], B_[0], B_[1])
            ssd_pass(P, 1, A_[0], A_[1], B_[0], B_[1], C_[0], C_[1])
            cur = c
        elif kind == "mix" and item[1] == 1:
            rwkv_pass(P, 0, A_[0], A_[1], A_[0], A_[1], B_[0], B_[1])
            rwkv_pass(P, 1, A_[0], A_[1], B_[0], B_[1], C_[0], C_[1])
            cur = c
        elif kind == "mix" and item[1] == 2:
            gla_pass(P, A_[0], A_[1], A_[0], A_[1], B_[0], B_[1])
            cur = b
        elif kind == "final":
            final_norm(P, bufs[cur][0], bufs[cur][1], out, "out")
    P.barrier("sp", [("out",)])
    global LAST_INPUT_NAMES
    LAST_INPUT_NAMES = list(P.inputs.keys())
    return P.finish()


def ret_perm():
    idx = []
    for part in range(2):
        for h in range(RET_H):
            base = part * 1024 + h * RET_DK
            idx += [base + 2 * i for i in range(128)] + [base + 2 * i + 1 for i in range(128)]
    return np.array(idx + list(range(2048, 6144)))


def host_consts(inputs):
    import ml_dtypes
    f = lambda a: np.ascontiguousarray(np.asarray(a, dtype=np.float32))
    c = {}
    c["c_ident"] = np.eye(128, dtype=np.float32).astype(ml_dtypes.bfloat16)
    nw = np.concatenate([f(inputs["norm_mix"]), f(inputs["norm_ffn"]), f(inputs["norm_final"])[None]], 0)
    c["c_normw"] = np.ascontiguousarray(nw.reshape(9, KC, 128).transpose(2, 0, 1))
    c["c_nfb"] = f(inputs["norm_final"])
    cw = f(inputs["ffn_conv_w"])
    cwl = cw.reshape(4, 3, 44, 128).transpose(0, 3, 2, 1)
    cb = f(inputs["ffn_conv_b"]).reshape(4, 44, 128).transpose(0, 2, 1)
    for li in range(4):
        c["ffn_w_up_%d" % li] = f(inputs["ffn_w_up"][li])
        c["ffn_w_down_%d" % li] = f(inputs["ffn_w_down"][li])
        c["c_ffn_cw_%d" % li] = np.ascontiguousarray(cwl[li])
        c["c_ffn_cb_%d" % li] = np.ascontiguousarray(cb[li])
    c["ret_w_in_p"] = np.ascontiguousarray(f(inputs["ret_w_in"][0])[:, ret_perm()])
    c["ret_w_out"] = f(inputs["ret_w_out"][0])
    inv = (1.0 / (np.float32(10000.0) ** np.linspace(0.0, 1.0, 128, dtype=np.float32))).astype(np.float32)
    ang = (np.arange(4096, dtype=np.float32)[None, :] * inv[:, None]).astype(np.float32)
    c["c_ret_cos"] = np.cos(ang).astype(np.float32)
    c["c_ret_sin"] = np.sin(ang).astype(np.float32)
    gam = 1.0 - 2.0 ** (-5.0 - np.arange(4, dtype=np.float64))
    s_ = np.arange(128)[:, None]
    l_ = np.arange(128)[None, :]
    decT = np.zeros((128, 4, 128), np.float64)
    for h in range(4):
        decT[:, h, :] = np.where(l_ >= s_, gam[h] ** (l_ - s_), 0.0) / 16.0
    c["c_ret_decT"] = decT.astype(np.float32)
    c["c_ret_gl"] = np.stack([gam[h] ** ((np.arange(TB) % 128) + 1) for h in range(4)]).astype(np.float32)
    c["c_ret_kdec"] = np.stack([gam[h] ** (127 - np.arange(128)) / 16.0 for h in range(4)], 1).astype(np.float32)
    c["ssd_w_in"] = f(inputs["ssd_w_in"][0])
    c["ssd_w_out"] = f(inputs["ssd_w_out"][0])
    c["c_ssd_cw"] = np.ascontiguousarray(f(inputs["ssd_conv_w"][0]).reshape(4, 32, 128).transpose(2, 1, 0))
    c["c_ssd_cb"] = np.ascontiguousarray(f(inputs["ssd_conv_b"][0]).reshape(32, 128).T)
    for n_ in ("ssd_dt_bias", "ssd_a_log", "ssd_d", "ssd_norm_w"):
        c[n_] = f(inputs[n_][0])
    c["c_SU"] = (s_ < l_).T.astype(np.float32).copy()
    for n_ in ("w_rkv", "w_out", "w1", "w2", "a1", "a2", "g1", "g2", "ln_w", "ln_b"):
        c["rwkv_" + n_] = f(inputs["rwkv_" + n_][0])
    vecs = [f(inputs["rwkv_mix"][0])[i_] for i_ in range(6)] + [f(inputs["rwkv_" + n_][0]).reshape(-1) for n_ in
                                                                 ("w0", "a0", "k_k", "k_a", "r_k")] + [np.zeros(1024, np.float32)]
    c["c_rwkv_vec"] = np.ascontiguousarray(np.stack(vecs).reshape(12, KC, 128).transpose(2, 0, 1))
    t64 = np.arange(64)[:, None]
    u64 = np.arange(64)[None, :]
    c["c_rwkv_masks"] = np.ascontiguousarray(np.stack([(u64 < t64), (t64 < u64), (t64 <= u64), (t64 == u64)], 1).astype(np.float32))
    E = np.zeros((128, 2), np.float32)
    E[:64, 0] = 1.0
    E[64:, 1] = 1.0
    c["c_rwkv_E"] = E.astype(ml_dtypes.bfloat16)
    c["c_rwkv_bo"] = (E @ E.T).astype(ml_dtypes.bfloat16)
    sm64 = np.ones(TB, np.float32)
    sm64[::64] = 0.0
    c["c_rwkv_scanm"] = sm64
    c["gla_w_in"] = f(inputs["gla_w_in"][0])
    c["gla_w_out"] = f(inputs["gla_w_out"][0])
    c["gla_w_gk2"] = f(inputs["gla_w_gk2"][0])
    c["c_gla_bgk"] = np.ascontiguousarray(f(inputs["gla_b_gk2"][0]).reshape(4, 128).T)
    c["c_gla_nw"] = np.ascontiguousarray(f(inputs["gla_norm_w"][0]).reshape(2, 128).T)
    c["c_maskT"] = (l_ >= s_).astype(np.float32)
    sm = np.ones(TB, np.float32)
    sm[::128] = 0.0
    c["c_scanm"] = sm
    return c


FULL_PLAN = [("mix", 0), ("ffn", 0), ("mix", 1), ("ffn", 1), ("mix", 2), ("ffn", 2), ("mix", 3), ("ffn", 3), ("final",)]
_CACHE = {}


def kernel(**inputs):
    T = 4096
    n_cores = 8
    if "nc" not in _CACHE:
        _CACHE["nc"] = build(T, FULL_PLAN)
        _CACHE["names"] = list(LAST_INPUT_NAMES)
    nc = _CACHE["nc"]
    names = _CACHE["names"]
    consts = host_consts(inputs)
    x = np.ascontiguousarray(np.asarray(inputs["x"], dtype=np.float32))
    shared = {n: consts[n] for n in names if n != "x"}
    in_maps = []
    for b in range(n_cores):
        m = dict(shared)
        m["x"] = np.ascontiguousarray(x[b])
        in_maps.append(m)
    res = run_bass_kernel_spmd(nc, in_maps, core_ids=list(range(n_cores)))
    return np.stack([np.asarray(r["out"], dtype=np.float32) for r in res.results], axis=0)
```

```python
import numpy as np
import concourse.bass as bass
import concourse.mybir as mybir
from concourse.bass_utils import run_bass_kernel_spmd

F32 = mybir.dt.float32
BF16 = mybir.dt.bfloat16
AF = mybir.ActivationFunctionType
ALU = mybir.AluOpType
AX = mybir.AxisListType

D = 1024
KC = 8
TB = 512
SCHEDULE = True
KEEP_ORDER = ()
PRIO = True
SOFT = True
EPS = 1e-5


class V:
    __slots__ = ("ap", "tok")

    def __init__(self, ap, tok):
        self.ap = ap
        self.tok = tok if isinstance(tok, tuple) else (tok,)


class _Op:
    __slots__ = ("eng", "fn", "reads", "writes", "dma_key", "deps", "inc", "val", "sem", "amt",
                 "odeps", "cost", "lat", "bar", "idx", "grp", "gend", "st", "bind", "prio")

    def __init__(self, eng, fn, reads, writes, dma_key, cost=300.0, lat=0.0):
        self.odeps = []
        self.cost = cost
        self.lat = lat
        self.bar = False
        self.idx = 0
        self.grp = None
        self.gend = True
        self.st = 0.0
        self.bind = None
        self.prio = False
        self.eng = eng
        self.fn = fn
        self.reads = reads
        self.writes = writes
        self.dma_key = dma_key
        self.deps = []
        self.inc = False
        self.val = 0
        self.sem = None
        self.amt = 1


class Sched:
    COMPUTE = ("pe", "act", "dve", "pool")

    def __init__(self, nc):
        self.nc = nc
        self.ops = []
        self.state = {}

    def op(self, eng, fn, reads=(), writes=(), dma_key=None, cost=300.0, lat=0.0):
        reads = [t if isinstance(t, tuple) else (t,) for t in reads]
        writes = [t if isinstance(t, tuple) else (t,) for t in writes]
        writes = [t[:2] if t[0] == "psb" else t for t in writes]
        writes += [t[:2] for t in reads if t[0] == "psb" and t[:2] not in writes]
        reads = [t for t in reads if t[0] != "psb"]
        o = _Op(eng, fn, reads, writes, dma_key, cost, lat)
        o.prio = getattr(self, "cur_prio", False)
        self._analyse(o)
        self.ops.append(o)
        return o

    @staticmethod
    def _conf(a, b):
        n = min(len(a), len(b))
        return a[:n] == b[:n]

    def _add_dep(self, o, p, kind):
        if p is None or p is o:
            return
        pd = p.dma_key is not None
        od = o.dma_key is not None
        if not pd and not od:
            if p.eng == "pe" and o.eng == "pe":
                o.odeps.append(p)
                return
            if p.eng == o.eng and kind != "RAW":
                o.odeps.append(p)
                return
        if pd and od and p.eng == o.eng and kind == "WAR" and False:
            return
        o.deps.append(p)

    def _analyse(self, o):
        st = self.state
        for tk in o.reads:
            root = st.setdefault(tk[0], {})
            for k, e in root.items():
                if self._conf(k, tk):
                    self._add_dep(o, e[0], "RAW")
            e = root.get(tk)
            if e is None:
                root[tk] = [None, [o]]
            else:
                e[1].append(o)
        for tk in o.writes:
            root = st.setdefault(tk[0], {})
            dead = []
            for k, e in root.items():
                if self._conf(k, tk):
                    self._add_dep(o, e[0], "WAW")
                    for r in e[1]:
                        self._add_dep(o, r, "WAR")
                    if len(k) > len(tk):
                        dead.append(k)
                    elif len(k) < len(tk):
                        pass
            for k in dead:
                del root[k]
            root[tk] = [o, []]

    def barrier(self):
        lasts = {}
        dmas = {}
        for o in self.ops:
            if o.dma_key is not None:
                dmas[o.dma_key] = o
            elif o.fn is not None:
                lasts[o.eng] = o
        new = []
        for eng in ("pe", "act", "dve", "pool", "sp"):
            b = _Op(eng, None, [], [], None)
            b.deps = [p for e, p in lasts.items() if e != eng] + list(dmas.values())
            b.bar = True
            new.append(b)
        self.ops.extend(new)
        self.state = {}

    def schedule(self, window=16, xlat=300.0):
        import bisect
        segs, cur = [], []
        for o in self.ops:
            if o.bar:
                if cur:
                    segs.append(cur)
                    cur = []
                segs.append([o])
            else:
                cur.append(o)
        if cur:
            segs.append(cur)
        out = []
        self.seg_times = []
        prev_lasts, prev_dmas = {}, {}
        for seg in segs:
            if len(seg) == 1:
                b = seg[0]
                if b.bar:
                    b.deps = [p_ for e_, p_ in prev_lasts.items() if e_ != b.eng] + list(prev_dmas.values())
                out.extend(seg)
                continue
            seg_start = len(out)
            lastof = {}
            for i, o in enumerate(seg):
                o.idx = i
                if o.eng in KEEP_ORDER:
                    if o.eng in lastof:
                        o.odeps.append(lastof[o.eng])
                    lastof[o.eng] = o
            inseg = set(id(o) for o in seg)
            groups = {}
            for o in seg:
                if o.grp is not None:
                    groups.setdefault(o.grp, []).append(o)
            for g, mem in groups.items():
                if len(mem) > 1:
                    ids = set(id(m) for m in mem)
                    first = mem[0]
                    for m in mem[1:]:
                        for d in m.deps + m.odeps:
                            if id(d) not in ids:
                                first.odeps.append(d)
            succ_pre = {}
            for o in seg:
                for d in o.deps:
                    if id(d) in inseg:
                        succ_pre.setdefault(id(d), []).append((o, True))
                for d in o.odeps:
                    if id(d) in inseg:
                        succ_pre.setdefault(id(d), []).append((o, False))
            pe_lock = None
            busy = {}
            use_prio = PRIO and seg[len(seg) // 2].prio
            blev = {}
            for o in reversed(seg):
                m = 0.0
                for s2, _h in succ_pre.get(id(o), ()):
                    v = blev[id(s2)]
                    if v > m:
                        m = v
                blev[id(o)] = m + o.cost + o.lat
            npred = {}
            succ = {}
            for o in seg:
                ds = [(d, True) for d in o.deps if id(d) in inseg] + [(d, False) for d in o.odeps if id(d) in inseg]
                npred[id(o)] = len(ds)
                for d, hard in ds:
                    succ.setdefault(id(d), []).append((o, hard))
            fin = {}
            rtime = {}
            ready = {e: [] for e in ("pe", "act", "dve", "pool", "sp")}
            free = {e: 0.0 for e in ready}
            for o in seg:
                if npred[id(o)] == 0:
                    rtime[id(o)] = 0.0
                    ready[o.eng].append((o.idx, o))
            for e in ready:
                ready[e].sort(key=lambda t: t[0])
            done = 0
            n = len(seg)
            while done < n:
                best = None
                for e, lst in ready.items():
                    fe = free[e]
                    cand = lst[:window]
                    if e == "pe" and pe_lock is not None:
                        cand = [t for t in lst if t[1].grp == pe_lock][:1]
                    for (ix, o) in cand:
                        st = rtime[id(o)]
                        if st < fe:
                            st = fe
                        key = (st, -blev[id(o)], ix) if use_prio else (st, ix)
                        if best is None or key < best[0]:
                            best = (key, o, st, ix)
                _k, o, st, ix = best
                lst = ready[o.eng]
                lst.pop(bisect.bisect_left(lst, (ix,), key=lambda t: (t[0],)))
                if o.eng == "pe" and o.grp is not None:
                    pe_lock = None if o.gend else o.grp
                free[o.eng] = st + o.cost
                busy[o.eng] = busy.get(o.eng, 0.0) + o.cost
                f = st + o.cost + o.lat
                fin[id(o)] = f
                o.st = st
                out.append(o)
                done += 1
                for s_, hard in succ.get(id(o), ()):
                    k = id(s_)
                    npred[k] -= 1
                    if hard or s_.eng != o.eng:
                        t_ = f + (xlat if s_.eng != o.eng else 0.0)
                    else:
                        t_ = st + o.cost
                    if rtime.get(k, 0.0) < t_:
                        rtime[k] = t_
                        s_.bind = o
                    if npred[k] == 0:
                        bisect.insort(ready[s_.eng], (s_.idx, s_), key=lambda t: t[0])
            self.seg_times.append((max(fin.values()) if fin else 0.0, dict(busy), len(seg)))
            prev_lasts, prev_dmas = {}, {}
            for o in out[seg_start:]:
                if o.dma_key is not None:
                    prev_dmas[o.dma_key] = o
                elif o.fn is not None:
                    prev_lasts[o.eng] = o
        assert len(out) == len(self.ops)
        self.ops = out

    def emit(self, block_ctx, sems):
        nc = self.nc
        for o in self.ops:
            for p in o.deps:
                p.inc = True
        cnt = {}
        for o in self.ops:
            if o.dma_key is not None:
                key = ("dma", o.dma_key)
                cnt[key] = cnt.get(key, 0) + 16
                o.val = cnt[key]
                o.sem = sems[key]
                o.amt = 16
                o.inc = True
            elif o.inc:
                cnt[o.eng] = cnt.get(o.eng, 0) + 1
                o.val = cnt[o.eng]
                o.sem = sems[o.eng]
        engs = {"pe": nc.tensor, "act": nc.scalar, "dve": nc.vector, "pool": nc.gpsimd, "sp": nc.sync}
        per_eng = {k: [] for k in engs}
        for o in self.ops:
            per_eng[o.eng].append(o)

        def run(engname):
            eng = engs[engname]
            waited = {}
            for o in per_eng[engname]:
                need = {}
                for p in o.deps:
                    sid = id(p.sem)
                    if need.get(sid, (None, 0))[1] < p.val:
                        need[sid] = (p.sem, p.val)
                for sid, (sem, val) in need.items():
                    if waited.get(sid, 0) < val:
                        eng.wait_ge(sem, val)
                        waited[sid] = val
                if o.fn is None:
                    continue
                ins = o.fn()
                if o.inc:
                    ins.then_inc(o.sem, o.amt)

        @block_ctx.tensor
        def _(e):
            run("pe")

        @block_ctx.scalar
        def _(e):
            run("act")

        @block_ctx.vector
        def _(e):
            run("dve")

        @block_ctx.gpsimd
        def _(e):
            run("pool")

        @block_ctx.sync
        def _(e):
            run("sp")


class Prog:
    def __init__(self, T, plan):
        self.T = T
        self.plan = plan
        self.nc = bass.Bass("TRN2", target_bir_lowering=False)
        self.S = Sched(self.nc)
        self.ctxs = []
        self.dma_keys = []
        self.inputs = {}
        self.psn = 0

    def dram_in(self, name, shape, dt=F32):
        t = self.nc.dram_tensor(name, list(shape), dt, kind="ExternalInput")
        self.inputs[name] = t
        return t.ap()

    def dram_out(self, name, shape, dt=F32):
        return self.nc.dram_tensor(name, list(shape), dt, kind="ExternalOutput").ap()

    def dram_scratch(self, name, shape, dt=F32):
        return self.nc.dram_tensor(name, list(shape), dt, kind="Internal").ap()

    def sb(self, name, shape, dt=F32):
        g = self.nc.sbuf_tensor(name, list(shape), dt)
        t = g.__enter__()
        self.ctxs.append(g)
        return t

    def ps(self, name, shape=(128, 512), dt=F32):
        g = self.nc.psum_tensor(name, list(shape), dt)
        t = g.__enter__()
        self.ctxs.append(g)
        return t

    def arena_init(self, nbytes):
        self.arena = self.sb("arena", [128, nbytes // 4], F32)
        self.arena_n = nbytes
        self.arena_off = 0

    def alloc(self, shape, dt=F32):
        n = 1
        for d in shape:
            n *= d
        esz = 4 if dt == F32 else 2
        nb = (n * esz + 63) // 64 * 64
        assert self.arena_off + nb <= self.arena_n, ("arena overflow", self.arena_off + nb, self.arena_n)
        a = self.arena[:, self.arena_off // 4:(self.arena_off + nb) // 4]
        self.arena_off += nb
        if dt != F32:
            a = a.bitcast(dt)
        a = a[:, 0:n]
        if len(shape) == 2:
            a = a.rearrange("p (a b) -> p a b", a=shape[0])
        elif len(shape) == 3:
            a = a.rearrange("p (a b c) -> p a b c", a=shape[0], b=shape[1])
        elif len(shape) == 4:
            a = a.rearrange("p (a b c d) -> p a b c d", a=shape[0], b=shape[1], c=shape[2])
        return a

    def arena_reset(self, mark=0):
        if getattr(self, "soft_next", False):
            self.soft_next = False
            self.arena_off = mark
            return
        self.S.barrier()
        self.arena_off = mark

    def _toks(self, *vs):
        return [v.tok for v in vs if isinstance(v, V)]

    @staticmethod
    def _n(ap):
        n = 1
        for d in list(ap.shape)[1:]:
            n *= int(d)
        return n

    def mm(self, out, lhsT, rhs, start=True, stop=True):
        nc = self.nc
        otok = out.tok
        if not (start and stop):
            otok = otok[:2]
        n = self._n(rhs.ap)
        mult = 4.0 if rhs.ap.dtype == F32 else 1.0
        o = self.S.op("pe", lambda: nc.tensor.matmul(out.ap, lhsT.ap, rhs.ap, start=start, stop=stop),
                      reads=self._toks(lhsT, rhs), writes=[otok], cost=mult * (max(64, n) * 0.42 + 20.0), lat=250.0)
        if start:
            self.gid = getattr(self, "gid", 0) + 1
        o.grp = self.gid
        o.gend = bool(stop)

    def transpose(self, out, in_, ident):
        nc = self.nc
        self.S.op("pe", lambda: nc.tensor.transpose(out.ap, in_.ap, ident.ap),
                  reads=self._toks(in_, ident), writes=self._toks(out), cost=80.0, lat=250.0)

    def act(self, out, in_, func, scale=None, bias=None, accum=None, extra_reads=()):
        nc = self.nc
        kw = {}
        if scale is not None:
            kw["scale"] = scale.ap if isinstance(scale, V) else scale
        if bias is not None:
            kw["bias"] = bias.ap if isinstance(bias, V) else bias
        if accum is not None:
            kw["accum_out"] = accum.ap
        w = self._toks(out) + (self._toks(accum) if accum is not None else [])
        self.S.op("act", lambda: nc.scalar.activation(out.ap, in_.ap, func, **kw),
                  reads=self._toks(in_, scale, bias) + list(extra_reads), writes=w,
                  cost=230.0 + 0.83 * self._n(in_.ap) + (90.0 if accum is not None else 0.0))

    def _e(self, eng):
        return {"dve": self.nc.vector, "pool": self.nc.gpsimd, "act": self.nc.scalar}[eng]

    def _c(self, eng, ap, per=1.04):
        n = self._n(ap)
        if eng == "pool":
            return 300.0 + 1.6 * n
        if eng == "act":
            return 230.0 + 0.83 * n
        return 120.0 + per * n

    def tt(self, eng, out, a, b, op):
        e = self._e(eng)
        self.S.op(eng, lambda: e.tensor_tensor(out.ap, a.ap, b.ap, op),
                  reads=self._toks(a, b), writes=self._toks(out), cost=self._c(eng, out.ap))

    def ts(self, eng, out, in_, s1, op0, s2=None, op1=None, accum=None):
        e = self._e(eng)
        a1 = s1.ap if isinstance(s1, V) else s1
        a2 = s2.ap if isinstance(s2, V) else s2
        kw = {}
        if op1 is not None:
            kw["op1"] = op1
        if accum is not None:
            kw["accum_out"] = accum.ap
        w = self._toks(out) + (self._toks(accum) if accum is not None else [])
        self.S.op(eng, lambda: e.tensor_scalar(out.ap, in_.ap, a1, a2, op0, **kw),
                  reads=self._toks(in_, s1, s2), writes=w, cost=self._c(eng, out.ap, 0.7))

    def stt(self, out, in0, scalar, in1, op0, op1):
        nc = self.nc
        sc = scalar.ap if isinstance(scalar, V) else scalar
        self.S.op("dve", lambda: nc.vector.scalar_tensor_tensor(out.ap, in0.ap, sc, in1.ap, op0, op1),
                  reads=self._toks(in0, scalar, in1), writes=self._toks(out), cost=self._c("dve", out.ap))

    def copy(self, eng, out, in_):
        if eng == "act":
            nc = self.nc
            self.S.op("act", lambda: nc.scalar.copy(out.ap, in_.ap), reads=self._toks(in_), writes=self._toks(out),
                      cost=self._c("act", out.ap))
        else:
            e = self._e(eng)
            self.S.op(eng, lambda: e.tensor_copy(out.ap, in_.ap), reads=self._toks(in_), writes=self._toks(out),
                      cost=self._c(eng, out.ap, 0.7))

    def memset(self, eng, out, val):
        e = self._e(eng)
        self.S.op(eng, lambda: e.memset(out.ap, val), writes=self._toks(out), cost=self._c(eng, out.ap, 0.7))

    def recip(self, out, in_):
        nc = self.nc
        self.S.op("dve", lambda: nc.vector.reciprocal(out.ap, in_.ap), reads=self._toks(in_), writes=self._toks(out),
                  cost=self._c("dve", out.ap, 8.4))

    def dma(self, q, out, in_, key):
        if key not in self.dma_keys:
            self.dma_keys.append(key)
        e = {"sp": self.nc.sync, "pool": self.nc.gpsimd, "act": self.nc.scalar}[q]
        nb = self._n(out.ap) * int(list(out.ap.shape)[0]) * (4 if out.ap.dtype == F32 else 2)
        self.S.op(q, lambda: e.dma_start(out=out.ap, in_=in_.ap), reads=self._toks(in_),
                  writes=self._toks(out), dma_key=key, cost=(400.0 if q == "pool" else 60.0), lat=2000.0 + nb / 100.0)

    def barrier(self, eng, toks):
        self.S.op(eng, None, reads=list(toks))

    def finish(self):
        nc = self.nc
        sems = {}
        gs = []
        for name in ("pe", "act", "dve", "pool"):
            g = nc.semaphore("sem_" + name)
            sems[name] = g.__enter__()
            gs.append(g)
        for i, k in enumerate(self.dma_keys):
            g = nc.semaphore("semd_%d" % i)
            sems[("dma", k)] = g.__enter__()
            gs.append(g)
        if SCHEDULE:
            self.S.schedule()
            self.seg_times = self.S.seg_times
        blk = nc.Block()
        b = blk.__enter__()
        self.S.emit(b, sems)
        blk.__exit__(None, None, None)
        for g in reversed(gs):
            g.__exit__(None, None, None)
        for g in reversed(self.ctxs):
            g.__exit__(None, None, None)
        return nc


FFN_H = 2816
FFN_NC = 22
ARENA_BYTES = 204 * 1024


def tile_rows(ap, blk, s):
    r0 = blk * TB + s * 128
    return ap[r0:r0 + 128, :]


def alloc_common(P):
    P.xt = [P.alloc([D]) for _ in range(2)]
    P.xr = [P.alloc([D]) for _ in range(2)]
    P.xnT = [P.alloc([KC, TB], BF16) for _ in range(2)]
    P.xs = P.alloc([D], BF16)
    P.junk = P.alloc([D], BF16)
    P.ss = [P.alloc([4]) for _ in range(2)]
    P.rstd = [P.alloc([4]) for _ in range(2)]
    P.xtn = 0
    P.xrn = 0


def norm_tile(P, src, srcname, blk, s, nidx, slot):
    psT = P.psb[7].bitcast(BF16)
    ss, rstd = P.ss[slot], P.rstd[slot]
    xi = P.xtn % 2
    P.xtn += 1
    xt = P.xt[xi]
    xtv = V(xt, ("xt", xi))
    P.dma("sp", xtv, V(tile_rows(src, blk, s), (srcname, blk, s)), key=("xt", xi))
    P.act(V(P.junk, "junk"), xtv, AF.Square, accum=V(ss[:, s:s + 1], ("ss", slot, s)))
    P.act(V(rstd[:, s:s + 1], ("rstd", slot, s)), V(ss[:, s:s + 1], ("ss", slot, s)), AF.Sqrt,
          scale=1.0 / D, bias=V(P.epsv, "epsv"))
    P.recip(V(rstd[:, s:s + 1], ("rstd", slot, s)), V(rstd[:, s:s + 1], ("rstd", slot, s)))
    P.ts("dve", V(P.xs, "xs"), xtv, V(rstd[:, s:s + 1], ("rstd", slot, s)), ALU.mult)
    for kc in range(KC):
        P.transpose(V(psT[:, kc * 128:(kc + 1) * 128], ("psb", 7)), V(P.xs[:, kc * 128:(kc + 1) * 128], "xs"),
                    V(P.ident, "ident"))
    P.tt("dve", V(P.xnT[slot][:, :, s * 128:(s + 1) * 128], ("xnT", slot, s)),
         V(psT.rearrange("p (k t) -> p k t", k=KC), ("psb", 7)),
         V(P.normw[:, nidx, :].unsqueeze(2).broadcast_to([128, KC, 128]), "normw"), ALU.mult)


def run_blocks(P, srcN, srcNname, nidx, stageB):
    nblk = P.T // TB
    for s in range(4):
        norm_tile(P, srcN, srcNname, 0, s, nidx, 0)
    for blk in range(nblk):
        pending = [(blk + 1, s) for s in range(4)] if blk + 1 < nblk else []

        def tick():
            if pending:
                b, s_ = pending.pop(0)
                norm_tile(P, srcN, srcNname, b, s_, nidx, b % 2)

        stageB(blk, blk % 2, tick)
        while pending:
            tick()


def out_stage(P, blk, srcR, srcRname, dst, dstname, lhs_fn, nk, wo, wotok):
    for s in range(4):
        xi = P.xrn % 2
        P.xrn += 1
        xr = P.xr[xi]
        xrv = V(xr, ("xr", xi))
        P.dma("sp", xrv, V(tile_rows(srcR, blk, s), (srcRname, blk, s)), key=("xr", xi))
        for half in range(2):
            b = 3 + (2 * s + half) % 2
            pd = V(P.psb[b], ("psb", b))
            for k in range(nk):
                P.mm(pd, lhs_fn(k, s), V(wo[:, k, half * 512:(half + 1) * 512], wotok),
                     start=(k == 0), stop=(k == nk - 1))
            xh = V(xr[:, half * 512:(half + 1) * 512], ("xr", xi))
            P.tt("dve", xh, pd, xh, ALU.add)
        P.dma("sp", V(tile_rows(dst, blk, s), (dstname, blk, s)), xrv, key=("xr", xi))


def ffn_pass(P, li, c0, c1, srcN, srcNname, srcR, srcRname, dst, dstname):
    P.S.cur_prio = False
    mark = P.arena_off
    alloc_common(P)
    nch = c1 - c0
    ncol = nch * 128
    nblk = P.T // TB
    w_up = P.win("ffn_w_up_%d" % li, [D, 2 * FFN_H])
    w_dn = P.win("ffn_w_down_%d" % li, [FFN_H, D])
    c_cw = P.win("c_ffn_cw_%d" % li, [128, 2 * FFN_NC, 3])
    c_cb = P.win("c_ffn_cb_%d" % li, [128, 2 * FFN_NC])
    wupv = P.alloc([KC, ncol], BF16)
    wupg = P.alloc([KC, ncol], BF16)
    wdn = P.alloc([nch, D], BF16)
    cw = P.alloc([2 * FFN_NC, 3])
    cb = P.alloc([2 * FFN_NC])
    hs = [P.alloc([2 * FFN_NC, 2]) for _ in range(2)]
    A = [P.alloc([TB]) for _ in range(6)]
    G = [P.alloc([TB]) for _ in range(4)]
    hid = P.alloc([nch, TB], BF16)
    upsrc = w_up.rearrange("(k p) n -> p k n", p=128)
    P.dma("pool", V(wupv, ("w", 0)), V(upsrc[:, :, c0 * 128:c1 * 128], "in_w"), key=("w", 0))
    P.dma("pool", V(wupg, ("w", 1)), V(upsrc[:, :, FFN_H + c0 * 128:FFN_H + c1 * 128], "in_w"), key=("w", 1))
    P.dma("pool", V(wdn, ("w", 2)), V(w_dn[c0 * 128:c1 * 128, :].rearrange("(c p) n -> p c n", p=128), "in_w"),
          key=("w", 2))
    P.dma("sp", V(cw, "cw"), V(c_cw, "in_w"), key="cw")
    P.dma("sp", V(cb, "cb"), V(c_cb, "in_w"), key="cb")
    P.memset("pool", V(hs[0], ("hs", 0)), 0.0)
    P.memset("pool", V(hs[1], ("hs", 1)), 0.0)

    def stageB(blk, slot, tick):
        xnT = P.xnT[slot]
        par = blk % 2
        ubanks = (0, 1, 2, 5, 6)
        tick_at = set(int(round(x)) for x in np.linspace(1, 2 * nch - 2, 4))
        for c in range(nch):
            for part in range(2):
                q = 2 * c + part
                if q in tick_at:
                    tick()
                cp = (c0 + c) + part * FFN_NC
                wsel = wupv if part == 0 else wupg
                wtok = ("w", part)
                bi = ubanks[q % 5]
                pu = V(P.psb[bi], ("psb", bi))
                for kc in range(KC):
                    P.mm(pu, V(wsel[:, kc, c * 128:(c + 1) * 128], wtok),
                         V(xnT[:, kc, :], ("xnT", slot)), start=(kc == 0), stop=(kc == KC - 1))
                ai = q % 6
                At = A[ai]
                atok = ("A", ai)
                P.act(V(At[:, 0:TB], atok), pu, AF.Identity,
                      scale=V(cw[:, cp, 2:3], "cw"), bias=V(cb[:, cp:cp + 1], "cb"))
                P.copy("act", V(hs[par][:, cp, :], ("hs", par, cp)), V(P.psb[bi][:, TB - 2:TB], ("psb", bi)))
                P.stt(V(At[:, 1:TB], atok), V(P.psb[bi][:, 0:TB - 1], ("psb", bi)), V(cw[:, cp, 1:2], "cw"),
                      V(At[:, 1:TB], atok), ALU.mult, ALU.add)
                P.stt(V(At[:, 2:TB], atok), V(P.psb[bi][:, 0:TB - 2], ("psb", bi)), V(cw[:, cp, 0:1], "cw"),
                      V(At[:, 2:TB], atok), ALU.mult, ALU.add)
                hp = V(hs[1 - par][:, cp, :], ("hs", 1 - par, cp))
                P.stt(V(At[:, 0:2], atok), hp, V(cw[:, cp, 0:1], "cw"), V(At[:, 0:2], atok), ALU.mult, ALU.add)
                P.stt(V(At[:, 0:1], atok), V(hs[1 - par][:, cp, 1:2], ("hs", 1 - par, cp)), V(cw[:, cp, 1:2], "cw"),
                      V(At[:, 0:1], atok), ALU.mult, ALU.add)
                if part == 0:
                    Aval, avtok = At, atok
                else:
                    Gt = G[c % 4]
                    gtok = ("G", c % 4)
                    P.act(V(Gt, gtok), V(At[:, 0:TB], atok), AF.Silu)
                    P.tt("pool", V(hid[:, c, :], ("hid", c)), V(Aval[:, 0:TB], avtok), V(Gt, gtok), ALU.mult)
        out_stage(P, blk, srcR, srcRname, dst, dstname,
                  lambda k, s: V(hid[:, k, s * 128:(s + 1) * 128], ("hid", k)), nch, wdn, ("w", 2))

    run_blocks(P, srcN, srcNname, 4 + li, stageB)
    P.arena_reset(mark)


def final_norm(P, src, srcname, dst, dstname):
    P.S.cur_prio = False
    mark = P.arena_off
    alloc_common(P)
    nfb = P.alloc([D])
    c_nf = P.win("c_nfb", [D])
    P.dma("sp", V(nfb, "nfb"), V(c_nf.partition_broadcast(128), "in_w"), key="const2")
    nblk = P.T // TB
    n = 0
    for blk in range(nblk):
        for s in range(4):
            xi = n % 2
            n += 1
            xt, xo = P.xt[xi], P.xr[xi]
            xtv = V(xt, ("xt", xi))
            P.dma("sp", xtv, V(tile_rows(src, blk, s), (srcname, blk, s)), key=("xt", xi))
            ssv = V(P.ss[xi][:, 0:1], ("ss", xi))
            rv = V(P.rstd[xi][:, 0:1], ("rstd", xi))
            P.act(V(P.junk, "junk"), xtv, AF.Square, accum=ssv)
            P.act(rv, ssv, AF.Sqrt, scale=1.0 / D, bias=V(P.epsv, "epsv"))
            P.recip(rv, rv)
            P.stt(V(xo, ("xr", xi)), xtv, rv, V(nfb, "nfb"), ALU.mult, ALU.mult)
            P.dma("sp", V(tile_rows(dst, blk, s), (dstname, blk, s)), V(xo, ("xr", xi)), key=("xr", xi))
    P.arena_reset(mark)


RET_H = 4
RET_DK = 256
RET_DV = 512


def retnet_pass(P, h0, srcN, srcNname, srcR, srcRname, dst, dstname):
    P.S.cur_prio = True
    mark = P.arena_off
    alloc_common(P)
    nblk = P.T // TB
    T = P.T
    w_in = P.win("ret_w_in_p", [D, 6144])
    w_out = P.win("ret_w_out", [2048, D])
    c_cos = P.win("c_ret_cos", [128, 4096])
    c_sin = P.win("c_ret_sin", [128, 4096])
    c_decT = P.win("c_ret_decT", [128, RET_H, 128])
    c_gl = P.win("c_ret_gl", [RET_H, TB])
    c_kdec = P.win("c_ret_kdec", [128, RET_H])
    wq = P.alloc([KC, 512], BF16)
    wk = P.alloc([KC, 512], BF16)
    wv = P.alloc([KC, 1024], BF16)
    wg = P.alloc([KC, 1024], BF16)
    wo = P.alloc([8, D], BF16)
    cos = P.alloc([TB])
    sin = P.alloc([TB])
    qT = P.alloc([2, 2, TB], BF16)
    kT = P.alloc([2, 2, TB], BF16)
    qg = P.alloc([2, 2, TB], BF16)
    rt = [P.alloc([TB]) for _ in range(4)]
    vt = P.alloc([4, 2, 512], BF16)
    khat = P.alloc([4, 2, 256], BF16)
    sg = P.alloc([8, TB], BF16)
    yT = P.alloc([8, TB], BF16)
    S = P.alloc([2, 2, 512])
    Sbf = P.alloc([2, 2, 512], BF16)
    decT = P.alloc([RET_H, 128])
    gl = P.alloc([2, TB])
    kdec = P.alloc([RET_H])
    PT = [P.alloc([128], BF16) for _ in range(4)]
    ysq = [P.alloc([512], BF16) for _ in range(4)]
    rs = [P.alloc([128]) for _ in range(4)]
    tmp = [P.alloc([4, 128]) for _ in range(4)]
    src = w_in.rearrange("(k p) n -> p k n", p=128)
    P.dma("pool", V(wq, ("w", 0)), V(src[:, :, h0 * 256:h0 * 256 + 512], "in_w"), key=("w", 0))
    P.dma("pool", V(wk, ("w", 1)), V(src[:, :, 1024 + h0 * 256:1024 + h0 * 256 + 512], "in_w"), key=("w", 1))
    P.dma("pool", V(wv, ("w", 2)), V(src[:, :, 2048 + h0 * 512:2048 + h0 * 512 + 1024], "in_w"), key=("w", 2))
    P.dma("pool", V(wg, ("w", 3)), V(src[:, :, 4096 + h0 * 512:4096 + h0 * 512 + 1024], "in_w"), key=("w", 3))
    P.dma("pool", V(wo, ("w", 4)), V(w_out[h0 * 512:h0 * 512 + 1024, :].rearrange("(c p) n -> p c n", p=128), "in_w"),
          key=("w", 4))
    P.dma("sp", V(decT, "decT"), V(c_decT, "in_w"), key="c0")
    P.dma("sp", V(kdec, "kdec"), V(c_kdec, "in_w"), key="c1")
    for hl in range(2):
        P.dma("sp", V(gl[:, hl, :], ("gl", hl)), V(c_gl[h0 + hl].partition_broadcast(128), "in_w"), key=("c2", hl))
    P.memset("dve", V(S, "S"), 0.0)
    P.memset("pool", V(Sbf, "Sbf"), 0.0)
    g128 = [float((1.0 - 2.0 ** (-5.0 - (h0 + hl))) ** 128) for hl in range(2)]
    pn = [0]

    def pbank():
        b = pn[0] % 3
        pn[0] += 1
        return V(P.psb[b], ("psb", b))

    def stageB(blk, slot, tick):
        xnT = P.xnT[slot]
        xv = V(xnT, ("xnT", slot))
        P.dma("sp", V(cos, "cos"), V(c_cos[:, blk * TB:(blk + 1) * TB], "in_w"), key="cos")
        P.dma("sp", V(sin, "sin"), V(c_sin[:, blk * TB:(blk + 1) * TB], "in_w"), key="sin")
        for (wsel, wtok, dstT, dname) in ((wq, ("w", 0), qT, "qT"), (wk, ("w", 1), kT, "kT")):
            for hl in range(2):
                p1 = pbank()
                for kc in range(KC):
                    P.mm(p1, V(wsel[:, kc, hl * 256:hl * 256 + 128], wtok), V(xnT[:, kc, :], ("xnT", slot)),
                         start=(kc == 0), stop=(kc == KC - 1))
                p2 = pbank()
                for kc in range(KC):
                    P.mm(p2, V(wsel[:, kc, hl * 256 + 128:hl * 256 + 256], wtok), V(xnT[:, kc, :], ("xnT", slot)),
                         start=(kc == 0), stop=(kc == KC - 1))
                r = [V(rt[i], ("rt", i)) for i in range(4)]
                P.tt("dve", r[0], p1, V(cos, "cos"), ALU.mult)
                P.tt("dve", r[1], p2, V(sin, "sin"), ALU.mult)
                P.tt("dve", r[2], p2, V(cos, "cos"), ALU.mult)
                P.tt("dve", r[3], p1, V(sin, "sin"), ALU.mult)
                P.tt("pool", V(dstT[:, hl, 0, :], (dname, hl, 0)), r[0], r[1], ALU.subtract)
                P.tt("pool", V(dstT[:, hl, 1, :], (dname, hl, 1)), r[2], r[3], ALU.add)
                if dname == "qT":
                    for e in range(2):
                        P.tt("pool", V(qg[:, hl, e, :], ("qg", hl, e)), V(qT[:, hl, e, :], ("qT", hl, e)),
                             V(gl[:, hl, :], ("gl", hl)), ALU.mult)
        tick()
        for hl in range(2):
            for j in range(4):
                pg = pbank()
                c = hl * 512 + j * 128
                for kc in range(KC):
                    P.mm(pg, V(wg[:, kc, c:c + 128], ("w", 3)), V(xnT[:, kc, :], ("xnT", slot)),
                         start=(kc == 0), stop=(kc == KC - 1))
                P.act(V(sg[:, hl * 4 + j, :], ("sg", hl, j)), pg, AF.Silu)
        tick()
        for c4 in range(4):
            for hl in range(2):
                pv = pbank()
                for kc in range(KC):
                    P.mm(pv, V(xnT[:, kc, c4 * 128:(c4 + 1) * 128], ("xnT", slot)),
                         V(wv[:, kc, hl * 512:(hl + 1) * 512], ("w", 2)), start=(kc == 0), stop=(kc == KC - 1))
                P.copy("act", V(vt[:, c4, hl, :], ("vt", c4, hl)), pv)
        tick()
        psT6 = P.psb[6].bitcast(BF16)
        for c4 in range(4):
            for hl in range(2):
                for e in range(2):
                    P.transpose(V(psT6[:, e * 128:(e + 1) * 128], ("psb", 6)),
                                V(kT[:, hl, e, c4 * 128:(c4 + 1) * 128], ("kT", hl, e)), V(P.ident, "ident"))
                P.act(V(khat[:, c4, hl, :], ("khat", c4, hl)), V(psT6[:, 0:256], ("psb", 6)), AF.Identity,
                      scale=V(kdec[:, h0 + hl:h0 + hl + 1], "kdec"))
        tick()
        n = 0
        for c4 in range(4):
            sl = slice(c4 * 128, (c4 + 1) * 128)
            for hl in range(2):
                h = h0 + hl
                i2 = n % 4
                n += 1
                psS = V(P.psb[3][:, 0:128], ("psb", 3))
                for e in range(2):
                    P.mm(psS, V(kT[:, hl, e, sl], ("kT", hl, e)), V(qT[:, hl, e, sl], ("qT", hl, e)),
                         start=(e == 0), stop=(e == 1))
                ptv = V(PT[i2], ("PT", i2))
                P.tt("dve", ptv, psS, V(decT[:, h, :], "decT"), ALU.mult)
                psO = P.psb[4]
                for j in range(4):
                    po = V(psO[:, j * 128:(j + 1) * 128], ("psb", 4))
                    P.mm(po, V(vt[:, c4, hl, j * 128:(j + 1) * 128], ("vt", c4, hl)), ptv, start=True, stop=False)
                    for e in range(2):
                        P.mm(po, V(Sbf[:, hl, e, j * 128:(j + 1) * 128], ("Sbf", hl, e)),
                             V(qg[:, hl, e, sl], ("qg", hl, e)), start=False, stop=(e == 1))
                pov = V(psO, ("psb", 4))
                yq = V(ysq[i2], ("ysq", i2))
                P.act(yq, pov, AF.Square)
                psN = V(P.psb[3][:, 128:256], ("psb", 3))
                for j in range(4):
                    P.mm(psN, V(P.ones, "ones"), V(ysq[i2][:, j * 128:(j + 1) * 128], ("ysq", i2)),
                         start=(j == 0), stop=(j == 3))
                rv = V(rs[i2], ("rs", i2))
                P.act(rv, psN, AF.Sqrt, scale=1.0 / RET_DV, bias=V(P.epsv, "epsv"))
                P.recip(rv, rv)
                tv = V(tmp[i2], ("tmp", i2))
                P.tt("dve", tv, V(psO.rearrange("p (j l) -> p j l", j=4), ("psb", 4)),
                     V(rs[i2].unsqueeze(1).broadcast_to([128, 4, 128]), ("rs", i2)), ALU.mult)
                P.tt("pool", V(yT[:, hl * 4:(hl + 1) * 4, sl], ("yT", hl, c4)), tv,
                     V(sg[:, hl * 4:(hl + 1) * 4, sl], ("sg", hl)), ALU.mult)
                for e in range(2):
                    pu = V(P.psb[5], ("psb", 5))
                    P.mm(pu, V(khat[:, c4, hl, e * 128:(e + 1) * 128], ("khat", c4, hl)),
                         V(vt[:, c4, hl, :], ("vt", c4, hl)), start=True, stop=True)
                    sv = V(S[:, hl, e, :], ("S", hl, e))
                    P.stt(sv, sv, g128[hl], pu, ALU.mult, ALU.add)
                    P.copy("pool", V(Sbf[:, hl, e, :], ("Sbf", hl, e)), sv)
        out_stage(P, blk, srcR, srcRname, dst, dstname,
                  lambda k, s: V(yT[:, k, s * 128:(s + 1) * 128], ("yT",)), 8, wo, ("w", 4))

    run_blocks(P, srcN, srcNname, 3, stageB)
    P.arena_reset(mark)


GLA_H = 4
GLA_DK = 128
GLA_DV = 256


def gla_pass(P, srcN, srcNname, srcR, srcRname, dst, dstname):
    P.S.cur_prio = True
    mark = P.arena_off
    alloc_common(P)
    nblk = P.T // TB
    w_in = P.win("gla_w_in", [D, 3088])
    w_out = P.win("gla_w_out", [D, D])
    w_gk2 = P.win("gla_w_gk2", [16, 512])
    c_bgk = P.win("c_gla_bgk", [128, GLA_H])
    c_nw = P.win("c_gla_nw", [128, 2])
    c_maskT = P.win("c_maskT", [128, 128])
    c_scanm = P.win("c_scanm", [TB])
    wq = P.alloc([KC, 512], BF16)
    wk = P.alloc([KC, 512], BF16)
    wv = P.alloc([KC, 1024], BF16)
    wg = P.alloc([KC, 1024], BF16)
    wgk = P.alloc([KC, 16], BF16)
    wo = P.alloc([8, D], BF16)
    wgk2 = P.alloc([512])
    bgk = P.alloc([GLA_H])
    nbgk = P.alloc([GLA_H])
    nw = P.alloc([2])
    maskT = P.alloc([128])
    scanm = P.alloc([TB])
    gkf = P.alloc([TB])
    Gp = P.alloc([GLA_H, TB])
    et = [P.alloc([TB]) for _ in range(2)]
    qT = P.alloc([GLA_H, TB], BF16)
    kT = P.alloc([GLA_H, TB], BF16)
    vt = P.alloc([4, 1024], BF16)
    khat = P.alloc([4, GLA_H, 128], BF16)
    sg = P.alloc([8, TB], BF16)
    yT = P.alloc([8, TB], BF16)
    S = P.alloc([GLA_H, 256])
    Sbf = P.alloc([GLA_H, 256], BF16)
    elast = P.alloc([GLA_H, 4])
    PT = [P.alloc([128], BF16) for _ in range(4)]
    ysq = [P.alloc([256], BF16) for _ in range(4)]
    rs = [P.alloc([128]) for _ in range(4)]
    tmp = [P.alloc([2, 128]) for _ in range(4)]
    src = w_in.rearrange("(k p) n -> p k n", p=128)
    P.dma("pool", V(wq, ("w", 0)), V(src[:, :, 0:512], "in_w"), key=("w", 0))
    P.dma("pool", V(wk, ("w", 1)), V(src[:, :, 512:1024], "in_w"), key=("w", 1))
    P.dma("pool", V(wv, ("w", 2)), V(src[:, :, 1024:2048], "in_w"), key=("w", 2))
    P.dma("pool", V(wg, ("w", 3)), V(src[:, :, 2048:3072], "in_w"), key=("w", 3))
    P.dma("pool", V(wgk, ("w", 5)), V(src[:, :, 3072:3088], "in_w"), key=("w", 5))
    P.dma("pool", V(wo, ("w", 4)), V(w_out.rearrange("(c p) n -> p c n", p=128), "in_w"), key=("w", 4))
    P.dma("sp", V(wgk2[0:16, :], "wgk2"), V(w_gk2, "in_w"), key="c0")
    P.dma("sp", V(bgk, "bgk"), V(c_bgk, "in_w"), key="c1")
    P.dma("sp", V(nw, "nw"), V(c_nw, "in_w"), key="c2")
    P.dma("sp", V(maskT, "maskT"), V(c_maskT, "in_w"), key="c3")
    P.dma("sp", V(scanm, "scanm"), V(c_scanm.partition_broadcast(128), "in_w"), key="c4")
    P.ts("dve", V(nbgk, "nbgk"), V(bgk, "bgk"), -1.0, ALU.mult)
    P.memset("dve", V(S, "S"), 0.0)
    P.memset("pool", V(Sbf, "Sbf"), 0.0)
    lnsc = float(np.log(GLA_DK ** -0.5))
    pn = [0]

    def pbank():
        b = pn[0] % 3
        pn[0] += 1
        return V(P.psb[b], ("psb", b))

    def stageB(blk, slot, tick):
        xnT = P.xnT[slot]
        xtok = ("xnT", slot)
        pg = pbank()
        for kc in range(KC):
            P.mm(V(pg.ap[0:16, :], pg.tok), V(wgk[:, kc, :], ("w", 5)), V(xnT[:, kc, :], xtok),
                 start=(kc == 0), stop=(kc == KC - 1))
        P.copy("act", V(gkf[0:16, :], "gkf"), V(pg.ap[0:16, :], pg.tok))
        for h in range(GLA_H):
            pp = pbank()
            P.mm(pp, V(wgk2[0:16, h * 128:(h + 1) * 128], "wgk2"), V(gkf[0:16, :], "gkf"), start=True, stop=True)
            e0 = V(et[0], ("et", 0))
            P.act(e0, pp, AF.Exp, scale=-1.0, bias=V(nbgk[:, h:h + 1], "nbgk"))
            P.act(e0, e0, AF.Ln, scale=1.0, bias=1.0)
            gph = V(Gp[:, h, :], ("Gp", h))
            nc = P.nc
            P.S.op("dve", (lambda o=gph.ap, a=scanm, b=et[0]: nc.vector.tensor_tensor_scan(o, a, b, 0.0, ALU.mult, ALU.add)),
                   reads=[("scanm",), ("et", 0)], writes=[gph.tok])
            pq = pbank()
            for kc in range(KC):
                P.mm(pq, V(wq[:, kc, h * 128:(h + 1) * 128], ("w", 0)), V(xnT[:, kc, :], xtok),
                     start=(kc == 0), stop=(kc == KC - 1))
            e1 = V(et[1], ("et", 1))
            P.act(e1, gph, AF.Exp, scale=-1.0 / 16.0, bias=lnsc)
            P.tt("dve", V(qT[:, h, :], ("qT", h)), pq, e1, ALU.mult)
            pk = pbank()
            for kc in range(KC):
                P.mm(pk, V(wk[:, kc, h * 128:(h + 1) * 128], ("w", 1)), V(xnT[:, kc, :], xtok),
                     start=(kc == 0), stop=(kc == KC - 1))
            P.act(e1, gph, AF.Exp, scale=1.0 / 16.0)
            P.tt("dve", V(kT[:, h, :], ("kT", h)), pk, e1, ALU.mult)
            P.act(V(elast[:, h, :], ("elast", h)),
                  V(Gp[:, h, :].rearrange("p (c l) -> p c l", c=4)[:, :, 127], ("Gp", h)), AF.Exp, scale=-1.0 / 16.0)
        tick()
        for c in range(8):
            pg2 = pbank()
            for kc in range(KC):
                P.mm(pg2, V(wg[:, kc, c * 128:(c + 1) * 128], ("w", 3)), V(xnT[:, kc, :], xtok),
                     start=(kc == 0), stop=(kc == KC - 1))
            P.act(V(sg[:, c, :], ("sg", c)), pg2, AF.Silu)
            P.ts("pool", V(sg[:, c, :], ("sg", c)), V(sg[:, c, :], ("sg", c)), V(nw[:, (c % 2):(c % 2) + 1], "nw"), ALU.mult)
        tick()
        for c4 in range(4):
            for half in range(2):
                pv = pbank()
                for kc in range(KC):
                    P.mm(pv, V(xnT[:, kc, c4 * 128:(c4 + 1) * 128], xtok),
                         V(wv[:, kc, half * 512:(half + 1) * 512], ("w", 2)), start=(kc == 0), stop=(kc == KC - 1))
                P.copy("act", V(vt[:, c4, half * 512:(half + 1) * 512], ("vt", c4, half)), pv)
        tick()
        psT6 = P.psb[6].bitcast(BF16)
        for c4 in range(4):
            for h in range(GLA_H):
                P.transpose(V(psT6[:, h * 128:(h + 1) * 128], ("psb", 6)),
                            V(kT[:, h, c4 * 128:(c4 + 1) * 128], ("kT", h)), V(P.ident, "ident"))
            P.copy("act", V(khat[:, c4, :, :], ("khat", c4)),
                   V(psT6[:, 0:512].rearrange("p (h d) -> p h d", h=GLA_H), ("psb", 6)))
        tick()
        n = 0
        for c4 in range(4):
            sl = slice(c4 * 128, (c4 + 1) * 128)
            for h in range(GLA_H):
                i2 = n % 4
                n += 1
                psS = V(P.psb[3][:, 0:128], ("psb", 3))
                P.mm(psS, V(kT[:, h, sl], ("kT", h)), V(qT[:, h, sl], ("qT", h)), start=True, stop=True)
                ptv = V(PT[i2], ("PT", i2))
                P.tt("dve", ptv, psS, V(maskT, "maskT"), ALU.mult)
                psO = P.psb[4]
                for j in range(2):
                    po = V(psO[:, j * 128:(j + 1) * 128], ("psb", 4))
                    vc = h * 256 + j * 128
                    P.mm(po, V(vt[:, c4, vc:vc + 128], ("vt", c4, vc // 512)), ptv, start=True, stop=False)
                    P.mm(po, V(Sbf[:, h, j * 128:(j + 1) * 128], ("Sbf", h)), V(qT[:, h, sl], ("qT", h)),
                         start=False, stop=True)
                pov = V(psO[:, 0:256], ("psb", 4))
                yq = V(ysq[i2], ("ysq", i2))
                P.act(yq, pov, AF.Square)
                psN = V(P.psb[3][:, 128:256], ("psb", 3))
                for j in range(2):
                    P.mm(psN, V(P.ones, "ones"), V(ysq[i2][:, j * 128:(j + 1) * 128], ("ysq", i2)),
                         start=(j == 0), stop=(j == 1))
                rv = V(rs[i2], ("rs", i2))
                P.act(rv, psN, AF.Sqrt, scale=1.0 / GLA_DV, bias=V(P.epsv, "epsv"))
                P.recip(rv, rv)
                tv = V(tmp[i2], ("tmp", i2))
                P.tt("dve", tv, V(psO[:, 0:256].rearrange("p (j l) -> p j l", j=2), ("psb", 4)),
                     V(rs[i2].unsqueeze(1).broadcast_to([128, 2, 128]), ("rs", i2)), ALU.mult)
                P.tt("pool", V(yT[:, h * 2:(h + 1) * 2, sl], ("yT", h, c4)), tv,
                     V(sg[:, h * 2:(h + 1) * 2, sl], ("sg",)), ALU.mult)
                pu = V(P.psb[5][:, 0:256], ("psb", 5))
                P.mm(pu, V(khat[:, c4, h, :], ("khat", c4)), V(vt[:, c4, h * 256:(h + 1) * 256], ("vt", c4, h // 2)),
                     start=True, stop=True)
                sv = V(S[:, h, :], ("S", h))
                P.tt("dve", sv, sv, pu, ALU.add)
                P.ts("dve", sv, sv, V(elast[:, h, c4:c4 + 1], ("elast", h)), ALU.mult)
                P.copy("pool", V(Sbf[:, h, :], ("Sbf", h)), sv)
        out_stage(P, blk, srcR, srcRname, dst, dstname,
                  lambda k, s: V(yT[:, k, s * 128:(s + 1) * 128], ("yT",)), 8, wo, ("w", 4))

    run_blocks(P, srcN, srcNname, 2, stageB)
    P.arena_reset(mark)


def ssd_pass(P, p, srcN, srcNname, srcR, srcRname, dst, dstname):
    P.S.cur_prio = True
    mark = P.arena_off
    alloc_common(P)
    nc = P.nc
    NSL = 4
    nblk = P.T // TB
    w_in = P.win("ssd_w_in", [D, 6176])
    w_out = P.win("ssd_w_out", [2048, D])
    c_cw = P.win("c_ssd_cw", [128, 32, 4])
    c_cb = P.win("c_ssd_cb", [128, 32])
    c_dtb = P.win("ssd_dt_bias", [32])
    c_alog = P.win("ssd_a_log", [32])
    c_dsk = P.win("ssd_d", [32])
    c_nw = P.win("ssd_norm_w", [2048])
    c_maskT = P.win("c_maskT", [128, 128])
    c_SU = P.win("c_SU", [128, 128])
    wz = P.alloc([KC, 1024], BF16)
    wxs = P.alloc([KC, 1024], BF16)
    wB = P.alloc([KC, 512], BF16)
    wC = P.alloc([KC, 512], BF16)
    wdt = P.alloc([KC, 16], BF16)
    wo = P.alloc([8, D], BF16)
    cw = P.alloc([32, 4])
    cb = P.alloc([32])
    spill = P.alloc([16, 3])
    dtb = P.alloc([16])
    abc = P.alloc([16])
    dsk = P.alloc([16])
    nwc = P.alloc([1024])
    maskT = P.alloc([128])
    SU = P.alloc([128])
    onesf = P.alloc([128])
    A = [P.alloc([TB + 3]) for _ in range(3)]
    xsT = P.alloc([8, TB], BF16)
    kT = P.alloc([4, TB], BF16)
    qT = P.alloc([4, TB], BF16)
    sz = P.alloc([4, 1024], BF16)
    dtt = P.alloc([16])
    ld = P.alloc([16])
    Gs = P.alloc([16])
    eG = P.alloc([16])
    eGl = P.alloc([16])
    wdec = P.alloc([16])
    tiny = P.alloc([16])
    R = [P.alloc([4, 128]) for _ in range(NSL)]
    dec = [P.alloc([4, 128]) for _ in range(NSL)]
    sm = [P.alloc([128]) for _ in range(NSL)]
    PT = [P.alloc([4, 128], BF16) for _ in range(NSL)]
    xk = [P.alloc([384], BF16) for _ in range(NSL)]
    vv = [P.alloc([256], BF16) for _ in range(NSL)]
    vh = [P.alloc([256], BF16) for _ in range(NSL)]
    ot = [P.alloc([256]) for _ in range(NSL)]
    t2 = [P.alloc([256]) for _ in range(NSL)]
    yv = [P.alloc([256]) for _ in range(NSL)]
    yn = [P.alloc([256], BF16) for _ in range(NSL)]
    ssq = [P.alloc([1]) for _ in range(NSL)]
    yT = P.alloc([8, TB], BF16)
    S = P.alloc([4, 256])
    Sbf = P.alloc([4, 256], BF16)
    src = w_in.rearrange("(k p) n -> p k n", p=128)
    P.dma("pool", V(wz, ("w", 0)), V(src[:, :, p * 1024:(p + 1) * 1024], "in_w"), key=("w", 0))
    P.dma("pool", V(wxs, ("w", 1)), V(src[:, :, 2048 + p * 1024:2048 + (p + 1) * 1024], "in_w"), key=("w", 1))
    P.dma("pool", V(wB, ("w", 2)), V(src[:, :, 4096 + p * 512:4096 + (p + 1) * 512], "in_w"), key=("w", 2))
    P.dma("pool", V(wC, ("w", 3)), V(src[:, :, 5120 + p * 512:5120 + (p + 1) * 512], "in_w"), key=("w", 3))
    P.dma("pool", V(wdt, ("w", 5)), V(src[:, :, 6144 + p * 16:6144 + (p + 1) * 16], "in_w"), key=("w", 5))
    P.dma("pool", V(wo, ("w", 4)), V(w_out[p * 1024:(p + 1) * 1024, :].rearrange("(c p) n -> p c n", p=128), "in_w"),
          key=("w", 4))
    P.dma("sp", V(cw, "cw"), V(c_cw, "in_w"), key="c0")
    P.dma("sp", V(cb, "cb"), V(c_cb, "in_w"), key="c1")
    P.dma("sp", V(dtb, "dtb"), V(c_dtb[p * 16:(p + 1) * 16].partition_broadcast(128), "in_w"), key="c2")
    P.dma("sp", V(abc, "abc"), V(c_alog[p * 16:(p + 1) * 16].partition_broadcast(128), "in_w"), key="c3")
    P.dma("sp", V(dsk, "dsk"), V(c_dsk[p * 16:(p + 1) * 16].partition_broadcast(128), "in_w"), key="c4")
    P.dma("sp", V(nwc, "nwc"), V(c_nw[p * 1024:(p + 1) * 1024].partition_broadcast(128), "in_w"), key="c5")
    P.dma("sp", V(maskT, "maskT"), V(c_maskT, "in_w"), key="c6")
    P.dma("sp", V(SU, "SU"), V(c_SU, "in_w"), key="c7")
    P.memset("pool", V(onesf, "onesf"), 1.0)
    P.memset("pool", V(spill, "spill"), 0.0)
    P.act(V(abc, "abc"), V(abc, "abc"), AF.Exp)
    P.ts("dve", V(abc, "abc"), V(abc, "abc"), -1.0, ALU.mult)
    P.memset("dve", V(S, "S"), 0.0)
    P.memset("pool", V(Sbf, "Sbf"), 0.0)
    pn = [0]

    def pbank():
        b = pn[0] % 3
        pn[0] += 1
        return V(P.psb[b], ("psb", b))

    def conv_chunk(lc, wsel, wtok, col, xnT, slot, dst):
        cc = (8 * p + lc) if lc < 8 else ((16 + 4 * p + lc - 8) if lc < 12 else (24 + 4 * p + lc - 12))
        pu = pbank()
        for kc in range(KC):
            P.mm(pu, V(wsel[:, kc, col:col + 128], wtok), V(xnT[:, kc, :], ("xnT", slot)),
                 start=(kc == 0), stop=(kc == KC - 1))
        ai = lc % 3
        At, atok = A[ai], ("A", ai)
        P.act(V(At[:, 0:TB], atok), pu, AF.Identity, scale=V(cw[:, cc, 3:4], "cw"), bias=V(cb[:, cc:cc + 1], "cb"))
        P.memset("pool", V(At[:, TB:TB + 3], atok), 0.0)
        for sh in (1, 2, 3):
            P.stt(V(At[:, sh:TB + sh], atok), pu, V(cw[:, cc, 3 - sh:4 - sh], "cw"), V(At[:, sh:TB + sh], atok),
                  ALU.mult, ALU.add)
        P.tt("pool", V(At[:, 0:3], atok), V(At[:, 0:3], atok), V(spill[:, lc, :], ("spill", lc)), ALU.add)
        P.copy("pool", V(spill[:, lc, :], ("spill", lc)), V(At[:, TB:TB + 3], atok))
        P.act(dst, V(At[:, 0:TB], atok), AF.Silu)

    def stageB(blk, slot, tick):
        xnT = P.xnT[slot]
        xtok = ("xnT", slot)
        for lc in range(8):
            conv_chunk(lc, wxs, ("w", 1), lc * 128, xnT, slot, V(xsT[:, lc, :], ("xsT", lc)))
        tick()
        for gl in range(4):
            conv_chunk(8 + gl, wB, ("w", 2), gl * 128, xnT, slot, V(kT[:, gl, :], ("kT", gl)))
            conv_chunk(12 + gl, wC, ("w", 3), gl * 128, xnT, slot, V(qT[:, gl, :], ("qT", gl)))
        tick()
        for c4 in range(4):
            for half in range(2):
                pz = pbank()
                for kc in range(KC):
                    P.mm(pz, V(xnT[:, kc, c4 * 128:(c4 + 1) * 128], xtok),
                         V(wz[:, kc, half * 512:(half + 1) * 512], ("w", 0)), start=(kc == 0), stop=(kc == KC - 1))
                P.act(V(sz[:, c4, half * 512:(half + 1) * 512], ("sz", c4, half)), pz, AF.Silu)
        tick()
        n = 0
        psT6 = P.psb[6].bitcast(BF16)
        for c4 in range(4):
            sl = slice(c4 * 128, (c4 + 1) * 128)
            if c4 == 2:
                tick()
            pdt = V(P.psb[4][:, 128:144], ("psb", 4, "d"))
            for kc in range(KC):
                P.mm(pdt, V(xnT[:, kc, sl], xtok), V(wdt[:, kc, :], ("w", 5)), start=(kc == 0), stop=(kc == KC - 1))
            tn = V(tiny, "tiny")
            P.tt("dve", tn, pdt, V(dtb, "dtb"), ALU.add)
            P.act(tn, tn, AF.Exp)
            P.act(V(dtt, "dtt"), tn, AF.Ln, scale=1.0, bias=1.0)
            P.tt("dve", V(ld, "ld"), V(dtt, "dtt"), V(abc, "abc"), ALU.mult)
            pG = V(P.psb[4][:, 144:160], ("psb", 4, "d"))
            P.mm(pG, V(maskT, "maskT"), V(ld, "ld"), start=True, stop=True)
            pGl = V(P.psb[4][:, 160:176], ("psb", 4, "d"))
            P.mm(pGl, V(onesf, "onesf"), V(ld, "ld"), start=True, stop=True)
            P.act(V(Gs, "Gs"), pG, AF.Identity)
            P.act(V(eG, "eG"), pG, AF.Exp)
            P.act(V(eGl, "eGl"), pGl, AF.Exp)
            P.tt("dve", tn, pGl, V(Gs, "Gs"), ALU.subtract)
            P.act(V(wdec, "wdec"), tn, AF.Exp)
            for gl in range(4):
                i2 = n % NSL
                n += 1
                hs = slice(gl * 4, gl * 4 + 4)
                Rv = V(R[i2], ("R", i2))
                P.tt("dve", Rv, V(maskT.unsqueeze(1).broadcast_to([128, 4, 128]), "maskT"),
                     V(ld[:, hs].unsqueeze(2).broadcast_to([128, 4, 128]), "ld"), ALU.mult)
                pSeg = V(P.psb[3], ("psb", 3))
                P.mm(pSeg, V(SU, "SU"), V(R[i2].rearrange("p h l -> p (h l)"), ("R", i2)), start=True, stop=True)
                dv_ = V(dec[i2], ("dec", i2))
                P.act(V(dec[i2].rearrange("p h l -> p (h l)"), ("dec", i2)), pSeg, AF.Exp)
                pS = V(P.psb[4][:, 0:128], ("psb", 4, "s"))
                P.mm(pS, V(kT[:, gl, sl], ("kT", gl)), V(qT[:, gl, sl], ("qT", gl)), start=True, stop=True)
                smv = V(sm[i2], ("sm", i2))
                P.tt("dve", smv, pS, V(maskT, "maskT"), ALU.mult)
                ptv = V(PT[i2], ("PT", i2))
                P.tt("pool", ptv, dv_, V(sm[i2].unsqueeze(1).broadcast_to([128, 4, 128]), ("sm", i2)), ALU.mult)
                P.transpose(V(psT6[:, 0:128], ("psb", 6, "a")), V(xsT[:, gl * 2, sl], ("xsT", gl * 2)), V(P.ident, "ident"))
                P.transpose(V(psT6[:, 128:256], ("psb", 6, "a")), V(xsT[:, gl * 2 + 1, sl], ("xsT", gl * 2 + 1)),
                            V(P.ident, "ident"))
                P.transpose(V(psT6[:, 256:384], ("psb", 6, "a")), V(kT[:, gl, sl], ("kT", gl)), V(P.ident, "ident"))
                xkv = V(xk[i2], ("xk", i2))
                P.copy("act", xkv, V(psT6[:, 0:384], ("psb", 6, "a")))
                xs4 = V(xk[i2][:, 0:256].rearrange("p (h d) -> p h d", h=4), ("xk", i2))
                v4 = V(vv[i2].rearrange("p (h d) -> p h d", h=4), ("vv", i2))
                P.tt("dve", v4, xs4, V(dtt[:, hs].unsqueeze(2).broadcast_to([128, 4, 64]), "dtt"), ALU.mult)
                vh4 = V(vh[i2].rearrange("p (h d) -> p h d", h=4), ("vh", i2))
                P.tt("pool", vh4, v4, V(wdec[:, hs].unsqueeze(2).broadcast_to([128, 4, 64]), "wdec"), ALU.mult)
                for hh in range(4):
                    P.mm(V(P.psb[5][:, hh * 64:(hh + 1) * 64], ("psb", 5, "a")), V(PT[i2][:, hh, :], ("PT", i2)),
                         V(vv[i2][:, hh * 64:(hh + 1) * 64], ("vv", i2)), start=True, stop=True)
                pB = V(P.psb[5][:, 256:512], ("psb", 5, "b"))
                P.mm(pB, V(qT[:, gl, sl], ("qT", gl)), V(Sbf[:, gl, :], ("Sbf", gl)), start=True, stop=True)
                o4 = V(ot[i2].rearrange("p (h d) -> p h d", h=4), ("ot", i2))
                P.tt("dve", o4, V(P.psb[5][:, 256:512].rearrange("p (h d) -> p h d", h=4), ("psb", 5, "b")),
                     V(eG[:, hs].unsqueeze(2).broadcast_to([128, 4, 64]), "eG"), ALU.mult)
                ov = V(ot[i2], ("ot", i2))
                P.tt("dve", ov, ov, V(P.psb[5][:, 0:256], ("psb", 5, "a")), ALU.add)
                t24 = V(t2[i2].rearrange("p (h d) -> p h d", h=4), ("t2", i2))
                P.tt("pool", t24, xs4, V(dsk[:, hs].unsqueeze(2).broadcast_to([128, 4, 64]), "dsk"), ALU.mult)
                P.tt("pool", ov, ov, V(t2[i2], ("t2", i2)), ALU.add)
                yvv = V(yv[i2], ("yv", i2))
                P.tt("pool", yvv, ov, V(sz[:, c4, gl * 256:(gl + 1) * 256], ("sz", c4, gl // 2)), ALU.mult)
                sq = V(ssq[i2], ("ssq", i2))
                P.act(V(P.junk[:, 0:256], "junk"), yvv, AF.Square, accum=sq)
                P.act(sq, sq, AF.Sqrt, scale=1.0 / 256.0, bias=V(P.epsv, "epsv"))
                P.recip(sq, sq)
                ynv = V(yn[i2], ("yn", i2))
                P.stt(ynv, yvv, sq, V(nwc[:, gl * 256:(gl + 1) * 256], "nwc"), ALU.mult, ALU.mult)
                for j in range(2):
                    P.transpose(V(psT6[:, 512 + j * 128:512 + (j + 1) * 128], ("psb", 6, "b")),
                                V(yn[i2][:, j * 128:(j + 1) * 128], ("yn", i2)), V(P.ident, "ident"))
                P.copy("act", V(yT[:, gl * 2:(gl + 1) * 2, sl], ("yT", gl, c4)),
                       V(psT6[:, 512:768].rearrange("p (j l) -> p j l", j=2), ("psb", 6, "b")))
                pU = V(P.psb[4][:, 256:512], ("psb", 4, "u"))
                P.mm(pU, V(xk[i2][:, 256:384], ("xk", i2)), V(vh[i2], ("vh", i2)), start=True, stop=True)
                s4 = V(S[:, gl, :].rearrange("p (h d) -> p h d", h=4), ("S", gl))
                P.tt("dve", s4, s4, V(eGl[:, hs].unsqueeze(2).broadcast_to([128, 4, 64]), "eGl"), ALU.mult)
                sv = V(S[:, gl, :], ("S", gl))
                P.tt("dve", sv, sv, pU, ALU.add)
                P.copy("pool", V(Sbf[:, gl, :], ("Sbf", gl)), sv)
        out_stage(P, blk, srcR, srcRname, dst, dstname,
                  lambda k, s: V(yT[:, k, s * 128:(s + 1) * 128], ("yT",)), 8, wo, ("w", 4))

    run_blocks(P, srcN, srcNname, 0, stageB)
    P.arena_reset(mark)


RW_C = 64
RW_GN_EPS = 64e-5


def rwkv_pass(P, p, srcN, srcNname, srcR, srcRname, dst, dstname):
    P.S.cur_prio = True
    mark = P.arena_off
    alloc_common(P)
    nc = P.nc
    nblk = P.T // TB
    NK = 4
    c0 = p * 512
    w_rkv = P.win("rwkv_w_rkv", [3, D, D])
    w_out = P.win("rwkv_w_out", [D, D])
    w1d, a1d, g1d = P.win("rwkv_w1", [D, 64]), P.win("rwkv_a1", [D, 64]), P.win("rwkv_g1", [D, 160])
    w2d, a2d, g2d = P.win("rwkv_w2", [64, D]), P.win("rwkv_a2", [64, D]), P.win("rwkv_g2", [160, D])
    c_vec = P.win("c_rwkv_vec", [128, 12, KC])
    c_lnw, c_lnb = P.win("rwkv_ln_w", [D]), P.win("rwkv_ln_b", [D])
    c_m = P.win("c_rwkv_masks", [64, 4, 64])
    c_E = P.win("c_rwkv_E", [128, 2], BF16)
    c_bo = P.win("c_rwkv_bo", [128, 128], BF16)
    c_scm = P.win("c_rwkv_scanm", [TB])
    Wr = P.alloc([KC, 512], BF16)
    Wk = P.alloc([KC, 512], BF16)
    Wv = P.alloc([KC, 512], BF16)
    wo = P.alloc([NK, D], BF16)
    w1 = P.alloc([KC, 64], BF16)
    a1 = P.alloc([KC, 64], BF16)
    g1 = P.alloc([KC, 160], BF16)
    w2 = P.alloc([512], BF16)
    a2 = P.alloc([512], BF16)
    g2A = P.alloc([512], BF16)
    g2B = P.alloc([512], BF16)
    vec = P.alloc([12, KC])
    omka = P.alloc([KC])
    nw0 = P.alloc([KC])
    lnw = P.alloc([512])
    lnb = P.alloc([512])
    msk = P.alloc([4, 64])
    identb = P.alloc([64], BF16)
    Eh = P.alloc([2], BF16)
    bo = P.alloc([128], BF16)
    scm = P.alloc([TB])
    epsg = P.alloc([1])
    tinyb = P.alloc([1])
    xx = P.alloc([KC, TB], BF16)
    xi = [P.alloc([KC, TB], BF16) for _ in range(2)]
    xlast = P.alloc([KC], BF16)
    h1 = P.alloc([TB], BF16)
    ha = P.alloc([TB], BF16)
    hgA = P.alloc([TB], BF16)
    hgB = P.alloc([TB], BF16)
    aTm = [P.alloc([NK, TB], BF16) for _ in range(2)]
    rTm = [P.alloc([NK, TB], BF16) for _ in range(2)]
    bT = P.alloc([NK, TB], BF16)
    kT = P.alloc([NK, TB], BF16)
    rkT = P.alloc([NK, TB], BF16)
    yT = P.alloc([NK, TB], BF16)
    WC = P.alloc([NK, 8])
    f32t = [P.alloc([TB]) for _ in range(8)]
    NS = 2
    LmS = [[P.alloc([8, 64], BF16) for _ in range(2)] for _ in range(NS)]
    LTmS = [[P.alloc([8, 64], BF16) for _ in range(2)] for _ in range(NS)]
    XTS = [[P.alloc([8, 64], BF16) for _ in range(2)] for _ in range(NS)]
    XTF = [P.alloc([8, 64], BF16) for _ in range(NS)]
    AkT = [P.alloc([8, 64], BF16) for _ in range(NS)]
    ArbT = [P.alloc([8, 64], BF16) for _ in range(NS)]
    ArkT = [P.alloc([8, 64], BF16) for _ in range(NS)]
    Zs = P.alloc([512], BF16)
    Us = P.alloc([512], BF16)
    Vtm = [P.alloc([512], BF16) for _ in range(2)]
    BKtm = P.alloc([2, 512], BF16)
    yc = P.alloc([512])
    sq = P.alloc([512])
    bon = P.alloc([512])
    ytm = P.alloc([512], BF16)
    st8 = [P.alloc([8]) for _ in range(4)]
    S = P.alloc([NK, 64])
    Sbf = P.alloc([NK, 64], BF16)
    wsrc = lambda i_: w_rkv[i_].rearrange("(k p) n -> p k n", p=128)
    P.dma("pool", V(Wr, ("w", 0)), V(wsrc(0)[:, :, c0:c0 + 512], "in_w"), key=("w", 0))
    P.dma("pool", V(Wk, ("w", 1)), V(wsrc(1)[:, :, c0:c0 + 512], "in_w"), key=("w", 1))
    P.dma("pool", V(Wv, ("w", 2)), V(wsrc(2)[:, :, c0:c0 + 512], "in_w"), key=("w", 2))
    P.dma("pool", V(wo, ("w", 3)), V(w_out[c0:c0 + 512, :].rearrange("(c p) n -> p c n", p=128), "in_w"), key=("w", 3))
    P.dma("pool", V(w1, ("w", 4)), V(w1d.rearrange("(k p) n -> p k n", p=128), "in_w"), key=("w", 4))
    P.dma("pool", V(a1, ("w", 5)), V(a1d.rearrange("(k p) n -> p k n", p=128), "in_w"), key=("w", 5))
    P.dma("pool", V(g1, ("w", 6)), V(g1d.rearrange("(k p) n -> p k n", p=128), "in_w"), key=("w", 6))
    P.dma("pool", V(w2[0:64, :], ("w", 7)), V(w2d[:, c0:c0 + 512], "in_w"), key=("w", 7))
    P.dma("pool", V(a2[0:64, :], ("w", 8)), V(a2d[:, c0:c0 + 512], "in_w"), key=("w", 8))
    P.dma("pool", V(g2A, ("w", 9)), V(g2d[0:128, c0:c0 + 512], "in_w"), key=("w", 9))
    P.dma("pool", V(g2B[0:32, :], ("w", 10)), V(g2d[128:160, c0:c0 + 512], "in_w"), key=("w", 10))
    P.dma("sp", V(vec, "vec"), V(c_vec, "in_w"), key="c0")
    P.dma("sp", V(lnw[0:64, :], "lnw"), V(c_lnw[c0:c0 + 512].partition_broadcast(64), "in_w"), key="c1")
    P.dma("sp", V(lnb[0:64, :], "lnb"), V(c_lnb[c0:c0 + 512].partition_broadcast(64), "in_w"), key="c2")
    P.dma("sp", V(msk[0:64, :, :], "msk"), V(c_m, "in_w"), key="c3")
    P.dma("sp", V(Eh, "Eh"), V(c_E, "in_w"), key="c4")
    P.dma("sp", V(bo, "bo"), V(c_bo, "in_w"), key="c5")
    P.dma("sp", V(scm, "scm"), V(c_scm.partition_broadcast(128), "in_w"), key="c6")
    P.ts("dve", V(omka, "omka"), V(vec[:, 9, :], "vec"), -1.0, ALU.mult, 1.0, ALU.add)
    P.ts("dve", V(nw0, "nw0"), V(vec[:, 6, :], "vec"), -1.0, ALU.mult)
    P.copy("dve", V(identb[0:64, :], "identb"), V(msk[0:64, 3, :], "msk"))
    P.memset("pool", V(epsg, "epsg"), RW_GN_EPS)
    P.memset("pool", V(tinyb, "tinyb"), 1e-24)
    P.memset("pool", V(xlast, "xlast"), 0.0)
    P.memset("dve", V(S, "S"), 0.0)
    P.memset("pool", V(Sbf, "Sbf"), 0.0)
    for e_ in range(2):
        P.memset("pool", V(aTm[e_], ("aT", e_)), 0.0)
        P.memset("pool", V(rTm[e_], ("rT", e_)), 0.0)
    pn = [0]

    def pbank():
        b = pn[0] % 3
        pn[0] += 1
        return V(P.psb[b], ("psb", b))

    def mask(i_):
        return V(msk[0:64, i_, :].unsqueeze(1).broadcast_to([64, 8, 64]), "msk")

    def vcol(i_, kc):
        return V(vec[:, i_, kc:kc + 1], "vec")

    def mix(i_, xnT, xtok, buf):
        o = xi[buf]
        for kc in range(KC):
            if kc % 2 == 0:
                P.stt(V(o[:, kc, :], ("xi", buf, kc)), V(xx[:, kc, :], ("xx", kc)), vcol(i_, kc),
                      V(xnT[:, kc, :], xtok), ALU.mult, ALU.add)
            else:
                P.ts("pool", V(o[:, kc, :], ("xi", buf, kc)), V(xx[:, kc, :], ("xx", kc)), vcol(i_, kc), ALU.mult)
                P.tt("pool", V(o[:, kc, :], ("xi", buf, kc)), V(o[:, kc, :], ("xi", buf, kc)), V(xnT[:, kc, :], xtok), ALU.add)
        return o, ("xi", buf)

    def stage1(blk, slot, tick):
        xnT = P.xnT[slot]
        xtok = ("xnT", slot)
        P.tt("dve", V(xx[:, :, 1:TB], "xx"), V(xnT[:, :, 0:TB - 1], xtok), V(xnT[:, :, 1:TB], xtok), ALU.subtract)
        P.tt("dve", V(xx[:, :, 0:1], "xx"), V(xlast.unsqueeze(2), "xlast"), V(xnT[:, :, 0:1], xtok), ALU.subtract)
        P.copy("pool", V(xlast.unsqueeze(2), "xlast"), V(xnT[:, :, TB - 1:TB], xtok))
        xw, xwtok = mix(1, xnT, xtok, 0)
        pw = pbank()
        for kc in range(KC):
            P.mm(V(pw.ap[0:64, :], pw.tok), V(w1[:, kc, :], ("w", 4)), V(xw[:, kc, :], xwtok), start=(kc == 0), stop=(kc == KC - 1))
        P.act(V(h1[0:64, :], "h1"), V(pw.ap[0:64, :], pw.tok), AF.Tanh)
        xa, xatok = mix(4, xnT, xtok, 1)
        pa = pbank()
        for kc in range(KC):
            P.mm(V(pa.ap[0:64, :], pa.tok), V(a1[:, kc, :], ("w", 5)), V(xa[:, kc, :], xatok), start=(kc == 0), stop=(kc == KC - 1))
        P.copy("act", V(ha[0:64, :], "ha"), V(pa.ap[0:64, :], pa.tok))
        xg, xgtok = mix(5, xnT, xtok, 0)
        pg = pbank()
        for kc in range(KC):
            P.mm(pg, V(g1[:, kc, 0:128], ("w", 6)), V(xg[:, kc, :], xgtok), start=(kc == 0), stop=(kc == KC - 1))
        P.act(V(hgA, "hgA"), pg, AF.Sigmoid)
        pg = pbank()
        for kc in range(KC):
            P.mm(V(pg.ap[0:32, :], pg.tok), V(g1[:, kc, 128:160], ("w", 6)), V(xg[:, kc, :], xgtok), start=(kc == 0), stop=(kc == KC - 1))
        P.act(V(hgB[0:32, :], "hgB"), V(pg.ap[0:32, :], pg.tok), AF.Sigmoid)
        tick()
        xk_, xktok = mix(2, xnT, xtok, 1)
        xr_, xrtok = mix(0, xnT, xtok, 0)
        for kc in range(NK):
            gk = 4 * p + kc
            if kc == 2:
                tick()
            cs = slice(kc * 128, (kc + 1) * 128)
            t = [V(f32t[j], ("f32t", j)) for j in range(8)]
            pz = pbank()
            P.mm(pz, V(w2[0:64, cs], ("w", 7)), V(h1[0:64, :], "h1"), start=True, stop=True)
            P.act(t[0], pz, AF.Exp, scale=-1.0, bias=V(nw0[:, gk:gk + 1], "nw0"))
            P.act(t[0], t[0], AF.Ln, scale=1.0, bias=1.0)
            P.act(t[0], t[0], AF.Exp, scale=-1.0, bias=-0.5)
            P.S.op("dve", (lambda o=f32t[1], a_=scm, b_=f32t[0]: nc.vector.tensor_tensor_scan(o, a_, b_, 0.0, ALU.mult, ALU.add)),
                   reads=[("scm",), ("f32t", 0)], writes=[("f32t", 1)])
            P.tt("pool", t[2], t[1], t[0], ALU.subtract)
            P.act(t[2], t[2], AF.Exp, scale=-1.0)
            P.act(t[3], t[1], AF.Exp, scale=1.0)
            P.act(t[1], t[1], AF.Exp, scale=-1.0)
            P.copy("pool", V(WC[:, kc, :], ("WC", kc)), V(f32t[1].rearrange("p (c l) -> p c l", c=8)[:, :, RW_C - 1], ("f32t", 1)))
            pa2 = pbank()
            P.mm(pa2, V(a2[0:64, cs], ("w", 8)), V(ha[0:64, :], "ha"), start=True, stop=True)
            P.act(t[4], pa2, AF.Sigmoid, scale=1.0, bias=vcol(7, gk))
            pk = pbank()
            for k8 in range(KC):
                P.mm(pk, V(Wk[:, k8, cs], ("w", 1)), V(xk_[:, k8, :], xktok), start=(k8 == 0), stop=(k8 == KC - 1))
            P.ts("dve", t[5], pk, vcol(8, gk), ALU.mult)
            P.act(V(P.junk[:, 0:TB], "junk"), t[5], AF.Square)
            pss = pbank()
            P.mm(pss, V(bo, "bo"), V(P.junk[:, 0:TB], "junk"), start=True, stop=True)
            P.act(t[6], pss, AF.Ln, scale=1.0, bias=V(tinyb, "tinyb"))
            P.act(t[6], t[6], AF.Exp, scale=-0.5)
            P.tt("dve", t[5], t[5], t[6], ALU.mult)
            for e_ in range(2):
                ps_ = slice(e_ * 64, (e_ + 1) * 64)
                P.stt(V(aTm[e_][ps_, kc, :], ("aT", e_, kc)), V(f32t[5][ps_, :], ("f32t", 5)), -1.0,
                      V(f32t[2][ps_, :], ("f32t", 2)), ALU.mult, ALU.mult)
            P.tt("pool", t[6], t[5], t[4], ALU.mult)
            P.tt("pool", V(bT[:, kc, :], ("bT", kc)), t[6], t[3], ALU.mult)
            P.ts("dve", t[4], t[4], vcol(9, gk), ALU.mult, V(omka[:, gk:gk + 1], "omka"), ALU.add)
            P.tt("dve", t[4], pk, t[4], ALU.mult)
            P.tt("pool", V(kT[:, kc, :], ("kT", kc)), t[4], t[3], ALU.mult)
            pr = pbank()
            for k8 in range(KC):
                P.mm(pr, V(Wr[:, k8, cs], ("w", 0)), V(xr_[:, k8, :], xrtok), start=(k8 == 0), stop=(k8 == KC - 1))
            for e_ in range(2):
                ps_ = slice(e_ * 64, (e_ + 1) * 64)
                P.tt("dve", V(rTm[e_][ps_, kc, :], ("rT", e_, kc)), V(pr.ap[ps_, :], pr.tok),
                     V(f32t[1][ps_, :], ("f32t", 1)), ALU.mult)
            P.stt(V(rkT[:, kc, :], ("rkT", kc)), pr, vcol(10, gk), t[4], ALU.mult, ALU.mult)
        xv_, xvtok = mix(3, xnT, xtok, 1)
        return xv_, xvtok

    def phaseAB(c8, sl, ab):
        Lm, LTm = LmS[ab], LTmS[ab]
        XT = XTS[ab] + [None, None]
        XT[2 + ab] = XTF[ab]
        lt = lambda nm, i_: (nm, ab, i_)
        banks = [V(P.psb[b][0:64, :], ("psb", b)) for b in range(5)]
        for hl in range(8):
            kc, e_ = hl // 2, hl % 2
            a_ = V(aTm[e_][:, kc, sl], ("aT", e_, kc))
            b_ = V(bT[:, kc, sl], ("bT", kc))
            k_ = V(kT[:, kc, sl], ("kT", kc))
            r_ = V(rTm[e_][:, kc, sl], ("rT", e_, kc))
            hs = slice(hl * 64, (hl + 1) * 64)
            for bi, (l_, r2) in enumerate(((a_, b_), (b_, a_), (k_, a_), (b_, r_), (k_, r_))):
                P.mm(V(P.psb[bi][0:64, hs], ("psb", bi)), l_, r2, start=True, stop=True)
        v8 = lambda ap_: ap_.rearrange("p (h s) -> p h s", h=8)
        P.tt("dve", V(Lm[0][0:64], lt("Lm", 0)), V(v8(P.psb[0][0:64, :]), ("psb", 0)), mask(0), ALU.mult)
        P.tt("dve", V(LTm[0][0:64], lt("LTm", 0)), V(v8(P.psb[1][0:64, :]), ("psb", 1)), mask(1), ALU.mult)
        P.tt("dve", V(AkT[ab][0:64], ("AkT", ab)), V(v8(P.psb[2][0:64, :]), ("psb", 2)), mask(1), ALU.mult)
        P.tt("dve", V(ArbT[ab][0:64], ("ArbT", ab)), V(v8(P.psb[3][0:64, :]), ("psb", 3)), mask(2), ALU.mult)
        P.tt("dve", V(ArkT[ab][0:64], ("ArkT", ab)), V(v8(P.psb[4][0:64, :]), ("psb", 4)), mask(2), ALU.mult)
        P.tt("pool", V(XT[0][0:64], lt("XT", 0)), V(LTm[0][0:64], lt("LTm", 0)), mask(3), ALU.add)
        cur, xc = 0, 0
        for lvl in range(5):
            nx = 1 - cur
            last = (lvl == 4)
            for hl in range(8):
                hs = slice(hl * 64, (hl + 1) * 64)
                P.mm(V(P.psb[0][0:64, hs], ("psb", 0)), V(LTm[cur][0:64, hl, :], lt("LTm", cur)),
                     V(Lm[cur][0:64, hl, :], lt("Lm", cur)), start=True, stop=True)
                if not last:
                    P.mm(V(P.psb[1][0:64, hs], ("psb", 1)), V(Lm[cur][0:64, hl, :], lt("Lm", cur)),
                         V(LTm[cur][0:64, hl, :], lt("LTm", cur)), start=True, stop=True)
            P.copy("act", V(Lm[nx][0:64], lt("Lm", nx)), V(v8(P.psb[0][0:64, :]), ("psb", 0)))
            if not last:
                P.copy("act", V(LTm[nx][0:64], lt("LTm", nx)), V(v8(P.psb[1][0:64, :]), ("psb", 1)))
            xn_ = (2 + ab) if last else (1 - xc)
            for hl in range(8):
                hs = slice(hl * 64, (hl + 1) * 64)
                P.mm(V(P.psb[2][0:64, hs], ("psb", 2)), V(Lm[nx][0:64, hl, :], lt("Lm", nx)),
                     V(XT[xc][0:64, hl, :], lt("XT", xc)), start=True, stop=False)
                P.mm(V(P.psb[2][0:64, hs], ("psb", 2)), V(identb[0:64, :], "identb"),
                     V(XT[xc][0:64, hl, :], lt("XT", xc)), start=False, stop=True)
            P.copy("act", V(XT[xn_][0:64], lt("XT", xn_)), V(v8(P.psb[2][0:64, :]), ("psb", 2)))
            cur = nx
            xc = xn_

    def phaseCD(blk, c8, sl, ab, xv_, xvtok):
        b5 = V(P.psb[5][0:64, :], ("psb", 5))
        vt_ = Vtm[c8 % 2]
        vtok = ("Vtm", c8 % 2)
        for k8 in range(KC):
            P.mm(b5, V(xv_[:, k8, sl], xvtok), V(Wv[:, k8, :], ("w", 2)), start=(k8 == 0), stop=(k8 == KC - 1))
        P.copy("act", V(vt_[0:64, :], vtok), b5)
        for hl in range(8):
            kc, e_ = hl // 2, hl % 2
            hs = slice(hl * 64, (hl + 1) * 64)
            P.mm(V(P.psb[5][0:64, hs], ("psb", 5)), V(aTm[e_][:, kc, sl], ("aT", e_, kc)),
                 V(Sbf[:, kc, :], ("Sbf", kc)), start=True, stop=False)
            P.mm(V(P.psb[5][0:64, hs], ("psb", 5)), V(AkT[ab][0:64, hl, :], ("AkT", ab)),
                 V(vt_[0:64, hs], vtok), start=False, stop=True)
        P.copy("act", V(Zs[0:64, :], "Zs"), b5)
        for hl in range(8):
            hs = slice(hl * 64, (hl + 1) * 64)
            P.mm(V(P.psb[5][0:64, hs], ("psb", 5)), V(XTF[ab][0:64, hl, :], ("XT", ab, 2 + ab)),
                 V(Zs[0:64, hs], "Zs"), start=True, stop=True)
        P.copy("act", V(Us[0:64, :], "Us"), b5)
        for hl in range(8):
            kc, e_ = hl // 2, hl % 2
            hs = slice(hl * 64, (hl + 1) * 64)
            P.mm(V(P.psb[5][0:64, hs], ("psb", 5)), V(rTm[e_][:, kc, sl], ("rT", e_, kc)),
                 V(Sbf[:, kc, :], ("Sbf", kc)), start=True, stop=False)
            P.mm(V(P.psb[5][0:64, hs], ("psb", 5)), V(ArbT[ab][0:64, hl, :], ("ArbT", ab)),
                 V(Us[0:64, hs], "Us"), start=False, stop=False)
            P.mm(V(P.psb[5][0:64, hs], ("psb", 5)), V(ArkT[ab][0:64, hl, :], ("ArkT", ab)),
                 V(vt_[0:64, hs], vtok), start=False, stop=True)
        y8 = P.psb[5][0:64, :].rearrange("p (h v) -> p h v", h=8)
        s0, s1 = V(st8[0][0:64, :], ("st8", 0)), V(st8[1][0:64, :], ("st8", 1))
        P.S.op("dve", (lambda o=st8[0][0:64, :], i_=y8: nc.vector.tensor_reduce(o, i_, AX.X, ALU.add)),
               reads=[("psb", 5)], writes=[("st8", 0)])
        P.ts("dve", s0, s0, -1.0 / 64.0, ALU.mult)
        ycv = V(yc[0:64, :], "yc")
        yc8 = yc[0:64, :].rearrange("p (h v) -> p h v", h=8)
        P.tt("dve", V(yc8, "yc"), V(y8, ("psb", 5)), V(st8[0][0:64, :].unsqueeze(2).broadcast_to([64, 8, 64]), ("st8", 0)), ALU.add)
        P.act(V(sq[0:64, :], "sq"), ycv, AF.Square)
        P.S.op("dve", (lambda o=st8[1][0:64, :], i_=sq[0:64, :].rearrange("p (h v) -> p h v", h=8): nc.vector.tensor_reduce(o, i_, AX.X, ALU.add)),
               reads=[("sq",)], writes=[("st8", 1)])
        P.act(s1, s1, AF.Ln, scale=1.0 / 64.0, bias=V(epsg[0:64, :], "epsg"))
        P.act(s1, s1, AF.Exp, scale=-0.5)
        P.tt("dve", V(yc8, "yc"), V(yc8, "yc"), V(st8[1][0:64, :].unsqueeze(2).broadcast_to([64, 8, 64]), ("st8", 1)), ALU.mult)
        P.tt("pool", ycv, ycv, V(lnw[0:64, :], "lnw"), ALU.mult)
        P.tt("pool", ycv, ycv, V(lnb[0:64, :], "lnb"), ALU.add)
        b7f = P.psb[7]
        pBs = V(b7f[0:64, 384:392], ("psb", 7, "s"))
        for kc in range(NK):
            P.mm(V(b7f[0:64, 384 + 2 * kc:386 + 2 * kc], ("psb", 7, "s")), V(rkT[:, kc, sl], ("rkT", kc)), V(Eh, "Eh"),
                 start=True, stop=True)
        s2 = V(st8[2][0:64, :], ("st8", 2))
        P.copy("act", s2, pBs)
        P.tt("pool", V(bon[0:64, :].rearrange("p (h v) -> p h v", h=8), "bon"),
             V(vt_[0:64, :].rearrange("p (h v) -> p h v", h=8), vtok),
             V(st8[2][0:64, :].unsqueeze(2).broadcast_to([64, 8, 64]), ("st8", 2)), ALU.mult)
        P.tt("pool", ycv, ycv, V(bon[0:64, :], "bon"), ALU.add)
        psT7 = P.psb[7].bitcast(BF16)
        for kc in range(NK):
            P.transpose(V(psT7[0:64, kc * 128:(kc + 1) * 128], ("psb", 7, "t")), V(bT[:, kc, sl], ("bT", kc)), V(P.ident, "ident"))
        P.copy("act", V(BKtm[0:64, 0, :], ("BKtm", 0)), V(psT7[0:64, 0:512], ("psb", 7, "t")))
        for kc in range(NK):
            P.transpose(V(psT7[0:64, kc * 128:(kc + 1) * 128], ("psb", 7, "t")), V(kT[:, kc, sl], ("kT", kc)), V(P.ident, "ident"))
        P.copy("act", V(BKtm[0:64, 1, :], ("BKtm", 1)), V(psT7[0:64, 0:512], ("psb", 7, "t")))
        for kc in range(NK):
            cs = slice(kc * 128, (kc + 1) * 128)
            P.mm(V(P.psb[6][:, cs], ("psb", 6)), V(BKtm[0:64, 0, cs], ("BKtm", 0)), V(Us[0:64, cs], "Us"), start=True, stop=False)
            P.mm(V(P.psb[6][:, cs], ("psb", 6)), V(BKtm[0:64, 1, cs], ("BKtm", 1)), V(vt_[0:64, cs], vtok), start=False, stop=True)
        for hp in range(2):
            ps_ = slice(hp * 64, (hp + 1) * 64)
            sv = V(S[ps_, :, :], ("S", hp))
            pst = V(P.psb[6][ps_, :].rearrange("p (k c) -> p k c", k=NK)[:, :, hp * 64:(hp + 1) * 64], ("psb", 6))
            P.tt("dve", sv, sv, pst, ALU.add)
            P.tt("dve", sv, sv, V(WC[ps_, :, c8:c8 + 1].broadcast_to([64, NK, 64]), ("WC",)), ALU.mult)
            P.copy("pool", V(Sbf[ps_, :, :], ("Sbf",)), sv)
        for (lh, rh, kk_) in ((V(hgA[:, sl], "hgA"), V(g2A, ("w", 9)), 0), (V(hgB[0:32, sl], "hgB"), V(g2B[0:32, :], ("w", 10)), 1)):
            P.mm(b5, lh, rh, start=(kk_ == 0), stop=(kk_ == 1))
        P.tt("dve", V(ytm[0:64, :], "ytm"), b5, ycv, ALU.mult)
        for kc in range(NK):
            P.transpose(V(psT7[:, 512 + kc * 64:512 + (kc + 1) * 64], ("psb", 7, "y")), V(ytm[0:64, kc * 128:(kc + 1) * 128], "ytm"),
                        V(P.ident[0:64, 0:64], "ident"))
        P.copy("act", V(yT[:, :, sl], ("yT", c8)), V(psT7[:, 512:768].rearrange("p (k t) -> p k t", k=NK), ("psb", 7, "y")))

    def stageB(blk, slot, tick):
        xv_, xvtok = stage1(blk, slot, tick)
        sls = [slice(c8 * RW_C, (c8 + 1) * RW_C) for c8 in range(8)]
        phaseAB(0, sls[0], 0)
        for c8 in range(8):
            if c8 + 1 < 8:
                phaseAB(c8 + 1, sls[c8 + 1], (c8 + 1) % 2)
            phaseCD(blk, c8, sls[c8], c8 % 2, xv_, xvtok)
            if c8 in (1, 4):
                tick()
        out_stage(P, blk, srcR, srcRname, dst, dstname,
                  lambda k, s: V(yT[:, k, s * 128:(s + 1) * 128], ("yT",)), NK, wo, ("w", 3))

    run_blocks(P, srcN, srcNname, 1, stageB)
    P.arena_reset(mark)


def build(T, plan):
    P = Prog(T, plan)
    P.w = {}

    def win(name, shape, dt=F32):
        if name not in P.w:
            P.w[name] = P.dram_in(name, shape, dt)
        return P.w[name]

    P.win = win
    x_in = P.dram_in("x", [T, D])
    out = P.dram_out("out", [T, D])
    P.arena_init(ARENA_BYTES)
    P.psb = [P.ps("psb%d" % i)[:, :] for i in range(8)]
    P.ident = P.alloc([128], BF16)
    P.ones = P.alloc([128], BF16)
    P.normw = P.alloc([9, KC])
    P.epsv = P.alloc([1])
    P.dma("sp", V(P.ident, "ident"), V(win("c_ident", [128, 128], BF16), "in_w"), key="const0")
    P.dma("sp", V(P.normw, "normw"), V(win("c_normw", [128, 9, KC]), "in_w"), key="const1")
    P.memset("pool", V(P.ones, "ones"), 1.0)
    P.memset("pool", V(P.epsv, "epsv"), EPS)
    P.S.barrier()
    scr = [P.dram_scratch("scr%d" % i, [T, D]) for i in range(3)]
    bufs = [(x_in, "x")] + [(scr[i], "scr%d" % i) for i in range(3)]
    cur = 0

    def nxt(*busy):
        for i in (1, 2, 3):
            if i not in busy:
                return i

    for item in plan:
        kind = item[0]
        a = cur
        b = nxt(a)
        c = nxt(a, b)
        A_, B_, C_ = bufs[a], bufs[b], bufs[c]
        if kind == "ffn":
            li = item[1]
            P.soft_next = SOFT
            ffn_pass(P, li, 0, 11, A_[0], A_[1], A_[0], A_[1], B_[0], B_[1])
            ffn_pass(P, li, 11, 22, A_[0], A_[1], B_[0], B_[1], C_[0], C_[1])
            cur = c
        elif kind == "mix" and item[1] == 3:
            P.soft_next = SOFT
            retnet_pass(P, 0, A_[0], A_[1], A_[0], A_[1], B_[0], B_[1])
            retnet_pass(P, 2, A_[0], A_[1], B_[0], B_[1], C_[0], C_[1])
            cur = c
        elif kind == "mix" and item[1] == 0:
            P.soft_next = SOFT
            ssd_pass(P, 0, A_[0], A_[1], A_[0], A_[1], B_[0], B_[1])
            ssd_pass(P, 1, A_[0], A_[1], B_[0], B_[1], C_[0], C_[1])
            cur = c
        elif kind == "mix" and item[1] == 1:
            P.soft_next = SOFT
            rwkv_pass(P, 0, A_[0], A_[1], A_[0], A_[1], B_[0], B_[1])
            rwkv_pass(P, 1, A_[0], A_[1], B_[0], B_[1], C_[0], C_[1])
            cur = c
        elif kind == "mix" and item[1] == 2:
            gla_pass(P, A_[0], A_[1], A_[0], A_[1], B_[0], B_[1])
            cur = b
        elif kind == "final":
            final_norm(P, bufs[cur][0], bufs[cur][1], out, "out")
    P.barrier("sp", [("out",)])
    global LAST_INPUT_NAMES
    LAST_INPUT_NAMES = list(P.inputs.keys())
    return P.finish()


def ret_perm():
    idx = []
    for part in range(2):
        for h in range(RET_H):
            base = part * 1024 + h * RET_DK
            idx += [base + 2 * i for i in range(128)] + [base + 2 * i + 1 for i in range(128)]
    return np.array(idx + list(range(2048, 6144)))


def host_consts(inputs):
    import ml_dtypes
    f = lambda a: np.ascontiguousarray(np.asarray(a, dtype=np.float32))
    c = {}
    c["c_ident"] = np.eye(128, dtype=np.float32).astype(ml_dtypes.bfloat16)
    nw = np.concatenate([f(inputs["norm_mix"]), f(inputs["norm_ffn"]), f(inputs["norm_final"])[None]], 0)
    c["c_normw"] = np.ascontiguousarray(nw.reshape(9, KC, 128).transpose(2, 0, 1))
    c["c_nfb"] = f(inputs["norm_final"])
    cw = f(inputs["ffn_conv_w"])
    cwl = cw.reshape(4, 3, 44, 128).transpose(0, 3, 2, 1)
    cb = f(inputs["ffn_conv_b"]).reshape(4, 44, 128).transpose(0, 2, 1)
    for li in range(4):
        c["ffn_w_up_%d" % li] = f(inputs["ffn_w_up"][li])
        c["ffn_w_down_%d" % li] = f(inputs["ffn_w_down"][li])
        c["c_ffn_cw_%d" % li] = np.ascontiguousarray(cwl[li])
        c["c_ffn_cb_%d" % li] = np.ascontiguousarray(cb[li])
    c["ret_w_in_p"] = np.ascontiguousarray(f(inputs["ret_w_in"][0])[:, ret_perm()])
    c["ret_w_out"] = f(inputs["ret_w_out"][0])
    inv = (1.0 / (np.float32(10000.0) ** np.linspace(0.0, 1.0, 128, dtype=np.float32))).astype(np.float32)
    ang = (np.arange(4096, dtype=np.float32)[None, :] * inv[:, None]).astype(np.float32)
    c["c_ret_cos"] = np.cos(ang).astype(np.float32)
    c["c_ret_sin"] = np.sin(ang).astype(np.float32)
    gam = 1.0 - 2.0 ** (-5.0 - np.arange(4, dtype=np.float64))
    s_ = np.arange(128)[:, None]
    l_ = np.arange(128)[None, :]
    decT = np.zeros((128, 4, 128), np.float64)
    for h in range(4):
        decT[:, h, :] = np.where(l_ >= s_, gam[h] ** (l_ - s_), 0.0) / 16.0
    c["c_ret_decT"] = decT.astype(np.float32)
    c["c_ret_gl"] = np.stack([gam[h] ** ((np.arange(TB) % 128) + 1) for h in range(4)]).astype(np.float32)
    c["c_ret_kdec"] = np.stack([gam[h] ** (127 - np.arange(128)) / 16.0 for h in range(4)], 1).astype(np.float32)
    c["ssd_w_in"] = f(inputs["ssd_w_in"][0])
    c["ssd_w_out"] = f(inputs["ssd_w_out"][0])
    c["c_ssd_cw"] = np.ascontiguousarray(f(inputs["ssd_conv_w"][0]).reshape(4, 32, 128).transpose(2, 1, 0))
    c["c_ssd_cb"] = np.ascontiguousarray(f(inputs["ssd_conv_b"][0]).reshape(32, 128).T)
    for n_ in ("ssd_dt_bias", "ssd_a_log", "ssd_d", "ssd_norm_w"):
        c[n_] = f(inputs[n_][0])
    c["c_SU"] = (s_ < l_).T.astype(np.float32).copy()
    for n_ in ("w_rkv", "w_out", "w1", "w2", "a1", "a2", "g1", "g2", "ln_w", "ln_b"):
        c["rwkv_" + n_] = f(inputs["rwkv_" + n_][0])
    vecs = [f(inputs["rwkv_mix"][0])[i_] for i_ in range(6)] + [f(inputs["rwkv_" + n_][0]).reshape(-1) for n_ in
                                                                 ("w0", "a0", "k_k", "k_a", "r_k")] + [np.zeros(1024, np.float32)]
    c["c_rwkv_vec"] = np.ascontiguousarray(np.stack(vecs).reshape(12, KC, 128).transpose(2, 0, 1))
    t64 = np.arange(64)[:, None]
    u64 = np.arange(64)[None, :]
    c["c_rwkv_masks"] = np.ascontiguousarray(np.stack([(u64 < t64), (t64 < u64), (t64 <= u64), (t64 == u64)], 1).astype(np.float32))
    E = np.zeros((128, 2), np.float32)
    E[:64, 0] = 1.0
    E[64:, 1] = 1.0
    c["c_rwkv_E"] = E.astype(ml_dtypes.bfloat16)
    c["c_rwkv_bo"] = (E @ E.T).astype(ml_dtypes.bfloat16)
    sm64 = np.ones(TB, np.float32)
    sm64[::64] = 0.0
    c["c_rwkv_scanm"] = sm64
    c["gla_w_in"] = f(inputs["gla_w_in"][0])
    c["gla_w_out"] = f(inputs["gla_w_out"][0])
    c["gla_w_gk2"] = f(inputs["gla_w_gk2"][0])
    c["c_gla_bgk"] = np.ascontiguousarray(f(inputs["gla_b_gk2"][0]).reshape(4, 128).T)
    c["c_gla_nw"] = np.ascontiguousarray(f(inputs["gla_norm_w"][0]).reshape(2, 128).T)
    c["c_maskT"] = (l_ >= s_).astype(np.float32)
    sm = np.ones(TB, np.float32)
    sm[::128] = 0.0
    c["c_scanm"] = sm
    return c


FULL_PLAN = [("mix", 0), ("ffn", 0), ("mix", 1), ("ffn", 1), ("mix", 2), ("ffn", 2), ("mix", 3), ("ffn", 3), ("final",)]
_CACHE = {}


def kernel(**inputs):
    T = 4096
    n_cores = 8
    if "nc" not in _CACHE:
        _CACHE["nc"] = build(T, FULL_PLAN)
        _CACHE["names"] = list(LAST_INPUT_NAMES)
    nc = _CACHE["nc"]
    names = _CACHE["names"]
    consts = host_consts(inputs)
    x = np.ascontiguousarray(np.asarray(inputs["x"], dtype=np.float32))
    shared = {n: consts[n] for n in names if n != "x"}
    in_maps = []
    for b in range(n_cores):
        m = dict(shared)
        m["x"] = np.ascontiguousarray(x[b])
        in_maps.append(m)
    res = run_bass_kernel_spmd(nc, in_maps, core_ids=list(range(n_cores)))
    return np.stack([np.asarray(r["out"], dtype=np.float32) for r in res.results], axis=0)
```

```python
import numpy as np
import concourse.bass as bass
import concourse.mybir as mybir
from concourse.bass_utils import run_bass_kernel_spmd

F32 = mybir.dt.float32
BF16 = mybir.dt.bfloat16
AF = mybir.ActivationFunctionType
ALU = mybir.AluOpType
AX = mybir.AxisListType

D = 1024
KC = 8
TB = 512
SCHEDULE = True
ACT_SETS = {AF.Exp: 'lnexp', AF.Ln: 'lnexp', AF.Silu: 'silu', AF.Sqrt: 'sqrt', AF.Sigmoid: 'sig', AF.Tanh: 'sig'}
ACT_SWITCH_NS = 1300.0
KEEP_ORDER = ()
PRIO = True
SOFT = True
EPS = 1e-5


class V:
    __slots__ = ("ap", "tok")

    def __init__(self, ap, tok):
        self.ap = ap
        self.tok = tok if isinstance(tok, tuple) else (tok,)


class _Op:
    __slots__ = ("eng", "fn", "reads", "writes", "dma_key", "deps", "inc", "val", "sem", "amt",
                 "odeps", "cost", "lat", "bar", "idx", "grp", "gend", "st", "bind", "prio", "tset")

    def __init__(self, eng, fn, reads, writes, dma_key, cost=300.0, lat=0.0):
        self.odeps = []
        self.cost = cost
        self.lat = lat
        self.bar = False
        self.idx = 0
        self.grp = None
        self.gend = True
        self.st = 0.0
        self.bind = None
        self.prio = False
        self.tset = None
        self.eng = eng
        self.fn = fn
        self.reads = reads
        self.writes = writes
        self.dma_key = dma_key
        self.deps = []
        self.inc = False
        self.val = 0
        self.sem = None
        self.amt = 1


class Sched:
    COMPUTE = ("pe", "act", "dve", "pool")

    def __init__(self, nc):
        self.nc = nc
        self.ops = []
        self.state = {}

    def op(self, eng, fn, reads=(), writes=(), dma_key=None, cost=300.0, lat=0.0):
        reads = [t if isinstance(t, tuple) else (t,) for t in reads]
        writes = [t if isinstance(t, tuple) else (t,) for t in writes]
        writes = [t[:2] if t[0] == "psb" else t for t in writes]
        writes += [t[:2] for t in reads if t[0] == "psb" and t[:2] not in writes]
        reads = [t for t in reads if t[0] != "psb"]
        o = _Op(eng, fn, reads, writes, dma_key, cost, lat)
        o.prio = getattr(self, "cur_prio", False)
        self._analyse(o)
        self.ops.append(o)
        return o

    @staticmethod
    def _conf(a, b):
        n = min(len(a), len(b))
        return a[:n] == b[:n]

    def _add_dep(self, o, p, kind):
        if p is None or p is o:
            return
        pd = p.dma_key is not None
        od = o.dma_key is not None
        if not pd and not od:
            if p.eng == "pe" and o.eng == "pe":
                o.odeps.append(p)
                return
            if p.eng == o.eng and kind != "RAW":
                o.odeps.append(p)
                return
        if pd and od and p.eng == o.eng and kind == "WAR" and False:
            return
        o.deps.append(p)

    def _analyse(self, o):
        st = self.state
        for tk in o.reads:
            root = st.setdefault(tk[0], {})
            for k, e in root.items():
                if self._conf(k, tk):
                    self._add_dep(o, e[0], "RAW")
            e = root.get(tk)
            if e is None:
                root[tk] = [None, [o]]
            else:
                e[1].append(o)
        for tk in o.writes:
            root = st.setdefault(tk[0], {})
            dead = []
            for k, e in root.items():
                if self._conf(k, tk):
                    self._add_dep(o, e[0], "WAW")
                    for r in e[1]:
                        self._add_dep(o, r, "WAR")
                    if len(k) > len(tk):
                        dead.append(k)
                    elif len(k) < len(tk):
                        pass
            for k in dead:
                del root[k]
            root[tk] = [o, []]

    def barrier(self):
        lasts = {}
        dmas = {}
        for o in self.ops:
            if o.dma_key is not None:
                dmas[o.dma_key] = o
            elif o.fn is not None:
                lasts[o.eng] = o
        new = []
        for eng in ("pe", "act", "dve", "pool", "sp"):
            b = _Op(eng, None, [], [], None)
            b.deps = [p for e, p in lasts.items() if e != eng] + list(dmas.values())
            b.bar = True
            new.append(b)
        self.ops.extend(new)
        self.state = {}

    def schedule(self, window=16, xlat=300.0):
        import bisect
        segs, cur = [], []
        for o in self.ops:
            if o.bar:
                if cur:
                    segs.append(cur)
                    cur = []
                segs.append([o])
            else:
                cur.append(o)
        if cur:
            segs.append(cur)
        out = []
        self.seg_times = []
        prev_lasts, prev_dmas = {}, {}
        for seg in segs:
            if len(seg) == 1:
                b = seg[0]
                if b.bar:
                    b.deps = [p_ for e_, p_ in prev_lasts.items() if e_ != b.eng] + list(prev_dmas.values())
                out.extend(seg)
                continue
            seg_start = len(out)
            lastof = {}
            for i, o in enumerate(seg):
                o.idx = i
                if o.eng in KEEP_ORDER:
                    if o.eng in lastof:
                        o.odeps.append(lastof[o.eng])
                    lastof[o.eng] = o
            inseg = set(id(o) for o in seg)
            groups = {}
            for o in seg:
                if o.grp is not None:
                    groups.setdefault(o.grp, []).append(o)
            for g, mem in groups.items():
                if len(mem) > 1:
                    ids = set(id(m) for m in mem)
                    first = mem[0]
                    for m in mem[1:]:
                        for d in m.deps + m.odeps:
                            if id(d) not in ids:
                                first.odeps.append(d)
            succ_pre = {}
            for o in seg:
                for d in o.deps:
                    if id(d) in inseg:
                        succ_pre.setdefault(id(d), []).append((o, True))
                for d in o.odeps:
                    if id(d) in inseg:
                        succ_pre.setdefault(id(d), []).append((o, False))
            pe_lock = None
            cur_tset = None
            busy = {}
            use_prio = PRIO and seg[len(seg) // 2].prio
            blev = {}
            for o in reversed(seg):
                m = 0.0
                for s2, _h in succ_pre.get(id(o), ()):
                    v = blev[id(s2)]
                    if v > m:
                        m = v
                blev[id(o)] = m + o.cost + o.lat
            npred = {}
            succ = {}
            for o in seg:
                ds = [(d, True) for d in o.deps if id(d) in inseg] + [(d, False) for d in o.odeps if id(d) in inseg]
                npred[id(o)] = len(ds)
                for d, hard in ds:
                    succ.setdefault(id(d), []).append((o, hard))
            fin = {}
            rtime = {}
            ready = {e: [] for e in ("pe", "act", "dve", "pool", "sp")}
            free = {e: 0.0 for e in ready}
            for o in seg:
                if npred[id(o)] == 0:
                    rtime[id(o)] = 0.0
                    ready[o.eng].append((o.idx, o))
            for e in ready:
                ready[e].sort(key=lambda t: t[0])
            done = 0
            n = len(seg)
            while done < n:
                best = None
                for e, lst in ready.items():
                    fe = free[e]
                    cand = lst[:window]
                    if e == "pe" and pe_lock is not None:
                        cand = [t for t in lst if t[1].grp == pe_lock][:1]
                    for (ix, o) in cand:
                        st = rtime[id(o)]
                        if st < fe:
                            st = fe
                        if o.tset is not None and o.tset != cur_tset:
                            st += ACT_SWITCH_NS
                        key = (st, -blev[id(o)], ix) if use_prio else (st, ix)
                        if best is None or key < best[0]:
                            best = (key, o, st, ix)
                _k, o, st, ix = best
                lst = ready[o.eng]
                lst.pop(bisect.bisect_left(lst, (ix,), key=lambda t: (t[0],)))
                if o.eng == "pe" and o.grp is not None:
                    pe_lock = None if o.gend else o.grp
                if o.tset is not None:
                    cur_tset = o.tset
                free[o.eng] = st + o.cost
                busy[o.eng] = busy.get(o.eng, 0.0) + o.cost
                f = st + o.cost + o.lat
                fin[id(o)] = f
                o.st = st
                out.append(o)
                done += 1
                for s_, hard in succ.get(id(o), ()):
                    k = id(s_)
                    npred[k] -= 1
                    if hard or s_.eng != o.eng:
                        t_ = f + (xlat if s_.eng != o.eng else 0.0)
                    else:
                        t_ = st + o.cost
                    if rtime.get(k, 0.0) < t_:
                        rtime[k] = t_
                        s_.bind = o
                    if npred[k] == 0:
                        bisect.insort(ready[s_.eng], (s_.idx, s_), key=lambda t: t[0])
            self.seg_times.append((max(fin.values()) if fin else 0.0, dict(busy), len(seg)))
            prev_lasts, prev_dmas = {}, {}
            for o in out[seg_start:]:
                if o.dma_key is not None:
                    prev_dmas[o.dma_key] = o
                elif o.fn is not None:
                    prev_lasts[o.eng] = o
        assert len(out) == len(self.ops)
        self.ops = out

    def emit(self, block_ctx, sems):
        nc = self.nc
        for o in self.ops:
            for p in o.deps:
                p.inc = True
        cnt = {}
        for o in self.ops:
            if o.dma_key is not None:
                key = ("dma", o.dma_key)
                cnt[key] = cnt.get(key, 0) + 16
                o.val = cnt[key]
                o.sem = sems[key]
                o.amt = 16
                o.inc = True
            elif o.inc:
                cnt[o.eng] = cnt.get(o.eng, 0) + 1
                o.val = cnt[o.eng]
                o.sem = sems[o.eng]
        engs = {"pe": nc.tensor, "act": nc.scalar, "dve": nc.vector, "pool": nc.gpsimd, "sp": nc.sync}
        per_eng = {k: [] for k in engs}
        for o in self.ops:
            per_eng[o.eng].append(o)

        def run(engname):
            eng = engs[engname]
            waited = {}
            for o in per_eng[engname]:
                need = {}
                for p in o.deps:
                    sid = id(p.sem)
                    if need.get(sid, (None, 0))[1] < p.val:
                        need[sid] = (p.sem, p.val)
                for sid, (sem, val) in need.items():
                    if waited.get(sid, 0) < val:
                        eng.wait_ge(sem, val)
                        waited[sid] = val
                if o.fn is None:
                    continue
                ins = o.fn()
                if o.inc:
                    ins.then_inc(o.sem, o.amt)

        @block_ctx.tensor
        def _(e):
            run("pe")

        @block_ctx.scalar
        def _(e):
            run("act")

        @block_ctx.vector
        def _(e):
            run("dve")

        @block_ctx.gpsimd
        def _(e):
            run("pool")

        @block_ctx.sync
        def _(e):
            run("sp")


class Prog:
    def __init__(self, T, plan):
        self.T = T
        self.plan = plan
        self.nc = bass.Bass("TRN2", target_bir_lowering=False)
        self.S = Sched(self.nc)
        self.ctxs = []
        self.dma_keys = []
        self.inputs = {}
        self.psn = 0

    def dram_in(self, name, shape, dt=F32):
        t = self.nc.dram_tensor(name, list(shape), dt, kind="ExternalInput")
        self.inputs[name] = t
        return t.ap()

    def dram_out(self, name, shape, dt=F32):
        return self.nc.dram_tensor(name, list(shape), dt, kind="ExternalOutput").ap()

    def dram_scratch(self, name, shape, dt=F32):
        return self.nc.dram_tensor(name, list(shape), dt, kind="Internal").ap()

    def sb(self, name, shape, dt=F32):
        g = self.nc.sbuf_tensor(name, list(shape), dt)
        t = g.__enter__()
        self.ctxs.append(g)
        return t

    def ps(self, name, shape=(128, 512), dt=F32):
        g = self.nc.psum_tensor(name, list(shape), dt)
        t = g.__enter__()
        self.ctxs.append(g)
        return t

    def arena_init(self, nbytes):
        self.arena = self.sb("arena", [128, nbytes // 4], F32)
        self.arena_n = nbytes
        self.arena_off = 0

    def alloc(self, shape, dt=F32):
        n = 1
        for d in shape:
            n *= d
        esz = 4 if dt == F32 else 2
        nb = (n * esz + 63) // 64 * 64
        assert self.arena_off + nb <= self.arena_n, ("arena overflow", self.arena_off + nb, self.arena_n)
        a = self.arena[:, self.arena_off // 4:(self.arena_off + nb) // 4]
        self.arena_off += nb
        if dt != F32:
            a = a.bitcast(dt)
        a = a[:, 0:n]
        if len(shape) == 2:
            a = a.rearrange("p (a b) -> p a b", a=shape[0])
        elif len(shape) == 3:
            a = a.rearrange("p (a b c) -> p a b c", a=shape[0], b=shape[1])
        elif len(shape) == 4:
            a = a.rearrange("p (a b c d) -> p a b c d", a=shape[0], b=shape[1], c=shape[2])
        return a

    def arena_reset(self, mark=0):
        if getattr(self, "soft_next", False):
            self.soft_next = False
            self.arena_off = mark
            return
        self.S.barrier()
        self.arena_off = mark

    def _toks(self, *vs):
        return [v.tok for v in vs if isinstance(v, V)]

    @staticmethod
    def _n(ap):
        n = 1
        for d in list(ap.shape)[1:]:
            n *= int(d)
        return n

    def mm(self, out, lhsT, rhs, start=True, stop=True):
        nc = self.nc
        otok = out.tok
        if not (start and stop):
            otok = otok[:2]
        n = self._n(rhs.ap)
        mult = 4.0 if rhs.ap.dtype == F32 else 1.0
        o = self.S.op("pe", lambda: nc.tensor.matmul(out.ap, lhsT.ap, rhs.ap, start=start, stop=stop),
                      reads=self._toks(lhsT, rhs), writes=[otok], cost=mult * (max(64, n) * 0.42 + 20.0), lat=250.0)
        if start:
            self.gid = getattr(self, "gid", 0) + 1
        o.grp = self.gid
        o.gend = bool(stop)

    def transpose(self, out, in_, ident):
        nc = self.nc
        self.S.op("pe", lambda: nc.tensor.transpose(out.ap, in_.ap, ident.ap),
                  reads=self._toks(in_, ident), writes=self._toks(out), cost=80.0, lat=250.0)

    def act(self, out, in_, func, scale=None, bias=None, accum=None, extra_reads=()):
        nc = self.nc
        kw = {}
        if scale is not None:
            kw["scale"] = scale.ap if isinstance(scale, V) else scale
        if bias is not None:
            kw["bias"] = bias.ap if isinstance(bias, V) else bias
        if accum is not None:
            kw["accum_out"] = accum.ap
        w = self._toks(out) + (self._toks(accum) if accum is not None else [])
        o = self.S.op("act", lambda: nc.scalar.activation(out.ap, in_.ap, func, **kw),
                      reads=self._toks(in_, scale, bias) + list(extra_reads), writes=w,
                      cost=230.0 + 0.83 * self._n(in_.ap) + (90.0 if accum is not None else 0.0))
        o.tset = ACT_SETS.get(func)

    def _e(self, eng):
        return {"dve": self.nc.vector, "pool": self.nc.gpsimd, "act": self.nc.scalar}[eng]

    def _c(self, eng, ap, per=1.04):
        n = self._n(ap)
        if eng == "pool":
            return 300.0 + 1.6 * n
        if eng == "act":
            return 230.0 + 0.83 * n
        return 120.0 + per * n

    def tt(self, eng, out, a, b, op):
        e = self._e(eng)
        self.S.op(eng, lambda: e.tensor_tensor(out.ap, a.ap, b.ap, op),
                  reads=self._toks(a, b), writes=self._toks(out), cost=self._c(eng, out.ap))

    def ts(self, eng, out, in_, s1, op0, s2=None, op1=None, accum=None):
        e = self._e(eng)
        a1 = s1.ap if isinstance(s1, V) else s1
        a2 = s2.ap if isinstance(s2, V) else s2
        kw = {}
        if op1 is not None:
            kw["op1"] = op1
        if accum is not None:
            kw["accum_out"] = accum.ap
        w = self._toks(out) + (self._toks(accum) if accum is not None else [])
        self.S.op(eng, lambda: e.tensor_scalar(out.ap, in_.ap, a1, a2, op0, **kw),
                  reads=self._toks(in_, s1, s2), writes=w, cost=self._c(eng, out.ap, 0.7))

    def stt(self, out, in0, scalar, in1, op0, op1):
        nc = self.nc
        sc = scalar.ap if isinstance(scalar, V) else scalar
        self.S.op("dve", lambda: nc.vector.scalar_tensor_tensor(out.ap, in0.ap, sc, in1.ap, op0, op1),
                  reads=self._toks(in0, scalar, in1), writes=self._toks(out), cost=self._c("dve", out.ap))

    def copy(self, eng, out, in_):
        if eng == "act":
            nc = self.nc
            self.S.op("act", lambda: nc.scalar.copy(out.ap, in_.ap), reads=self._toks(in_), writes=self._toks(out),
                      cost=self._c("act", out.ap))
        else:
            e = self._e(eng)
            self.S.op(eng, lambda: e.tensor_copy(out.ap, in_.ap), reads=self._toks(in_), writes=self._toks(out),
                      cost=self._c(eng, out.ap, 0.7))

    def memset(self, eng, out, val):
        e = self._e(eng)
        self.S.op(eng, lambda: e.memset(out.ap, val), writes=self._toks(out), cost=self._c(eng, out.ap, 0.7))

    def recip(self, out, in_):
        nc = self.nc
        self.S.op("dve", lambda: nc.vector.reciprocal(out.ap, in_.ap), reads=self._toks(in_), writes=self._toks(out),
                  cost=self._c("dve", out.ap, 8.4))

    def dma(self, q, out, in_, key):
        if key not in self.dma_keys:
            self.dma_keys.append(key)
        e = {"sp": self.nc.sync, "pool": self.nc.gpsimd, "act": self.nc.scalar}[q]
        nb = self._n(out.ap) * int(list(out.ap.shape)[0]) * (4 if out.ap.dtype == F32 else 2)
        self.S.op(q, lambda: e.dma_start(out=out.ap, in_=in_.ap), reads=self._toks(in_),
                  writes=self._toks(out), dma_key=key, cost=(400.0 if q == "pool" else 60.0), lat=2000.0 + nb / 100.0)

    def barrier(self, eng, toks):
        self.S.op(eng, None, reads=list(toks))

    def finish(self):
        nc = self.nc
        sems = {}
        gs = []
        for name in ("pe", "act", "dve", "pool"):
            g = nc.semaphore("sem_" + name)
            sems[name] = g.__enter__()
            gs.append(g)
        for i, k in enumerate(self.dma_keys):
            g = nc.semaphore("semd_%d" % i)
            sems[("dma", k)] = g.__enter__()
            gs.append(g)
        if SCHEDULE:
            self.S.schedule()
            self.seg_times = self.S.seg_times
        blk = nc.Block()
        b = blk.__enter__()
        self.S.emit(b, sems)
        blk.__exit__(None, None, None)
        for g in reversed(gs):
            g.__exit__(None, None, None)
        for g in reversed(self.ctxs):
            g.__exit__(None, None, None)
        return nc


FFN_H = 2816
FFN_NC = 22
ARENA_BYTES = 204 * 1024


def tile_rows(ap, blk, s):
    r0 = blk * TB + s * 128
    return ap[r0:r0 + 128, :]


def alloc_common(P):
    P.xt = [P.alloc([D]) for _ in range(2)]
    P.xr = [P.alloc([D]) for _ in range(2)]
    P.xnT = [P.alloc([KC, TB], BF16) for _ in range(2)]
    P.xs = P.alloc([D], BF16)
    P.junk = P.alloc([D], BF16)
    P.ss = [P.alloc([4]) for _ in range(2)]
    P.rstd = [P.alloc([4]) for _ in range(2)]
    P.xtn = 0
    P.xrn = 0


def norm_tile(P, src, srcname, blk, s, nidx, slot):
    psT = P.psb[7].bitcast(BF16)
    ss, rstd = P.ss[slot], P.rstd[slot]
    xi = P.xtn % 2
    P.xtn += 1
    xt = P.xt[xi]
    xtv = V(xt, ("xt", xi))
    P.dma("sp", xtv, V(tile_rows(src, blk, s), (srcname, blk, s)), key=("xt", xi))
    P.act(V(P.junk, "junk"), xtv, AF.Square, accum=V(ss[:, s:s + 1], ("ss", slot, s)))
    P.ts("dve", V(rstd[:, s:s + 1], ("rstd", slot, s)), V(ss[:, s:s + 1], ("ss", slot, s)), 1.0 / D, ALU.mult, EPS, ALU.add)
    P.tt("pool", V(rstd[:, s:s + 1], ("rstd", slot, s)), V(rstd[:, s:s + 1], ("rstd", slot, s)), V(P.neghalf, "neghalf"), ALU.pow)
    P.ts("dve", V(P.xs, "xs"), xtv, V(rstd[:, s:s + 1], ("rstd", slot, s)), ALU.mult)
    for kc in range(KC):
        P.transpose(V(psT[:, kc * 128:(kc + 1) * 128], ("psb", 7)), V(P.xs[:, kc * 128:(kc + 1) * 128], "xs"),
                    V(P.ident, "ident"))
    P.tt("dve", V(P.xnT[slot][:, :, s * 128:(s + 1) * 128], ("xnT", slot, s)),
         V(psT.rearrange("p (k t) -> p k t", k=KC), ("psb", 7)),
         V(P.normw[:, nidx, :].unsqueeze(2).broadcast_to([128, KC, 128]), "normw"), ALU.mult)


def run_blocks(P, srcN, srcNname, nidx, stageB):
    nblk = P.T // TB
    for s in range(4):
        norm_tile(P, srcN, srcNname, 0, s, nidx, 0)
    for blk in range(nblk):
        pending = [(blk + 1, s) for s in range(4)] if blk + 1 < nblk else []

        def tick():
            if pending:
                b, s_ = pending.pop(0)
                norm_tile(P, srcN, srcNname, b, s_, nidx, b % 2)

        stageB(blk, blk % 2, tick)
        while pending:
            tick()


def out_stage(P, blk, srcR, srcRname, dst, dstname, lhs_fn, nk, wo, wotok):
    for s in range(4):
        xi = P.xrn % 2
        P.xrn += 1
        xr = P.xr[xi]
        xrv = V(xr, ("xr", xi))
        P.dma("sp", xrv, V(tile_rows(srcR, blk, s), (srcRname, blk, s)), key=("xr", xi))
        for half in range(2):
            b = 3 + (2 * s + half) % 2
            pd = V(P.psb[b], ("psb", b))
            for k in range(nk):
                P.mm(pd, lhs_fn(k, s), V(wo[:, k, half * 512:(half + 1) * 512], wotok),
                     start=(k == 0), stop=(k == nk - 1))
            xh = V(xr[:, half * 512:(half + 1) * 512], ("xr", xi))
            P.tt("dve", xh, pd, xh, ALU.add)
        P.dma("sp", V(tile_rows(dst, blk, s), (dstname, blk, s)), xrv, key=("xr", xi))


def ffn_pass(P, li, c0, c1, srcN, srcNname, srcR, srcRname, dst, dstname):
    P.S.cur_prio = False
    mark = P.arena_off
    alloc_common(P)
    nch = c1 - c0
    ncol = nch * 128
    nblk = P.T // TB
    w_up = P.win("ffn_w_up_%d" % li, [D, 2 * FFN_H])
    w_dn = P.win("ffn_w_down_%d" % li, [FFN_H, D])
    c_cw = P.win("c_ffn_cw_%d" % li, [128, 2 * FFN_NC, 3])
    c_cb = P.win("c_ffn_cb_%d" % li, [128, 2 * FFN_NC])
    wupv = P.alloc([KC, ncol], BF16)
    wupg = P.alloc([KC, ncol], BF16)
    wdn = P.alloc([nch, D], BF16)
    cw = P.alloc([2 * FFN_NC, 3])
    cb = P.alloc([2 * FFN_NC])
    hs = [P.alloc([2 * FFN_NC, 2]) for _ in range(2)]
    A = [P.alloc([TB]) for _ in range(6)]
    G = [P.alloc([TB]) for _ in range(4)]
    hid = P.alloc([nch, TB], BF16)
    upsrc = w_up.rearrange("(k p) n -> p k n", p=128)
    P.dma("pool", V(wupv, ("w", 0)), V(upsrc[:, :, c0 * 128:c1 * 128], "in_w"), key=("w", 0))
    P.dma("pool", V(wupg, ("w", 1)), V(upsrc[:, :, FFN_H + c0 * 128:FFN_H + c1 * 128], "in_w"), key=("w", 1))
    P.dma("pool", V(wdn, ("w", 2)), V(w_dn[c0 * 128:c1 * 128, :].rearrange("(c p) n -> p c n", p=128), "in_w"),
          key=("w", 2))
    P.dma("sp", V(cw, "cw"), V(c_cw, "in_w"), key="cw")
    P.dma("sp", V(cb, "cb"), V(c_cb, "in_w"), key="cb")
    P.memset("pool", V(hs[0], ("hs", 0)), 0.0)
    P.memset("pool", V(hs[1], ("hs", 1)), 0.0)

    def stageB(blk, slot, tick):
        xnT = P.xnT[slot]
        par = blk % 2
        ubanks = (0, 1, 2, 5, 6)
        tick_at = set(int(round(x)) for x in np.linspace(1, 2 * nch - 2, 4))
        for c in range(nch):
            for part in range(2):
                q = 2 * c + part
                if q in tick_at:
                    tick()
                cp = (c0 + c) + part * FFN_NC
                wsel = wupv if part == 0 else wupg
                wtok = ("w", part)
                bi = ubanks[q % 5]
                pu = V(P.psb[bi], ("psb", bi))
                for kc in range(KC):
                    P.mm(pu, V(wsel[:, kc, c * 128:(c + 1) * 128], wtok),
                         V(xnT[:, kc, :], ("xnT", slot)), start=(kc == 0), stop=(kc == KC - 1))
                ai = q % 6
                At = A[ai]
                atok = ("A", ai)
                P.act(V(At[:, 0:TB], atok), pu, AF.Identity,
                      scale=V(cw[:, cp, 2:3], "cw"), bias=V(cb[:, cp:cp + 1], "cb"))
                P.copy("act", V(hs[par][:, cp, :], ("hs", par, cp)), V(P.psb[bi][:, TB - 2:TB], ("psb", bi)))
                P.stt(V(At[:, 1:TB], atok), V(P.psb[bi][:, 0:TB - 1], ("psb", bi)), V(cw[:, cp, 1:2], "cw"),
                      V(At[:, 1:TB], atok), ALU.mult, ALU.add)
                P.stt(V(At[:, 2:TB], atok), V(P.psb[bi][:, 0:TB - 2], ("psb", bi)), V(cw[:, cp, 0:1], "cw"),
                      V(At[:, 2:TB], atok), ALU.mult, ALU.add)
                hp = V(hs[1 - par][:, cp, :], ("hs", 1 - par, cp))
                P.stt(V(At[:, 0:2], atok), hp, V(cw[:, cp, 0:1], "cw"), V(At[:, 0:2], atok), ALU.mult, ALU.add)
                P.stt(V(At[:, 0:1], atok), V(hs[1 - par][:, cp, 1:2], ("hs", 1 - par, cp)), V(cw[:, cp, 1:2], "cw"),
                      V(At[:, 0:1], atok), ALU.mult, ALU.add)
                if part == 0:
                    Aval, avtok = At, atok
                else:
                    Gt = G[c % 4]
                    gtok = ("G", c % 4)
                    P.act(V(Gt, gtok), V(At[:, 0:TB], atok), AF.Silu)
                    P.tt("pool", V(hid[:, c, :], ("hid", c)), V(Aval[:, 0:TB], avtok), V(Gt, gtok), ALU.mult)
        out_stage(P, blk, srcR, srcRname, dst, dstname,
                  lambda k, s: V(hid[:, k, s * 128:(s + 1) * 128], ("hid", k)), nch, wdn, ("w", 2))

    run_blocks(P, srcN, srcNname, 4 + li, stageB)
    P.arena_reset(mark)


def final_norm(P, src, srcname, dst, dstname):
    P.S.cur_prio = False
    mark = P.arena_off
    alloc_common(P)
    nfb = P.alloc([D])
    c_nf = P.win("c_nfb", [D])
    P.dma("sp", V(nfb, "nfb"), V(c_nf.partition_broadcast(128), "in_w"), key="const2")
    nblk = P.T // TB
    n = 0
    for blk in range(nblk):
        for s in range(4):
            xi = n % 2
            n += 1
            xt, xo = P.xt[xi], P.xr[xi]
            xtv = V(xt, ("xt", xi))
            P.dma("sp", xtv, V(tile_rows(src, blk, s), (srcname, blk, s)), key=("xt", xi))
            ssv = V(P.ss[xi][:, 0:1], ("ss", xi))
            rv = V(P.rstd[xi][:, 0:1], ("rstd", xi))
            P.act(V(P.junk, "junk"), xtv, AF.Square, accum=ssv)
            P.act(rv, ssv, AF.Sqrt, scale=1.0 / D, bias=V(P.epsv, "epsv"))
            P.recip(rv, rv)
            P.stt(V(xo, ("xr", xi)), xtv, rv, V(nfb, "nfb"), ALU.mult, ALU.mult)
            P.dma("sp", V(tile_rows(dst, blk, s), (dstname, blk, s)), V(xo, ("xr", xi)), key=("xr", xi))
    P.arena_reset(mark)


RET_H = 4
RET_DK = 256
RET_DV = 512


def retnet_pass(P, h0, srcN, srcNname, srcR, srcRname, dst, dstname):
    P.S.cur_prio = True
    mark = P.arena_off
    alloc_common(P)
    nblk = P.T // TB
    T = P.T
    w_in = P.win("ret_w_in_p", [D, 6144])
    w_out = P.win("ret_w_out", [2048, D])
    c_cos = P.win("c_ret_cos", [128, 4096])
    c_sin = P.win("c_ret_sin", [128, 4096])
    c_decT = P.win("c_ret_decT", [128, RET_H, 128])
    c_gl = P.win("c_ret_gl", [RET_H, TB])
    c_kdec = P.win("c_ret_kdec", [128, RET_H])
    wq = P.alloc([KC, 512], BF16)
    wk = P.alloc([KC, 512], BF16)
    wv = P.alloc([KC, 1024], BF16)
    wg = P.alloc([KC, 1024], BF16)
    wo = P.alloc([8, D], BF16)
    cos = P.alloc([TB])
    sin = P.alloc([TB])
    qT = P.alloc([2, 2, TB], BF16)
    kT = P.alloc([2, 2, TB], BF16)
    qg = P.alloc([2, 2, TB], BF16)
    rt = [P.alloc([TB]) for _ in range(4)]
    vt = P.alloc([4, 2, 512], BF16)
    khat = P.alloc([4, 2, 256], BF16)
    sg = P.alloc([8, TB], BF16)
    yT = P.alloc([8, TB], BF16)
    S = P.alloc([2, 2, 512])
    Sbf = P.alloc([2, 2, 512], BF16)
    decT = P.alloc([RET_H, 128])
    gl = P.alloc([2, TB])
    kdec = P.alloc([RET_H])
    PT = [P.alloc([128], BF16) for _ in range(4)]
    ysq = [P.alloc([512], BF16) for _ in range(4)]
    rs = [P.alloc([128]) for _ in range(4)]
    tmp = [P.alloc([4, 128]) for _ in range(4)]
    src = w_in.rearrange("(k p) n -> p k n", p=128)
    P.dma("pool", V(wq, ("w", 0)), V(src[:, :, h0 * 256:h0 * 256 + 512], "in_w"), key=("w", 0))
    P.dma("pool", V(wk, ("w", 1)), V(src[:, :, 1024 + h0 * 256:1024 + h0 * 256 + 512], "in_w"), key=("w", 1))
    P.dma("pool", V(wv, ("w", 2)), V(src[:, :, 2048 + h0 * 512:2048 + h0 * 512 + 1024], "in_w"), key=("w", 2))
    P.dma("pool", V(wg, ("w", 3)), V(src[:, :, 4096 + h0 * 512:4096 + h0 * 512 + 1024], "in_w"), key=("w", 3))
    P.dma("pool", V(wo, ("w", 4)), V(w_out[h0 * 512:h0 * 512 + 1024, :].rearrange("(c p) n -> p c n", p=128), "in_w"),
          key=("w", 4))
    P.dma("sp", V(decT, "decT"), V(c_decT, "in_w"), key="c0")
    P.dma("sp", V(kdec, "kdec"), V(c_kdec, "in_w"), key="c1")
    for hl in range(2):
        P.dma("sp", V(gl[:, hl, :], ("gl", hl)), V(c_gl[h0 + hl].partition_broadcast(128), "in_w"), key=("c2", hl))
    P.memset("dve", V(S, "S"), 0.0)
    P.memset("pool", V(Sbf, "Sbf"), 0.0)
    g128 = [float((1.0 - 2.0 ** (-5.0 - (h0 + hl))) ** 128) for hl in range(2)]
    pn = [0]

    def pbank():
        b = pn[0] % 3
        pn[0] += 1
        return V(P.psb[b], ("psb", b))

    def stageB(blk, slot, tick):
        xnT = P.xnT[slot]
        xv = V(xnT, ("xnT", slot))
        P.dma("sp", V(cos, "cos"), V(c_cos[:, blk * TB:(blk + 1) * TB], "in_w"), key="cos")
        P.dma("sp", V(sin, "sin"), V(c_sin[:, blk * TB:(blk + 1) * TB], "in_w"), key="sin")
        for (wsel, wtok, dstT, dname) in ((wq, ("w", 0), qT, "qT"), (wk, ("w", 1), kT, "kT")):
            for hl in range(2):
                p1 = pbank()
                for kc in range(KC):
                    P.mm(p1, V(wsel[:, kc, hl * 256:hl * 256 + 128], wtok), V(xnT[:, kc, :], ("xnT", slot)),
                         start=(kc == 0), stop=(kc == KC - 1))
                p2 = pbank()
                for kc in range(KC):
                    P.mm(p2, V(wsel[:, kc, hl * 256 + 128:hl * 256 + 256], wtok), V(xnT[:, kc, :], ("xnT", slot)),
                         start=(kc == 0), stop=(kc == KC - 1))
                r = [V(rt[i], ("rt", i)) for i in range(4)]
                P.tt("dve", r[0], p1, V(cos, "cos"), ALU.mult)
                P.tt("dve", r[1], p2, V(sin, "sin"), ALU.mult)
                P.tt("dve", r[2], p2, V(cos, "cos"), ALU.mult)
                P.tt("dve", r[3], p1, V(sin, "sin"), ALU.mult)
                P.tt("pool", V(dstT[:, hl, 0, :], (dname, hl, 0)), r[0], r[1], ALU.subtract)
                P.tt("pool", V(dstT[:, hl, 1, :], (dname, hl, 1)), r[2], r[3], ALU.add)
                if dname == "qT":
                    for e in range(2):
                        P.tt("pool", V(qg[:, hl, e, :], ("qg", hl, e)), V(qT[:, hl, e, :], ("qT", hl, e)),
                             V(gl[:, hl, :], ("gl", hl)), ALU.mult)
        tick()
        for hl in range(2):
            for j in range(4):
                pg = pbank()
                c = hl * 512 + j * 128
                for kc in range(KC):
                    P.mm(pg, V(wg[:, kc, c:c + 128], ("w", 3)), V(xnT[:, kc, :], ("xnT", slot)),
                         start=(kc == 0), stop=(kc == KC - 1))
                P.act(V(sg[:, hl * 4 + j, :], ("sg", hl, j)), pg, AF.Silu)
        tick()
        for c4 in range(4):
            for hl in range(2):
                pv = pbank()
                for kc in range(KC):
                    P.mm(pv, V(xnT[:, kc, c4 * 128:(c4 + 1) * 128], ("xnT", slot)),
                         V(wv[:, kc, hl * 512:(hl + 1) * 512], ("w", 2)), start=(kc == 0), stop=(kc == KC - 1))
                P.copy("act", V(vt[:, c4, hl, :], ("vt", c4, hl)), pv)
        tick()
        psT6 = P.psb[6].bitcast(BF16)
        for c4 in range(4):
            for hl in range(2):
                for e in range(2):
                    P.transpose(V(psT6[:, e * 128:(e + 1) * 128], ("psb", 6)),
                                V(kT[:, hl, e, c4 * 128:(c4 + 1) * 128], ("kT", hl, e)), V(P.ident, "ident"))
                P.act(V(khat[:, c4, hl, :], ("khat", c4, hl)), V(psT6[:, 0:256], ("psb", 6)), AF.Identity,
                      scale=V(kdec[:, h0 + hl:h0 + hl + 1], "kdec"))
        tick()
        n = 0
        for c4 in range(4):
            sl = slice(c4 * 128, (c4 + 1) * 128)
            for hl in range(2):
                h = h0 + hl
                i2 = n % 4
                n += 1
                psS = V(P.psb[3][:, 0:128], ("psb", 3))
                for e in range(2):
                    P.mm(psS, V(kT[:, hl, e, sl], ("kT", hl, e)), V(qT[:, hl, e, sl], ("qT", hl, e)),
                         start=(e == 0), stop=(e == 1))
                ptv = V(PT[i2], ("PT", i2))
                P.tt("dve", ptv, psS, V(decT[:, h, :], "decT"), ALU.mult)
                psO = P.psb[4]
                for j in range(4):
                    po = V(psO[:, j * 128:(j + 1) * 128], ("psb", 4))
                    P.mm(po, V(vt[:, c4, hl, j * 128:(j + 1) * 128], ("vt", c4, hl)), ptv, start=True, stop=False)
                    for e in range(2):
                        P.mm(po, V(Sbf[:, hl, e, j * 128:(j + 1) * 128], ("Sbf", hl, e)),
                             V(qg[:, hl, e, sl], ("qg", hl, e)), start=False, stop=(e == 1))
                pov = V(psO, ("psb", 4))
                yq = V(ysq[i2], ("ysq", i2))
                P.act(yq, pov, AF.Square)
                psN = V(P.psb[3][:, 128:256], ("psb", 3))
                for j in range(4):
                    P.mm(psN, V(P.ones, "ones"), V(ysq[i2][:, j * 128:(j + 1) * 128], ("ysq", i2)),
                         start=(j == 0), stop=(j == 3))
                rv = V(rs[i2], ("rs", i2))
                P.act(rv, psN, AF.Sqrt, scale=1.0 / RET_DV, bias=V(P.epsv, "epsv"))
                P.recip(rv, rv)
                tv = V(tmp[i2], ("tmp", i2))
                P.tt("dve", tv, V(psO.rearrange("p (j l) -> p j l", j=4), ("psb", 4)),
                     V(rs[i2].unsqueeze(1).broadcast_to([128, 4, 128]), ("rs", i2)), ALU.mult)
                P.tt("pool", V(yT[:, hl * 4:(hl + 1) * 4, sl], ("yT", hl, c4)), tv,
                     V(sg[:, hl * 4:(hl + 1) * 4, sl], ("sg", hl)), ALU.mult)
                for e in range(2):
                    pu = V(P.psb[5], ("psb", 5))
                    P.mm(pu, V(khat[:, c4, hl, e * 128:(e + 1) * 128], ("khat", c4, hl)),
                         V(vt[:, c4, hl, :], ("vt", c4, hl)), start=True, stop=True)
                    sv = V(S[:, hl, e, :], ("S", hl, e))
                    P.stt(sv, sv, g128[hl], pu, ALU.mult, ALU.add)
                    P.copy("pool", V(Sbf[:, hl, e, :], ("Sbf", hl, e)), sv)
        out_stage(P, blk, srcR, srcRname, dst, dstname,
                  lambda k, s: V(yT[:, k, s * 128:(s + 1) * 128], ("yT",)), 8, wo, ("w", 4))

    run_blocks(P, srcN, srcNname, 3, stageB)
    P.arena_reset(mark)


GLA_H = 4
GLA_DK = 128
GLA_DV = 256


def gla_pass(P, srcN, srcNname, srcR, srcRname, dst, dstname):
    P.S.cur_prio = True
    mark = P.arena_off
    alloc_common(P)
    nblk = P.T // TB
    w_in = P.win("gla_w_in", [D, 3088])
    w_out = P.win("gla_w_out", [D, D])
    w_gk2 = P.win("gla_w_gk2", [16, 512])
    c_bgk = P.win("c_gla_bgk", [128, GLA_H])
    c_nw = P.win("c_gla_nw", [128, 2])
    c_maskT = P.win("c_maskT", [128, 128])
    c_scanm = P.win("c_scanm", [TB])
    wq = P.alloc([KC, 512], BF16)
    wk = P.alloc([KC, 512], BF16)
    wv = P.alloc([KC, 1024], BF16)
    wg = P.alloc([KC, 1024], BF16)
    wgk = P.alloc([KC, 16], BF16)
    wo = P.alloc([8, D], BF16)
    wgk2 = P.alloc([512])
    bgk = P.alloc([GLA_H])
    nbgk = P.alloc([GLA_H])
    nw = P.alloc([2])
    maskT = P.alloc([128])
    scanm = P.alloc([TB])
    gkf = P.alloc([TB])
    Gp = P.alloc([GLA_H, TB])
    et = [P.alloc([TB]) for _ in range(2)]
    qT = P.alloc([GLA_H, TB], BF16)
    kT = P.alloc([GLA_H, TB], BF16)
    vt = P.alloc([4, 1024], BF16)
    khat = P.alloc([4, GLA_H, 128], BF16)
    sg = P.alloc([8, TB], BF16)
    yT = P.alloc([8, TB], BF16)
    S = P.alloc([GLA_H, 256])
    Sbf = P.alloc([GLA_H, 256], BF16)
    elast = P.alloc([GLA_H, 4])
    PT = [P.alloc([128], BF16) for _ in range(4)]
    ysq = [P.alloc([256], BF16) for _ in range(4)]
    rs = [P.alloc([128]) for _ in range(4)]
    tmp = [P.alloc([2, 128]) for _ in range(4)]
    src = w_in.rearrange("(k p) n -> p k n", p=128)
    P.dma("pool", V(wq, ("w", 0)), V(src[:, :, 0:512], "in_w"), key=("w", 0))
    P.dma("pool", V(wk, ("w", 1)), V(src[:, :, 512:1024], "in_w"), key=("w", 1))
    P.dma("pool", V(wv, ("w", 2)), V(src[:, :, 1024:2048], "in_w"), key=("w", 2))
    P.dma("pool", V(wg, ("w", 3)), V(src[:, :, 2048:3072], "in_w"), key=("w", 3))
    P.dma("pool", V(wgk, ("w", 5)), V(src[:, :, 3072:3088], "in_w"), key=("w", 5))
    P.dma("pool", V(wo, ("w", 4)), V(w_out.rearrange("(c p) n -> p c n", p=128), "in_w"), key=("w", 4))
    P.dma("sp", V(wgk2[0:16, :], "wgk2"), V(w_gk2, "in_w"), key="c0")
    P.dma("sp", V(bgk, "bgk"), V(c_bgk, "in_w"), key="c1")
    P.dma("sp", V(nw, "nw"), V(c_nw, "in_w"), key="c2")
    P.dma("sp", V(maskT, "maskT"), V(c_maskT, "in_w"), key="c3")
    P.dma("sp", V(scanm, "scanm"), V(c_scanm.partition_broadcast(128), "in_w"), key="c4")
    P.ts("dve", V(nbgk, "nbgk"), V(bgk, "bgk"), -1.0, ALU.mult)
    P.memset("dve", V(S, "S"), 0.0)
    P.memset("pool", V(Sbf, "Sbf"), 0.0)
    lnsc = float(np.log(GLA_DK ** -0.5))
    pn = [0]

    def pbank():
        b = pn[0] % 3
        pn[0] += 1
        return V(P.psb[b], ("psb", b))

    def stageB(blk, slot, tick):
        xnT = P.xnT[slot]
        xtok = ("xnT", slot)
        pg = pbank()
        for kc in range(KC):
            P.mm(V(pg.ap[0:16, :], pg.tok), V(wgk[:, kc, :], ("w", 5)), V(xnT[:, kc, :], xtok),
                 start=(kc == 0), stop=(kc == KC - 1))
        P.copy("act", V(gkf[0:16, :], "gkf"), V(pg.ap[0:16, :], pg.tok))
        for h in range(GLA_H):
            pp = pbank()
            P.mm(pp, V(wgk2[0:16, h * 128:(h + 1) * 128], "wgk2"), V(gkf[0:16, :], "gkf"), start=True, stop=True)
            e0 = V(et[0], ("et", 0))
            P.act(e0, pp, AF.Exp, scale=-1.0, bias=V(nbgk[:, h:h + 1], "nbgk"))
            P.act(e0, e0, AF.Ln, scale=1.0, bias=1.0)
            gph = V(Gp[:, h, :], ("Gp", h))
            nc = P.nc
            P.S.op("dve", (lambda o=gph.ap, a=scanm, b=et[0]: nc.vector.tensor_tensor_scan(o, a, b, 0.0, ALU.mult, ALU.add)),
                   reads=[("scanm",), ("et", 0)], writes=[gph.tok])
            pq = pbank()
            for kc in range(KC):
                P.mm(pq, V(wq[:, kc, h * 128:(h + 1) * 128], ("w", 0)), V(xnT[:, kc, :], xtok),
                     start=(kc == 0), stop=(kc == KC - 1))
            e1 = V(et[1], ("et", 1))
            P.act(e1, gph, AF.Exp, scale=-1.0 / 16.0, bias=lnsc)
            P.tt("dve", V(qT[:, h, :], ("qT", h)), pq, e1, ALU.mult)
            pk = pbank()
            for kc in range(KC):
                P.mm(pk, V(wk[:, kc, h * 128:(h + 1) * 128], ("w", 1)), V(xnT[:, kc, :], xtok),
                     start=(kc == 0), stop=(kc == KC - 1))
            P.act(e1, gph, AF.Exp, scale=1.0 / 16.0)
            P.tt("dve", V(kT[:, h, :], ("kT", h)), pk, e1, ALU.mult)
            P.act(V(elast[:, h, :], ("elast", h)),
                  V(Gp[:, h, :].rearrange("p (c l) -> p c l", c=4)[:, :, 127], ("Gp", h)), AF.Exp, scale=-1.0 / 16.0)
        tick()
        for c in range(8):
            pg2 = pbank()
            for kc in range(KC):
                P.mm(pg2, V(wg[:, kc, c * 128:(c + 1) * 128], ("w", 3)), V(xnT[:, kc, :], xtok),
                     start=(kc == 0), stop=(kc == KC - 1))
            P.act(V(sg[:, c, :], ("sg", c)), pg2, AF.Silu)
            P.ts("pool", V(sg[:, c, :], ("sg", c)), V(sg[:, c, :], ("sg", c)), V(nw[:, (c % 2):(c % 2) + 1], "nw"), ALU.mult)
        tick()
        for c4 in range(4):
            for half in range(2):
                pv = pbank()
                for kc in range(KC):
                    P.mm(pv, V(xnT[:, kc, c4 * 128:(c4 + 1) * 128], xtok),
                         V(wv[:, kc, half * 512:(half + 1) * 512], ("w", 2)), start=(kc == 0), stop=(kc == KC - 1))
                P.copy("act", V(vt[:, c4, half * 512:(half + 1) * 512], ("vt", c4, half)), pv)
        tick()
        psT6 = P.psb[6].bitcast(BF16)
        for c4 in range(4):
            for h in range(GLA_H):
                P.transpose(V(psT6[:, h * 128:(h + 1) * 128], ("psb", 6)),
                            V(kT[:, h, c4 * 128:(c4 + 1) * 128], ("kT", h)), V(P.ident, "ident"))
            P.copy("act", V(khat[:, c4, :, :], ("khat", c4)),
                   V(psT6[:, 0:512].rearrange("p (h d) -> p h d", h=GLA_H), ("psb", 6)))
        tick()
        n = 0
        for c4 in range(4):
            sl = slice(c4 * 128, (c4 + 1) * 128)
            for h in range(GLA_H):
                i2 = n % 4
                n += 1
                psS = V(P.psb[3][:, 0:128], ("psb", 3))
                P.mm(psS, V(kT[:, h, sl], ("kT", h)), V(qT[:, h, sl], ("qT", h)), start=True, stop=True)
                ptv = V(PT[i2], ("PT", i2))
                P.tt("dve", ptv, psS, V(maskT, "maskT"), ALU.mult)
                psO = P.psb[4]
                for j in range(2):
                    po = V(psO[:, j * 128:(j + 1) * 128], ("psb", 4))
                    vc = h * 256 + j * 128
                    P.mm(po, V(vt[:, c4, vc:vc + 128], ("vt", c4, vc // 512)), ptv, start=True, stop=False)
                    P.mm(po, V(Sbf[:, h, j * 128:(j + 1) * 128], ("Sbf", h)), V(qT[:, h, sl], ("qT", h)),
                         start=False, stop=True)
                pov = V(psO[:, 0:256], ("psb", 4))
                yq = V(ysq[i2], ("ysq", i2))
                P.act(yq, pov, AF.Square)
                psN = V(P.psb[3][:, 128:256], ("psb", 3))
                for j in range(2):
                    P.mm(psN, V(P.ones, "ones"), V(ysq[i2][:, j * 128:(j + 1) * 128], ("ysq", i2)),
                         start=(j == 0), stop=(j == 1))
                rv = V(rs[i2], ("rs", i2))
                P.act(rv, psN, AF.Sqrt, scale=1.0 / GLA_DV, bias=V(P.epsv, "epsv"))
                P.recip(rv, rv)
                tv = V(tmp[i2], ("tmp", i2))
                P.tt("dve", tv, V(psO[:, 0:256].rearrange("p (j l) -> p j l", j=2), ("psb", 4)),
                     V(rs[i2].unsqueeze(1).broadcast_to([128, 2, 128]), ("rs", i2)), ALU.mult)
                P.tt("pool", V(yT[:, h * 2:(h + 1) * 2, sl], ("yT", h, c4)), tv,
                     V(sg[:, h * 2:(h + 1) * 2, sl], ("sg",)), ALU.mult)
                pu = V(P.psb[5][:, 0:256], ("psb", 5))
                P.mm(pu, V(khat[:, c4, h, :], ("khat", c4)), V(vt[:, c4, h * 256:(h + 1) * 256], ("vt", c4, h // 2)),
                     start=True, stop=True)
                sv = V(S[:, h, :], ("S", h))
                P.tt("dve", sv, sv, pu, ALU.add)
                P.ts("dve", sv, sv, V(elast[:, h, c4:c4 + 1], ("elast", h)), ALU.mult)
                P.copy("pool", V(Sbf[:, h, :], ("Sbf", h)), sv)
        out_stage(P, blk, srcR, srcRname, dst, dstname,
                  lambda k, s: V(yT[:, k, s * 128:(s + 1) * 128], ("yT",)), 8, wo, ("w", 4))

    run_blocks(P, srcN, srcNname, 2, stageB)
    P.arena_reset(mark)


def ssd_pass(P, p, srcN, srcNname, srcR, srcRname, dst, dstname):
    P.S.cur_prio = True
    mark = P.arena_off
    alloc_common(P)
    nc = P.nc
    NSL = 4
    nblk = P.T // TB
    w_in = P.win("ssd_w_in", [D, 6176])
    w_out = P.win("ssd_w_out", [2048, D])
    c_cw = P.win("c_ssd_cw", [128, 32, 4])
    c_cb = P.win("c_ssd_cb", [128, 32])
    c_dtb = P.win("ssd_dt_bias", [32])
    c_alog = P.win("ssd_a_log", [32])
    c_dsk = P.win("ssd_d", [32])
    c_nw = P.win("ssd_norm_w", [2048])
    c_maskT = P.win("c_maskT", [128, 128])
    c_SU = P.win("c_SU", [128, 128])
    wz = P.alloc([KC, 1024], BF16)
    wxs = P.alloc([KC, 1024], BF16)
    wB = P.alloc([KC, 512], BF16)
    wC = P.alloc([KC, 512], BF16)
    wdt = P.alloc([KC, 16], BF16)
    wo = P.alloc([8, D], BF16)
    cw = P.alloc([32, 4])
    cb = P.alloc([32])
    spill = P.alloc([16, 3])
    dtb = P.alloc([16])
    abc = P.alloc([16])
    dsk = P.alloc([16])
    nwc = P.alloc([1024])
    maskT = P.alloc([128])
    SU = P.alloc([128])
    onesf = P.alloc([128])
    A = [P.alloc([TB + 3]) for _ in range(3)]
    xsT = P.alloc([8, TB], BF16)
    kT = P.alloc([4, TB], BF16)
    qT = P.alloc([4, TB], BF16)
    sz = P.alloc([4, 1024], BF16)
    dtt = P.alloc([16])
    ld = P.alloc([16])
    Gs = P.alloc([16])
    eG = P.alloc([16])
    eGl = P.alloc([16])
    wdec = P.alloc([16])
    tiny = P.alloc([16])
    R = [P.alloc([4, 128]) for _ in range(NSL)]
    dec = [P.alloc([4, 128]) for _ in range(NSL)]
    sm = [P.alloc([128]) for _ in range(NSL)]
    PT = [P.alloc([4, 128], BF16) for _ in range(NSL)]
    xk = [P.alloc([384], BF16) for _ in range(NSL)]
    vv = [P.alloc([256], BF16) for _ in range(NSL)]
    vh = [P.alloc([256], BF16) for _ in range(NSL)]
    ot = [P.alloc([256]) for _ in range(NSL)]
    t2 = [P.alloc([256]) for _ in range(NSL)]
    yv = [P.alloc([256]) for _ in range(NSL)]
    yn = [P.alloc([256], BF16) for _ in range(NSL)]
    ssq = [P.alloc([1]) for _ in range(NSL)]
    yT = P.alloc([8, TB], BF16)
    S = P.alloc([4, 256])
    Sbf = P.alloc([4, 256], BF16)
    src = w_in.rearrange("(k p) n -> p k n", p=128)
    P.dma("pool", V(wz, ("w", 0)), V(src[:, :, p * 1024:(p + 1) * 1024], "in_w"), key=("w", 0))
    P.dma("pool", V(wxs, ("w", 1)), V(src[:, :, 2048 + p * 1024:2048 + (p + 1) * 1024], "in_w"), key=("w", 1))
    P.dma("pool", V(wB, ("w", 2)), V(src[:, :, 4096 + p * 512:4096 + (p + 1) * 512], "in_w"), key=("w", 2))
    P.dma("pool", V(wC, ("w", 3)), V(src[:, :, 5120 + p * 512:5120 + (p + 1) * 512], "in_w"), key=("w", 3))
    P.dma("pool", V(wdt, ("w", 5)), V(src[:, :, 6144 + p * 16:6144 + (p + 1) * 16], "in_w"), key=("w", 5))
    P.dma("pool", V(wo, ("w", 4)), V(w_out[p * 1024:(p + 1) * 1024, :].rearrange("(c p) n -> p c n", p=128), "in_w"),
          key=("w", 4))
    P.dma("sp", V(cw, "cw"), V(c_cw, "in_w"), key="c0")
    P.dma("sp", V(cb, "cb"), V(c_cb, "in_w"), key="c1")
    P.dma("sp", V(dtb, "dtb"), V(c_dtb[p * 16:(p + 1) * 16].partition_broadcast(128), "in_w"), key="c2")
    P.dma("sp", V(abc, "abc"), V(c_alog[p * 16:(p + 1) * 16].partition_broadcast(128), "in_w"), key="c3")
    P.dma("sp", V(dsk, "dsk"), V(c_dsk[p * 16:(p + 1) * 16].partition_broadcast(128), "in_w"), key="c4")
    P.dma("sp", V(nwc, "nwc"), V(c_nw[p * 1024:(p + 1) * 1024].partition_broadcast(128), "in_w"), key="c5")
    P.dma("sp", V(maskT, "maskT"), V(c_maskT, "in_w"), key="c6")
    P.dma("sp", V(SU, "SU"), V(c_SU, "in_w"), key="c7")
    P.memset("pool", V(onesf, "onesf"), 1.0)
    P.memset("pool", V(spill, "spill"), 0.0)
    P.act(V(abc, "abc"), V(abc, "abc"), AF.Exp)
    P.ts("dve", V(abc, "abc"), V(abc, "abc"), -1.0, ALU.mult)
    P.memset("dve", V(S, "S"), 0.0)
    P.memset("pool", V(Sbf, "Sbf"), 0.0)
    pn = [0]

    def pbank():
        b = pn[0] % 3
        pn[0] += 1
        return V(P.psb[b], ("psb", b))

    def conv_chunk(lc, wsel, wtok, col, xnT, slot, dst):
        cc = (8 * p + lc) if lc < 8 else ((16 + 4 * p + lc - 8) if lc < 12 else (24 + 4 * p + lc - 12))
        pu = pbank()
        for kc in range(KC):
            P.mm(pu, V(wsel[:, kc, col:col + 128], wtok), V(xnT[:, kc, :], ("xnT", slot)),
                 start=(kc == 0), stop=(kc == KC - 1))
        ai = lc % 3
        At, atok = A[ai], ("A", ai)
        P.act(V(At[:, 0:TB], atok), pu, AF.Identity, scale=V(cw[:, cc, 3:4], "cw"), bias=V(cb[:, cc:cc + 1], "cb"))
        P.memset("pool", V(At[:, TB:TB + 3], atok), 0.0)
        for sh in (1, 2, 3):
            P.stt(V(At[:, sh:TB + sh], atok), pu, V(cw[:, cc, 3 - sh:4 - sh], "cw"), V(At[:, sh:TB + sh], atok),
                  ALU.mult, ALU.add)
        P.tt("pool", V(At[:, 0:3], atok), V(At[:, 0:3], atok), V(spill[:, lc, :], ("spill", lc)), ALU.add)
        P.copy("pool", V(spill[:, lc, :], ("spill", lc)), V(At[:, TB:TB + 3], atok))
        P.act(dst, V(At[:, 0:TB], atok), AF.Silu)

    def stageB(blk, slot, tick):
        xnT = P.xnT[slot]
        xtok = ("xnT", slot)
        for lc in range(8):
            conv_chunk(lc, wxs, ("w", 1), lc * 128, xnT, slot, V(xsT[:, lc, :], ("xsT", lc)))
        tick()
        for gl in range(4):
            conv_chunk(8 + gl, wB, ("w", 2), gl * 128, xnT, slot, V(kT[:, gl, :], ("kT", gl)))
            conv_chunk(12 + gl, wC, ("w", 3), gl * 128, xnT, slot, V(qT[:, gl, :], ("qT", gl)))
        tick()
        for c4 in range(4):
            for half in range(2):
                pz = pbank()
                for kc in range(KC):
                    P.mm(pz, V(xnT[:, kc, c4 * 128:(c4 + 1) * 128], xtok),
                         V(wz[:, kc, half * 512:(half + 1) * 512], ("w", 0)), start=(kc == 0), stop=(kc == KC - 1))
                P.act(V(sz[:, c4, half * 512:(half + 1) * 512], ("sz", c4, half)), pz, AF.Silu)
        tick()
        n = 0
        psT6 = P.psb[6].bitcast(BF16)
        for c4 in range(4):
            sl = slice(c4 * 128, (c4 + 1) * 128)
            if c4 == 2:
                tick()
            pdt = V(P.psb[4][:, 128:144], ("psb", 4, "d"))
            for kc in range(KC):
                P.mm(pdt, V(xnT[:, kc, sl], xtok), V(wdt[:, kc, :], ("w", 5)), start=(kc == 0), stop=(kc == KC - 1))
            tn = V(tiny, "tiny")
            P.tt("dve", tn, pdt, V(dtb, "dtb"), ALU.add)
            P.act(tn, tn, AF.Exp)
            P.act(V(dtt, "dtt"), tn, AF.Ln, scale=1.0, bias=1.0)
            P.tt("dve", V(ld, "ld"), V(dtt, "dtt"), V(abc, "abc"), ALU.mult)
            pG = V(P.psb[4][:, 144:160], ("psb", 4, "d"))
            P.mm(pG, V(maskT, "maskT"), V(ld, "ld"), start=True, stop=True)
            pGl = V(P.psb[4][:, 160:176], ("psb", 4, "d"))
            P.mm(pGl, V(onesf, "onesf"), V(ld, "ld"), start=True, stop=True)
            P.act(V(Gs, "Gs"), pG, AF.Identity)
            P.act(V(eG, "eG"), pG, AF.Exp)
            P.act(V(eGl, "eGl"), pGl, AF.Exp)
            P.tt("dve", tn, pGl, V(Gs, "Gs"), ALU.subtract)
            P.act(V(wdec, "wdec"), tn, AF.Exp)
            for gl in range(4):
                i2 = n % NSL
                n += 1
                hs = slice(gl * 4, gl * 4 + 4)
                Rv = V(R[i2], ("R", i2))
                P.tt("dve", Rv, V(maskT.unsqueeze(1).broadcast_to([128, 4, 128]), "maskT"),
                     V(ld[:, hs].unsqueeze(2).broadcast_to([128, 4, 128]), "ld"), ALU.mult)
                pSeg = V(P.psb[3], ("psb", 3))
                P.mm(pSeg, V(SU, "SU"), V(R[i2].rearrange("p h l -> p (h l)"), ("R", i2)), start=True, stop=True)
                dv_ = V(dec[i2], ("dec", i2))
                P.act(V(dec[i2].rearrange("p h l -> p (h l)"), ("dec", i2)), pSeg, AF.Exp)
                pS = V(P.psb[4][:, 0:128], ("psb", 4, "s"))
                P.mm(pS, V(kT[:, gl, sl], ("kT", gl)), V(qT[:, gl, sl], ("qT", gl)), start=True, stop=True)
                smv = V(sm[i2], ("sm", i2))
                P.tt("dve", smv, pS, V(maskT, "maskT"), ALU.mult)
                ptv = V(PT[i2], ("PT", i2))
                P.tt("pool", ptv, dv_, V(sm[i2].unsqueeze(1).broadcast_to([128, 4, 128]), ("sm", i2)), ALU.mult)
                P.transpose(V(psT6[:, 0:128], ("psb", 6, "a")), V(xsT[:, gl * 2, sl], ("xsT", gl * 2)), V(P.ident, "ident"))
                P.transpose(V(psT6[:, 128:256], ("psb", 6, "a")), V(xsT[:, gl * 2 + 1, sl], ("xsT", gl * 2 + 1)),
                            V(P.ident, "ident"))
                P.transpose(V(psT6[:, 256:384], ("psb", 6, "a")), V(kT[:, gl, sl], ("kT", gl)), V(P.ident, "ident"))
                xkv = V(xk[i2], ("xk", i2))
                P.copy("act", xkv, V(psT6[:, 0:384], ("psb", 6, "a")))
                xs4 = V(xk[i2][:, 0:256].rearrange("p (h d) -> p h d", h=4), ("xk", i2))
                v4 = V(vv[i2].rearrange("p (h d) -> p h d", h=4), ("vv", i2))
                P.tt("dve", v4, xs4, V(dtt[:, hs].unsqueeze(2).broadcast_to([128, 4, 64]), "dtt"), ALU.mult)
                vh4 = V(vh[i2].rearrange("p (h d) -> p h d", h=4), ("vh", i2))
                P.tt("pool", vh4, v4, V(wdec[:, hs].unsqueeze(2).broadcast_to([128, 4, 64]), "wdec"), ALU.mult)
                for hh in range(4):
                    P.mm(V(P.psb[5][:, hh * 64:(hh + 1) * 64], ("psb", 5, "a")), V(PT[i2][:, hh, :], ("PT", i2)),
                         V(vv[i2][:, hh * 64:(hh + 1) * 64], ("vv", i2)), start=True, stop=True)
                pB = V(P.psb[5][:, 256:512], ("psb", 5, "b"))
                P.mm(pB, V(qT[:, gl, sl], ("qT", gl)), V(Sbf[:, gl, :], ("Sbf", gl)), start=True, stop=True)
                o4 = V(ot[i2].rearrange("p (h d) -> p h d", h=4), ("ot", i2))
                P.tt("dve", o4, V(P.psb[5][:, 256:512].rearrange("p (h d) -> p h d", h=4), ("psb", 5, "b")),
                     V(eG[:, hs].unsqueeze(2).broadcast_to([128, 4, 64]), "eG"), ALU.mult)
                ov = V(ot[i2], ("ot", i2))
                P.tt("dve", ov, ov, V(P.psb[5][:, 0:256], ("psb", 5, "a")), ALU.add)
                t24 = V(t2[i2].rearrange("p (h d) -> p h d", h=4), ("t2", i2))
                P.tt("pool", t24, xs4, V(dsk[:, hs].unsqueeze(2).broadcast_to([128, 4, 64]), "dsk"), ALU.mult)
                P.tt("pool", ov, ov, V(t2[i2], ("t2", i2)), ALU.add)
                yvv = V(yv[i2], ("yv", i2))
                P.tt("pool", yvv, ov, V(sz[:, c4, gl * 256:(gl + 1) * 256], ("sz", c4, gl // 2)), ALU.mult)
                sq = V(ssq[i2], ("ssq", i2))
                P.act(V(P.junk[:, 0:256], "junk"), yvv, AF.Square, accum=sq)
                P.act(sq, sq, AF.Sqrt, scale=1.0 / 256.0, bias=V(P.epsv, "epsv"))
                P.recip(sq, sq)
                ynv = V(yn[i2], ("yn", i2))
                P.stt(ynv, yvv, sq, V(nwc[:, gl * 256:(gl + 1) * 256], "nwc"), ALU.mult, ALU.mult)
                for j in range(2):
                    P.transpose(V(psT6[:, 512 + j * 128:512 + (j + 1) * 128], ("psb", 6, "b")),
                                V(yn[i2][:, j * 128:(j + 1) * 128], ("yn", i2)), V(P.ident, "ident"))
                P.copy("act", V(yT[:, gl * 2:(gl + 1) * 2, sl], ("yT", gl, c4)),
                       V(psT6[:, 512:768].rearrange("p (j l) -> p j l", j=2), ("psb", 6, "b")))
                pU = V(P.psb[4][:, 256:512], ("psb", 4, "u"))
                P.mm(pU, V(xk[i2][:, 256:384], ("xk", i2)), V(vh[i2], ("vh", i2)), start=True, stop=True)
                s4 = V(S[:, gl, :].rearrange("p (h d) -> p h d", h=4), ("S", gl))
                P.tt("dve", s4, s4, V(eGl[:, hs].unsqueeze(2).broadcast_to([128, 4, 64]), "eGl"), ALU.mult)
                sv = V(S[:, gl, :], ("S", gl))
                P.tt("dve", sv, sv, pU, ALU.add)
                P.copy("pool", V(Sbf[:, gl, :], ("Sbf", gl)), sv)
        out_stage(P, blk, srcR, srcRname, dst, dstname,
                  lambda k, s: V(yT[:, k, s * 128:(s + 1) * 128], ("yT",)), 8, wo, ("w", 4))

    run_blocks(P, srcN, srcNname, 0, stageB)
    P.arena_reset(mark)


RW_C = 64
RW_GN_EPS = 64e-5


def rwkv_pass(P, p, srcN, srcNname, srcR, srcRname, dst, dstname):
    P.S.cur_prio = True
    mark = P.arena_off
    alloc_common(P)
    nc = P.nc
    nblk = P.T // TB
    NK = 4
    c0 = p * 512
    w_rkv = P.win("rwkv_w_rkv", [3, D, D])
    w_out = P.win("rwkv_w_out", [D, D])
    w1d, a1d, g1d = P.win("rwkv_w1", [D, 64]), P.win("rwkv_a1", [D, 64]), P.win("rwkv_g1", [D, 160])
    w2d, a2d, g2d = P.win("rwkv_w2", [64, D]), P.win("rwkv_a2", [64, D]), P.win("rwkv_g2", [160, D])
    c_vec = P.win("c_rwkv_vec", [128, 12, KC])
    c_lnw, c_lnb = P.win("rwkv_ln_w", [D]), P.win("rwkv_ln_b", [D])
    c_m = P.win("c_rwkv_masks", [64, 4, 64])
    c_E = P.win("c_rwkv_E", [128, 2], BF16)
    c_bo = P.win("c_rwkv_bo", [128, 128], BF16)
    c_scm = P.win("c_rwkv_scanm", [TB])
    Wr = P.alloc([KC, 512], BF16)
    Wk = P.alloc([KC, 512], BF16)
    Wv = P.alloc([KC, 512], BF16)
    wo = P.alloc([NK, D], BF16)
    w1 = P.alloc([KC, 64], BF16)
    a1 = P.alloc([KC, 64], BF16)
    g1 = P.alloc([KC, 160], BF16)
    w2 = P.alloc([512], BF16)
    a2 = P.alloc([512], BF16)
    g2A = P.alloc([512], BF16)
    g2B = P.alloc([512], BF16)
    vec = P.alloc([12, KC])
    omka = P.alloc([KC])
    nw0 = P.alloc([KC])
    lnw = P.alloc([512])
    lnb = P.alloc([512])
    msk = P.alloc([4, 64])
    identb = P.alloc([64], BF16)
    Eh = P.alloc([2], BF16)
    bo = P.alloc([128], BF16)
    scm = P.alloc([TB])
    epsg = P.alloc([1])
    tinyb = P.alloc([1])
    xx = P.alloc([KC, TB], BF16)
    xi = [P.alloc([KC, TB], BF16) for _ in range(2)]
    xlast = P.alloc([KC], BF16)
    h1 = P.alloc([TB], BF16)
    ha = P.alloc([TB], BF16)
    hgA = P.alloc([TB], BF16)
    hgB = P.alloc([TB], BF16)
    aTm = [P.alloc([NK, TB], BF16) for _ in range(2)]
    rTm = [P.alloc([NK, TB], BF16) for _ in range(2)]
    bT = P.alloc([NK, TB], BF16)
    kT = P.alloc([NK, TB], BF16)
    rkT = P.alloc([NK, TB], BF16)
    yT = P.alloc([NK, TB], BF16)
    WC = P.alloc([NK, 8])
    f32t = [P.alloc([TB]) for _ in range(8)]
    NS = 2
    LmS = [[P.alloc([8, 64], BF16) for _ in range(2)] for _ in range(NS)]
    LTmS = [[P.alloc([8, 64], BF16) for _ in range(2)] for _ in range(NS)]
    XTS = [[P.alloc([8, 64], BF16) for _ in range(2)] for _ in range(NS)]
    XTF = [P.alloc([8, 64], BF16) for _ in range(NS)]
    AkT = [P.alloc([8, 64], BF16) for _ in range(NS)]
    ArbT = [P.alloc([8, 64], BF16) for _ in range(NS)]
    ArkT = [P.alloc([8, 64], BF16) for _ in range(NS)]
    Zs = P.alloc([512], BF16)
    Us = P.alloc([512], BF16)
    Vtm = [P.alloc([512], BF16) for _ in range(2)]
    BKtm = P.alloc([2, 512], BF16)
    yc = P.alloc([512])
    sq = P.alloc([512])
    bon = P.alloc([512])
    ytm = P.alloc([512], BF16)
    st8 = [P.alloc([8]) for _ in range(4)]
    S = P.alloc([NK, 64])
    Sbf = P.alloc([NK, 64], BF16)
    wsrc = lambda i_: w_rkv[i_].rearrange("(k p) n -> p k n", p=128)
    P.dma("pool", V(Wr, ("w", 0)), V(wsrc(0)[:, :, c0:c0 + 512], "in_w"), key=("w", 0))
    P.dma("pool", V(Wk, ("w", 1)), V(wsrc(1)[:, :, c0:c0 + 512], "in_w"), key=("w", 1))
    P.dma("pool", V(Wv, ("w", 2)), V(wsrc(2)[:, :, c0:c0 + 512], "in_w"), key=("w", 2))
    P.dma("pool", V(wo, ("w", 3)), V(w_out[c0:c0 + 512, :].rearrange("(c p) n -> p c n", p=128), "in_w"), key=("w", 3))
    P.dma("pool", V(w1, ("w", 4)), V(w1d.rearrange("(k p) n -> p k n", p=128), "in_w"), key=("w", 4))
    P.dma("pool", V(a1, ("w", 5)), V(a1d.rearrange("(k p) n -> p k n", p=128), "in_w"), key=("w", 5))
    P.dma("pool", V(g1, ("w", 6)), V(g1d.rearrange("(k p) n -> p k n", p=128), "in_w"), key=("w", 6))
    P.dma("pool", V(w2[0:64, :], ("w", 7)), V(w2d[:, c0:c0 + 512], "in_w"), key=("w", 7))
    P.dma("pool", V(a2[0:64, :], ("w", 8)), V(a2d[:, c0:c0 + 512], "in_w"), key=("w", 8))
    P.dma("pool", V(g2A, ("w", 9)), V(g2d[0:128, c0:c0 + 512], "in_w"), key=("w", 9))
    P.dma("pool", V(g2B[0:32, :], ("w", 10)), V(g2d[128:160, c0:c0 + 512], "in_w"), key=("w", 10))
    P.dma("sp", V(vec, "vec"), V(c_vec, "in_w"), key="c0")
    P.dma("sp", V(lnw[0:64, :], "lnw"), V(c_lnw[c0:c0 + 512].partition_broadcast(64), "in_w"), key="c1")
    P.dma("sp", V(lnb[0:64, :], "lnb"), V(c_lnb[c0:c0 + 512].partition_broadcast(64), "in_w"), key="c2")
    P.dma("sp", V(msk[0:64, :, :], "msk"), V(c_m, "in_w"), key="c3")
    P.dma("sp", V(Eh, "Eh"), V(c_E, "in_w"), key="c4")
    P.dma("sp", V(bo, "bo"), V(c_bo, "in_w"), key="c5")
    P.dma("sp", V(scm, "scm"), V(c_scm.partition_broadcast(128), "in_w"), key="c6")
    P.ts("dve", V(omka, "omka"), V(vec[:, 9, :], "vec"), -1.0, ALU.mult, 1.0, ALU.add)
    P.ts("dve", V(nw0, "nw0"), V(vec[:, 6, :], "vec"), -1.0, ALU.mult)
    P.copy("dve", V(identb[0:64, :], "identb"), V(msk[0:64, 3, :], "msk"))
    P.memset("pool", V(epsg, "epsg"), RW_GN_EPS)
    P.memset("pool", V(tinyb, "tinyb"), 1e-24)
    P.memset("pool", V(xlast, "xlast"), 0.0)
    P.memset("dve", V(S, "S"), 0.0)
    P.memset("pool", V(Sbf, "Sbf"), 0.0)
    for e_ in range(2):
        P.memset("pool", V(aTm[e_], ("aT", e_)), 0.0)
        P.memset("pool", V(rTm[e_], ("rT", e_)), 0.0)
    pn = [0]

    def pbank():
        b = pn[0] % 3
        pn[0] += 1
        return V(P.psb[b], ("psb", b))

    def mask(i_):
        return V(msk[0:64, i_, :].unsqueeze(1).broadcast_to([64, 8, 64]), "msk")

    def vcol(i_, kc):
        return V(vec[:, i_, kc:kc + 1], "vec")

    def mix(i_, xnT, xtok, buf):
        o = xi[buf]
        for kc in range(KC):
            if kc % 2 == 0:
                P.stt(V(o[:, kc, :], ("xi", buf, kc)), V(xx[:, kc, :], ("xx", kc)), vcol(i_, kc),
                      V(xnT[:, kc, :], xtok), ALU.mult, ALU.add)
            else:
                P.ts("pool", V(o[:, kc, :], ("xi", buf, kc)), V(xx[:, kc, :], ("xx", kc)), vcol(i_, kc), ALU.mult)
                P.tt("pool", V(o[:, kc, :], ("xi", buf, kc)), V(o[:, kc, :], ("xi", buf, kc)), V(xnT[:, kc, :], xtok), ALU.add)
        return o, ("xi", buf)

    def stage1(blk, slot, tick):
        xnT = P.xnT[slot]
        xtok = ("xnT", slot)
        P.tt("dve", V(xx[:, :, 1:TB], "xx"), V(xnT[:, :, 0:TB - 1], xtok), V(xnT[:, :, 1:TB], xtok), ALU.subtract)
        P.tt("dve", V(xx[:, :, 0:1], "xx"), V(xlast.unsqueeze(2), "xlast"), V(xnT[:, :, 0:1], xtok), ALU.subtract)
        P.copy("pool", V(xlast.unsqueeze(2), "xlast"), V(xnT[:, :, TB - 1:TB], xtok))
        xw, xwtok = mix(1, xnT, xtok, 0)
        pw = pbank()
        for kc in range(KC):
            P.mm(V(pw.ap[0:64, :], pw.tok), V(w1[:, kc, :], ("w", 4)), V(xw[:, kc, :], xwtok), start=(kc == 0), stop=(kc == KC - 1))
        P.act(V(h1[0:64, :], "h1"), V(pw.ap[0:64, :], pw.tok), AF.Tanh)
        xa, xatok = mix(4, xnT, xtok, 1)
        pa = pbank()
        for kc in range(KC):
            P.mm(V(pa.ap[0:64, :], pa.tok), V(a1[:, kc, :], ("w", 5)), V(xa[:, kc, :], xatok), start=(kc == 0), stop=(kc == KC - 1))
        P.copy("act", V(ha[0:64, :], "ha"), V(pa.ap[0:64, :], pa.tok))
        xg, xgtok = mix(5, xnT, xtok, 0)
        pg = pbank()
        for kc in range(KC):
            P.mm(pg, V(g1[:, kc, 0:128], ("w", 6)), V(xg[:, kc, :], xgtok), start=(kc == 0), stop=(kc == KC - 1))
        P.act(V(hgA, "hgA"), pg, AF.Sigmoid)
        pg = pbank()
        for kc in range(KC):
            P.mm(V(pg.ap[0:32, :], pg.tok), V(g1[:, kc, 128:160], ("w", 6)), V(xg[:, kc, :], xgtok), start=(kc == 0), stop=(kc == KC - 1))
        P.act(V(hgB[0:32, :], "hgB"), V(pg.ap[0:32, :], pg.tok), AF.Sigmoid)
        tick()
        xk_, xktok = mix(2, xnT, xtok, 1)
        xr_, xrtok = mix(0, xnT, xtok, 0)
        for kc in range(NK):
            gk = 4 * p + kc
            if kc == 2:
                tick()
            cs = slice(kc * 128, (kc + 1) * 128)
            t = [V(f32t[j], ("f32t", j)) for j in range(8)]
            pz = pbank()
            P.mm(pz, V(w2[0:64, cs], ("w", 7)), V(h1[0:64, :], "h1"), start=True, stop=True)
            P.act(t[0], pz, AF.Exp, scale=-1.0, bias=V(nw0[:, gk:gk + 1], "nw0"))
            P.act(t[0], t[0], AF.Ln, scale=1.0, bias=1.0)
            P.act(t[0], t[0], AF.Exp, scale=-1.0, bias=-0.5)
            P.S.op("dve", (lambda o=f32t[1], a_=scm, b_=f32t[0]: nc.vector.tensor_tensor_scan(o, a_, b_, 0.0, ALU.mult, ALU.add)),
                   reads=[("scm",), ("f32t", 0)], writes=[("f32t", 1)])
            P.tt("pool", t[2], t[1], t[0], ALU.subtract)
            P.act(t[2], t[2], AF.Exp, scale=-1.0)
            P.act(t[3], t[1], AF.Exp, scale=1.0)
            P.act(t[1], t[1], AF.Exp, scale=-1.0)
            P.copy("pool", V(WC[:, kc, :], ("WC", kc)), V(f32t[1].rearrange("p (c l) -> p c l", c=8)[:, :, RW_C - 1], ("f32t", 1)))
            pa2 = pbank()
            P.mm(pa2, V(a2[0:64, cs], ("w", 8)), V(ha[0:64, :], "ha"), start=True, stop=True)
            P.act(t[4], pa2, AF.Sigmoid, scale=1.0, bias=vcol(7, gk))
            pk = pbank()
            for k8 in range(KC):
                P.mm(pk, V(Wk[:, k8, cs], ("w", 1)), V(xk_[:, k8, :], xktok), start=(k8 == 0), stop=(k8 == KC - 1))
            P.ts("dve", t[5], pk, vcol(8, gk), ALU.mult)
            P.act(V(P.junk[:, 0:TB], "junk"), t[5], AF.Square)
            pss = pbank()
            P.mm(pss, V(bo, "bo"), V(P.junk[:, 0:TB], "junk"), start=True, stop=True)
            P.act(t[6], pss, AF.Ln, scale=1.0, bias=V(tinyb, "tinyb"))
            P.act(t[6], t[6], AF.Exp, scale=-0.5)
            P.tt("dve", t[5], t[5], t[6], ALU.mult)
            for e_ in range(2):
                ps_ = slice(e_ * 64, (e_ + 1) * 64)
                P.stt(V(aTm[e_][ps_, kc, :], ("aT", e_, kc)), V(f32t[5][ps_, :], ("f32t", 5)), -1.0,
                      V(f32t[2][ps_, :], ("f32t", 2)), ALU.mult, ALU.mult)
            P.tt("pool", t[6], t[5], t[4], ALU.mult)
            P.tt("pool", V(bT[:, kc, :], ("bT", kc)), t[6], t[3], ALU.mult)
            P.ts("dve", t[4], t[4], vcol(9, gk), ALU.mult, V(omka[:, gk:gk + 1], "omka"), ALU.add)
            P.tt("dve", t[4], pk, t[4], ALU.mult)
            P.tt("pool", V(kT[:, kc, :], ("kT", kc)), t[4], t[3], ALU.mult)
            pr = pbank()
            for k8 in range(KC):
                P.mm(pr, V(Wr[:, k8, cs], ("w", 0)), V(xr_[:, k8, :], xrtok), start=(k8 == 0), stop=(k8 == KC - 1))
            for e_ in range(2):
                ps_ = slice(e_ * 64, (e_ + 1) * 64)
                P.tt("dve", V(rTm[e_][ps_, kc, :], ("rT", e_, kc)), V(pr.ap[ps_, :], pr.tok),
                     V(f32t[1][ps_, :], ("f32t", 1)), ALU.mult)
            P.stt(V(rkT[:, kc, :], ("rkT", kc)), pr, vcol(10, gk), t[4], ALU.mult, ALU.mult)
        xv_, xvtok = mix(3, xnT, xtok, 1)
        return xv_, xvtok

    def phaseAB(c8, sl, ab):
        Lm, LTm = LmS[ab], LTmS[ab]
        XT = XTS[ab] + [None, None]
        XT[2 + ab] = XTF[ab]
        lt = lambda nm, i_: (nm, ab, i_)
        banks = [V(P.psb[b][0:64, :], ("psb", b)) for b in range(5)]
        for hl in range(8):
            kc, e_ = hl // 2, hl % 2
            a_ = V(aTm[e_][:, kc, sl], ("aT", e_, kc))
            b_ = V(bT[:, kc, sl], ("bT", kc))
            k_ = V(kT[:, kc, sl], ("kT", kc))
            r_ = V(rTm[e_][:, kc, sl], ("rT", e_, kc))
            hs = slice(hl * 64, (hl + 1) * 64)
            for bi, (l_, r2) in enumerate(((a_, b_), (b_, a_), (k_, a_), (b_, r_), (k_, r_))):
                P.mm(V(P.psb[bi][0:64, hs], ("psb", bi)), l_, r2, start=True, stop=True)
        v8 = lambda ap_: ap_.rearrange("p (h s) -> p h s", h=8)
        P.tt("dve", V(Lm[0][0:64], lt("Lm", 0)), V(v8(P.psb[0][0:64, :]), ("psb", 0)), mask(0), ALU.mult)
        P.tt("dve", V(LTm[0][0:64], lt("LTm", 0)), V(v8(P.psb[1][0:64, :]), ("psb", 1)), mask(1), ALU.mult)
        P.tt("dve", V(AkT[ab][0:64], ("AkT", ab)), V(v8(P.psb[2][0:64, :]), ("psb", 2)), mask(1), ALU.mult)
        P.tt("dve", V(ArbT[ab][0:64], ("ArbT", ab)), V(v8(P.psb[3][0:64, :]), ("psb", 3)), mask(2), ALU.mult)
        P.tt("dve", V(ArkT[ab][0:64], ("ArkT", ab)), V(v8(P.psb[4][0:64, :]), ("psb", 4)), mask(2), ALU.mult)
        P.tt("pool", V(XT[0][0:64], lt("XT", 0)), V(LTm[0][0:64], lt("LTm", 0)), mask(3), ALU.add)
        cur, xc = 0, 0
        for lvl in range(5):
            nx = 1 - cur
            last = (lvl == 4)
            for hl in range(8):
                hs = slice(hl * 64, (hl + 1) * 64)
                P.mm(V(P.psb[0][0:64, hs], ("psb", 0)), V(LTm[cur][0:64, hl, :], lt("LTm", cur)),
                     V(Lm[cur][0:64, hl, :], lt("Lm", cur)), start=True, stop=True)
                if not last:
                    P.mm(V(P.psb[1][0:64, hs], ("psb", 1)), V(Lm[cur][0:64, hl, :], lt("Lm", cur)),
                         V(LTm[cur][0:64, hl, :], lt("LTm", cur)), start=True, stop=True)
            P.copy("act", V(Lm[nx][0:64], lt("Lm", nx)), V(v8(P.psb[0][0:64, :]), ("psb", 0)))
            if not last:
                P.copy("act", V(LTm[nx][0:64], lt("LTm", nx)), V(v8(P.psb[1][0:64, :]), ("psb", 1)))
            xn_ = (2 + ab) if last else (1 - xc)
            for hl in range(8):
                hs = slice(hl * 64, (hl + 1) * 64)
                P.mm(V(P.psb[2][0:64, hs], ("psb", 2)), V(Lm[nx][0:64, hl, :], lt("Lm", nx)),
                     V(XT[xc][0:64, hl, :], lt("XT", xc)), start=True, stop=False)
                P.mm(V(P.psb[2][0:64, hs], ("psb", 2)), V(identb[0:64, :], "identb"),
                     V(XT[xc][0:64, hl, :], lt("XT", xc)), start=False, stop=True)
            P.copy("act", V(XT[xn_][0:64], lt("XT", xn_)), V(v8(P.psb[2][0:64, :]), ("psb", 2)))
            cur = nx
            xc = xn_

    def phaseCD(blk, c8, sl, ab, xv_, xvtok):
        b5 = V(P.psb[5][0:64, :], ("psb", 5))
        vt_ = Vtm[c8 % 2]
        vtok = ("Vtm", c8 % 2)
        for k8 in range(KC):
            P.mm(b5, V(xv_[:, k8, sl], xvtok), V(Wv[:, k8, :], ("w", 2)), start=(k8 == 0), stop=(k8 == KC - 1))
        P.copy("act", V(vt_[0:64, :], vtok), b5)
        for hl in range(8):
            kc, e_ = hl // 2, hl % 2
            hs = slice(hl * 64, (hl + 1) * 64)
            P.mm(V(P.psb[5][0:64, hs], ("psb", 5)), V(aTm[e_][:, kc, sl], ("aT", e_, kc)),
                 V(Sbf[:, kc, :], ("Sbf", kc)), start=True, stop=False)
            P.mm(V(P.psb[5][0:64, hs], ("psb", 5)), V(AkT[ab][0:64, hl, :], ("AkT", ab)),
                 V(vt_[0:64, hs], vtok), start=False, stop=True)
        P.copy("act", V(Zs[0:64, :], "Zs"), b5)
        for hl in range(8):
            hs = slice(hl * 64, (hl + 1) * 64)
            P.mm(V(P.psb[5][0:64, hs], ("psb", 5)), V(XTF[ab][0:64, hl, :], ("XT", ab, 2 + ab)),
                 V(Zs[0:64, hs], "Zs"), start=True, stop=True)
        P.copy("act", V(Us[0:64, :], "Us"), b5)
        for hl in range(8):
            kc, e_ = hl // 2, hl % 2
            hs = slice(hl * 64, (hl + 1) * 64)
            P.mm(V(P.psb[5][0:64, hs], ("psb", 5)), V(rTm[e_][:, kc, sl], ("rT", e_, kc)),
                 V(Sbf[:, kc, :], ("Sbf", kc)), start=True, stop=False)
            P.mm(V(P.psb[5][0:64, hs], ("psb", 5)), V(ArbT[ab][0:64, hl, :], ("ArbT", ab)),
                 V(Us[0:64, hs], "Us"), start=False, stop=False)
            P.mm(V(P.psb[5][0:64, hs], ("psb", 5)), V(ArkT[ab][0:64, hl, :], ("ArkT", ab)),
                 V(vt_[0:64, hs], vtok), start=False, stop=True)
        y8 = P.psb[5][0:64, :].rearrange("p (h v) -> p h v", h=8)
        s0, s1 = V(st8[0][0:64, :], ("st8", 0)), V(st8[1][0:64, :], ("st8", 1))
        P.S.op("dve", (lambda o=st8[0][0:64, :], i_=y8: nc.vector.tensor_reduce(o, i_, AX.X, ALU.add)),
               reads=[("psb", 5)], writes=[("st8", 0)])
        P.ts("dve", s0, s0, -1.0 / 64.0, ALU.mult)
        ycv = V(yc[0:64, :], "yc")
        yc8 = yc[0:64, :].rearrange("p (h v) -> p h v", h=8)
        P.tt("dve", V(yc8, "yc"), V(y8, ("psb", 5)), V(st8[0][0:64, :].unsqueeze(2).broadcast_to([64, 8, 64]), ("st8", 0)), ALU.add)
        P.act(V(sq[0:64, :], "sq"), ycv, AF.Square)
        P.S.op("dve", (lambda o=st8[1][0:64, :], i_=sq[0:64, :].rearrange("p (h v) -> p h v", h=8): nc.vector.tensor_reduce(o, i_, AX.X, ALU.add)),
               reads=[("sq",)], writes=[("st8", 1)])
        P.act(s1, s1, AF.Ln, scale=1.0 / 64.0, bias=V(epsg[0:64, :], "epsg"))
        P.act(s1, s1, AF.Exp, scale=-0.5)
        P.tt("dve", V(yc8, "yc"), V(yc8, "yc"), V(st8[1][0:64, :].unsqueeze(2).broadcast_to([64, 8, 64]), ("st8", 1)), ALU.mult)
        P.tt("pool", ycv, ycv, V(lnw[0:64, :], "lnw"), ALU.mult)
        P.tt("pool", ycv, ycv, V(lnb[0:64, :], "lnb"), ALU.add)
        b7f = P.psb[7]
        pBs = V(b7f[0:64, 384:392], ("psb", 7, "s"))
        for kc in range(NK):
            P.mm(V(b7f[0:64, 384 + 2 * kc:386 + 2 * kc], ("psb", 7, "s")), V(rkT[:, kc, sl], ("rkT", kc)), V(Eh, "Eh"),
                 start=True, stop=True)
        s2 = V(st8[2][0:64, :], ("st8", 2))
        P.copy("act", s2, pBs)
        P.tt("pool", V(bon[0:64, :].rearrange("p (h v) -> p h v", h=8), "bon"),
             V(vt_[0:64, :].rearrange("p (h v) -> p h v", h=8), vtok),
             V(st8[2][0:64, :].unsqueeze(2).broadcast_to([64, 8, 64]), ("st8", 2)), ALU.mult)
        P.tt("pool", ycv, ycv, V(bon[0:64, :], "bon"), ALU.add)
        psT7 = P.psb[7].bitcast(BF16)
        for kc in range(NK):
            P.transpose(V(psT7[0:64, kc * 128:(kc + 1) * 128], ("psb", 7, "t")), V(bT[:, kc, sl], ("bT", kc)), V(P.ident, "ident"))
        P.copy("act", V(BKtm[0:64, 0, :], ("BKtm", 0)), V(psT7[0:64, 0:512], ("psb", 7, "t")))
        for kc in range(NK):
            P.transpose(V(psT7[0:64, kc * 128:(kc + 1) * 128], ("psb", 7, "t")), V(kT[:, kc, sl], ("kT", kc)), V(P.ident, "ident"))
        P.copy("act", V(BKtm[0:64, 1, :], ("BKtm", 1)), V(psT7[0:64, 0:512], ("psb", 7, "t")))
        for kc in range(NK):
            cs = slice(kc * 128, (kc + 1) * 128)
            P.mm(V(P.psb[6][:, cs], ("psb", 6)), V(BKtm[0:64, 0, cs], ("BKtm", 0)), V(Us[0:64, cs], "Us"), start=True, stop=False)
            P.mm(V(P.psb[6][:, cs], ("psb", 6)), V(BKtm[0:64, 1, cs], ("BKtm", 1)), V(vt_[0:64, cs], vtok), start=False, stop=True)
        for hp in range(2):
            ps_ = slice(hp * 64, (hp + 1) * 64)
            sv = V(S[ps_, :, :], ("S", hp))
            pst = V(P.psb[6][ps_, :].rearrange("p (k c) -> p k c", k=NK)[:, :, hp * 64:(hp + 1) * 64], ("psb", 6))
            P.tt("dve", sv, sv, pst, ALU.add)
            P.tt("dve", sv, sv, V(WC[ps_, :, c8:c8 + 1].broadcast_to([64, NK, 64]), ("WC",)), ALU.mult)
            P.copy("pool", V(Sbf[ps_, :, :], ("Sbf",)), sv)
        for (lh, rh, kk_) in ((V(hgA[:, sl], "hgA"), V(g2A, ("w", 9)), 0), (V(hgB[0:32, sl], "hgB"), V(g2B[0:32, :], ("w", 10)), 1)):
            P.mm(b5, lh, rh, start=(kk_ == 0), stop=(kk_ == 1))
        P.tt("dve", V(ytm[0:64, :], "ytm"), b5, ycv, ALU.mult)
        for kc in range(NK):
            P.transpose(V(psT7[:, 512 + kc * 64:512 + (kc + 1) * 64], ("psb", 7, "y")), V(ytm[0:64, kc * 128:(kc + 1) * 128], "ytm"),
                        V(P.ident[0:64, 0:64], "ident"))
        P.copy("act", V(yT[:, :, sl], ("yT", c8)), V(psT7[:, 512:768].rearrange("p (k t) -> p k t", k=NK), ("psb", 7, "y")))

    def stageB(blk, slot, tick):
        xv_, xvtok = stage1(blk, slot, tick)
        sls = [slice(c8 * RW_C, (c8 + 1) * RW_C) for c8 in range(8)]
        phaseAB(0, sls[0], 0)
        for c8 in range(8):
            if c8 + 1 < 8:
                phaseAB(c8 + 1, sls[c8 + 1], (c8 + 1) % 2)
            phaseCD(blk, c8, sls[c8], c8 % 2, xv_, xvtok)
            if c8 in (1, 4):
                tick()
        out_stage(P, blk, srcR, srcRname, dst, dstname,
                  lambda k, s: V(yT[:, k, s * 128:(s + 1) * 128], ("yT",)), NK, wo, ("w", 3))

    run_blocks(P, srcN, srcNname, 1, stageB)
    P.arena_reset(mark)


def build(T, plan):
    P = Prog(T, plan)
    P.w = {}

    def win(name, shape, dt=F32):
        if name not in P.w:
            P.w[name] = P.dram_in(name, shape, dt)
        return P.w[name]

    P.win = win
    x_in = P.dram_in("x", [T, D])
    out = P.dram_out("out", [T, D])
    P.arena_init(ARENA_BYTES)
    P.psb = [P.ps("psb%d" % i)[:, :] for i in range(8)]
    P.ident = P.alloc([128], BF16)
    P.ones = P.alloc([128], BF16)
    P.normw = P.alloc([9, KC])
    P.epsv = P.alloc([1])
    P.dma("sp", V(P.ident, "ident"), V(win("c_ident", [128, 128], BF16), "in_w"), key="const0")
    P.dma("sp", V(P.normw, "normw"), V(win("c_normw", [128, 9, KC]), "in_w"), key="const1")
    P.memset("pool", V(P.ones, "ones"), 1.0)
    P.memset("pool", V(P.epsv, "epsv"), EPS)
    P.neghalf = P.alloc([1])
    P.memset("pool", V(P.neghalf, "neghalf"), -0.5)
    P.S.barrier()
    scr = [P.dram_scratch("scr%d" % i, [T, D]) for i in range(3)]
    bufs = [(x_in, "x")] + [(scr[i], "scr%d" % i) for i in range(3)]
    cur = 0

    def nxt(*busy):
        for i in (1, 2, 3):
            if i not in busy:
                return i

    for item in plan:
        kind = item[0]
        a = cur
        b = nxt(a)
        c = nxt(a, b)
        A_, B_, C_ = bufs[a], bufs[b], bufs[c]
        if kind == "ffn":
            li = item[1]
            P.soft_next = SOFT
            ffn_pass(P, li, 0, 11, A_[0], A_[1], A_[0], A_[1], B_[0], B_[1])
            ffn_pass(P, li, 11, 22, A_[0], A_[1], B_[0], B_[1], C_[0], C_[1])
            cur = c
        elif kind == "mix" and item[1] == 3:
            P.soft_next = SOFT
            retnet_pass(P, 0, A_[0], A_[1], A_[0], A_[1], B_[0], B_[1])
            retnet_pass(P, 2, A_[0], A_[1], B_[0], B_[1], C_[0], C_[1])
            cur = c
        elif kind == "mix" and item[1] == 0:
            P.soft_next = SOFT
            ssd_pass(P, 0, A_[0], A_[1], A_[0], A_[1], B_[0], B_[1])
            ssd_pass(P, 1, A_[0], A_[1], B_[0], B_[1], C_[0], C_[1])
            cur = c
        elif kind == "mix" and item[1] == 1:
            P.soft_next = SOFT
            rwkv_pass(P, 0, A_[0], A_[1], A_[0], A_[1], B_[0], B_[1])
            rwkv_pass(P, 1, A_[0], A_[1], B_[0], B_[1], C_[0], C_[1])
            cur = c
        elif kind == "mix" and item[1] == 2:
            gla_pass(P, A_[0], A_[1], A_[0], A_[1], B_[0], B_[1])
            cur = b
        elif kind == "final":
            final_norm(P, bufs[cur][0], bufs[cur][1], out, "out")
    P.barrier("sp", [("out",)])
    global LAST_INPUT_NAMES
    LAST_INPUT_NAMES = list(P.inputs.keys())
    return P.finish()


def ret_perm():
    idx = []
    for part in range(2):
        for h in range(RET_H):
            base = part * 1024 + h * RET_DK
            idx += [base + 2 * i for i in range(128)] + [base + 2 * i + 1 for i in range(128)]
    return np.array(idx + list(range(2048, 6144)))


def host_consts(inputs):
    import ml_dtypes
    f = lambda a: np.ascontiguousarray(np.asarray(a, dtype=np.float32))
    c = {}
    c["c_ident"] = np.eye(128, dtype=np.float32).astype(ml_dtypes.bfloat16)
    nw = np.concatenate([f(inputs["norm_mix"]), f(inputs["norm_ffn"]), f(inputs["norm_final"])[None]], 0)
    c["c_normw"] = np.ascontiguousarray(nw.reshape(9, KC, 128).transpose(2, 0, 1))
    c["c_nfb"] = f(inputs["norm_final"])
    cw = f(inputs["ffn_conv_w"])
    cwl = cw.reshape(4, 3, 44, 128).transpose(0, 3, 2, 1)
    cb = f(inputs["ffn_conv_b"]).reshape(4, 44, 128).transpose(0, 2, 1)
    for li in range(4):
        c["ffn_w_up_%d" % li] = f(inputs["ffn_w_up"][li])
        c["ffn_w_down_%d" % li] = f(inputs["ffn_w_down"][li])
        c["c_ffn_cw_%d" % li] = np.ascontiguousarray(cwl[li])
        c["c_ffn_cb_%d" % li] = np.ascontiguousarray(cb[li])
    c["ret_w_in_p"] = np.ascontiguousarray(f(inputs["ret_w_in"][0])[:, ret_perm()])
    c["ret_w_out"] = f(inputs["ret_w_out"][0])
    inv = (1.0 / (np.float32(10000.0) ** np.linspace(0.0, 1.0, 128, dtype=np.float32))).astype(np.float32)
    ang = (np.arange(4096, dtype=np.float32)[None, :] * inv[:, None]).astype(np.float32)
    c["c_ret_cos"] = np.cos(ang).astype(np.float32)
    c["c_ret_sin"] = np.sin(ang).astype(np.float32)
    gam = 1.0 - 2.0 ** (-5.0 - np.arange(4, dtype=np.float64))
    s_ = np.arange(128)[:, None]
    l_ = np.arange(128)[None, :]
    decT = np.zeros((128, 4, 128), np.float64)
    for h in range(4):
        decT[:, h, :] = np.where(l_ >= s_, gam[h] ** (l_ - s_), 0.0) / 16.0
    c["c_ret_decT"] = decT.astype(np.float32)
    c["c_ret_gl"] = np.stack([gam[h] ** ((np.arange(TB) % 128) + 1) for h in range(4)]).astype(np.float32)
    c["c_ret_kdec"] = np.stack([gam[h] ** (127 - np.arange(128)) / 16.0 for h in range(4)], 1).astype(np.float32)
    c["ssd_w_in"] = f(inputs["ssd_w_in"][0])
    c["ssd_w_out"] = f(inputs["ssd_w_out"][0])
    c["c_ssd_cw"] = np.ascontiguousarray(f(inputs["ssd_conv_w"][0]).reshape(4, 32, 128).transpose(2, 1, 0))
    c["c_ssd_cb"] = np.ascontiguousarray(f(inputs["ssd_conv_b"][0]).reshape(32, 128).T)
    for n_ in ("ssd_dt_bias", "ssd_a_log", "ssd_d", "ssd_norm_w"):
        c[n_] = f(inputs[n_][0])
    c["c_SU"] = (s_ < l_).T.astype(np.float32).copy()
    for n_ in ("w_rkv", "w_out", "w1", "w2", "a1", "a2", "g1", "g2", "ln_w", "ln_b"):
        c["rwkv_" + n_] = f(inputs["rwkv_" + n_][0])
    vecs = [f(inputs["rwkv_mix"][0])[i_] for i_ in range(6)] + [f(inputs["rwkv_" + n_][0]).reshape(-1) for n_ in
                                                                 ("w0", "a0", "k_k", "k_a", "r_k")] + [np.zeros(1024, np.float32)]
    c["c_rwkv_vec"] = np.ascontiguousarray(np.stack(vecs).reshape(12, KC, 128).transpose(2, 0, 1))
    t64 = np.arange(64)[:, None]
    u64 = np.arange(64)[None, :]
    c["c_rwkv_masks"] = np.ascontiguousarray(np.stack([(u64 < t64), (t64 < u64), (t64 <= u64), (t64 == u64)], 1).astype(np.float32))
    E = np.zeros((128, 2), np.float32)
    E[:64, 0] = 1.0
    E[64:, 1] = 1.0
    c["c_rwkv_E"] = E.astype(ml_dtypes.bfloat16)
    c["c_rwkv_bo"] = (E @ E.T).astype(ml_dtypes.bfloat16)
    sm64 = np.ones(TB, np.float32)
    sm64[::64] = 0.0
    c["c_rwkv_scanm"] = sm64
    c["gla_w_in"] = f(inputs["gla_w_in"][0])
    c["gla_w_out"] = f(inputs["gla_w_out"][0])
    c["gla_w_gk2"] = f(inputs["gla_w_gk2"][0])
    c["c_gla_bgk"] = np.ascontiguousarray(f(inputs["gla_b_gk2"][0]).reshape(4, 128).T)
    c["c_gla_nw"] = np.ascontiguousarray(f(inputs["gla_norm_w"][0]).reshape(2, 128).T)
    c["c_maskT"] = (l_ >= s_).astype(np.float32)
    sm = np.ones(TB, np.float32)
    sm[::128] = 0.0
    c["c_scanm"] = sm
    return c


FULL_PLAN = [("mix", 0), ("ffn", 0), ("mix", 1), ("ffn", 1), ("mix", 2), ("ffn", 2), ("mix", 3), ("ffn", 3), ("final",)]
_CACHE = {}


def kernel(**inputs):
    T = 4096
    n_cores = 8
    if "nc" not in _CACHE:
        _CACHE["nc"] = build(T, FULL_PLAN)
        _CACHE["names"] = list(LAST_INPUT_NAMES)
    nc = _CACHE["nc"]
    names = _CACHE["names"]
    consts = host_consts(inputs)
    x = np.ascontiguousarray(np.asarray(inputs["x"], dtype=np.float32))
    shared = {n: consts[n] for n in names if n != "x"}
    in_maps = []
    for b in range(n_cores):
        m = dict(shared)
        m["x"] = np.ascontiguousarray(x[b])
        in_maps.append(m)
    res = run_bass_kernel_spmd(nc, in_maps, core_ids=list(range(n_cores)))
    return np.stack([np.asarray(r["out"], dtype=np.float32) for r in res.results], axis=0)
```

```python
import numpy as np
import concourse.bass as bass
import concourse.mybir as mybir
from concourse.bass_utils import run_bass_kernel_spmd

F32 = mybir.dt.float32
BF16 = mybir.dt.bfloat16
AF = mybir.ActivationFunctionType
ALU = mybir.AluOpType
AX = mybir.AxisListType

D = 1024
KC = 8
TB = 512
SCHEDULE = True
KEEP_ORDER = ()
PRIO = True
SOFT = True
EPS = 1e-5


class V:
    __slots__ = ("ap", "tok")

    def __init__(self, ap, tok):
        self.ap = ap
        self.tok = tok if isinstance(tok, tuple) else (tok,)


class _Op:
    __slots__ = ("eng", "fn", "reads", "writes", "dma_key", "deps", "inc", "val", "sem", "amt",
                 "odeps", "cost", "lat", "bar", "idx", "grp", "gend", "st", "bind", "prio")

    def __init__(self, eng, fn, reads, writes, dma_key, cost=300.0, lat=0.0):
        self.odeps = []
        self.cost = cost
        self.lat = lat
        self.bar = False
        self.idx = 0
        self.grp = None
        self.gend = True
        self.st = 0.0
        self.bind = None
        self.prio = False
        self.eng = eng
        self.fn = fn
        self.reads = reads
        self.writes = writes
        self.dma_key = dma_key
        self.deps = []
        self.inc = False
        self.val = 0
        self.sem = None
        self.amt = 1


class Sched:
    COMPUTE = ("pe", "act", "dve", "pool")

    def __init__(self, nc):
        self.nc = nc
        self.ops = []
        self.state = {}

    def op(self, eng, fn, reads=(), writes=(), dma_key=None, cost=300.0, lat=0.0):
        reads = [t if isinstance(t, tuple) else (t,) for t in reads]
        writes = [t if isinstance(t, tuple) else (t,) for t in writes]
        writes = [t[:2] if t[0] == "psb" else t for t in writes]
        writes += [t[:2] for t in reads if t[0] == "psb" and t[:2] not in writes]
        reads = [t for t in reads if t[0] != "psb"]
        o = _Op(eng, fn, reads, writes, dma_key, cost, lat)
        o.prio = getattr(self, "cur_prio", False)
        self._analyse(o)
        self.ops.append(o)
        return o

    @staticmethod
    def _conf(a, b):
        n = min(len(a), len(b))
        return a[:n] == b[:n]

    def _add_dep(self, o, p, kind):
        if p is None or p is o:
            return
        pd = p.dma_key is not None
        od = o.dma_key is not None
        if not pd and not od:
            if p.eng == "pe" and o.eng == "pe":
                o.odeps.append(p)
                return
            if p.eng == o.eng and kind != "RAW":
                o.odeps.append(p)
                return
        if pd and od and p.eng == o.eng and kind == "WAR" and False:
            return
        o.deps.append(p)

    def _analyse(self, o):
        st = self.state
        for tk in o.reads:
            root = st.setdefault(tk[0], {})
            for k, e in root.items():
                if self._conf(k, tk):
                    self._add_dep(o, e[0], "RAW")
            e = root.get(tk)
            if e is None:
                root[tk] = [None, [o]]
            else:
                e[1].append(o)
        for tk in o.writes:
            root = st.setdefault(tk[0], {})
            dead = []
            for k, e in root.items():
                if self._conf(k, tk):
                    self._add_dep(o, e[0], "WAW")
                    for r in e[1]:
                        self._add_dep(o, r, "WAR")
                    if len(k) > len(tk):
                        dead.append(k)
                    elif len(k) < len(tk):
                        pass
            for k in dead:
                del root[k]
            root[tk] = [o, []]

    def barrier(self):
        lasts = {}
        dmas = {}
        for o in self.ops:
            if o.dma_key is not None:
                dmas[o.dma_key] = o
            elif o.fn is not None:
                lasts[o.eng] = o
        new = []
        for eng in ("pe", "act", "dve", "pool", "sp"):
            b = _Op(eng, None, [], [], None)
            b.deps = [p for e, p in lasts.items() if e != eng] + list(dmas.values())
            b.bar = True
            new.append(b)
        self.ops.extend(new)
        self.state = {}

    def schedule(self, window=16, xlat=300.0):
        import bisect
        segs, cur = [], []
        for o in self.ops:
            if o.bar:
                if cur:
                    segs.append(cur)
                    cur = []
                segs.append([o])
            else:
                cur.append(o)
        if cur:
            segs.append(cur)
        out = []
        self.seg_times = []
        prev_lasts, prev_dmas = {}, {}
        for seg in segs:
            if len(seg) == 1:
                b = seg[0]
                if b.bar:
                    b.deps = [p_ for e_, p_ in prev_lasts.items() if e_ != b.eng] + list(prev_dmas.values())
                out.extend(seg)
                continue
            seg_start = len(out)
            lastof = {}
            for i, o in enumerate(seg):
                o.idx = i
                if o.eng in KEEP_ORDER:
                    if o.eng in lastof:
                        o.odeps.append(lastof[o.eng])
                    lastof[o.eng] = o
            inseg = set(id(o) for o in seg)
            groups = {}
            for o in seg:
                if o.grp is not None:
                    groups.setdefault(o.grp, []).append(o)
            for g, mem in groups.items():
                if len(mem) > 1:
                    ids = set(id(m) for m in mem)
                    first = mem[0]
                    for m in mem[1:]:
                        for d in m.deps + m.odeps:
                            if id(d) not in ids:
                                first.odeps.append(d)
            succ_pre = {}
            for o in seg:
                for d in o.deps:
                    if id(d) in inseg:
                        succ_pre.setdefault(id(d), []).append((o, True))
                for d in o.odeps:
                    if id(d) in inseg:
                        succ_pre.setdefault(id(d), []).append((o, False))
            pe_lock = None
            busy = {}
            use_prio = PRIO and seg[len(seg) // 2].prio
            blev = {}
            for o in reversed(seg):
                m = 0.0
                for s2, _h in succ_pre.get(id(o), ()):
                    v = blev[id(s2)]
                    if v > m:
                        m = v
                blev[id(o)] = m + o.cost + o.lat
            npred = {}
            succ = {}
            for o in seg:
                ds = [(d, True) for d in o.deps if id(d) in inseg] + [(d, False) for d in o.odeps if id(d) in inseg]
                npred[id(o)] = len(ds)
                for d, hard in ds:
                    succ.setdefault(id(d), []).append((o, hard))
            fin = {}
            rtime = {}
            ready = {e: [] for e in ("pe", "act", "dve", "pool", "sp")}
            free = {e: 0.0 for e in ready}
            for o in seg:
                if npred[id(o)] == 0:
                    rtime[id(o)] = 0.0
                    ready[o.eng].append((o.idx, o))
            for e in ready:
                ready[e].sort(key=lambda t: t[0])
            done = 0
            n = len(seg)
            while done < n:
                best = None
                for e, lst in ready.items():
                    fe = free[e]
                    cand = lst[:window]
                    if e == "pe" and pe_lock is not None:
                        cand = [t for t in lst if t[1].grp == pe_lock][:1]
                    for (ix, o) in cand:
                        st = rtime[id(o)]
                        if st < fe:
                            st = fe
                        key = (st, -blev[id(o)], ix) if use_prio else (st, ix)
                        if best is None or key < best[0]:
                            best = (key, o, st, ix)
                _k, o, st, ix = best
                lst = ready[o.eng]
                lst.pop(bisect.bisect_left(lst, (ix,), key=lambda t: (t[0],)))
                if o.eng == "pe" and o.grp is not None:
                    pe_lock = None if o.gend else o.grp
                free[o.eng] = st + o.cost
                busy[o.eng] = busy.get(o.eng, 0.0) + o.cost
                f = st + o.cost + o.lat
                fin[id(o)] = f
                o.st = st
                out.append(o)
                done += 1
                for s_, hard in succ.get(id(o), ()):
                    k = id(s_)
                    npred[k] -= 1
                    if hard or s_.eng != o.eng:
                        t_ = f + (xlat if s_.eng != o.eng else 0.0)
                    else:
                        t_ = st + o.cost
                    if rtime.get(k, 0.0) < t_:
                        rtime[k] = t_
                        s_.bind = o
                    if npred[k] == 0:
                        bisect.insort(ready[s_.eng], (s_.idx, s_), key=lambda t: t[0])
            self.seg_times.append((max(fin.values()) if fin else 0.0, dict(busy), len(seg)))
            prev_lasts, prev_dmas = {}, {}
            for o in out[seg_start:]:
                if o.dma_key is not None:
                    prev_dmas[o.dma_key] = o
                elif o.fn is not None:
                    prev_lasts[o.eng] = o
        assert len(out) == len(self.ops)
        self.ops = out

    def emit(self, block_ctx, sems):
        nc = self.nc
        for o in self.ops:
            for p in o.deps:
                p.inc = True
        cnt = {}
        for o in self.ops:
            if o.dma_key is not None:
                key = ("dma", o.dma_key)
                cnt[key] = cnt.get(key, 0) + 16
                o.val = cnt[key]
                o.sem = sems[key]
                o.amt = 16
                o.inc = True
            elif o.inc:
                cnt[o.eng] = cnt.get(o.eng, 0) + 1
                o.val = cnt[o.eng]
                o.sem = sems[o.eng]
        engs = {"pe": nc.tensor, "act": nc.scalar, "dve": nc.vector, "pool": nc.gpsimd, "sp": nc.sync}
        per_eng = {k: [] for k in engs}
        for o in self.ops:
            per_eng[o.eng].append(o)

        def run(engname):
            eng = engs[engname]
            waited = {}
            for o in per_eng[engname]:
                need = {}
                for p in o.deps:
                    sid = id(p.sem)
                    if need.get(sid, (None, 0))[1] < p.val:
                        need[sid] = (p.sem, p.val)
                for sid, (sem, val) in need.items():
                    if waited.get(sid, 0) < val:
                        eng.wait_ge(sem, val)
                        waited[sid] = val
                if o.fn is None:
                    continue
                ins = o.fn()
                if o.inc:
                    ins.then_inc(o.sem, o.amt)

        @block_ctx.tensor
        def _(e):
            run("pe")

        @block_ctx.scalar
        def _(e):
            run("act")

        @block_ctx.vector
        def _(e):
            run("dve")

        @block_ctx.gpsimd
        def _(e):
            run("pool")

        @block_ctx.sync
        def _(e):
            run("sp")


class Prog:
    def __init__(self, T, plan):
        self.T = T
        self.plan = plan
        self.nc = bass.Bass("TRN2", target_bir_lowering=False)
        self.S = Sched(self.nc)
        self.ctxs = []
        self.dma_keys = []
        self.inputs = {}
        self.psn = 0

    def dram_in(self, name, shape, dt=F32):
        t = self.nc.dram_tensor(name, list(shape), dt, kind="ExternalInput")
        self.inputs[name] = t
        return t.ap()

    def dram_out(self, name, shape, dt=F32):
        return self.nc.dram_tensor(name, list(shape), dt, kind="ExternalOutput").ap()

    def dram_scratch(self, name, shape, dt=F32):
        return self.nc.dram_tensor(name, list(shape), dt, kind="Internal").ap()

    def sb(self, name, shape, dt=F32):
        g = self.nc.sbuf_tensor(name, list(shape), dt)
        t = g.__enter__()
        self.ctxs.append(g)
        return t

    def ps(self, name, shape=(128, 512), dt=F32):
        g = self.nc.psum_tensor(name, list(shape), dt)
        t = g.__enter__()
        self.ctxs.append(g)
        return t

    def arena_init(self, nbytes):
        self.arena = self.sb("arena", [128, nbytes // 4], F32)
        self.arena_n = nbytes
        self.arena_off = 0

    def alloc(self, shape, dt=F32):
        n = 1
        for d in shape:
            n *= d
        esz = 4 if dt == F32 else 2
        nb = (n * esz + 63) // 64 * 64
        assert self.arena_off + nb <= self.arena_n, ("arena overflow", self.arena_off + nb, self.arena_n)
        a = self.arena[:, self.arena_off // 4:(self.arena_off + nb) // 4]
        self.arena_off += nb
        if dt != F32:
            a = a.bitcast(dt)
        a = a[:, 0:n]
        if len(shape) == 2:
            a = a.rearrange("p (a b) -> p a b", a=shape[0])
        elif len(shape) == 3:
            a = a.rearrange("p (a b c) -> p a b c", a=shape[0], b=shape[1])
        elif len(shape) == 4:
            a = a.rearrange("p (a b c d) -> p a b c d", a=shape[0], b=shape[1], c=shape[2])
        return a

    def arena_reset(self, mark=0):
        if getattr(self, "soft_next", False):
            self.soft_next = False
            self.arena_off = mark
            return
        self.S.barrier()
        self.arena_off = mark

    def _toks(self, *vs):
        return [v.tok for v in vs if isinstance(v, V)]

    @staticmethod
    def _n(ap):
        n = 1
        for d in list(ap.shape)[1:]:
            n *= int(d)
        return n

    def mm(self, out, lhsT, rhs, start=True, stop=True):
        nc = self.nc
        otok = out.tok
        if not (start and stop):
            otok = otok[:2]
        n = self._n(rhs.ap)
        mult = 4.0 if rhs.ap.dtype == F32 else 1.0
        o = self.S.op("pe", lambda: nc.tensor.matmul(out.ap, lhsT.ap, rhs.ap, start=start, stop=stop),
                      reads=self._toks(lhsT, rhs), writes=[otok], cost=mult * (max(64, n) * 0.42 + 20.0), lat=250.0)
        if start:
            self.gid = getattr(self, "gid", 0) + 1
        o.grp = self.gid
        o.gend = bool(stop)

    def transpose(self, out, in_, ident):
        nc = self.nc
        self.S.op("pe", lambda: nc.tensor.transpose(out.ap, in_.ap, ident.ap),
                  reads=self._toks(in_, ident), writes=self._toks(out), cost=80.0, lat=250.0)

    def act(self, out, in_, func, scale=None, bias=None, accum=None, extra_reads=()):
        nc = self.nc
        kw = {}
        if scale is not None:
            kw["scale"] = scale.ap if isinstance(scale, V) else scale
        if bias is not None:
            kw["bias"] = bias.ap if isinstance(bias, V) else bias
        if accum is not None:
            kw["accum_out"] = accum.ap
        w = self._toks(out) + (self._toks(accum) if accum is not None else [])
        self.S.op("act", lambda: nc.scalar.activation(out.ap, in_.ap, func, **kw),
                  reads=self._toks(in_, scale, bias) + list(extra_reads), writes=w,
                  cost=230.0 + 0.83 * self._n(in_.ap) + (90.0 if accum is not None else 0.0))

    def _e(self, eng):
        return {"dve": self.nc.vector, "pool": self.nc.gpsimd, "act": self.nc.scalar}[eng]

    def _c(self, eng, ap, per=1.04):
        n = self._n(ap)
        if eng == "pool":
            return 300.0 + 1.6 * n
        if eng == "act":
            return 230.0 + 0.83 * n
        return 120.0 + per * n

    def tt(self, eng, out, a, b, op):
        e = self._e(eng)
        self.S.op(eng, lambda: e.tensor_tensor(out.ap, a.ap, b.ap, op),
                  reads=self._toks(a, b), writes=self._toks(out), cost=self._c(eng, out.ap))

    def ts(self, eng, out, in_, s1, op0, s2=None, op1=None, accum=None):
        e = self._e(eng)
        a1 = s1.ap if isinstance(s1, V) else s1
        a2 = s2.ap if isinstance(s2, V) else s2
        kw = {}
        if op1 is not None:
            kw["op1"] = op1
        if accum is not None:
            kw["accum_out"] = accum.ap
        w = self._toks(out) + (self._toks(accum) if accum is not None else [])
        self.S.op(eng, lambda: e.tensor_scalar(out.ap, in_.ap, a1, a2, op0, **kw),
                  reads=self._toks(in_, s1, s2), writes=w, cost=self._c(eng, out.ap, 0.7))

    def stt(self, out, in0, scalar, in1, op0, op1):
        nc = self.nc
        sc = scalar.ap if isinstance(scalar, V) else scalar
        self.S.op("dve", lambda: nc.vector.scalar_tensor_tensor(out.ap, in0.ap, sc, in1.ap, op0, op1),
                  reads=self._toks(in0, scalar, in1), writes=self._toks(out), cost=self._c("dve", out.ap))

    def copy(self, eng, out, in_):
        if eng == "act":
            nc = self.nc
            self.S.op("act", lambda: nc.scalar.copy(out.ap, in_.ap), reads=self._toks(in_), writes=self._toks(out),
                      cost=self._c("act", out.ap))
        else:
            e = self._e(eng)
            self.S.op(eng, lambda: e.tensor_copy(out.ap, in_.ap), reads=self._toks(in_), writes=self._toks(out),
                      cost=self._c(eng, out.ap, 0.7))

    def memset(self, eng, out, val):
        e = self._e(eng)
        self.S.op(eng, lambda: e.memset(out.ap, val), writes=self._toks(out), cost=self._c(eng, out.ap, 0.7))

    def recip(self, out, in_):
        nc = self.nc
        self.S.op("dve", lambda: nc.vector.reciprocal(out.ap, in_.ap), reads=self._toks(in_), writes=self._toks(out),
                  cost=self._c("dve", out.ap, 8.4))

    def dma(self, q, out, in_, key):
        if key not in self.dma_keys:
            self.dma_keys.append(key)
        e = {"sp": self.nc.sync, "pool": self.nc.gpsimd, "act": self.nc.scalar}[q]
        nb = self._n(out.ap) * int(list(out.ap.shape)[0]) * (4 if out.ap.dtype == F32 else 2)
        self.S.op(q, lambda: e.dma_start(out=out.ap, in_=in_.ap), reads=self._toks(in_),
                  writes=self._toks(out), dma_key=key, cost=(400.0 if q == "pool" else 60.0), lat=2000.0 + nb / 100.0)

    def barrier(self, eng, toks):
        self.S.op(eng, None, reads=list(toks))

    def finish(self):
        nc = self.nc
        sems = {}
        gs = []
        for name in ("pe", "act", "dve", "pool"):
            g = nc.semaphore("sem_" + name)
            sems[name] = g.__enter__()
            gs.append(g)
        for i, k in enumerate(self.dma_keys):
            g = nc.semaphore("semd_%d" % i)
            sems[("dma", k)] = g.__enter__()
            gs.append(g)
        if SCHEDULE:
            self.S.schedule()
            self.seg_times = self.S.seg_times
        blk = nc.Block()
        b = blk.__enter__()
        self.S.emit(b, sems)
        blk.__exit__(None, None, None)
        for g in reversed(gs):
            g.__exit__(None, None, None)
        for g in reversed(self.ctxs):
            g.__exit__(None, None, None)
        return nc


FFN_H = 2816
FFN_NC = 22
ARENA_BYTES = 204 * 1024


def tile_rows(ap, blk, s):
    r0 = blk * TB + s * 128
    return ap[r0:r0 + 128, :]


def alloc_common(P):
    P.xt = [P.alloc([D]) for _ in range(2)]
    P.xr = [P.alloc([D]) for _ in range(2)]
    P.xnT = [P.alloc([KC, TB], BF16) for _ in range(2)]
    P.xs = P.alloc([D], BF16)
    P.junk = P.alloc([D], BF16)
    P.ss = [P.alloc([4]) for _ in range(2)]
    P.rstd = [P.alloc([4]) for _ in range(2)]
    P.xtn = 0
    P.xrn = 0


def norm_tile(P, src, srcname, blk, s, nidx, slot):
    psT = P.psb[7].bitcast(BF16)
    ss, rstd = P.ss[slot], P.rstd[slot]
    xi = P.xtn % 2
    P.xtn += 1
    xt = P.xt[xi]
    xtv = V(xt, ("xt", xi))
    P.dma("sp", xtv, V(tile_rows(src, blk, s), (srcname, blk, s)), key=("xt", xi))
    P.act(V(P.junk, "junk"), xtv, AF.Square, accum=V(ss[:, s:s + 1], ("ss", slot, s)))
    P.act(V(rstd[:, s:s + 1], ("rstd", slot, s)), V(ss[:, s:s + 1], ("ss", slot, s)), AF.Sqrt,
          scale=1.0 / D, bias=V(P.epsv, "epsv"))
    P.recip(V(rstd[:, s:s + 1], ("rstd", slot, s)), V(rstd[:, s:s + 1], ("rstd", slot, s)))
    P.ts("dve", V(P.xs, "xs"), xtv, V(rstd[:, s:s + 1], ("rstd", slot, s)), ALU.mult)
    for kc in range(KC):
        P.transpose(V(psT[:, kc * 128:(kc + 1) * 128], ("psb", 7)), V(P.xs[:, kc * 128:(kc + 1) * 128], "xs"),
                    V(P.ident, "ident"))
    P.tt("dve", V(P.xnT[slot][:, :, s * 128:(s + 1) * 128], ("xnT", slot, s)),
         V(psT.rearrange("p (k t) -> p k t", k=KC), ("psb", 7)),
         V(P.normw[:, nidx, :].unsqueeze(2).broadcast_to([128, KC, 128]), "normw"), ALU.mult)


def run_blocks(P, srcN, srcNname, nidx, stageB):
    nblk = P.T // TB
    for s in range(4):
        norm_tile(P, srcN, srcNname, 0, s, nidx, 0)
    for blk in range(nblk):
        pending = [(blk + 1, s) for s in range(4)] if blk + 1 < nblk else []

        def tick():
            if pending:
                b, s_ = pending.pop(0)
                norm_tile(P, srcN, srcNname, b, s_, nidx, b % 2)

        stageB(blk, blk % 2, tick)
        while pending:
            tick()


def out_stage(P, blk, srcR, srcRname, dst, dstname, lhs_fn, nk, wo, wotok):
    for s in range(4):
        xi = P.xrn % 2
        P.xrn += 1
        xr = P.xr[xi]
        xrv = V(xr, ("xr", xi))
        P.dma("sp", xrv, V(tile_rows(srcR, blk, s), (srcRname, blk, s)), key=("xr", xi))
        for half in range(2):
            b = 3 + (2 * s + half) % 2
            pd = V(P.psb[b], ("psb", b))
            for k in range(nk):
                P.mm(pd, lhs_fn(k, s), V(wo[:, k, half * 512:(half + 1) * 512], wotok),
                     start=(k == 0), stop=(k == nk - 1))
            xh = V(xr[:, half * 512:(half + 1) * 512], ("xr", xi))
            P.tt("dve", xh, pd, xh, ALU.add)
        P.dma("sp", V(tile_rows(dst, blk, s), (dstname, blk, s)), xrv, key=("xr", xi))


def ffn_pass(P, li, c0, c1, srcN, srcNname, srcR, srcRname, dst, dstname):
    P.S.cur_prio = False
    mark = P.arena_off
    alloc_common(P)
    nch = c1 - c0
    ncol = nch * 128
    nblk = P.T // TB
    w_up = P.win("ffn_w_up_%d" % li, [D, 2 * FFN_H])
    w_dn = P.win("ffn_w_down_%d" % li, [FFN_H, D])
    c_cw = P.win("c_ffn_cw_%d" % li, [128, 2 * FFN_NC, 3])
    c_cb = P.win("c_ffn_cb_%d" % li, [128, 2 * FFN_NC])
    wupv = P.alloc([KC, ncol], BF16)
    wupg = P.alloc([KC, ncol], BF16)
    wdn = P.alloc([nch, D], BF16)
    cw = P.alloc([2 * FFN_NC, 3])
    cb = P.alloc([2 * FFN_NC])
    hs = [P.alloc([2 * FFN_NC, 2]) for _ in range(2)]
    A = [P.alloc([TB]) for _ in range(6)]
    G = [P.alloc([TB]) for _ in range(4)]
    hid = P.alloc([nch, TB], BF16)
    upsrc = w_up.rearrange("(k p) n -> p k n", p=128)
    P.dma("pool", V(wupv, ("w", 0)), V(upsrc[:, :, c0 * 128:c1 * 128], "in_w"), key=("w", 0))
    P.dma("pool", V(wupg, ("w", 1)), V(upsrc[:, :, FFN_H + c0 * 128:FFN_H + c1 * 128], "in_w"), key=("w", 1))
    P.dma("pool", V(wdn, ("w", 2)), V(w_dn[c0 * 128:c1 * 128, :].rearrange("(c p) n -> p c n", p=128), "in_w"),
          key=("w", 2))
    P.dma("sp", V(cw, "cw"), V(c_cw, "in_w"), key="cw")
    P.dma("sp", V(cb, "cb"), V(c_cb, "in_w"), key="cb")
    P.memset("pool", V(hs[0], ("hs", 0)), 0.0)
    P.memset("pool", V(hs[1], ("hs", 1)), 0.0)

    def stageB(blk, slot, tick):
        xnT = P.xnT[slot]
        par = blk % 2
        ubanks = (0, 1, 2, 5, 6)
        tick_at = set(int(round(x)) for x in np.linspace(1, 2 * nch - 2, 4))
        for c in range(nch):
            for part in range(2):
                q = 2 * c + part
                if q in tick_at:
                    tick()
                cp = (c0 + c) + part * FFN_NC
                wsel = wupv if part == 0 else wupg
                wtok = ("w", part)
                bi = ubanks[q % 5]
                pu = V(P.psb[bi], ("psb", bi))
                for kc in range(KC):
                    P.mm(pu, V(wsel[:, kc, c * 128:(c + 1) * 128], wtok),
                         V(xnT[:, kc, :], ("xnT", slot)), start=(kc == 0), stop=(kc == KC - 1))
                ai = q % 6
                At = A[ai]
                atok = ("A", ai)
                P.act(V(At[:, 0:TB], atok), pu, AF.Identity,
                      scale=V(cw[:, cp, 2:3], "cw"), bias=V(cb[:, cp:cp + 1], "cb"))
                P.copy("act", V(hs[par][:, cp, :], ("hs", par, cp)), V(P.psb[bi][:, TB - 2:TB], ("psb", bi)))
                P.stt(V(At[:, 1:TB], atok), V(P.psb[bi][:, 0:TB - 1], ("psb", bi)), V(cw[:, cp, 1:2], "cw"),
                      V(At[:, 1:TB], atok), ALU.mult, ALU.add)
                P.stt(V(At[:, 2:TB], atok), V(P.psb[bi][:, 0:TB - 2], ("psb", bi)), V(cw[:, cp, 0:1], "cw"),
                      V(At[:, 2:TB], atok), ALU.mult, ALU.add)
                hp = V(hs[1 - par][:, cp, :], ("hs", 1 - par, cp))
                P.stt(V(At[:, 0:2], atok), hp, V(cw[:, cp, 0:1], "cw"), V(At[:, 0:2], atok), ALU.mult, ALU.add)
                P.stt(V(At[:, 0:1], atok), V(hs[1 - par][:, cp, 1:2], ("hs", 1 - par, cp)), V(cw[:, cp, 1:2], "cw"),
                      V(At[:, 0:1], atok), ALU.mult, ALU.add)
                if part == 0:
                    Aval, avtok = At, atok
                else:
                    Gt = G[c % 4]
                    gtok = ("G", c % 4)
                    P.act(V(Gt, gtok), V(At[:, 0:TB], atok), AF.Silu)
                    P.tt("pool", V(hid[:, c, :], ("hid", c)), V(Aval[:, 0:TB], avtok), V(Gt, gtok), ALU.mult)
        out_stage(P, blk, srcR, srcRname, dst, dstname,
                  lambda k, s: V(hid[:, k, s * 128:(s + 1) * 128], ("hid", k)), nch, wdn, ("w", 2))

    run_blocks(P, srcN, srcNname, 4 + li, stageB)
    P.arena_reset(mark)


def final_norm(P, src, srcname, dst, dstname):
    P.S.cur_prio = False
    mark = P.arena_off
    alloc_common(P)
    nfb = P.alloc([D])
    c_nf = P.win("c_nfb", [D])
    P.dma("sp", V(nfb, "nfb"), V(c_nf.partition_broadcast(128), "in_w"), key="const2")
    nblk = P.T // TB
    n = 0
    for blk in range(nblk):
        for s in range(4):
            xi = n % 2
            n += 1
            xt, xo = P.xt[xi], P.xr[xi]
            xtv = V(xt, ("xt", xi))
            P.dma("sp", xtv, V(tile_rows(src, blk, s), (srcname, blk, s)), key=("xt", xi))
            ssv = V(P.ss[xi][:, 0:1], ("ss", xi))
            rv = V(P.rstd[xi][:, 0:1], ("rstd", xi))
            P.act(V(P.junk, "junk"), xtv, AF.Square, accum=ssv)
            P.act(rv, ssv, AF.Sqrt, scale=1.0 / D, bias=V(P.epsv, "epsv"))
            P.recip(rv, rv)
            P.stt(V(xo, ("xr", xi)), xtv, rv, V(nfb, "nfb"), ALU.mult, ALU.mult)
            P.dma("sp", V(tile_rows(dst, blk, s), (dstname, blk, s)), V(xo, ("xr", xi)), key=("xr", xi))
    P.arena_reset(mark)


RET_H = 4
RET_DK = 256
RET_DV = 512


def retnet_pass(P, h0, srcN, srcNname, srcR, srcRname, dst, dstname):
    P.S.cur_prio = True
    mark = P.arena_off
    alloc_common(P)
    nblk = P.T // TB
    T = P.T
    w_in = P.win("ret_w_in_p", [D, 6144])
    w_out = P.win("ret_w_out", [2048, D])
    c_cos = P.win("c_ret_cos", [128, 4096])
    c_sin = P.win("c_ret_sin", [128, 4096])
    c_decT = P.win("c_ret_decT", [128, RET_H, 128])
    c_gl = P.win("c_ret_gl", [RET_H, TB])
    c_kdec = P.win("c_ret_kdec", [128, RET_H])
    wq = P.alloc([KC, 512], BF16)
    wk = P.alloc([KC, 512], BF16)
    wv = P.alloc([KC, 1024], BF16)
    wg = P.alloc([KC, 1024], BF16)
    wo = P.alloc([8, D], BF16)
    cos = P.alloc([TB])
    sin = P.alloc([TB])
    qT = P.alloc([2, 2, TB], BF16)
    kT = P.alloc([2, 2, TB], BF16)
    qg = P.alloc([2, 2, TB], BF16)
    rt = [P.alloc([TB]) for _ in range(4)]
    vt = P.alloc([4, 2, 512], BF16)
    khat = P.alloc([4, 2, 256], BF16)
    sg = P.alloc([8, TB], BF16)
    yT = P.alloc([8, TB], BF16)
    S = P.alloc([2, 2, 512])
    Sbf = P.alloc([2, 2, 512], BF16)
    decT = P.alloc([RET_H, 128])
    gl = P.alloc([2, TB])
    kdec = P.alloc([RET_H])
    PT = [P.alloc([128], BF16) for _ in range(4)]
    ysq = [P.alloc([512], BF16) for _ in range(4)]
    rs = [P.alloc([128]) for _ in range(4)]
    tmp = [P.alloc([4, 128]) for _ in range(4)]
    src = w_in.rearrange("(k p) n -> p k n", p=128)
    P.dma("pool", V(wq, ("w", 0)), V(src[:, :, h0 * 256:h0 * 256 + 512], "in_w"), key=("w", 0))
    P.dma("pool", V(wk, ("w", 1)), V(src[:, :, 1024 + h0 * 256:1024 + h0 * 256 + 512], "in_w"), key=("w", 1))
    P.dma("pool", V(wv, ("w", 2)), V(src[:, :, 2048 + h0 * 512:2048 + h0 * 512 + 1024], "in_w"), key=("w", 2))
    P.dma("pool", V(wg, ("w", 3)), V(src[:, :, 4096 + h0 * 512:4096 + h0 * 512 + 1024], "in_w"), key=("w", 3))
    P.dma("pool", V(wo, ("w", 4)), V(w_out[h0 * 512:h0 * 512 + 1024, :].rearrange("(c p) n -> p c n", p=128), "in_w"),
          key=("w", 4))
    P.dma("sp", V(decT, "decT"), V(c_decT, "in_w"), key="c0")
    P.dma("sp", V(kdec, "kdec"), V(c_kdec, "in_w"), key="c1")
    for hl in range(2):
        P.dma("sp", V(gl[:, hl, :], ("gl", hl)), V(c_gl[h0 + hl].partition_broadcast(128), "in_w"), key=("c2", hl))
    P.memset("dve", V(S, "S"), 0.0)
    P.memset("pool", V(Sbf, "Sbf"), 0.0)
    g128 = [float((1.0 - 2.0 ** (-5.0 - (h0 + hl))) ** 128) for hl in range(2)]
    pn = [0]

    def pbank():
        b = pn[0] % 3
        pn[0] += 1
        return V(P.psb[b], ("psb", b))

    def stageB(blk, slot, tick):
        xnT = P.xnT[slot]
        xv = V(xnT, ("xnT", slot))
        P.dma("sp", V(cos, "cos"), V(c_cos[:, blk * TB:(blk + 1) * TB], "in_w"), key="cos")
        P.dma("sp", V(sin, "sin"), V(c_sin[:, blk * TB:(blk + 1) * TB], "in_w"), key="sin")
        for (wsel, wtok, dstT, dname) in ((wq, ("w", 0), qT, "qT"), (wk, ("w", 1), kT, "kT")):
            for hl in range(2):
                p1 = pbank()
                for kc in range(KC):
                    P.mm(p1, V(wsel[:, kc, hl * 256:hl * 256 + 128], wtok), V(xnT[:, kc, :], ("xnT", slot)),
                         start=(kc == 0), stop=(kc == KC - 1))
                p2 = pbank()
                for kc in range(KC):
                    P.mm(p2, V(wsel[:, kc, hl * 256 + 128:hl * 256 + 256], wtok), V(xnT[:, kc, :], ("xnT", slot)),
                         start=(kc == 0), stop=(kc == KC - 1))
                r = [V(rt[i], ("rt", i)) for i in range(4)]
                P.tt("dve", r[0], p1, V(cos, "cos"), ALU.mult)
                P.tt("dve", r[1], p2, V(sin, "sin"), ALU.mult)
                P.tt("dve", r[2], p2, V(cos, "cos"), ALU.mult)
                P.tt("dve", r[3], p1, V(sin, "sin"), ALU.mult)
                P.tt("pool", V(dstT[:, hl, 0, :], (dname, hl, 0)), r[0], r[1], ALU.subtract)
                P.tt("pool", V(dstT[:, hl, 1, :], (dname, hl, 1)), r[2], r[3], ALU.add)
                if dname == "qT":
                    for e in range(2):
                        P.tt("pool", V(qg[:, hl, e, :], ("qg", hl, e)), V(qT[:, hl, e, :], ("qT", hl, e)),
                             V(gl[:, hl, :], ("gl", hl)), ALU.mult)
        tick()
        for hl in range(2):
            for j in range(4):
                pg = pbank()
                c = hl * 512 + j * 128
                for kc in range(KC):
                    P.mm(pg, V(wg[:, kc, c:c + 128], ("w", 3)), V(xnT[:, kc, :], ("xnT", slot)),
                         start=(kc == 0), stop=(kc == KC - 1))
                P.act(V(sg[:, hl * 4 + j, :], ("sg", hl, j)), pg, AF.Silu)
        tick()
        for c4 in range(4):
            for hl in range(2):
                pv = pbank()
                for kc in range(KC):
                    P.mm(pv, V(xnT[:, kc, c4 * 128:(c4 + 1) * 128], ("xnT", slot)),
                         V(wv[:, kc, hl * 512:(hl + 1) * 512], ("w", 2)), start=(kc == 0), stop=(kc == KC - 1))
                P.copy("act", V(vt[:, c4, hl, :], ("vt", c4, hl)), pv)
        tick()
        psT6 = P.psb[6].bitcast(BF16)
        for c4 in range(4):
            for hl in range(2):
                for e in range(2):
                    P.transpose(V(psT6[:, e * 128:(e + 1) * 128], ("psb", 6)),
                                V(kT[:, hl, e, c4 * 128:(c4 + 1) * 128], ("kT", hl, e)), V(P.ident, "ident"))
                P.act(V(khat[:, c4, hl, :], ("khat", c4, hl)), V(psT6[:, 0:256], ("psb", 6)), AF.Identity,
                      scale=V(kdec[:, h0 + hl:h0 + hl + 1], "kdec"))
        tick()
        n = 0
        for c4 in range(4):
            sl = slice(c4 * 128, (c4 + 1) * 128)
            for hl in range(2):
                h = h0 + hl
                i2 = n % 4
                n += 1
                psS = V(P.psb[3][:, 0:128], ("psb", 3))
                for e in range(2):
                    P.mm(psS, V(kT[:, hl, e, sl], ("kT", hl, e)), V(qT[:, hl, e, sl], ("qT", hl, e)),
                         start=(e == 0), stop=(e == 1))
                ptv = V(PT[i2], ("PT", i2))
                P.tt("dve", ptv, psS, V(decT[:, h, :], "decT"), ALU.mult)
                psO = P.psb[4]
                for j in range(4):
                    po = V(psO[:, j * 128:(j + 1) * 128], ("psb", 4))
                    P.mm(po, V(vt[:, c4, hl, j * 128:(j + 1) * 128], ("vt", c4, hl)), ptv, start=True, stop=False)
                    for e in range(2):
                        P.mm(po, V(Sbf[:, hl, e, j * 128:(j + 1) * 128], ("Sbf", hl, e)),
                             V(qg[:, hl, e, sl], ("qg", hl, e)), start=False, stop=(e == 1))
                pov = V(psO, ("psb", 4))
                yq = V(ysq[i2], ("ysq", i2))
                P.act(yq, pov, AF.Square)
                psN = V(P.psb[3][:, 128:256], ("psb", 3))
                for j in range(4):
                    P.mm(psN, V(P.ones, "ones"), V(ysq[i2][:, j * 128:(j + 1) * 128], ("ysq", i2)),
                         start=(j == 0), stop=(j == 3))
                rv = V(rs[i2], ("rs", i2))
                P.act(rv, psN, AF.Ln, scale=1.0 / RET_DV, bias=V(P.epsv, "epsv"))
                P.act(rv, rv, AF.Exp, scale=-0.5)
                tv = V(tmp[i2], ("tmp", i2))
                P.tt("dve", tv, V(psO.rearrange("p (j l) -> p j l", j=4), ("psb", 4)),
                     V(rs[i2].unsqueeze(1).broadcast_to([128, 4, 128]), ("rs", i2)), ALU.mult)
                P.tt("pool", V(yT[:, hl * 4:(hl + 1) * 4, sl], ("yT", hl, c4)), tv,
                     V(sg[:, hl * 4:(hl + 1) * 4, sl], ("sg", hl)), ALU.mult)
                for e in range(2):
                    pu = V(P.psb[5], ("psb", 5))
                    P.mm(pu, V(khat[:, c4, hl, e * 128:(e + 1) * 128], ("khat", c4, hl)),
                         V(vt[:, c4, hl, :], ("vt", c4, hl)), start=True, stop=True)
                    sv = V(S[:, hl, e, :], ("S", hl, e))
                    P.stt(sv, sv, g128[hl], pu, ALU.mult, ALU.add)
                    P.copy("pool", V(Sbf[:, hl, e, :], ("Sbf", hl, e)), sv)
        out_stage(P, blk, srcR, srcRname, dst, dstname,
                  lambda k, s: V(yT[:, k, s * 128:(s + 1) * 128], ("yT",)), 8, wo, ("w", 4))

    run_blocks(P, srcN, srcNname, 3, stageB)
    P.arena_reset(mark)


GLA_H = 4
GLA_DK = 128
GLA_DV = 256


def gla_pass(P, srcN, srcNname, srcR, srcRname, dst, dstname):
    P.S.cur_prio = True
    mark = P.arena_off
    alloc_common(P)
    nblk = P.T // TB
    w_in = P.win("gla_w_in", [D, 3088])
    w_out = P.win("gla_w_out", [D, D])
    w_gk2 = P.win("gla_w_gk2", [16, 512])
    c_bgk = P.win("c_gla_bgk", [128, GLA_H])
    c_nw = P.win("c_gla_nw", [128, 2])
    c_maskT = P.win("c_maskT", [128, 128])
    c_scanm = P.win("c_scanm", [TB])
    wq = P.alloc([KC, 512], BF16)
    wk = P.alloc([KC, 512], BF16)
    wv = P.alloc([KC, 1024], BF16)
    wg = P.alloc([KC, 1024], BF16)
    wgk = P.alloc([KC, 16], BF16)
    wo = P.alloc([8, D], BF16)
    wgk2 = P.alloc([512])
    bgk = P.alloc([GLA_H])
    nbgk = P.alloc([GLA_H])
    nw = P.alloc([2])
    maskT = P.alloc([128])
    scanm = P.alloc([TB])
    gkf = P.alloc([TB])
    Gp = P.alloc([GLA_H, TB])
    et = [P.alloc([TB]) for _ in range(2)]
    qT = P.alloc([GLA_H, TB], BF16)
    kT = P.alloc([GLA_H, TB], BF16)
    vt = P.alloc([4, 1024], BF16)
    khat = P.alloc([4, GLA_H, 128], BF16)
    sg = P.alloc([8, TB], BF16)
    yT = P.alloc([8, TB], BF16)
    S = P.alloc([GLA_H, 256])
    Sbf = P.alloc([GLA_H, 256], BF16)
    elast = P.alloc([GLA_H, 4])
    PT = [P.alloc([128], BF16) for _ in range(4)]
    ysq = [P.alloc([256], BF16) for _ in range(4)]
    rs = [P.alloc([128]) for _ in range(4)]
    tmp = [P.alloc([2, 128]) for _ in range(4)]
    src = w_in.rearrange("(k p) n -> p k n", p=128)
    P.dma("pool", V(wq, ("w", 0)), V(src[:, :, 0:512], "in_w"), key=("w", 0))
    P.dma("pool", V(wk, ("w", 1)), V(src[:, :, 512:1024], "in_w"), key=("w", 1))
    P.dma("pool", V(wv, ("w", 2)), V(src[:, :, 1024:2048], "in_w"), key=("w", 2))
    P.dma("pool", V(wg, ("w", 3)), V(src[:, :, 2048:3072], "in_w"), key=("w", 3))
    P.dma("pool", V(wgk, ("w", 5)), V(src[:, :, 3072:3088], "in_w"), key=("w", 5))
    P.dma("pool", V(wo, ("w", 4)), V(w_out.rearrange("(c p) n -> p c n", p=128), "in_w"), key=("w", 4))
    P.dma("sp", V(wgk2[0:16, :], "wgk2"), V(w_gk2, "in_w"), key="c0")
    P.dma("sp", V(bgk, "bgk"), V(c_bgk, "in_w"), key="c1")
    P.dma("sp", V(nw, "nw"), V(c_nw, "in_w"), key="c2")
    P.dma("sp", V(maskT, "maskT"), V(c_maskT, "in_w"), key="c3")
    P.dma("sp", V(scanm, "scanm"), V(c_scanm.partition_broadcast(128), "in_w"), key="c4")
    P.ts("dve", V(nbgk, "nbgk"), V(bgk, "bgk"), -1.0, ALU.mult)
    P.memset("dve", V(S, "S"), 0.0)
    P.memset("pool", V(Sbf, "Sbf"), 0.0)
    lnsc = float(np.log(GLA_DK ** -0.5))
    pn = [0]

    def pbank():
        b = pn[0] % 3
        pn[0] += 1
        return V(P.psb[b], ("psb", b))

    def stageB(blk, slot, tick):
        xnT = P.xnT[slot]
        xtok = ("xnT", slot)
        pg = pbank()
        for kc in range(KC):
            P.mm(V(pg.ap[0:16, :], pg.tok), V(wgk[:, kc, :], ("w", 5)), V(xnT[:, kc, :], xtok),
                 start=(kc == 0), stop=(kc == KC - 1))
        P.copy("act", V(gkf[0:16, :], "gkf"), V(pg.ap[0:16, :], pg.tok))
        for h in range(GLA_H):
            pp = pbank()
            P.mm(pp, V(wgk2[0:16, h * 128:(h + 1) * 128], "wgk2"), V(gkf[0:16, :], "gkf"), start=True, stop=True)
            e0 = V(et[0], ("et", 0))
            P.act(e0, pp, AF.Exp, scale=-1.0, bias=V(nbgk[:, h:h + 1], "nbgk"))
            P.act(e0, e0, AF.Ln, scale=1.0, bias=1.0)
            gph = V(Gp[:, h, :], ("Gp", h))
            nc = P.nc
            P.S.op("dve", (lambda o=gph.ap, a=scanm, b=et[0]: nc.vector.tensor_tensor_scan(o, a, b, 0.0, ALU.mult, ALU.add)),
                   reads=[("scanm",), ("et", 0)], writes=[gph.tok])
            pq = pbank()
            for kc in range(KC):
                P.mm(pq, V(wq[:, kc, h * 128:(h + 1) * 128], ("w", 0)), V(xnT[:, kc, :], xtok),
                     start=(kc == 0), stop=(kc == KC - 1))
            e1 = V(et[1], ("et", 1))
            P.act(e1, gph, AF.Exp, scale=-1.0 / 16.0, bias=lnsc)
            P.tt("dve", V(qT[:, h, :], ("qT", h)), pq, e1, ALU.mult)
            pk = pbank()
            for kc in range(KC):
                P.mm(pk, V(wk[:, kc, h * 128:(h + 1) * 128], ("w", 1)), V(xnT[:, kc, :], xtok),
                     start=(kc == 0), stop=(kc == KC - 1))
            P.act(e1, gph, AF.Exp, scale=1.0 / 16.0)
            P.tt("dve", V(kT[:, h, :], ("kT", h)), pk, e1, ALU.mult)
            P.act(V(elast[:, h, :], ("elast", h)),
                  V(Gp[:, h, :].rearrange("p (c l) -> p c l", c=4)[:, :, 127], ("Gp", h)), AF.Exp, scale=-1.0 / 16.0)
        tick()
        for c in range(8):
            pg2 = pbank()
            for kc in range(KC):
                P.mm(pg2, V(wg[:, kc, c * 128:(c + 1) * 128], ("w", 3)), V(xnT[:, kc, :], xtok),
                     start=(kc == 0), stop=(kc == KC - 1))
            P.act(V(sg[:, c, :], ("sg", c)), pg2, AF.Silu)
            P.ts("pool", V(sg[:, c, :], ("sg", c)), V(sg[:, c, :], ("sg", c)), V(nw[:, (c % 2):(c % 2) + 1], "nw"), ALU.mult)
        tick()
        for c4 in range(4):
            for half in range(2):
                pv = pbank()
                for kc in range(KC):
                    P.mm(pv, V(xnT[:, kc, c4 * 128:(c4 + 1) * 128], xtok),
                         V(wv[:, kc, half * 512:(half + 1) * 512], ("w", 2)), start=(kc == 0), stop=(kc == KC - 1))
                P.copy("act", V(vt[:, c4, half * 512:(half + 1) * 512], ("vt", c4, half)), pv)
        tick()
        psT6 = P.psb[6].bitcast(BF16)
        for c4 in range(4):
            for h in range(GLA_H):
                P.transpose(V(psT6[:, h * 128:(h + 1) * 128], ("psb", 6)),
                            V(kT[:, h, c4 * 128:(c4 + 1) * 128], ("kT", h)), V(P.ident, "ident"))
            P.copy("act", V(khat[:, c4, :, :], ("khat", c4)),
                   V(psT6[:, 0:512].rearrange("p (h d) -> p h d", h=GLA_H), ("psb", 6)))
        tick()
        n = 0
        for c4 in range(4):
            sl = slice(c4 * 128, (c4 + 1) * 128)
            for h in range(GLA_H):
                i2 = n % 4
                n += 1
                psS = V(P.psb[3][:, 0:128], ("psb", 3))
                P.mm(psS, V(kT[:, h, sl], ("kT", h)), V(qT[:, h, sl], ("qT", h)), start=True, stop=True)
                ptv = V(PT[i2], ("PT", i2))
                P.tt("dve", ptv, psS, V(maskT, "maskT"), ALU.mult)
                psO = P.psb[4]
                for j in range(2):
                    po = V(psO[:, j * 128:(j + 1) * 128], ("psb", 4))
                    vc = h * 256 + j * 128
                    P.mm(po, V(vt[:, c4, vc:vc + 128], ("vt", c4, vc // 512)), ptv, start=True, stop=False)
                    P.mm(po, V(Sbf[:, h, j * 128:(j + 1) * 128], ("Sbf", h)), V(qT[:, h, sl], ("qT", h)),
                         start=False, stop=True)
                pov = V(psO[:, 0:256], ("psb", 4))
                yq = V(ysq[i2], ("ysq", i2))
                P.act(yq, pov, AF.Square)
                psN = V(P.psb[3][:, 128:256], ("psb", 3))
                for j in range(2):
                    P.mm(psN, V(P.ones, "ones"), V(ysq[i2][:, j * 128:(j + 1) * 128], ("ysq", i2)),
                         start=(j == 0), stop=(j == 1))
                rv = V(rs[i2], ("rs", i2))
                P.act(rv, psN, AF.Ln, scale=1.0 / GLA_DV, bias=V(P.epsv, "epsv"))
                P.act(rv, rv, AF.Exp, scale=-0.5)
                tv = V(tmp[i2], ("tmp", i2))
                P.tt("dve", tv, V(psO[:, 0:256].rearrange("p (j l) -> p j l", j=2), ("psb", 4)),
                     V(rs[i2].unsqueeze(1).broadcast_to([128, 2, 128]), ("rs", i2)), ALU.mult)
                P.tt("pool", V(yT[:, h * 2:(h + 1) * 2, sl], ("yT", h, c4)), tv,
                     V(sg[:, h * 2:(h + 1) * 2, sl], ("sg",)), ALU.mult)
                pu = V(P.psb[5][:, 0:256], ("psb", 5))
                P.mm(pu, V(khat[:, c4, h, :], ("khat", c4)), V(vt[:, c4, h * 256:(h + 1) * 256], ("vt", c4, h // 2)),
                     start=True, stop=True)
                sv = V(S[:, h, :], ("S", h))
                P.tt("dve", sv, sv, pu, ALU.add)
                P.ts("dve", sv, sv, V(elast[:, h, c4:c4 + 1], ("elast", h)), ALU.mult)
                P.copy("pool", V(Sbf[:, h, :], ("Sbf", h)), sv)
        out_stage(P, blk, srcR, srcRname, dst, dstname,
                  lambda k, s: V(yT[:, k, s * 128:(s + 1) * 128], ("yT",)), 8, wo, ("w", 4))

    run_blocks(P, srcN, srcNname, 2, stageB)
    P.arena_reset(mark)


def ssd_pass(P, p, srcN, srcNname, srcR, srcRname, dst, dstname):
    P.S.cur_prio = True
    mark = P.arena_off
    alloc_common(P)
    nc = P.nc
    NSL = 4
    nblk = P.T // TB
    w_in = P.win("ssd_w_in", [D, 6176])
    w_out = P.win("ssd_w_out", [2048, D])
    c_cw = P.win("c_ssd_cw", [128, 32, 4])
    c_cb = P.win("c_ssd_cb", [128, 32])
    c_dtb = P.win("ssd_dt_bias", [32])
    c_alog = P.win("ssd_a_log", [32])
    c_dsk = P.win("ssd_d", [32])
    c_nw = P.win("ssd_norm_w", [2048])
    c_maskT = P.win("c_maskT", [128, 128])
    c_SU = P.win("c_SU", [128, 128])
    wz = P.alloc([KC, 1024], BF16)
    wxs = P.alloc([KC, 1024], BF16)
    wB = P.alloc([KC, 512], BF16)
    wC = P.alloc([KC, 512], BF16)
    wdt = P.alloc([KC, 16], BF16)
    wo = P.alloc([8, D], BF16)
    cw = P.alloc([32, 4])
    cb = P.alloc([32])
    spill = P.alloc([16, 3])
    dtb = P.alloc([16])
    abc = P.alloc([16])
    dsk = P.alloc([16])
    nwc = P.alloc([1024])
    maskT = P.alloc([128])
    SU = P.alloc([128])
    onesf = P.alloc([128])
    A = [P.alloc([TB + 3]) for _ in range(3)]
    xsT = P.alloc([8, TB], BF16)
    kT = P.alloc([4, TB], BF16)
    qT = P.alloc([4, TB], BF16)
    sz = P.alloc([4, 1024], BF16)
    dtt = P.alloc([16])
    ld = P.alloc([16])
    Gs = P.alloc([16])
    eG = P.alloc([16])
    eGl = P.alloc([16])
    wdec = P.alloc([16])
    tiny = P.alloc([16])
    R = [P.alloc([4, 128]) for _ in range(NSL)]
    dec = [P.alloc([4, 128]) for _ in range(NSL)]
    sm = [P.alloc([128]) for _ in range(NSL)]
    PT = [P.alloc([4, 128], BF16) for _ in range(NSL)]
    xk = [P.alloc([384], BF16) for _ in range(NSL)]
    vv = [P.alloc([256], BF16) for _ in range(NSL)]
    vh = [P.alloc([256], BF16) for _ in range(NSL)]
    ot = [P.alloc([256]) for _ in range(NSL)]
    t2 = [P.alloc([256]) for _ in range(NSL)]
    yv = [P.alloc([256]) for _ in range(NSL)]
    yn = [P.alloc([256], BF16) for _ in range(NSL)]
    ssq = [P.alloc([1]) for _ in range(NSL)]
    yT = P.alloc([8, TB], BF16)
    S = P.alloc([4, 256])
    Sbf = P.alloc([4, 256], BF16)
    src = w_in.rearrange("(k p) n -> p k n", p=128)
    P.dma("pool", V(wz, ("w", 0)), V(src[:, :, p * 1024:(p + 1) * 1024], "in_w"), key=("w", 0))
    P.dma("pool", V(wxs, ("w", 1)), V(src[:, :, 2048 + p * 1024:2048 + (p + 1) * 1024], "in_w"), key=("w", 1))
    P.dma("pool", V(wB, ("w", 2)), V(src[:, :, 4096 + p * 512:4096 + (p + 1) * 512], "in_w"), key=("w", 2))
    P.dma("pool", V(wC, ("w", 3)), V(src[:, :, 5120 + p * 512:5120 + (p + 1) * 512], "in_w"), key=("w", 3))
    P.dma("pool", V(wdt, ("w", 5)), V(src[:, :, 6144 + p * 16:6144 + (p + 1) * 16], "in_w"), key=("w", 5))
    P.dma("pool", V(wo, ("w", 4)), V(w_out[p * 1024:(p + 1) * 1024, :].rearrange("(c p) n -> p c n", p=128), "in_w"),
          key=("w", 4))
    P.dma("sp", V(cw, "cw"), V(c_cw, "in_w"), key="c0")
    P.dma("sp", V(cb, "cb"), V(c_cb, "in_w"), key="c1")
    P.dma("sp", V(dtb, "dtb"), V(c_dtb[p * 16:(p + 1) * 16].partition_broadcast(128), "in_w"), key="c2")
    P.dma("sp", V(abc, "abc"), V(c_alog[p * 16:(p + 1) * 16].partition_broadcast(128), "in_w"), key="c3")
    P.dma("sp", V(dsk, "dsk"), V(c_dsk[p * 16:(p + 1) * 16].partition_broadcast(128), "in_w"), key="c4")
    P.dma("sp", V(nwc, "nwc"), V(c_nw[p * 1024:(p + 1) * 1024].partition_broadcast(128), "in_w"), key="c5")
    P.dma("sp", V(maskT, "maskT"), V(c_maskT, "in_w"), key="c6")
    P.dma("sp", V(SU, "SU"), V(c_SU, "in_w"), key="c7")
    P.memset("pool", V(onesf, "onesf"), 1.0)
    P.memset("pool", V(spill, "spill"), 0.0)
    P.act(V(abc, "abc"), V(abc, "abc"), AF.Exp)
    P.ts("dve", V(abc, "abc"), V(abc, "abc"), -1.0, ALU.mult)
    P.memset("dve", V(S, "S"), 0.0)
    P.memset("pool", V(Sbf, "Sbf"), 0.0)
    pn = [0]

    def pbank():
        b = pn[0] % 3
        pn[0] += 1
        return V(P.psb[b], ("psb", b))

    def conv_chunk(lc, wsel, wtok, col, xnT, slot, dst):
        cc = (8 * p + lc) if lc < 8 else ((16 + 4 * p + lc - 8) if lc < 12 else (24 + 4 * p + lc - 12))
        pu = pbank()
        for kc in range(KC):
            P.mm(pu, V(wsel[:, kc, col:col + 128], wtok), V(xnT[:, kc, :], ("xnT", slot)),
                 start=(kc == 0), stop=(kc == KC - 1))
        ai = lc % 3
        At, atok = A[ai], ("A", ai)
        P.act(V(At[:, 0:TB], atok), pu, AF.Identity, scale=V(cw[:, cc, 3:4], "cw"), bias=V(cb[:, cc:cc + 1], "cb"))
        P.memset("pool", V(At[:, TB:TB + 3], atok), 0.0)
        for sh in (1, 2, 3):
            P.stt(V(At[:, sh:TB + sh], atok), pu, V(cw[:, cc, 3 - sh:4 - sh], "cw"), V(At[:, sh:TB + sh], atok),
                  ALU.mult, ALU.add)
        P.tt("pool", V(At[:, 0:3], atok), V(At[:, 0:3], atok), V(spill[:, lc, :], ("spill", lc)), ALU.add)
        P.copy("pool", V(spill[:, lc, :], ("spill", lc)), V(At[:, TB:TB + 3], atok))
        P.act(dst, V(At[:, 0:TB], atok), AF.Silu)

    def stageB(blk, slot, tick):
        xnT = P.xnT[slot]
        xtok = ("xnT", slot)
        for lc in range(8):
            conv_chunk(lc, wxs, ("w", 1), lc * 128, xnT, slot, V(xsT[:, lc, :], ("xsT", lc)))
        tick()
        for gl in range(4):
            conv_chunk(8 + gl, wB, ("w", 2), gl * 128, xnT, slot, V(kT[:, gl, :], ("kT", gl)))
            conv_chunk(12 + gl, wC, ("w", 3), gl * 128, xnT, slot, V(qT[:, gl, :], ("qT", gl)))
        tick()
        for c4 in range(4):
            for half in range(2):
                pz = pbank()
                for kc in range(KC):
                    P.mm(pz, V(xnT[:, kc, c4 * 128:(c4 + 1) * 128], xtok),
                         V(wz[:, kc, half * 512:(half + 1) * 512], ("w", 0)), start=(kc == 0), stop=(kc == KC - 1))
                P.act(V(sz[:, c4, half * 512:(half + 1) * 512], ("sz", c4, half)), pz, AF.Silu)
        tick()
        n = 0
        psT6 = P.psb[6].bitcast(BF16)
        for c4 in range(4):
            sl = slice(c4 * 128, (c4 + 1) * 128)
            if c4 == 2:
                tick()
            pdt = V(P.psb[4][:, 128:144], ("psb", 4, "d"))
            for kc in range(KC):
                P.mm(pdt, V(xnT[:, kc, sl], xtok), V(wdt[:, kc, :], ("w", 5)), start=(kc == 0), stop=(kc == KC - 1))
            tn = V(tiny, "tiny")
            P.tt("dve", tn, pdt, V(dtb, "dtb"), ALU.add)
            P.act(tn, tn, AF.Exp)
            P.act(V(dtt, "dtt"), tn, AF.Ln, scale=1.0, bias=1.0)
            P.tt("dve", V(ld, "ld"), V(dtt, "dtt"), V(abc, "abc"), ALU.mult)
            pG = V(P.psb[4][:, 144:160], ("psb", 4, "d"))
            P.mm(pG, V(maskT, "maskT"), V(ld, "ld"), start=True, stop=True)
            pGl = V(P.psb[4][:, 160:176], ("psb", 4, "d"))
            P.mm(pGl, V(onesf, "onesf"), V(ld, "ld"), start=True, stop=True)
            P.act(V(Gs, "Gs"), pG, AF.Identity)
            P.act(V(eG, "eG"), pG, AF.Exp)
            P.act(V(eGl, "eGl"), pGl, AF.Exp)
            P.tt("dve", tn, pGl, V(Gs, "Gs"), ALU.subtract)
            P.act(V(wdec, "wdec"), tn, AF.Exp)
            for gl in range(4):
                i2 = n % NSL
                n += 1
                hs = slice(gl * 4, gl * 4 + 4)
                Rv = V(R[i2], ("R", i2))
                P.tt("dve", Rv, V(maskT.unsqueeze(1).broadcast_to([128, 4, 128]), "maskT"),
                     V(ld[:, hs].unsqueeze(2).broadcast_to([128, 4, 128]), "ld"), ALU.mult)
                pSeg = V(P.psb[3], ("psb", 3))
                P.mm(pSeg, V(SU, "SU"), V(R[i2].rearrange("p h l -> p (h l)"), ("R", i2)), start=True, stop=True)
                dv_ = V(dec[i2], ("dec", i2))
                P.act(V(dec[i2].rearrange("p h l -> p (h l)"), ("dec", i2)), pSeg, AF.Exp)
                pS = V(P.psb[4][:, 0:128], ("psb", 4, "s"))
                P.mm(pS, V(kT[:, gl, sl], ("kT", gl)), V(qT[:, gl, sl], ("qT", gl)), start=True, stop=True)
                smv = V(sm[i2], ("sm", i2))
                P.tt("dve", smv, pS, V(maskT, "maskT"), ALU.mult)
                ptv = V(PT[i2], ("PT", i2))
                P.tt("pool", ptv, dv_, V(sm[i2].unsqueeze(1).broadcast_to([128, 4, 128]), ("sm", i2)), ALU.mult)
                P.transpose(V(psT6[:, 0:128], ("psb", 6, "a")), V(xsT[:, gl * 2, sl], ("xsT", gl * 2)), V(P.ident, "ident"))
                P.transpose(V(psT6[:, 128:256], ("psb", 6, "a")), V(xsT[:, gl * 2 + 1, sl], ("xsT", gl * 2 + 1)),
                            V(P.ident, "ident"))
                P.transpose(V(psT6[:, 256:384], ("psb", 6, "a")), V(kT[:, gl, sl], ("kT", gl)), V(P.ident, "ident"))
                xkv = V(xk[i2], ("xk", i2))
                P.copy("act", xkv, V(psT6[:, 0:384], ("psb", 6, "a")))
                xs4 = V(xk[i2][:, 0:256].rearrange("p (h d) -> p h d", h=4), ("xk", i2))
                v4 = V(vv[i2].rearrange("p (h d) -> p h d", h=4), ("vv", i2))
                P.tt("dve", v4, xs4, V(dtt[:, hs].unsqueeze(2).broadcast_to([128, 4, 64]), "dtt"), ALU.mult)
                vh4 = V(vh[i2].rearrange("p (h d) -> p h d", h=4), ("vh", i2))
                P.tt("pool", vh4, v4, V(wdec[:, hs].unsqueeze(2).broadcast_to([128, 4, 64]), "wdec"), ALU.mult)
                for hh in range(4):
                    P.mm(V(P.psb[5][:, hh * 64:(hh + 1) * 64], ("psb", 5, "a")), V(PT[i2][:, hh, :], ("PT", i2)),
                         V(vv[i2][:, hh * 64:(hh + 1) * 64], ("vv", i2)), start=True, stop=True)
                pB = V(P.psb[5][:, 256:512], ("psb", 5, "b"))
                P.mm(pB, V(qT[:, gl, sl], ("qT", gl)), V(Sbf[:, gl, :], ("Sbf", gl)), start=True, stop=True)
                o4 = V(ot[i2].rearrange("p (h d) -> p h d", h=4), ("ot", i2))
                P.tt("dve", o4, V(P.psb[5][:, 256:512].rearrange("p (h d) -> p h d", h=4), ("psb", 5, "b")),
                     V(eG[:, hs].unsqueeze(2).broadcast_to([128, 4, 64]), "eG"), ALU.mult)
                ov = V(ot[i2], ("ot", i2))
                P.tt("dve", ov, ov, V(P.psb[5][:, 0:256], ("psb", 5, "a")), ALU.add)
                t24 = V(t2[i2].rearrange("p (h d) -> p h d", h=4), ("t2", i2))
                P.tt("pool", t24, xs4, V(dsk[:, hs].unsqueeze(2).broadcast_to([128, 4, 64]), "dsk"), ALU.mult)
                P.tt("pool", ov, ov, V(t2[i2], ("t2", i2)), ALU.add)
                yvv = V(yv[i2], ("yv", i2))
                P.tt("pool", yvv, ov, V(sz[:, c4, gl * 256:(gl + 1) * 256], ("sz", c4, gl // 2)), ALU.mult)
                sq = V(ssq[i2], ("ssq", i2))
                P.act(V(P.junk[:, 0:256], "junk"), yvv, AF.Square, accum=sq)
                P.act(sq, sq, AF.Sqrt, scale=1.0 / 256.0, bias=V(P.epsv, "epsv"))
                P.recip(sq, sq)
                ynv = V(yn[i2], ("yn", i2))
                P.stt(ynv, yvv, sq, V(nwc[:, gl * 256:(gl + 1) * 256], "nwc"), ALU.mult, ALU.mult)
                for j in range(2):
                    P.transpose(V(psT6[:, 512 + j * 128:512 + (j + 1) * 128], ("psb", 6, "b")),
                                V(yn[i2][:, j * 128:(j + 1) * 128], ("yn", i2)), V(P.ident, "ident"))
                P.copy("act", V(yT[:, gl * 2:(gl + 1) * 2, sl], ("yT", gl, c4)),
                       V(psT6[:, 512:768].rearrange("p (j l) -> p j l", j=2), ("psb", 6, "b")))
                pU = V(P.psb[4][:, 256:512], ("psb", 4, "u"))
                P.mm(pU, V(xk[i2][:, 256:384], ("xk", i2)), V(vh[i2], ("vh", i2)), start=True, stop=True)
                s4 = V(S[:, gl, :].rearrange("p (h d) -> p h d", h=4), ("S", gl))
                P.tt("dve", s4, s4, V(eGl[:, hs].unsqueeze(2).broadcast_to([128, 4, 64]), "eGl"), ALU.mult)
                sv = V(S[:, gl, :], ("S", gl))
                P.tt("dve", sv, sv, pU, ALU.add)
                P.copy("pool", V(Sbf[:, gl, :], ("Sbf", gl)), sv)
        out_stage(P, blk, srcR, srcRname, dst, dstname,
                  lambda k, s: V(yT[:, k, s * 128:(s + 1) * 128], ("yT",)), 8, wo, ("w", 4))

    run_blocks(P, srcN, srcNname, 0, stageB)
    P.arena_reset(mark)


RW_C = 64
RW_GN_EPS = 64e-5


def rwkv_pass(P, p, srcN, srcNname, srcR, srcRname, dst, dstname):
    P.S.cur_prio = True
    mark = P.arena_off
    alloc_common(P)
    nc = P.nc
    nblk = P.T // TB
    NK = 4
    c0 = p * 512
    w_rkv = P.win("rwkv_w_rkv", [3, D, D])
    w_out = P.win("rwkv_w_out", [D, D])
    w1d, a1d, g1d = P.win("rwkv_w1", [D, 64]), P.win("rwkv_a1", [D, 64]), P.win("rwkv_g1", [D, 160])
    w2d, a2d, g2d = P.win("rwkv_w2", [64, D]), P.win("rwkv_a2", [64, D]), P.win("rwkv_g2", [160, D])
    c_vec = P.win("c_rwkv_vec", [128, 12, KC])
    c_lnw, c_lnb = P.win("rwkv_ln_w", [D]), P.win("rwkv_ln_b", [D])
    c_m = P.win("c_rwkv_masks", [64, 4, 64])
    c_E = P.win("c_rwkv_E", [128, 2], BF16)
    c_bo = P.win("c_rwkv_bo", [128, 128], BF16)
    c_scm = P.win("c_rwkv_scanm", [TB])
    Wr = P.alloc([KC, 512], BF16)
    Wk = P.alloc([KC, 512], BF16)
    Wv = P.alloc([KC, 512], BF16)
    wo = P.alloc([NK, D], BF16)
    w1 = P.alloc([KC, 64], BF16)
    a1 = P.alloc([KC, 64], BF16)
    g1 = P.alloc([KC, 160], BF16)
    w2 = P.alloc([512], BF16)
    a2 = P.alloc([512], BF16)
    g2A = P.alloc([512], BF16)
    g2B = P.alloc([512], BF16)
    vec = P.alloc([12, KC])
    omka = P.alloc([KC])
    nw0 = P.alloc([KC])
    lnw = P.alloc([512])
    lnb = P.alloc([512])
    msk = P.alloc([4, 64])
    identb = P.alloc([64], BF16)
    Eh = P.alloc([2], BF16)
    bo = P.alloc([128], BF16)
    scm = P.alloc([TB])
    epsg = P.alloc([1])
    tinyb = P.alloc([1])
    xx = P.alloc([KC, TB], BF16)
    xi = [P.alloc([KC, TB], BF16) for _ in range(2)]
    xlast = P.alloc([KC], BF16)
    h1 = P.alloc([TB], BF16)
    ha = P.alloc([TB], BF16)
    hgA = P.alloc([TB], BF16)
    hgB = P.alloc([TB], BF16)
    aTm = [P.alloc([NK, TB], BF16) for _ in range(2)]
    rTm = [P.alloc([NK, TB], BF16) for _ in range(2)]
    bT = P.alloc([NK, TB], BF16)
    kT = P.alloc([NK, TB], BF16)
    rkT = P.alloc([NK, TB], BF16)
    yT = P.alloc([NK, TB], BF16)
    WC = P.alloc([NK, 8])
    f32t = [P.alloc([TB]) for _ in range(8)]
    NS = 2
    LmS = [[P.alloc([8, 64], BF16) for _ in range(2)] for _ in range(NS)]
    LTmS = [[P.alloc([8, 64], BF16) for _ in range(2)] for _ in range(NS)]
    XTS = [[P.alloc([8, 64], BF16) for _ in range(2)] for _ in range(NS)]
    XTF = [P.alloc([8, 64], BF16) for _ in range(NS)]
    AkT = [P.alloc([8, 64], BF16) for _ in range(NS)]
    ArbT = [P.alloc([8, 64], BF16) for _ in range(NS)]
    ArkT = [P.alloc([8, 64], BF16) for _ in range(NS)]
    Zs = P.alloc([512], BF16)
    Us = P.alloc([512], BF16)
    Vtm = [P.alloc([512], BF16) for _ in range(2)]
    BKtm = P.alloc([2, 512], BF16)
    yc = P.alloc([512])
    sq = P.alloc([512])
    bon = P.alloc([512])
    ytm = P.alloc([512], BF16)
    st8 = [P.alloc([8]) for _ in range(4)]
    S = P.alloc([NK, 64])
    Sbf = P.alloc([NK, 64], BF16)
    wsrc = lambda i_: w_rkv[i_].rearrange("(k p) n -> p k n", p=128)
    P.dma("pool", V(Wr, ("w", 0)), V(wsrc(0)[:, :, c0:c0 + 512], "in_w"), key=("w", 0))
    P.dma("pool", V(Wk, ("w", 1)), V(wsrc(1)[:, :, c0:c0 + 512], "in_w"), key=("w", 1))
    P.dma("pool", V(Wv, ("w", 2)), V(wsrc(2)[:, :, c0:c0 + 512], "in_w"), key=("w", 2))
    P.dma("pool", V(wo, ("w", 3)), V(w_out[c0:c0 + 512, :].rearrange("(c p) n -> p c n", p=128), "in_w"), key=("w", 3))
    P.dma("pool", V(w1, ("w", 4)), V(w1d.rearrange("(k p) n -> p k n", p=128), "in_w"), key=("w", 4))
    P.dma("pool", V(a1, ("w", 5)), V(a1d.rearrange("(k p) n -> p k n", p=128), "in_w"), key=("w", 5))
    P.dma("pool", V(g1, ("w", 6)), V(g1d.rearrange("(k p) n -> p k n", p=128), "in_w"), key=("w", 6))
    P.dma("pool", V(w2[0:64, :], ("w", 7)), V(w2d[:, c0:c0 + 512], "in_w"), key=("w", 7))
    P.dma("pool", V(a2[0:64, :], ("w", 8)), V(a2d[:, c0:c0 + 512], "in_w"), key=("w", 8))
    P.dma("pool", V(g2A, ("w", 9)), V(g2d[0:128, c0:c0 + 512], "in_w"), key=("w", 9))
    P.dma("pool", V(g2B[0:32, :], ("w", 10)), V(g2d[128:160, c0:c0 + 512], "in_w"), key=("w", 10))
    P.dma("sp", V(vec, "vec"), V(c_vec, "in_w"), key="c0")
    P.dma("sp", V(lnw[0:64, :], "lnw"), V(c_lnw[c0:c0 + 512].partition_broadcast(64), "in_w"), key="c1")
    P.dma("sp", V(lnb[0:64, :], "lnb"), V(c_lnb[c0:c0 + 512].partition_broadcast(64), "in_w"), key="c2")
    P.dma("sp", V(msk[0:64, :, :], "msk"), V(c_m, "in_w"), key="c3")
    P.dma("sp", V(Eh, "Eh"), V(c_E, "in_w"), key="c4")
    P.dma("sp", V(bo, "bo"), V(c_bo, "in_w"), key="c5")
    P.dma("sp", V(scm, "scm"), V(c_scm.partition_broadcast(128), "in_w"), key="c6")
    P.ts("dve", V(omka, "omka"), V(vec[:, 9, :], "vec"), -1.0, ALU.mult, 1.0, ALU.add)
    P.ts("dve", V(nw0, "nw0"), V(vec[:, 6, :], "vec"), -1.0, ALU.mult)
    P.copy("dve", V(identb[0:64, :], "identb"), V(msk[0:64, 3, :], "msk"))
    P.memset("pool", V(epsg, "epsg"), RW_GN_EPS)
    P.memset("pool", V(tinyb, "tinyb"), 1e-24)
    P.memset("pool", V(xlast, "xlast"), 0.0)
    P.memset("dve", V(S, "S"), 0.0)
    P.memset("pool", V(Sbf, "Sbf"), 0.0)
    for e_ in range(2):
        P.memset("pool", V(aTm[e_], ("aT", e_)), 0.0)
        P.memset("pool", V(rTm[e_], ("rT", e_)), 0.0)
    pn = [0]

    def pbank():
        b = pn[0] % 3
        pn[0] += 1
        return V(P.psb[b], ("psb", b))

    def mask(i_):
        return V(msk[0:64, i_, :].unsqueeze(1).broadcast_to([64, 8, 64]), "msk")

    def vcol(i_, kc):
        return V(vec[:, i_, kc:kc + 1], "vec")

    def mix(i_, xnT, xtok, buf):
        o = xi[buf]
        for kc in range(KC):
            if kc % 2 == 0:
                P.stt(V(o[:, kc, :], ("xi", buf, kc)), V(xx[:, kc, :], ("xx", kc)), vcol(i_, kc),
                      V(xnT[:, kc, :], xtok), ALU.mult, ALU.add)
            else:
                P.ts("pool", V(o[:, kc, :], ("xi", buf, kc)), V(xx[:, kc, :], ("xx", kc)), vcol(i_, kc), ALU.mult)
                P.tt("pool", V(o[:, kc, :], ("xi", buf, kc)), V(o[:, kc, :], ("xi", buf, kc)), V(xnT[:, kc, :], xtok), ALU.add)
        return o, ("xi", buf)

    def stage1(blk, slot, tick):
        xnT = P.xnT[slot]
        xtok = ("xnT", slot)
        P.tt("dve", V(xx[:, :, 1:TB], "xx"), V(xnT[:, :, 0:TB - 1], xtok), V(xnT[:, :, 1:TB], xtok), ALU.subtract)
        P.tt("dve", V(xx[:, :, 0:1], "xx"), V(xlast.unsqueeze(2), "xlast"), V(xnT[:, :, 0:1], xtok), ALU.subtract)
        P.copy("pool", V(xlast.unsqueeze(2), "xlast"), V(xnT[:, :, TB - 1:TB], xtok))
        xw, xwtok = mix(1, xnT, xtok, 0)
        pw = pbank()
        for kc in range(KC):
            P.mm(V(pw.ap[0:64, :], pw.tok), V(w1[:, kc, :], ("w", 4)), V(xw[:, kc, :], xwtok), start=(kc == 0), stop=(kc == KC - 1))
        P.act(V(h1[0:64, :], "h1"), V(pw.ap[0:64, :], pw.tok), AF.Tanh)
        xa, xatok = mix(4, xnT, xtok, 1)
        pa = pbank()
        for kc in range(KC):
            P.mm(V(pa.ap[0:64, :], pa.tok), V(a1[:, kc, :], ("w", 5)), V(xa[:, kc, :], xatok), start=(kc == 0), stop=(kc == KC - 1))
        P.copy("act", V(ha[0:64, :], "ha"), V(pa.ap[0:64, :], pa.tok))
        xg, xgtok = mix(5, xnT, xtok, 0)
        pg = pbank()
        for kc in range(KC):
            P.mm(pg, V(g1[:, kc, 0:128], ("w", 6)), V(xg[:, kc, :], xgtok), start=(kc == 0), stop=(kc == KC - 1))
        P.act(V(hgA, "hgA"), pg, AF.Sigmoid)
        pg = pbank()
        for kc in range(KC):
            P.mm(V(pg.ap[0:32, :], pg.tok), V(g1[:, kc, 128:160], ("w", 6)), V(xg[:, kc, :], xgtok), start=(kc == 0), stop=(kc == KC - 1))
        P.act(V(hgB[0:32, :], "hgB"), V(pg.ap[0:32, :], pg.tok), AF.Sigmoid)
        tick()
        xk_, xktok = mix(2, xnT, xtok, 1)
        xr_, xrtok = mix(0, xnT, xtok, 0)
        for kc in range(NK):
            gk = 4 * p + kc
            if kc == 2:
                tick()
            cs = slice(kc * 128, (kc + 1) * 128)
            t = [V(f32t[j], ("f32t", j)) for j in range(8)]
            pz = pbank()
            P.mm(pz, V(w2[0:64, cs], ("w", 7)), V(h1[0:64, :], "h1"), start=True, stop=True)
            P.act(t[0], pz, AF.Exp, scale=-1.0, bias=V(nw0[:, gk:gk + 1], "nw0"))
            P.act(t[0], t[0], AF.Ln, scale=1.0, bias=1.0)
            P.act(t[0], t[0], AF.Exp, scale=-1.0, bias=-0.5)
            P.S.op("dve", (lambda o=f32t[1], a_=scm, b_=f32t[0]: nc.vector.tensor_tensor_scan(o, a_, b_, 0.0, ALU.mult, ALU.add)),
                   reads=[("scm",), ("f32t", 0)], writes=[("f32t", 1)])
            P.tt("pool", t[2], t[1], t[0], ALU.subtract)
            P.act(t[2], t[2], AF.Exp, scale=-1.0)
            P.act(t[3], t[1], AF.Exp, scale=1.0)
            P.act(t[1], t[1], AF.Exp, scale=-1.0)
            P.copy("pool", V(WC[:, kc, :], ("WC", kc)), V(f32t[1].rearrange("p (c l) -> p c l", c=8)[:, :, RW_C - 1], ("f32t", 1)))
            pa2 = pbank()
            P.mm(pa2, V(a2[0:64, cs], ("w", 8)), V(ha[0:64, :], "ha"), start=True, stop=True)
            P.act(t[4], pa2, AF.Sigmoid, scale=1.0, bias=vcol(7, gk))
            pk = pbank()
            for k8 in range(KC):
                P.mm(pk, V(Wk[:, k8, cs], ("w", 1)), V(xk_[:, k8, :], xktok), start=(k8 == 0), stop=(k8 == KC - 1))
            P.ts("dve", t[5], pk, vcol(8, gk), ALU.mult)
            P.act(V(P.junk[:, 0:TB], "junk"), t[5], AF.Square)
            pss = pbank()
            P.mm(pss, V(bo, "bo"), V(P.junk[:, 0:TB], "junk"), start=True, stop=True)
            P.act(t[6], pss, AF.Ln, scale=1.0, bias=V(tinyb, "tinyb"))
            P.act(t[6], t[6], AF.Exp, scale=-0.5)
            P.tt("dve", t[5], t[5], t[6], ALU.mult)
            for e_ in range(2):
                ps_ = slice(e_ * 64, (e_ + 1) * 64)
                P.stt(V(aTm[e_][ps_, kc, :], ("aT", e_, kc)), V(f32t[5][ps_, :], ("f32t", 5)), -1.0,
                      V(f32t[2][ps_, :], ("f32t", 2)), ALU.mult, ALU.mult)
            P.tt("pool", t[6], t[5], t[4], ALU.mult)
            P.tt("pool", V(bT[:, kc, :], ("bT", kc)), t[6], t[3], ALU.mult)
            P.ts("dve", t[4], t[4], vcol(9, gk), ALU.mult, V(omka[:, gk:gk + 1], "omka"), ALU.add)
            P.tt("dve", t[4], pk, t[4], ALU.mult)
            P.tt("pool", V(kT[:, kc, :], ("kT", kc)), t[4], t[3], ALU.mult)
            pr = pbank()
            for k8 in range(KC):
                P.mm(pr, V(Wr[:, k8, cs], ("w", 0)), V(xr_[:, k8, :], xrtok), start=(k8 == 0), stop=(k8 == KC - 1))
            for e_ in range(2):
                ps_ = slice(e_ * 64, (e_ + 1) * 64)
                P.tt("dve", V(rTm[e_][ps_, kc, :], ("rT", e_, kc)), V(pr.ap[ps_, :], pr.tok),
                     V(f32t[1][ps_, :], ("f32t", 1)), ALU.mult)
            P.stt(V(rkT[:, kc, :], ("rkT", kc)), pr, vcol(10, gk), t[4], ALU.mult, ALU.mult)
        xv_, xvtok = mix(3, xnT, xtok, 1)
        return xv_, xvtok

    def phaseAB(c8, sl, ab):
        Lm, LTm = LmS[ab], LTmS[ab]
        XT = XTS[ab] + [None, None]
        XT[2 + ab] = XTF[ab]
        lt = lambda nm, i_: (nm, ab, i_)
        banks = [V(P.psb[b][0:64, :], ("psb", b)) for b in range(5)]
        for hl in range(8):
            kc, e_ = hl // 2, hl % 2
            a_ = V(aTm[e_][:, kc, sl], ("aT", e_, kc))
            b_ = V(bT[:, kc, sl], ("bT", kc))
            k_ = V(kT[:, kc, sl], ("kT", kc))
            r_ = V(rTm[e_][:, kc, sl], ("rT", e_, kc))
            hs = slice(hl * 64, (hl + 1) * 64)
            for bi, (l_, r2) in enumerate(((a_, b_), (b_, a_), (k_, a_), (b_, r_), (k_, r_))):
                P.mm(V(P.psb[bi][0:64, hs], ("psb", bi)), l_, r2, start=True, stop=True)
        v8 = lambda ap_: ap_.rearrange("p (h s) -> p h s", h=8)
        P.tt("dve", V(Lm[0][0:64], lt("Lm", 0)), V(v8(P.psb[0][0:64, :]), ("psb", 0)), mask(0), ALU.mult)
        P.tt("dve", V(LTm[0][0:64], lt("LTm", 0)), V(v8(P.psb[1][0:64, :]), ("psb", 1)), mask(1), ALU.mult)
        P.tt("dve", V(AkT[ab][0:64], ("AkT", ab)), V(v8(P.psb[2][0:64, :]), ("psb", 2)), mask(1), ALU.mult)
        P.tt("dve", V(ArbT[ab][0:64], ("ArbT", ab)), V(v8(P.psb[3][0:64, :]), ("psb", 3)), mask(2), ALU.mult)
        P.tt("dve", V(ArkT[ab][0:64], ("ArkT", ab)), V(v8(P.psb[4][0:64, :]), ("psb", 4)), mask(2), ALU.mult)
        P.tt("pool", V(XT[0][0:64], lt("XT", 0)), V(LTm[0][0:64], lt("LTm", 0)), mask(3), ALU.add)
        cur, xc = 0, 0
        for lvl in range(5):
            nx = 1 - cur
            last = (lvl == 4)
            for hl in range(8):
                hs = slice(hl * 64, (hl + 1) * 64)
                P.mm(V(P.psb[0][0:64, hs], ("psb", 0)), V(LTm[cur][0:64, hl, :], lt("LTm", cur)),
                     V(Lm[cur][0:64, hl, :], lt("Lm", cur)), start=True, stop=True)
                if not last:
                    P.mm(V(P.psb[1][0:64, hs], ("psb", 1)), V(Lm[cur][0:64, hl, :], lt("Lm", cur)),
                         V(LTm[cur][0:64, hl, :], lt("LTm", cur)), start=True, stop=True)
            P.copy("act", V(Lm[nx][0:64], lt("Lm", nx)), V(v8(P.psb[0][0:64, :]), ("psb", 0)))
            if not last:
                P.copy("act", V(LTm[nx][0:64], lt("LTm", nx)), V(v8(P.psb[1][0:64, :]), ("psb", 1)))
            xn_ = (2 + ab) if last else (1 - xc)
            for hl in range(8):
                hs = slice(hl * 64, (hl + 1) * 64)
                P.mm(V(P.psb[2][0:64, hs], ("psb", 2)), V(Lm[nx][0:64, hl, :], lt("Lm", nx)),
                     V(XT[xc][0:64, hl, :], lt("XT", xc)), start=True, stop=False)
                P.mm(V(P.psb[2][0:64, hs], ("psb", 2)), V(identb[0:64, :], "identb"),
                     V(XT[xc][0:64, hl, :], lt("XT", xc)), start=False, stop=True)
            P.copy("act", V(XT[xn_][0:64], lt("XT", xn_)), V(v8(P.psb[2][0:64, :]), ("psb", 2)))
            cur = nx
            xc = xn_

    def phaseCD(blk, c8, sl, ab, xv_, xvtok):
        b5 = V(P.psb[5][0:64, :], ("psb", 5))
        vt_ = Vtm[c8 % 2]
        vtok = ("Vtm", c8 % 2)
        for k8 in range(KC):
            P.mm(b5, V(xv_[:, k8, sl], xvtok), V(Wv[:, k8, :], ("w", 2)), start=(k8 == 0), stop=(k8 == KC - 1))
        P.copy("act", V(vt_[0:64, :], vtok), b5)
        for hl in range(8):
            kc, e_ = hl // 2, hl % 2
            hs = slice(hl * 64, (hl + 1) * 64)
            P.mm(V(P.psb[5][0:64, hs], ("psb", 5)), V(aTm[e_][:, kc, sl], ("aT", e_, kc)),
                 V(Sbf[:, kc, :], ("Sbf", kc)), start=True, stop=False)
            P.mm(V(P.psb[5][0:64, hs], ("psb", 5)), V(AkT[ab][0:64, hl, :], ("AkT", ab)),
                 V(vt_[0:64, hs], vtok), start=False, stop=True)
        P.copy("act", V(Zs[0:64, :], "Zs"), b5)
        for hl in range(8):
            hs = slice(hl * 64, (hl + 1) * 64)
            P.mm(V(P.psb[5][0:64, hs], ("psb", 5)), V(XTF[ab][0:64, hl, :], ("XT", ab, 2 + ab)),
                 V(Zs[0:64, hs], "Zs"), start=True, stop=True)
        P.copy("act", V(Us[0:64, :], "Us"), b5)
        for hl in range(8):
            kc, e_ = hl // 2, hl % 2
            hs = slice(hl * 64, (hl + 1) * 64)
            P.mm(V(P.psb[5][0:64, hs], ("psb", 5)), V(rTm[e_][:, kc, sl], ("rT", e_, kc)),
                 V(Sbf[:, kc, :], ("Sbf", kc)), start=True, stop=False)
            P.mm(V(P.psb[5][0:64, hs], ("psb", 5)), V(ArbT[ab][0:64, hl, :], ("ArbT", ab)),
                 V(Us[0:64, hs], "Us"), start=False, stop=False)
            P.mm(V(P.psb[5][0:64, hs], ("psb", 5)), V(ArkT[ab][0:64, hl, :], ("ArkT", ab)),
                 V(vt_[0:64, hs], vtok), start=False, stop=True)
        y8 = P.psb[5][0:64, :].rearrange("p (h v) -> p h v", h=8)
        s0, s1 = V(st8[0][0:64, :], ("st8", 0)), V(st8[1][0:64, :], ("st8", 1))
        P.S.op("dve", (lambda o=st8[0][0:64, :], i_=y8: nc.vector.tensor_reduce(o, i_, AX.X, ALU.add)),
               reads=[("psb", 5)], writes=[("st8", 0)])
        P.ts("dve", s0, s0, -1.0 / 64.0, ALU.mult)
        ycv = V(yc[0:64, :], "yc")
        yc8 = yc[0:64, :].rearrange("p (h v) -> p h v", h=8)
        P.tt("dve", V(yc8, "yc"), V(y8, ("psb", 5)), V(st8[0][0:64, :].unsqueeze(2).broadcast_to([64, 8, 64]), ("st8", 0)), ALU.add)
        P.act(V(sq[0:64, :], "sq"), ycv, AF.Square)
        P.S.op("dve", (lambda o=st8[1][0:64, :], i_=sq[0:64, :].rearrange("p (h v) -> p h v", h=8): nc.vector.tensor_reduce(o, i_, AX.X, ALU.add)),
               reads=[("sq",)], writes=[("st8", 1)])
        P.act(s1, s1, AF.Ln, scale=1.0 / 64.0, bias=V(epsg[0:64, :], "epsg"))
        P.act(s1, s1, AF.Exp, scale=-0.5)
        P.tt("dve", V(yc8, "yc"), V(yc8, "yc"), V(st8[1][0:64, :].unsqueeze(2).broadcast_to([64, 8, 64]), ("st8", 1)), ALU.mult)
        P.tt("pool", ycv, ycv, V(lnw[0:64, :], "lnw"), ALU.mult)
        P.tt("pool", ycv, ycv, V(lnb[0:64, :], "lnb"), ALU.add)
        b7f = P.psb[7]
        pBs = V(b7f[0:64, 384:392], ("psb", 7, "s"))
        for kc in range(NK):
            P.mm(V(b7f[0:64, 384 + 2 * kc:386 + 2 * kc], ("psb", 7, "s")), V(rkT[:, kc, sl], ("rkT", kc)), V(Eh, "Eh"),
                 start=True, stop=True)
        s2 = V(st8[2][0:64, :], ("st8", 2))
        P.copy("act", s2, pBs)
        P.tt("pool", V(bon[0:64, :].rearrange("p (h v) -> p h v", h=8), "bon"),
             V(vt_[0:64, :].rearrange("p (h v) -> p h v", h=8), vtok),
             V(st8[2][0:64, :].unsqueeze(2).broadcast_to([64, 8, 64]), ("st8", 2)), ALU.mult)
        P.tt("pool", ycv, ycv, V(bon[0:64, :], "bon"), ALU.add)
        psT7 = P.psb[7].bitcast(BF16)
        for kc in range(NK):
            P.transpose(V(psT7[0:64, kc * 128:(kc + 1) * 128], ("psb", 7, "t")), V(bT[:, kc, sl], ("bT", kc)), V(P.ident, "ident"))
        P.copy("act", V(BKtm[0:64, 0, :], ("BKtm", 0)), V(psT7[0:64, 0:512], ("psb", 7, "t")))
        for kc in range(NK):
            P.transpose(V(psT7[0:64, kc * 128:(kc + 1) * 128], ("psb", 7, "t")), V(kT[:, kc, sl], ("kT", kc)), V(P.ident, "ident"))
        P.copy("act", V(BKtm[0:64, 1, :], ("BKtm", 1)), V(psT7[0:64, 0:512], ("psb", 7, "t")))
        for kc in range(NK):
            cs = slice(kc * 128, (kc + 1) * 128)
            P.mm(V(P.psb[6][:, cs], ("psb", 6)), V(BKtm[0:64, 0, cs], ("BKtm", 0)), V(Us[0:64, cs], "Us"), start=True, stop=False)
            P.mm(V(P.psb[6][:, cs], ("psb", 6)), V(BKtm[0:64, 1, cs], ("BKtm", 1)), V(vt_[0:64, cs], vtok), start=False, stop=True)
        for hp in range(2):
            ps_ = slice(hp * 64, (hp + 1) * 64)
            sv = V(S[ps_, :, :], ("S", hp))
            pst = V(P.psb[6][ps_, :].rearrange("p (k c) -> p k c", k=NK)[:, :, hp * 64:(hp + 1) * 64], ("psb", 6))
            P.tt("dve", sv, sv, pst, ALU.add)
            P.tt("dve", sv, sv, V(WC[ps_, :, c8:c8 + 1].broadcast_to([64, NK, 64]), ("WC",)), ALU.mult)
            P.copy("pool", V(Sbf[ps_, :, :], ("Sbf",)), sv)
        for (lh, rh, kk_) in ((V(hgA[:, sl], "hgA"), V(g2A, ("w", 9)), 0), (V(hgB[0:32, sl], "hgB"), V(g2B[0:32, :], ("w", 10)), 1)):
            P.mm(b5, lh, rh, start=(kk_ == 0), stop=(kk_ == 1))
        P.tt("dve", V(ytm[0:64, :], "ytm"), b5, ycv, ALU.mult)
        for kc in range(NK):
            P.transpose(V(psT7[:, 512 + kc * 64:512 + (kc + 1) * 64], ("psb", 7, "y")), V(ytm[0:64, kc * 128:(kc + 1) * 128], "ytm"),
                        V(P.ident[0:64, 0:64], "ident"))
        P.copy("act", V(yT[:, :, sl], ("yT", c8)), V(psT7[:, 512:768].rearrange("p (k t) -> p k t", k=NK), ("psb", 7, "y")))

    def stageB(blk, slot, tick):
        xv_, xvtok = stage1(blk, slot, tick)
        sls = [slice(c8 * RW_C, (c8 + 1) * RW_C) for c8 in range(8)]
        phaseAB(0, sls[0], 0)
        for c8 in range(8):
            if c8 + 1 < 8:
                phaseAB(c8 + 1, sls[c8 + 1], (c8 + 1) % 2)
            phaseCD(blk, c8, sls[c8], c8 % 2, xv_, xvtok)
            if c8 in (1, 4):
                tick()
        out_stage(P, blk, srcR, srcRname, dst, dstname,
                  lambda k, s: V(yT[:, k, s * 128:(s + 1) * 128], ("yT",)), NK, wo, ("w", 3))

    run_blocks(P, srcN, srcNname, 1, stageB)
    P.arena_reset(mark)


def build(T, plan):
    P = Prog(T, plan)
    P.w = {}

    def win(name, shape, dt=F32):
        if name not in P.w:
            P.w[name] = P.dram_in(name, shape, dt)
        return P.w[name]

    P.win = win
    x_in = P.dram_in("x", [T, D])
    out = P.dram_out("out", [T, D])
    P.arena_init(ARENA_BYTES)
    P.psb = [P.ps("psb%d" % i)[:, :] for i in range(8)]
    P.ident = P.alloc([128], BF16)
    P.ones = P.alloc([128], BF16)
    P.normw = P.alloc([9, KC])
    P.epsv = P.alloc([1])
    P.dma("sp", V(P.ident, "ident"), V(win("c_ident", [128, 128], BF16), "in_w"), key="const0")
    P.dma("sp", V(P.normw, "normw"), V(win("c_normw", [128, 9, KC]), "in_w"), key="const1")
    P.memset("pool", V(P.ones, "ones"), 1.0)
    P.memset("pool", V(P.epsv, "epsv"), EPS)
    P.S.barrier()
    scr = [P.dram_scratch("scr%d" % i, [T, D]) for i in range(3)]
    bufs = [(x_in, "x")] + [(scr[i], "scr%d" % i) for i in range(3)]
    cur = 0

    def nxt(*busy):
        for i in (1, 2, 3):
            if i not in busy:
                return i

    for item in plan:
        kind = item[0]
        a = cur
        b = nxt(a)
        c = nxt(a, b)
        A_, B_, C_ = bufs[a], bufs[b], bufs[c]
        if kind == "ffn":
            li = item[1]
            P.soft_next = SOFT
            ffn_pass(P, li, 0, 11, A_[0], A_[1], A_[0], A_[1], B_[0], B_[1])
            ffn_pass(P, li, 11, 22, A_[0], A_[1], B_[0], B_[1], C_[0], C_[1])
            cur = c
        elif kind == "mix" and item[1] == 3:
            P.soft_next = SOFT
            retnet_pass(P, 0, A_[0], A_[1], A_[0], A_[1], B_[0], B_[1])
            retnet_pass(P, 2, A_[0], A_[1], B_[0], B_[1], C_[0], C_[1])
            cur = c
        elif kind == "mix" and item[1] == 0:
            P.soft_next = SOFT
            ssd_pass(P, 0, A_[0], A_[1], A_[0], A_[1], B_[0], B_[1])
            ssd_pass(P, 1, A_[0], A_[1], B_[0], B_[1], C_[0], C_[1])
            cur = c
        elif kind == "mix" and item[1] == 1:
            P.soft_next = SOFT
            rwkv_pass(P, 0, A_[0], A_[1], A_[0], A_[1], B_[0], B_[1])
            rwkv_pass(P, 1, A_[0], A_[1], B_[0], B_[1], C_[0], C_[1])
            cur = c
        elif kind == "mix" and item[1] == 2:
            gla_pass(P, A_[0], A_[1], A_[0], A_[1], B_[0], B_[1])
            cur = b
        elif kind == "final":
            final_norm(P, bufs[cur][0], bufs[cur][1], out, "out")
    P.barrier("sp", [("out",)])
    global LAST_INPUT_NAMES
    LAST_INPUT_NAMES = list(P.inputs.keys())
    return P.finish()


def ret_perm():
    idx = []
    for part in range(2):
        for h in range(RET_H):
            base = part * 1024 + h * RET_DK
            idx += [base + 2 * i for i in range(128)] + [base + 2 * i + 1 for i in range(128)]
    return np.array(idx + list(range(2048, 6144)))


def host_consts(inputs):
    import ml_dtypes
    f = lambda a: np.ascontiguousarray(np.asarray(a, dtype=np.float32))
    c = {}
    c["c_ident"] = np.eye(128, dtype=np.float32).astype(ml_dtypes.bfloat16)
    nw = np.concatenate([f(inputs["norm_mix"]), f(inputs["norm_ffn"]), f(inputs["norm_final"])[None]], 0)
    c["c_normw"] = np.ascontiguousarray(nw.reshape(9, KC, 128).transpose(2, 0, 1))
    c["c_nfb"] = f(inputs["norm_final"])
    cw = f(inputs["ffn_conv_w"])
    cwl = cw.reshape(4, 3, 44, 128).transpose(0, 3, 2, 1)
    cb = f(inputs["ffn_conv_b"]).reshape(4, 44, 128).transpose(0, 2, 1)
    for li in range(4):
        c["ffn_w_up_%d" % li] = f(inputs["ffn_w_up"][li])
        c["ffn_w_down_%d" % li] = f(inputs["ffn_w_down"][li])
        c["c_ffn_cw_%d" % li] = np.ascontiguousarray(cwl[li])
        c["c_ffn_cb_%d" % li] = np.ascontiguousarray(cb[li])
    c["ret_w_in_p"] = np.ascontiguousarray(f(inputs["ret_w_in"][0])[:, ret_perm()])
    c["ret_w_out"] = f(inputs["ret_w_out"][0])
    inv = (1.0 / (np.float32(10000.0) ** np.linspace(0.0, 1.0, 128, dtype=np.float32))).astype(np.float32)
    ang = (np.arange(4096, dtype=np.float32)[None, :] * inv[:, None]).astype(np.float32)
    c["c_ret_cos"] = np.cos(ang).astype(np.float32)
    c["c_ret_sin"] = np.sin(ang).astype(np.float32)
    gam = 1.0 - 2.0 ** (-5.0 - np.arange(4, dtype=np.float64))
    s_ = np.arange(128)[:, None]
    l_ = np.arange(128)[None, :]
    decT = np.zeros((128, 4, 128), np.float64)
    for h in range(4):
        decT[:, h, :] = np.where(l_ >= s_, gam[h] ** (l_ - s_), 0.0) / 16.0
    c["c_ret_decT"] = decT.astype(np.float32)
    c["c_ret_gl"] = np.stack([gam[h] ** ((np.arange(TB) % 128) + 1) for h in range(4)]).astype(np.float32)
    c["c_ret_kdec"] = np.stack([gam[h] ** (127 - np.arange(128)) / 16.0 for h in range(4)], 1).astype(np.float32)
    c["ssd_w_in"] = f(inputs["ssd_w_in"][0])
    c["ssd_w_out"] = f(inputs["ssd_w_out"][0])
    c["c_ssd_cw"] = np.ascontiguousarray(f(inputs["ssd_conv_w"][0]).reshape(4, 32, 128).transpose(2, 1, 0))
    c["c_ssd_cb"] = np.ascontiguousarray(f(inputs["ssd_conv_b"][0]).reshape(32, 128).T)
    for n_ in ("ssd_dt_bias", "ssd_a_log", "ssd_d", "ssd_norm_w"):
        c[n_] = f(inputs[n_][0])
    c["c_SU"] = (s_ < l_).T.astype(np.float32).copy()
    for n_ in ("w_rkv", "w_out", "w1", "w2", "a1", "a2", "g1", "g2", "ln_w", "ln_b"):
        c["rwkv_" + n_] = f(inputs["rwkv_" + n_][0])
    vecs = [f(inputs["rwkv_mix"][0])[i_] for i_ in range(6)] + [f(inputs["rwkv_" + n_][0]).reshape(-1) for n_ in
                                                                 ("w0", "a0", "k_k", "k_a", "r_k")] + [np.zeros(1024, np.float32)]
    c["c_rwkv_vec"] = np.ascontiguousarray(np.stack(vecs).reshape(12, KC, 128).transpose(2, 0, 1))
    t64 = np.arange(64)[:, None]
    u64 = np.arange(64)[None, :]
    c["c_rwkv_masks"] = np.ascontiguousarray(np.stack([(u64 < t64), (t64 < u64), (t64 <= u64), (t64 == u64)], 1).astype(np.float32))
    E = np.zeros((128, 2), np.float32)
    E[:64, 0] = 1.0
    E[64:, 1] = 1.0
    c["c_rwkv_E"] = E.astype(ml_dtypes.bfloat16)
    c["c_rwkv_bo"] = (E @ E.T).astype(ml_dtypes.bfloat16)
    sm64 = np.ones(TB, np.float32)
    sm64[::64] = 0.0
    c["c_rwkv_scanm"] = sm64
    c["gla_w_in"] = f(inputs["gla_w_in"][0])
    c["gla_w_out"] = f(inputs["gla_w_out"][0])
    c["gla_w_gk2"] = f(inputs["gla_w_gk2"][0])
    c["c_gla_bgk"] = np.ascontiguousarray(f(inputs["gla_b_gk2"][0]).reshape(4, 128).T)
    c["c_gla_nw"] = np.ascontiguousarray(f(inputs["gla_norm_w"][0]).reshape(2, 128).T)
    c["c_maskT"] = (l_ >= s_).astype(np.float32)
    sm = np.ones(TB, np.float32)
    sm[::128] = 0.0
    c["c_scanm"] = sm
    return c


FULL_PLAN = [("mix", 0), ("ffn", 0), ("mix", 1), ("ffn", 1), ("mix", 2), ("ffn", 2), ("mix", 3), ("ffn", 3), ("final",)]
_CACHE = {}


def kernel(**inputs):
    T = 4096
    n_cores = 8
    if "nc" not in _CACHE:
        _CACHE["nc"] = build(T, FULL_PLAN)
        _CACHE["names"] = list(LAST_INPUT_NAMES)
    nc = _CACHE["nc"]
    names = _CACHE["names"]
    consts = host_consts(inputs)
    x = np.ascontiguousarray(np.asarray(inputs["x"], dtype=np.float32))
    shared = {n: consts[n] for n in names if n != "x"}
    in_maps = []
    for b in range(n_cores):
        m = dict(shared)
        m["x"] = np.ascontiguousarray(x[b])
        in_maps.append(m)
    res = run_bass_kernel_spmd(nc, in_maps, core_ids=list(range(n_cores)))
    return np.stack([np.asarray(r["out"], dtype=np.float32) for r in res.results], axis=0)
```
